# Optimizing a Trainium2 kernel written in Bass

```python
import jax, jax.numpy as jnp
from jax import lax
import numpy as np

D_MODEL = 1024
BATCH = 4
SEQ = 8192
DEPTH = 2

GRID_W = 64
CTX_LEN = 256
N_EVEN = (DEPTH + 1) // 2
N_ODD = DEPTH // 2
N_MOD = 6

POOL_WINDOWS = (2, 4, 8, 16)
POOL_GROUPS = len(POOL_WINDOWS)
D_POOL = D_MODEL // 2
POOL_GW = D_POOL // POOL_GROUPS
D_CONV = D_MODEL // 2
CONV_W = 31
AB_IN = D_POOL + 2 * D_CONV
AB_MIX = D_POOL + D_CONV
MLSTM_HEADS = 4
MLSTM_DH = D_MODEL // 8
D_MLSTM = MLSTM_HEADS * MLSTM_DH
MLSTM_CHUNK = 128
N_GATE = 4 * MLSTM_HEADS
ATT_QH = 8
ATT_KVH = 2
ATT_DH = D_MODEL // 16
ATT_GROUP = ATT_QH // ATT_KVH
ATT_BLOCK = 128
ROPE_THETA = 10000.0
ROPE_F = ATT_DH // 4
CD_SPLITS = (D_MLSTM, D_MLSTM, D_MLSTM, D_MLSTM, N_GATE, ATT_QH * ATT_DH, ATT_KVH * ATT_DH, ATT_KVH * ATT_DH)
CD_IN = sum(CD_SPLITS)
CD_CUTS = tuple(sum(CD_SPLITS[:i + 1]) for i in range(len(CD_SPLITS) - 1))
CD_MIX = D_MLSTM + ATT_QH * ATT_DH
PEER_HEADS = 8
PEER_NKEYS = 128
PEER_N = PEER_NKEYS * PEER_NKEYS
PEER_TOPK = 16
PEER_DKEY = 256
PEER_BLOCK = 128
DN_ALPHA = (2 * DEPTH) ** 0.25
DN_BETA = (8 * DEPTH) ** -0.25
LN_EPS = 1e-5
RMS_EPS = 1e-6

kernel_name = "hybrid_pool_conv_mlstm_gqa_peer_trunk"

F32 = jnp.float32


def layer_norm(x, g, b):
    xf = x.astype(F32)
    mu = jnp.mean(xf, -1, keepdims=True)
    var = jnp.mean(jnp.square(xf - mu), -1, keepdims=True)
    return ((xf - mu) * lax.rsqrt(var + LN_EPS) * g + b).astype(x.dtype)


def rms_norm(x, g):
    xf = x.astype(F32)
    return (xf * lax.rsqrt(jnp.mean(xf * xf, -1, keepdims=True) + RMS_EPS) * g).astype(x.dtype)


def multiscale_pool(xa, w_pool, ls_pool):
    T = xa.shape[1]
    xf = xa.astype(F32)
    cs = jnp.concatenate([jnp.zeros_like(xf[:, :1]), jnp.cumsum(xf, axis=1)], axis=1)
    t = jnp.arange(T)
    outs = []
    for g, w in enumerate(POOL_WINDOWS):
        lo = jnp.clip(t - w // 2, 0, T)
        hi = jnp.clip(t + w // 2, 0, T)
        sl = slice(g * POOL_GW, (g + 1) * POOL_GW)
        csg = cs[:, :, sl]
        mean = (csg[:, hi] - csg[:, lo]) / (hi - lo).astype(F32)[None, :, None]
        outs.append((mean - xf[:, :, sl]).astype(xa.dtype) @ w_pool[g])
    return jnp.concatenate(outs, axis=-1) * ls_pool


def conformer_conv(xb, conv_w, conv_b, ln_g, ln_b):
    a, gt = jnp.split(xb, 2, axis=-1)
    u = a * jax.nn.sigmoid(gt)
    y = lax.conv_general_dilated(u, conv_w[:, None, :], window_strides=(1,),
                                 padding=[(CONV_W // 2, CONV_W // 2)],
                                 dimension_numbers=('NWC', 'WIO', 'NWC'),
                                 feature_group_count=D_CONV) + conv_b
    return jax.nn.silu(layer_norm(y, ln_g, ln_b))


def mixer_ab(h, w_in, w_pool, ls_pool, conv_w, conv_b, cln_g, cln_b, w_out):
    z = h @ w_in
    ya = multiscale_pool(z[..., :D_POOL], w_pool, ls_pool)
    yb = conformer_conv(z[..., D_POOL:], conv_w, conv_b, cln_g, cln_b)
    return jnp.concatenate([ya, yb], axis=-1) @ w_out


def mlstm_chunkwise(q, k, v, ig, fg, state):
    B_, H, T, _ = q.shape
    L = MLSTM_CHUNK
    nc = T // L

    def chunks(a):
        return jnp.moveaxis(a.reshape(B_, H, nc, L, *a.shape[3:]), 2, 0)

    causal = jnp.tril(jnp.ones((L, L), dtype=bool))

    def step(carry, inp):
        C, n, m = carry
        qj, kj, vj, ij, fj = inp
        b = jnp.cumsum(jax.nn.log_sigmoid(fj), axis=-1)
        dmat = jnp.where(causal, b[..., :, None] - b[..., None, :] + ij[..., None, :], -jnp.inf)
        inter = b + m[..., None]
        m_t = jnp.maximum(inter, jnp.max(dmat, -1))
        w_inter = jnp.exp(inter - m_t)
        s = jnp.einsum('bhid,bhjd->bhij', qj, kj) * jnp.exp(dmat - m_t[..., None])
        num = jnp.einsum('bhij,bhjv->bhiv', s, vj) + w_inter[..., None] * jnp.einsum('bhid,bhdv->bhiv', qj, C)
        den = jnp.sum(s, -1) + w_inter * jnp.einsum('bhid,bhd->bhi', qj, n)
        h = num / jnp.maximum(jnp.abs(den), jnp.exp(-m_t))[..., None]
        b_last = b[..., -1]
        g_log = b_last[..., None] - b + ij
        m_new = jnp.maximum(b_last + m, jnp.max(g_log, -1))
        wk = jnp.exp(g_log - m_new[..., None])
        decay = jnp.exp(b_last + m - m_new)
        C_new = decay[..., None, None] * C + jnp.einsum('bhj,bhjd,bhjv->bhdv', wk, kj, vj)
        n_new = decay[..., None] * n + jnp.einsum('bhj,bhjd->bhd', wk, kj)
        return (C_new, n_new, m_new), h

    state, hs = lax.scan(step, state, tuple(chunks(a) for a in (q, k, v, ig, fg)))
    return jnp.moveaxis(hs, 0, 2).reshape(B_, H, T, -1), state


def mlstm_direction(q, k, v, ig, fg, state, reverse):
    if reverse:
        q, k, v, ig, fg = (jnp.flip(a, axis=2) for a in (q, k, v, ig, fg))
    h, state = mlstm_chunkwise(q, k, v, ig, fg, state)
    if reverse:
        h = jnp.flip(h, axis=2)
    return h, state


def mlstm_inputs(zq, zk, zv, zg, gate_b):
    B_, T, _ = zq.shape

    def heads(a):
        return a.reshape(B_, T, MLSTM_HEADS, MLSTM_DH).transpose(0, 2, 1, 3).astype(F32)

    g = (zg.astype(F32).reshape(B_, T, 4, MLSTM_HEADS) + gate_b.astype(F32)).transpose(2, 0, 3, 1)
    return heads(zq), heads(zk) * (MLSTM_DH ** -0.5), heads(zv), g


def mlstm_out(h, o_pre, norm_g):
    B_, H, T, dv = h.shape
    mu = jnp.mean(h, -1, keepdims=True)
    var = jnp.mean(jnp.square(h - mu), -1, keepdims=True)
    hn = ((h - mu) * lax.rsqrt(var + LN_EPS)).transpose(0, 2, 1, 3).reshape(B_, T, H * dv) * norm_g
    return (hn * jax.nn.sigmoid(o_pre.astype(F32))).astype(o_pre.dtype)


def mlstm_zero_state(b_):
    return (jnp.zeros((b_, MLSTM_HEADS, MLSTM_DH, MLSTM_DH), F32),
            jnp.zeros((b_, MLSTM_HEADS, MLSTM_DH), F32),
            jnp.zeros((b_, MLSTM_HEADS), F32))


def axial_rope_tables(row, col):
    freqs = ROPE_THETA ** (-jnp.arange(ROPE_F, dtype=F32) / ROPE_F)
    ar = row.astype(F32)[:, None] * freqs
    ac = col.astype(F32)[:, None] * freqs
    return (jnp.cos(ar), jnp.sin(ar), jnp.cos(ac), jnp.sin(ac))


def rope_1d(x, cos, sin):
    x1, x2 = jnp.split(x, 2, axis=-1)
    cos, sin = cos[:, None, :], sin[:, None, :]
    return jnp.concatenate([x1 * cos - x2 * sin, x1 * sin + x2 * cos], axis=-1)


def apply_axial_rope(x, tabs):
    cr, sr, cc, sc = tabs
    xr, xc = jnp.split(x, 2, axis=-1)
    return jnp.concatenate([rope_1d(xr, cr, sr), rope_1d(xc, cc, sc)], axis=-1).astype(x.dtype)


def attn_inputs(zq, zk, zv, qn_g, kn_g, rope):
    B_, T, _ = zq.shape
    q = rms_norm(zq.reshape(B_, T, ATT_QH, ATT_DH), qn_g)
    k = rms_norm(zk.reshape(B_, T, ATT_KVH, ATT_DH), kn_g)
    v = zv.reshape(B_, T, ATT_KVH, ATT_DH)
    if rope is not None:
        q, k = apply_axial_rope(q, rope), apply_axial_rope(k, rope)
    return q, k, v


def blocked_attention(q, k, v):
    B_, Tq, _, _ = q.shape
    nb = Tq // ATT_BLOCK
    qb = q.reshape(B_, nb, ATT_BLOCK, ATT_KVH, ATT_GROUP, ATT_DH).transpose(1, 0, 2, 3, 4, 5)
    scale = ATT_DH ** -0.5

    def block(qi):
        s = jnp.einsum('bqhgd,bkhd->bhgqk', qi, k).astype(F32) * scale
        p = jax.nn.softmax(s, axis=-1).astype(v.dtype)
        return jnp.einsum('bhgqk,bkhd->bqhgd', p, v)

    o = lax.map(block, qb)
    return o.transpose(1, 0, 2, 3, 4, 5).reshape(B_, Tq, ATT_QH * ATT_DH)


def mixer_cd(h_l, h_c, w_in, gate_b, norm_g, qn_g, kn_g, w_out, rope, ctx_out):
    qm_l, km_l, vm_l, om_l, gt_l, qa_l, ka_l, va_l = jnp.split(h_l @ w_in, CD_CUTS, axis=-1)
    qm_c, km_c, vm_c, om_c, gt_c, qa_c, ka_c, va_c = jnp.split(h_c @ w_in, CD_CUTS, axis=-1)
    Qc, Kc, Vc, Gc = mlstm_inputs(qm_c, km_c, vm_c, gt_c, gate_b)
    Ql, Kl, Vl, Gl = mlstm_inputs(qm_l, km_l, vm_l, gt_l, gate_b)
    zero = mlstm_zero_state(h_c.shape[0])
    hcf, st_f = mlstm_direction(Qc, Kc, Vc, Gc[0], Gc[1], zero, False)
    hcb, st_b = mlstm_direction(Qc, Kc, Vc, Gc[2], Gc[3], zero, True)
    hlf, _ = mlstm_direction(Ql, Kl, Vl, Gl[0], Gl[1], st_f, False)
    hlb, _ = mlstm_direction(Ql, Kl, Vl, Gl[2], Gl[3], st_b, True)
    q_l, k_l, v_l = attn_inputs(qa_l, ka_l, va_l, qn_g, kn_g, rope)
    q_c, k_c, v_c = attn_inputs(qa_c, ka_c, va_c, qn_g, kn_g, None)
    y_la = blocked_attention(q_l, jnp.concatenate([k_c, k_l], 1), jnp.concatenate([v_c, v_l], 1))
    y_l = jnp.concatenate([mlstm_out(hlf + hlb, om_l, norm_g), y_la], axis=-1) @ w_out
    if ctx_out:
        y_ca = blocked_attention(q_c, k_c, v_c)
        y_c = jnp.concatenate([mlstm_out(hcf + hcb, om_c, norm_g), y_ca], axis=-1) @ w_out
    else:
        y_c = None
    return y_l, y_c


def peer_ffn(h, w_q, sub_keys, u_tab, v_tab):
    shape = h.shape
    tok = h.reshape(-1, PEER_BLOCK, D_MODEL)
    half = PEER_DKEY // 2
    kk = PEER_TOPK * PEER_TOPK

    def block(hb):
        q = (hb @ w_q).reshape(PEER_BLOCK, PEER_HEADS, 2, half)
        s = jnp.einsum('nhpd,pkd->nhpk', q, sub_keys).astype(F32)
        sv, si = lax.top_k(s, PEER_TOPK)
        cand = (sv[:, :, 0, :, None] + sv[:, :, 1, None, :]).reshape(PEER_BLOCK, PEER_HEADS, kk)
        cidx = (si[:, :, 0, :, None] * PEER_NKEYS + si[:, :, 1, None, :]).reshape(PEER_BLOCK, PEER_HEADS, kk)
        top_s, pos = lax.top_k(cand, PEER_TOPK)
        eidx = jnp.take_along_axis(cidx, pos, axis=-1)
        gate = jax.nn.softmax(top_s, axis=-1)
        act = jax.nn.gelu(jnp.einsum('nd,nhkd->nhk', hb, u_tab[eidx]).astype(F32), approximate=False)
        w = (gate * act).astype(hb.dtype)
        return jnp.einsum('nhk,nhkd->nd', w, v_tab[eidx])

    return lax.map(block, tok).reshape(shape)


def setup_inputs(seed: int = 0) -> dict:
    key = jax.random.key(seed)
    ks = jax.random.split(key, 32)

    def nrm(k, shape, s):
        return jax.random.normal(k, shape, F32) * s

    gate_i = nrm(ks[20], (N_ODD, 2, MLSTM_HEADS), 0.1)
    gate_f = 3.0 + 3.0 * jax.random.uniform(ks[21], (N_ODD, 2, MLSTM_HEADS), F32)
    mlstm_gate_b = jnp.stack([gate_i[:, 0], gate_f[:, 0], gate_i[:, 1], gate_f[:, 1]], axis=1)
    return {
        "x": nrm(ks[0], (BATCH, SEQ, D_MODEL), 1.0),
        "c": nrm(ks[1], (BATCH, D_MODEL), 1.0),
        "ctx": nrm(ks[2], (BATCH, CTX_LEN, D_MODEL), 1.0),
        "c_ctx": nrm(ks[3], (D_MODEL,), 1.0),
        "mod_w": nrm(ks[4], (DEPTH, D_MODEL, N_MOD * D_MODEL), 0.5 * D_MODEL ** -0.5),
        "mod_b": nrm(ks[5], (DEPTH, N_MOD * D_MODEL), 0.02),
        "ln_g": 1.0 + nrm(ks[6], (DEPTH, 2, D_MODEL), 0.02),
        "ln_b": nrm(ks[7], (DEPTH, 2, D_MODEL), 0.02),
        "ab_w_in": nrm(ks[8], (N_EVEN, D_MODEL, AB_IN), D_MODEL ** -0.5),
        "pool_w": nrm(ks[9], (N_EVEN, POOL_GROUPS, POOL_GW, POOL_GW), POOL_GW ** -0.5),
        "pool_ls": 1.0 + nrm(ks[10], (N_EVEN, D_POOL), 0.1),
        "conv_w": nrm(ks[11], (N_EVEN, CONV_W, D_CONV), CONV_W ** -0.5),
        "conv_b": nrm(ks[12], (N_EVEN, D_CONV), 0.02),
        "conv_ln_g": 1.0 + nrm(ks[13], (N_EVEN, D_CONV), 0.02),
        "conv_ln_b": nrm(ks[14], (N_EVEN, D_CONV), 0.02),
        "ab_w_out": nrm(ks[15], (N_EVEN, AB_MIX, D_MODEL), DN_BETA * AB_MIX ** -0.5),
        "cd_w_in": nrm(ks[16], (N_ODD, D_MODEL, CD_IN), D_MODEL ** -0.5),
        "mlstm_gate_b": mlstm_gate_b,
        "mlstm_norm_g": 1.0 + nrm(ks[17], (N_ODD, D_MLSTM), 0.02),
        "q_norm_g": 1.0 + nrm(ks[18], (N_ODD, ATT_DH), 0.02),
        "k_norm_g": 1.0 + nrm(ks[19], (N_ODD, ATT_DH), 0.02),
        "cd_w_out": nrm(ks[22], (N_ODD, CD_MIX, D_MODEL), DN_BETA * CD_MIX ** -0.5),
        "peer_w_q": nrm(ks[23], (DEPTH, D_MODEL, PEER_HEADS * PEER_DKEY), D_MODEL ** -0.5),
        "peer_keys": nrm(ks[24], (DEPTH, 2, PEER_NKEYS, PEER_DKEY // 2), (PEER_DKEY // 2) ** -0.5),
        "peer_u": nrm(ks[25], (DEPTH, PEER_N, D_MODEL), D_MODEL ** -0.5),
        "peer_v": nrm(ks[26], (DEPTH, PEER_N, D_MODEL), DN_BETA),
    }


def reference(x, c, ctx, c_ctx, mod_w, mod_b, ln_g, ln_b,
              ab_w_in, pool_w, pool_ls, conv_w, conv_b, conv_ln_g, conv_ln_b, ab_w_out,
              cd_w_in, mlstm_gate_b, mlstm_norm_g, q_norm_g, k_norm_g, cd_w_out,
              peer_w_q, peer_keys, peer_u, peer_v):
    n_lat = x.shape[1]
    ROWS = n_lat // GRID_W
    row = jnp.repeat(jnp.arange(ROWS), GRID_W)
    col = jnp.tile(jnp.arange(GRID_W), ROWS)
    rope = axial_rope_tables(row, col)
    x_l, x_c = x, ctx
    for l in range(DEPTH):
        ctx_out = l < DEPTH - 1
        j = l // 2
        mod_l = (jax.nn.silu(c) @ mod_w[l] + mod_b[l])[:, None, :]
        mod_c = (jax.nn.silu(c_ctx) @ mod_w[l] + mod_b[l])[None, None, :]
        sh1, sc1, g1, sh2, sc2, g2 = jnp.split(mod_l, N_MOD, axis=-1)
        csh1, csc1, cg1, csh2, csc2, cg2 = jnp.split(mod_c, N_MOD, axis=-1)
        h_l = x_l * (1 + sc1) + sh1
        if l % 2 == 0:
            ab = (ab_w_in[j], pool_w[j], pool_ls[j], conv_w[j], conv_b[j], conv_ln_g[j], conv_ln_b[j], ab_w_out[j])
            y_l = mixer_ab(h_l, *ab)
            y_c = mixer_ab(x_c * (1 + csc1) + csh1, *ab) if ctx_out else None
        else:
            y_l, y_c = mixer_cd(h_l, x_c * (1 + csc1) + csh1, cd_w_in[j], mlstm_gate_b[j], mlstm_norm_g[j],
                                q_norm_g[j], k_norm_g[j], cd_w_out[j], rope, ctx_out)
        x_l = layer_norm(DN_ALPHA * x_l + g1 * y_l, ln_g[l, 0], ln_b[l, 0])
        f_l = peer_ffn(x_l * (1 + sc2) + sh2, peer_w_q[l], peer_keys[l], peer_u[l], peer_v[l])
        x_l = layer_norm(DN_ALPHA * x_l + g2 * f_l, ln_g[l, 1], ln_b[l, 1])
        if ctx_out:
            x_c = layer_norm(DN_ALPHA * x_c + cg1 * y_c, ln_g[l, 0], ln_b[l, 0])
            f_c = peer_ffn(x_c * (1 + csc2) + csh2, peer_w_q[l], peer_keys[l], peer_u[l], peer_v[l])
            x_c = layer_norm(DN_ALPHA * x_c + cg2 * f_c, ln_g[l, 1], ln_b[l, 1])
    return x_l
```

```python
import numpy as np
import ml_dtypes
from contextlib import ExitStack
import concourse.bass as bass
import concourse.mybir as mybir
from concourse.bass_utils import run_bass_kernel_spmd

F32 = mybir.dt.float32
BF16 = mybir.dt.bfloat16
I32 = mybir.dt.int32
U32 = mybir.dt.uint32
AF = mybir.ActivationFunctionType
ALU = mybir.AluOpType
AX = mybir.AxisListType

ENG = {'pe': 'tensor', 'act': 'scalar', 'dve': 'vector', 'pool': 'gpsimd', 'sp': 'sync'}


class Prog:
    def __init__(self, nc, ndma=20):
        self.nc = nc
        self.stack = ExitStack()
        self.sem = {e: self.stack.enter_context(nc.semaphore(f"s_{e}")) for e in ENG}
        self.cnt = {e: 0 for e in ENG}
        self.ndma = ndma
        self.dsem = [self.stack.enter_context(nc.semaphore(f"d{i}")) for i in range(ndma)]
        self.dcnt = [0] * ndma
        self.dnext = 0
        self.seen = {e: {} for e in ENG}
        self.lastw = {}
        self.readers = {}
        self.pending = {e: [] for e in ENG}
        self.nops = 0

    def eng(self, e):
        return getattr(self.nc, ENG[e])

    def _wait(self, e, key, val):
        if val <= 0:
            return
        if self.seen[e].get(key, 0) >= val:
            return
        self.seen[e][key] = val
        sem = self.sem[key] if isinstance(key, str) else self.dsem[key]
        self.eng(e).wait_ge(sem, val)

    def _deps(self, e, reads, writes, sync_same):
        toks = {}
        for r in reads:
            t = self.lastw.get(r)
            if t is not None:
                toks[t[0]] = max(toks.get(t[0], 0), t[1])
        for w in writes:
            t = self.lastw.get(w)
            if t is not None:
                toks[t[0]] = max(toks.get(t[0], 0), t[1])
            for k, v in self.readers.get(w, {}).items():
                toks[k] = max(toks.get(k, 0), v)
        for k, v in toks.items():
            if k == e and not sync_same:
                continue
            self._wait(e, k, v)

    def _record(self, tok, reads, writes):
        for r in reads:
            d = self.readers.setdefault(r, {})
            d[tok[0]] = max(d.get(tok[0], 0), tok[1])
        for w in writes:
            self.lastw[w] = tok
            self.readers[w] = {}

    def op(self, e, fn, reads=(), writes=(), sync_same=None, inc=True):
        if sync_same is None:
            sync_same = (e != 'pe')
        self._deps(e, reads, writes, sync_same)
        ins = fn(self.eng(e))
        self.nops += 1
        if not inc:
            self.pending[e].append((tuple(reads), tuple(writes)))
            return ins
        self.cnt[e] += 1
        ins.then_inc(self.sem[e], 1)
        tok = (e, self.cnt[e])
        for (r, w) in self.pending[e]:
            self._record(tok, r, w)
        self.pending[e] = []
        self._record(tok, reads, writes)
        return ins

    def dma(self, e, out, in_, reads=(), writes=(), fn=None, **kw):
        s = self.dnext
        self.dnext = (s + 1) % self.ndma
        self._wait(e, s, self.dcnt[s] * 16)
        self._deps(e, reads, writes, True)
        if fn is None:
            ins = self.eng(e).dma_start(out=out, in_=in_, **kw)
        else:
            ins = fn(self.eng(e))
        self.nops += 1
        self.dcnt[s] += 1
        ins.then_inc(self.dsem[s], 16)
        tok = (s, self.dcnt[s] * 16)
        self._record(tok, reads, writes)
        return ins

    def barrier(self):
        for e in ENG:
            for s_ in range(self.ndma):
                self._wait(e, s_, self.dcnt[s_] * 16)
            for k in ENG:
                if k != e:
                    self._wait(e, k, self.cnt[k])

    def finish(self, e='sp'):
        for s in range(self.ndma):
            self._wait(e, s, self.dcnt[s] * 16)
        for k in ENG:
            if k != e:
                self._wait(e, k, self.cnt[k])
        self.stack.close()


D = 1024
KC = 8
NT = 256
HALO = 16
WT = NT + 2 * HALO
ALPHA = 4.0 ** 0.25
LN_EPS = 1e-5
POOL_WINDOWS = (2, 4, 8, 16)


class Ctx:
    def __init__(self, name="k"):
        self.nc = bass.Bass("TRN2", target_bir_lowering=False)
        self.P = Prog(self.nc)
        self.es = ExitStack()
        self.n = 0

    def sb(self, name, shape, dt=F32):
        return self.es.enter_context(self.nc.sbuf_tensor("s_" + name, list(shape), dt))

    def ps(self, name, shape, dt=F32):
        return self.es.enter_context(self.nc.psum_tensor("p_" + name, list(shape), dt))

    def din(self, name, shape, dt=F32):
        return self.nc.dram_tensor(name, list(shape), dt, kind="ExternalInput").ap()

    def dout(self, name, shape, dt=F32):
        return self.nc.dram_tensor(name, list(shape), dt, kind="ExternalOutput").ap()

    def close(self):
        self.P.finish('sp')
        self.es.close()


def fap(t, off, dims):
    fs = 1
    for s in t.shape[1:]:
        fs *= s
    return bass.AP(t, off, [[fs, t.shape[0]]] + [list(d) for d in dims])


def load_cast(K, dst_bf, src_ap, stage, rname, wname, eng='dve', q='sp'):
    P = K.P
    P.dma(q, stage, src_ap, writes=[rname])
    if eng == 'dve':
        P.op('dve', lambda e: e.tensor_copy(out=dst_bf, in_=stage), reads=[rname], writes=[wname])
    else:
        P.op('act', lambda e: e.activation(out=dst_bf, in_=stage, func=AF.Copy), reads=[rname], writes=[wname])


def mod_setup(K, cvec_d, modw_d, modb_d, j0, j1, psm, tmp=None):
    nc, P = K.nc, K.P
    nj = j1 - j0
    cv = K.sb("cv", [128, KC, 2])
    sc = K.sb("sc", [128, KC, 2])
    mb = K.sb("mb", [128, 48])
    modT = K.sb("modT", [128, nj, 2])
    tmp = tmp if tmp is not None else K.es
    mwblk = [tmp.enter_context(K.nc.sbuf_tensor(f"mwblk{i}", [128, KC, 512], F32)) for i in range(2)]
    P.dma('sp', cv[:], cvec_d, writes=['cv'])
    P.dma('sp', mb[:], modb_d, writes=['mb'])
    P.op('act', lambda e: e.activation(out=sc[:], in_=cv[:], func=AF.Silu), reads=['cv'], writes=['sc'])
    assert j0 % 4 == 0 and j1 % 4 == 0
    for bi, blk in enumerate(range(j0 // 4, j1 // 4)):
        buf = mwblk[bi % 2]
        bn = f"mwblk{bi % 2}"
        P.dma('sp', buf[:], modw_d[:, blk * 512:(blk + 1) * 512].rearrange("(k p) f -> p k f", p=128), writes=[bn])
        for jj in range(4):
            j = blk * 4 + jj - j0
            for k in range(KC):
                P.op('pe', lambda e, k=k, jj=jj, j=j: e.matmul(psm[:, 2 * j:2 * j + 2], lhsT=buf[:, k, jj * 128:(jj + 1) * 128],
                                                               rhs=sc[:, k, :], start=(k == 0), stop=(k == KC - 1)),
                     reads=[bn, 'sc'], writes=['psm'], inc=(k == KC - 1))
    P.op('dve', lambda e: e.tensor_tensor(out=modT[:], in0=psm[:, 0:2 * nj].rearrange("p (j c) -> p j c", c=2),
                                          in1=fap(mb, j0, [[1, nj], [0, 2]]), op=ALU.add),
         reads=['psm', 'mb'], writes=['modT'])
    return modT


def build_mixa(ntiles, mcols):
    K = Ctx()
    nc, P = K.nc, K.P
    xt_d = K.din("xtiles", [ntiles, D, WT])
    cm_d = K.din("cmask", [ntiles, 128, WT])
    rc_d = K.din("rcorr", [ntiles, 128, 4, NT])
    cvec_d = K.din("cvec", [128, KC, 2])
    modw_d = K.din("mod_w", [D, 6144])
    modb_d = K.din("mod_bT", [128, 48])
    win_d = K.din("w_in", [D, 1536])
    poolw_d = K.din("pool_w", [4, 128, 128])
    poolls_d = K.din("pool_lsT", [128, 4])
    convw_d = K.din("conv_wT", [128, 4, 31])
    convb_d = K.din("conv_bT", [128, 4])
    clng_d = K.din("cln_gT", [128, 4])
    clnb_d = K.din("cln_bT", [128, 4])
    wout_d = K.din("w_out", [D, D])
    lng_d = K.din("ln_gT", [128, KC])
    lnb_d = K.din("ln_bT", [128, KC])
    out_d = K.dout("x1T", [ntiles, D, NT])

    psA = [K.ps(f"psA{i}", [128, 512]) for i in range(2)]
    psp = K.ps("psp", [128, 512])
    psmean = K.ps("psmean", [128, 512])
    pssq = K.ps("pssq", [128, 512])
    psm = K.ps("psm", [128, 512])

    ones512 = K.sb("ones512", [128, 128])
    ones1024 = K.sb("ones1024", [128, 128])
    P.op('dve', lambda e: e.memset(ones512[:], 1.0 / 512), writes=['ones512'])
    P.op('dve', lambda e: e.memset(ones1024[:], 1.0 / 1024), writes=['ones1024'])
    small = {}
    for nm, d, shp in [("pls", poolls_d, [128, 4]), ("cw", convw_d, [128, 4, 31]), ("cb", convb_d, [128, 4]),
                       ("clg", clng_d, [128, 4]), ("clb", clnb_d, [128, 4]), ("lng", lng_d, [128, KC]),
                       ("lnb", lnb_d, [128, KC])]:
        t = K.sb(nm, shp)
        P.dma('sp', t[:], d, writes=[nm])
        small[nm] = t
    pls, cw, cb, clg, clb, lng, lnb = [small[n] for n in ("pls", "cw", "cb", "clg", "clb", "lng", "lnb")]

    modT = mod_setup(K, cvec_d, modw_d, modb_d, 0, 24, psm)
    sc1p = K.sb("sc1p", [128, KC, 2])
    g1a = K.sb("g1a", [128, KC, 2])
    P.op('dve', lambda e: e.tensor_scalar(out=sc1p[:], in0=modT[:, 8:16, :], scalar1=1.0, scalar2=None, op0=ALU.add),
         reads=['modT'], writes=['sc1p'])
    P.op('dve', lambda e: e.tensor_scalar(out=g1a[:], in0=modT[:, 16:24, :], scalar1=1.0 / ALPHA, scalar2=None, op0=ALU.mult),
         reads=['modT'], writes=['g1a'])

    stage = [K.sb(f"stage{i}", [128, 1536]) for i in range(2)]
    win_b = K.sb("win_b", [128, KC, 1536], BF16)
    wout_b = K.sb("wout_b", [128, KC, D], BF16)
    poolw_b = K.sb("poolw_b", [128, 4, 128], BF16)
    si = 0
    for k in range(KC):
        load_cast(K, win_b[:, k, :], win_d[k * 128:(k + 1) * 128, :], stage[si % 2][:, :], f"stage{si % 2}", "win_b",
                  eng='dve' if k % 2 == 0 else 'act')
        si += 1
    for k in range(KC):
        load_cast(K, wout_b[:, k, :], wout_d[k * 128:(k + 1) * 128, :], stage[si % 2][:, 0:D], f"stage{si % 2}", "wout_b",
                  eng='dve' if k % 2 == 0 else 'act')
        si += 1
    for g in range(4):
        load_cast(K, poolw_b[:, g, :], poolw_d[g], stage[si % 2][:, 0:128], f"stage{si % 2}", "poolw_b")
        si += 1

    xt = [K.sb(f"xt{i}", [128, KC, WT]) for i in range(2)]
    cm = [K.sb(f"cm{i}", [128, WT]) for i in range(2)]
    rc = [K.sb(f"rc{i}", [128, 4, NT]) for i in range(2)]
    hT = K.sb("hT", [128, KC, WT], BF16)
    zp = K.sb("zp", [128, 4, WT])
    ua = K.sb("ua", [128, 4, WT])
    sg = K.sb("sg", [128, WT])
    u = K.sb("u", [128, 4, WT])
    pa = K.sb("pa", [128, WT])
    pb = K.sb("pb", [128, WT])
    dif = K.sb("dif", [128, NT], BF16)
    yc = K.sb("yc", [128, 4, NT])
    sqt = [K.sb(f"sqt{i}", [128, NT]) for i in range(2)]
    mean_sb = K.sb("mean_sb", [128, NT])
    rstd = K.sb("rstd", [128, NT])
    yn = K.sb("yn", [128, NT])
    ycat = K.sb("ycat", [128, KC, NT], BF16)
    vt = K.sb("vt", [128, KC, NT])
    xo = [K.sb(f"xo{i}", [128, KC, NT]) for i in range(2)]

    def ln_stats(src, nchunk, ones, onm, srcname, eps):
        for c in range(nchunk):
            P.op('pe', lambda e, c=c: e.matmul(psmean[:, 0:NT], lhsT=ones[:], rhs=src[:, c, :], start=(c == 0), stop=(c == nchunk - 1)),
                 reads=[onm, srcname], writes=['psmean'], inc=(c == nchunk - 1))
        for c in range(nchunk):
            s = sqt[c % 2]
            sn = f"sqt{c % 2}"
            P.op('act', lambda e, c=c, s=s: e.activation(out=s[:], in_=src[:, c, :], func=AF.Square), reads=[srcname], writes=[sn])
            P.op('pe', lambda e, c=c, s=s: e.matmul(pssq[:, 0:NT], lhsT=ones[:], rhs=s[:], start=(c == 0), stop=(c == nchunk - 1)),
                 reads=[onm, sn], writes=['pssq'])
        P.op('act', lambda e: e.activation(out=mean_sb[:], in_=psmean[:, 0:NT], func=AF.Copy), reads=['psmean'], writes=['mean_sb'])
        P.op('dve', lambda e: e.tensor_tensor(out=rstd[:], in0=mean_sb[:], in1=mean_sb[:], op=ALU.mult), reads=['mean_sb'], writes=['rstd'])
        P.op('dve', lambda e: e.tensor_tensor(out=rstd[:], in0=pssq[:, 0:NT], in1=rstd[:], op=ALU.subtract), reads=['pssq', 'rstd'], writes=['rstd'])
        P.op('dve', lambda e: e.tensor_scalar(out=rstd[:], in0=rstd[:], scalar1=eps, scalar2=None, op0=ALU.add),
             reads=['rstd'], writes=['rstd'])
        P.op('act', lambda e: e.activation(out=rstd[:], in_=rstd[:], func=AF.Sqrt), reads=['rstd'], writes=['rstd'])
        P.op('dve', lambda e: e.reciprocal(out=rstd[:], in_=rstd[:]), reads=['rstd'], writes=['rstd'])

    for t in range(ntiles):
        col = mcols[t]
        b = t % 2
        X, CM, RC = xt[b], cm[b], rc[b]
        xn, cmn, rcn = f"xt{b}", f"cm{b}", f"rc{b}"
        P.dma('sp', X[:], xt_d[t].rearrange("(c p) n -> p c n", p=128), writes=[xn])
        P.dma('sp', CM[:], cm_d[t], writes=[cmn])
        P.dma('sp', RC[:], rc_d[t], writes=[rcn])
        for c in range(KC):
            P.op('act', lambda e, c=c: e.activation(out=hT[:, c, :], in_=X[:, c, :], func=AF.Identity,
                                                    scale=sc1p[:, c, col:col + 1], bias=modT[:, c, col:col + 1]),
                 reads=[xn, 'sc1p', 'modT'], writes=['hT'])
        for oc in range(12):
            ps = psA[oc % 2]
            pn = f"psA{oc % 2}"
            for k in range(KC):
                P.op('pe', lambda e, k=k, oc=oc, ps=ps: e.matmul(ps[:, 0:WT], lhsT=win_b[:, k, oc * 128:(oc + 1) * 128], rhs=hT[:, k, :],
                                                                 start=(k == 0), stop=(k == KC - 1)),
                     reads=['win_b', 'hT'], writes=[pn], inc=(k == KC - 1))
            if oc < 4:
                P.op('dve', lambda e, oc=oc, ps=ps: e.tensor_tensor(out=zp[:, oc, :], in0=ps[:, 0:WT], in1=CM[:], op=ALU.mult),
                     reads=[pn, cmn], writes=['zp'])
            elif oc < 8:
                P.op('act', lambda e, oc=oc, ps=ps: e.activation(out=ua[:, oc - 4, :], in_=ps[:, 0:WT], func=AF.Copy),
                     reads=[pn], writes=['ua'])
            else:
                P.op('act', lambda e, ps=ps: e.activation(out=sg[:], in_=ps[:, 0:WT], func=AF.Sigmoid), reads=[pn], writes=['sg'])
                P.op('dve', lambda e: e.tensor_tensor(out=sg[:], in0=sg[:], in1=CM[:], op=ALU.mult), reads=['sg', cmn], writes=['sg'])
                P.op('dve', lambda e, oc=oc: e.tensor_tensor(out=u[:, oc - 8, :], in0=ua[:, oc - 8, :], in1=sg[:], op=ALU.mult),
                     reads=['ua', 'sg'], writes=['u'])
        for g in range(4):
            src = zp[:, g, :]
            srcn = 'zp'
            lo, hi = 0, WT
            bufs = [(pa, 'pa'), (pb, 'pb')]
            dst, dn = bufs[0]
            P.op('dve', lambda e, dst=dst, g=g: e.tensor_tensor(out=dst[:, 1:WT], in0=zp[:, g, 0:WT - 1], in1=zp[:, g, 1:WT], op=ALU.add),
                 reads=['zp'], writes=[dn])
            cur, cn = dst, dn
            lo, hi = 1, WT
            sh = 1
            for lvl in range(g):
                dst, dn = bufs[(lvl + 1) % 2]
                nlo, nhi = lo + sh, hi - sh
                P.op('dve', lambda e, dst=dst, cur=cur, nlo=nlo, nhi=nhi, sh=sh: e.tensor_tensor(
                    out=dst[:, nlo:nhi], in0=cur[:, nlo - sh:nhi - sh], in1=cur[:, nlo + sh:nhi + sh], op=ALU.add),
                    reads=[cn], writes=[dn])
                cur, cn = dst, dn
                lo, hi = nlo, nhi
                sh *= 2
            assert lo <= HALO and hi >= HALO + NT
            oth, on = bufs[0] if cur is bufs[1][0] else bufs[1]
            P.op('dve', lambda e, cur=cur, oth=oth, g=g: e.tensor_tensor(out=oth[:, 0:NT], in0=cur[:, HALO:HALO + NT], in1=RC[:, g, :], op=ALU.mult),
                 reads=[cn, rcn], writes=[on])
            P.op('dve', lambda e, oth=oth, g=g: e.tensor_tensor(out=dif[:], in0=oth[:, 0:NT], in1=zp[:, g, HALO:HALO + NT], op=ALU.subtract),
                 reads=[on, 'zp'], writes=['dif'])
            P.op('pe', lambda e, g=g: e.matmul(psp[:, 0:NT], lhsT=poolw_b[:, g, :], rhs=dif[:], start=True, stop=True),
                 reads=['poolw_b', 'dif'], writes=['psp'])
            P.op('act', lambda e, g=g: e.activation(out=ycat[:, g, :], in_=psp[:, 0:NT], func=AF.Copy, scale=pls[:, g:g + 1]),
                 reads=['psp', 'pls'], writes=['ycat'])
        for c in range(4):
            P.op('dve', lambda e, c=c: e.tensor_scalar(out=yc[:, c, :], in0=u[:, c, 1:1 + NT], scalar1=cw[:, c, 0:1], scalar2=cb[:, c:c + 1],
                                                       op0=ALU.mult, op1=ALU.add), reads=['u', 'cw', 'cb'], writes=['yc'])
            for k in range(1, 31):
                P.op('dve', lambda e, c=c, k=k: e.scalar_tensor_tensor(out=yc[:, c, :], in0=u[:, c, k + 1:k + 1 + NT], scalar=cw[:, c, k:k + 1],
                                                                      in1=yc[:, c, :], op0=ALU.mult, op1=ALU.add),
                     reads=['u', 'cw', 'yc'], writes=['yc'])
        ln_stats(yc, 4, ones512, 'ones512', 'yc', LN_EPS)
        for c in range(4):
            P.op('dve', lambda e, c=c: e.tensor_tensor(out=yn[:], in0=yc[:, c, :], in1=mean_sb[:], op=ALU.subtract),
                 reads=['yc', 'mean_sb'], writes=['yn'])
            P.op('dve', lambda e: e.tensor_tensor(out=yn[:], in0=yn[:], in1=rstd[:], op=ALU.mult), reads=['yn', 'rstd'], writes=['yn'])
            P.op('act', lambda e, c=c: e.activation(out=ycat[:, 4 + c, :], in_=yn[:], func=AF.Silu, scale=clg[:, c:c + 1], bias=clb[:, c:c + 1]),
                 reads=['yn', 'clg', 'clb'], writes=['ycat'])
        for oc in range(KC):
            ps = psA[oc % 2]
            pn = f"psA{oc % 2}"
            for k in range(KC):
                P.op('pe', lambda e, k=k, oc=oc, ps=ps: e.matmul(ps[:, 0:NT], lhsT=wout_b[:, k, oc * 128:(oc + 1) * 128], rhs=ycat[:, k, :],
                                                                 start=(k == 0), stop=(k == KC - 1)),
                     reads=['wout_b', 'ycat'], writes=[pn], inc=(k == KC - 1))
            P.op('dve', lambda e, oc=oc, ps=ps: e.scalar_tensor_tensor(out=vt[:, oc, :], in0=ps[:, 0:NT], scalar=g1a[:, oc, col:col + 1],
                                                                      in1=X[:, oc, HALO:HALO + NT], op0=ALU.mult, op1=ALU.add),
                 reads=[pn, 'g1a', xn], writes=['vt'])
        ln_stats(vt, KC, ones1024, 'ones1024', 'vt', LN_EPS / (ALPHA * ALPHA))
        XO = xo[b]
        xon = f"xo{b}"
        for oc in range(KC):
            P.op('dve', lambda e, oc=oc: e.tensor_tensor(out=vt[:, oc, :], in0=vt[:, oc, :], in1=mean_sb[:], op=ALU.subtract),
                 reads=['vt', 'mean_sb'], writes=['vt'])
            P.op('dve', lambda e, oc=oc: e.tensor_tensor(out=vt[:, oc, :], in0=vt[:, oc, :], in1=rstd[:], op=ALU.mult),
                 reads=['vt', 'rstd'], writes=['vt'])
            P.op('act', lambda e, oc=oc: e.activation(out=XO[:, oc, :], in_=vt[:, oc, :], func=AF.Identity, scale=lng[:, oc:oc + 1], bias=lnb[:, oc:oc + 1]),
                 reads=['vt', 'lng', 'lnb'], writes=[xon])
        P.dma('sp', out_d[t].rearrange("(c p) n -> p c n", p=128), XO[:], reads=[xon], writes=['out'])
    K.close()
    return nc


NSLOT = 128
NEG = -1.0e30


def build_peer(nsub, mcols):
    K = Ctx()
    nc, P = K.nc, K.P
    x1T_d = K.din("x1T", [nsub, D, 128])
    x1_d = K.din("x1", [nsub, 128, D])
    cvec_d = K.din("cvec", [128, KC, 2])
    modw_d = K.din("mod_w", [D, 6144])
    modb_d = K.din("mod_bT", [128, 48])
    wq_d = K.din("w_q", [D, 2048])
    keysT_d = K.din("keysT", [128, 2, 128])
    U_d = K.din("peer_u", [16384, D])
    V_d = K.din("peer_v", [16384, D])
    lng_d = K.din("ln_g", [1, D])
    lnb_d = K.din("ln_b", [1, D])
    ident_d = K.din("ident", [128, 128])
    iota_d = K.din("iota16", [128, 16])
    out_d = K.dout("x2", [nsub, 128, D])

    psq = [K.ps(f"psq{i}", [128, 512]) for i in range(2)]
    psS = K.ps("psS", [128, 2048])
    psm = K.ps("psm", [128, 512])
    psr = K.ps("psr", [128, 512])

    ident = K.sb("ident", [128, 128])
    iota16 = K.sb("iota16", [128, 16])
    ones = K.sb("ones", [128, 128])
    P.dma('sp', ident[:], ident_d, writes=['ident'])
    P.dma('sp', iota16[:], iota_d, writes=['iota16'])
    P.op('dve', lambda e: e.memset(ones[:], 1.0), writes=['ones'])
    lng = K.sb("lng", [128, D])
    lnb = K.sb("lnb", [128, D])
    P.dma('sp', lng[:], bass.AP(lng_d.tensor, 0, [[0, 128], [1, D]]), writes=['lng'])
    P.dma('sp', lnb[:], bass.AP(lnb_d.tensor, 0, [[0, 128], [1, D]]), writes=['lnb'])

    tmp = ExitStack()
    sc2p = K.sb("sc2p", [128, KC, 2])
    g2a = K.sb("g2a", [128, KC, 2])
    dg = K.sb("dg", [128, 128])
    reps = {(vn, col): K.sb(f"rep_{vn}{col}", [128, D]) for vn in ("sh2", "sc2p", "g2a") for col in range(2)}
    wq_b = K.sb("wq_b", [128, KC, 2048], BF16)
    keysT = K.sb("keysT", [128, 2, 128], BF16)
    modT = mod_setup(K, cvec_d, modw_d, modb_d, 24, 48, psm, tmp=tmp)
    stage = [tmp.enter_context(nc.sbuf_tensor(f"stage{i}", [128, 2048], F32)) for i in range(2)]
    P.op('dve', lambda e: e.tensor_scalar(out=sc2p[:], in0=modT[:, 8:16, :], scalar1=1.0, scalar2=None, op0=ALU.add),
         reads=['modT'], writes=['sc2p'])
    P.op('dve', lambda e: e.tensor_scalar(out=g2a[:], in0=modT[:, 16:24, :], scalar1=1.0 / ALPHA, scalar2=None, op0=ALU.mult),
         reads=['modT'], writes=['g2a'])
    rep = {}
    for vn, src, j0 in (("sh2", modT, 0), ("sc2p", sc2p, 0), ("g2a", g2a, 0)):
        for col in range(2):
            r = reps[(vn, col)]
            for c in range(KC):
                P.op('dve', lambda e, c=c, src=src, col=col: e.tensor_scalar(out=dg[:], in0=ident[:], scalar1=src[:, j0 + c, col:col + 1], scalar2=None, op0=ALU.mult),
                     reads=['ident', 'modT', 'sc2p', 'g2a'], writes=['dg'])
                P.op('pe', lambda e: e.matmul(psr[:, 0:128], lhsT=ones[:], rhs=dg[:], start=True, stop=True), reads=['ones', 'dg'], writes=['psr'])
                P.op('act', lambda e, c=c, r=r: e.activation(out=r[:, c * 128:(c + 1) * 128], in_=psr[:, 0:128], func=AF.Copy),
                     reads=['psr'], writes=['rep'])
            rep[(vn, col)] = r

    for k in range(KC):
        load_cast(K, wq_b[:, k, :], wq_d[k * 128:(k + 1) * 128, :], stage[k % 2][:], f"stage{k % 2}", "wq_b", eng='dve' if k % 2 == 0 else 'act')
    load_cast(K, keysT[:], keysT_d, stage[0][:, 0:256].rearrange("p (a b) -> p a b", a=2), "stage0", "keysT")
    P.barrier()
    tmp.close()

    x1T = [K.sb(f"x1T{i}", [128, KC, 128]) for i in range(2)]
    x1t = [K.sb(f"x1t{i}", [128, D]) for i in range(2)]
    h2T = K.sb("h2T", [128, KC, 128], BF16)
    qT = K.sb("qT", [128, 16, 128], BF16)
    S = K.sb("S", [128, 16, 128])
    S2 = K.sb("S2", [128, 16, 128])
    sv = K.sb("sv", [128, 16, 16])
    si = K.sb("si", [128, 16, 16], U32)
    sif = K.sb("sif", [128, 16, 16])
    cand = K.sb("cand", [128, 8, 256])
    cand2 = K.sb("cand2", [128, 8, 256])
    ts = K.sb("ts", [128, 8, 16])
    pos = K.sb("pos", [128, 8, 16], U32)
    pa = K.sb("pa_", [128, 8, 16], U32)
    pbb = K.sb("pb_", [128, 8, 16], U32)
    af = K.sb("af", [128, 8, 16])
    bf = K.sb("bf", [128, 8, 16])
    oh = K.sb("oh", [128, 8, 16, 16])
    isel = K.sb("isel", [128, 8, 16])
    jsel = K.sb("jsel", [128, 8, 16])
    eidf = K.sb("eidf", [128, 128])
    eidx = K.sb("eidx", [128, 128], I32)
    ex = K.sb("ex", [128, 8, 16])
    Z = K.sb("Z", [128, 8])
    gate = K.sb("gate", [128, 128])
    A = K.sb("A", [128, 128])
    wgt = K.sb("wgt", [128, 128])
    h2t = K.sb("h2t", [128, D])
    NB = 4
    rows = [K.sb(f"rows{i}", [128, D]) for i in range(NB)]
    acc = K.sb("acc", [128, D])
    stats = K.sb("stats", [128, 2, 6])
    mv = K.sb("mv", [128, 2])
    rs = K.sb("rs", [128, 1])
    xo = [K.sb(f"xo{i}", [128, D]) for i in range(2)]
    rbi = 0

    for s in range(nsub):
        col = mcols[s]
        b = s % 2
        XT, XK = x1T[b], x1t[b]
        xtn, xkn = f"x1T{b}", f"x1t{b}"
        P.dma('sp', XT[:], x1T_d[s].rearrange("(c p) n -> p c n", p=128), writes=[xtn])
        P.dma('sp', XK[:], x1_d[s], writes=[xkn])
        for c in range(KC):
            P.op('act', lambda e, c=c: e.activation(out=h2T[:, c, :], in_=XT[:, c, :], func=AF.Identity,
                                                    scale=sc2p[:, c, col:col + 1], bias=modT[:, c, col:col + 1]),
                 reads=[xtn, 'sc2p', 'modT'], writes=['h2T'])
        for hp in range(16):
            ps = psq[hp % 2]
            pn = f"psq{hp % 2}"
            for k in range(KC):
                P.op('pe', lambda e, k=k, hp=hp, ps=ps: e.matmul(ps[:, 0:128], lhsT=wq_b[:, k, hp * 128:(hp + 1) * 128], rhs=h2T[:, k, :],
                                                                 start=(k == 0), stop=(k == KC - 1)),
                     reads=['wq_b', 'h2T'], writes=[pn], inc=(k == KC - 1))
            if hp % 2 == 0:
                P.op('act', lambda e, hp=hp, ps=ps: e.activation(out=qT[:, hp, :], in_=ps[:, 0:128], func=AF.Copy), reads=[pn], writes=['qT'])
            else:
                P.op('dve', lambda e, hp=hp, ps=ps: e.tensor_copy(out=qT[:, hp, :], in_=ps[:, 0:128]), reads=[pn], writes=['qT'])
        for hp in range(16):
            P.op('pe', lambda e, hp=hp: e.matmul(psS[:, hp * 128:(hp + 1) * 128], lhsT=qT[:, hp, :], rhs=keysT[:, hp % 2, :], start=True, stop=True),
                 reads=['qT', 'keysT'], writes=['psS'], inc=(hp == 15))
        for q4 in range(4):
            P.op('act', lambda e, q4=q4: e.activation(out=S[:, 4 * q4:4 * q4 + 4, :], in_=psS[:, 512 * q4:512 * q4 + 512].rearrange("p (a b) -> p a b", a=4), func=AF.Copy),
                 reads=['psS'], writes=['S'])
        for hp in range(16):
            P.op('dve', lambda e, hp=hp: e.max(out=sv[:, hp, 0:8], in_=S[:, hp, :]), reads=['S'], writes=['sv'])
            P.op('dve', lambda e, hp=hp: e.max_index(out=si[:, hp, 0:8], in_max=sv[:, hp, 0:8], in_values=S[:, hp, :]), reads=['S', 'sv'], writes=['si'])
            P.op('dve', lambda e, hp=hp: e.match_replace(out=S2[:, hp, :], in_to_replace=sv[:, hp, 0:8], in_values=S[:, hp, :], imm_value=NEG),
                 reads=['S', 'sv'], writes=['S2'])
            P.op('dve', lambda e, hp=hp: e.max(out=sv[:, hp, 8:16], in_=S2[:, hp, :]), reads=['S2'], writes=['sv'])
            P.op('dve', lambda e, hp=hp: e.max_index(out=si[:, hp, 8:16], in_max=sv[:, hp, 8:16], in_values=S2[:, hp, :]), reads=['S2', 'sv'], writes=['si'])
        P.op('dve', lambda e: e.tensor_copy(out=sif[:], in_=si[:]), reads=['si'], writes=['sif'])
        P.op('dve', lambda e: e.tensor_tensor(out=cand[:].rearrange("p h (a b) -> p h a b", a=16),
                                              in0=fap(sv, 0, [[32, 8], [1, 16], [0, 16]]), in1=fap(sv, 16, [[32, 8], [0, 16], [1, 16]]), op=ALU.add),
             reads=['sv'], writes=['cand'])
        for h in range(8):
            P.op('dve', lambda e, h=h: e.max(out=ts[:, h, 0:8], in_=cand[:, h, :]), reads=['cand'], writes=['ts'])
            P.op('dve', lambda e, h=h: e.max_index(out=pos[:, h, 0:8], in_max=ts[:, h, 0:8], in_values=cand[:, h, :]), reads=['cand', 'ts'], writes=['pos'])
            P.op('dve', lambda e, h=h: e.match_replace(out=cand2[:, h, :], in_to_replace=ts[:, h, 0:8], in_values=cand[:, h, :], imm_value=NEG),
                 reads=['cand', 'ts'], writes=['cand2'])
            P.op('dve', lambda e, h=h: e.max(out=ts[:, h, 8:16], in_=cand2[:, h, :]), reads=['cand2'], writes=['ts'])
            P.op('dve', lambda e, h=h: e.max_index(out=pos[:, h, 8:16], in_max=ts[:, h, 8:16], in_values=cand2[:, h, :]), reads=['cand2', 'ts'], writes=['pos'])
        P.op('dve', lambda e: e.tensor_single_scalar(out=pa[:], in_=pos[:], scalar=4, op=ALU.logical_shift_right), reads=['pos'], writes=['pa'])
        P.op('dve', lambda e: e.tensor_single_scalar(out=pbb[:], in_=pos[:], scalar=15, op=ALU.bitwise_and), reads=['pos'], writes=['pb'])
        P.op('dve', lambda e: e.tensor_copy(out=af[:], in_=pa[:]), reads=['pa'], writes=['af'])
        P.op('dve', lambda e: e.tensor_copy(out=bf[:], in_=pbb[:]), reads=['pb'], writes=['bf'])
        for (src, p_, dst, dn) in ((af, 0, isel, 'isel'), (bf, 1, jsel, 'jsel')):
            P.op('dve', lambda e, src=src: e.tensor_tensor(out=oh[:], in0=fap(src, 0, [[16, 8], [1, 16], [0, 16]]),
                                                           in1=fap(iota16, 0, [[0, 8], [0, 16], [1, 16]]), op=ALU.is_equal),
                 reads=['af', 'bf', 'iota16'], writes=['oh'])
            P.op('dve', lambda e, p_=p_: e.tensor_tensor(out=oh[:], in0=oh[:], in1=fap(sif, 16 * p_, [[32, 8], [0, 16], [1, 16]]), op=ALU.mult),
                 reads=['oh', 'sif'], writes=['oh'])
            P.op('dve', lambda e, dst=dst: e.tensor_reduce(out=dst[:], in_=oh[:], axis=AX.X, op=ALU.add), reads=['oh'], writes=[dn])
        P.op('dve', lambda e: e.scalar_tensor_tensor(out=eidf[:], in0=isel[:].rearrange("p h k -> p (h k)"), scalar=128.0,
                                                     in1=jsel[:].rearrange("p h k -> p (h k)"), op0=ALU.mult, op1=ALU.add),
             reads=['isel', 'jsel'], writes=['eidf'])
        P.op('dve', lambda e: e.tensor_copy(out=eidx[:], in_=eidf[:]), reads=['eidf'], writes=['eidx'])
        P.op('dve', lambda e: e.tensor_tensor(out=ex[:], in0=ts[:], in1=fap(ts, 0, [[16, 8], [0, 16]]), op=ALU.subtract), reads=['ts'], writes=['ex'])
        P.op('act', lambda e: e.activation(out=ex[:], in_=ex[:], func=AF.Exp), reads=['ex'], writes=['ex'])
        P.op('dve', lambda e: e.tensor_reduce(out=Z[:], in_=ex[:], axis=AX.X, op=ALU.add), reads=['ex'], writes=['Z'])
        P.op('dve', lambda e: e.reciprocal(out=Z[:], in_=Z[:]), reads=['Z'], writes=['Z'])
        P.op('dve', lambda e: e.tensor_tensor(out=gate[:].rearrange("p (h k) -> p h k", h=8), in0=ex[:], in1=fap(Z, 0, [[1, 8], [0, 16]]), op=ALU.mult),
             reads=['ex', 'Z'], writes=['gate'])
        P.op('dve', lambda e: e.tensor_tensor(out=h2t[:], in0=XK[:], in1=rep[("sc2p", col)][:], op=ALU.mult), reads=[xkn, 'rep'], writes=['h2t'])
        P.op('dve', lambda e: e.tensor_tensor(out=h2t[:], in0=h2t[:], in1=rep[("sh2", col)][:], op=ALU.add), reads=['h2t', 'rep'], writes=['h2t'])
        for slot in range(NSLOT):
            r = rows[rbi % NB]
            rn = f"rows{rbi % NB}"
            rbi += 1
            P.dma('pool', None, None, reads=['eidx'], writes=[rn],
                  fn=lambda e, r=r, slot=slot: e.indirect_dma_start(out=r[:], out_offset=None, in_=U_d,
                                                                     in_offset=bass.IndirectOffsetOnAxis(ap=eidx[:, slot:slot + 1], axis=0)))
            P.op('dve', lambda e, r=r, slot=slot: e.scalar_tensor_tensor(out=r[:], in0=r[:], scalar=1.0, in1=h2t[:], op0=ALU.mult, op1=ALU.mult,
                                                                         accum_out=A[:, slot:slot + 1]),
                 reads=[rn, 'h2t'], writes=[rn, 'A'])
        P.op('act', lambda e: e.activation(out=wgt[:], in_=A[:], func=AF.Gelu), reads=['A'], writes=['wgt'])
        P.op('dve', lambda e: e.tensor_tensor(out=wgt[:], in0=wgt[:], in1=gate[:], op=ALU.mult), reads=['wgt', 'gate'], writes=['wgt'])
        for slot in range(NSLOT):
            r = rows[rbi % NB]
            rn = f"rows{rbi % NB}"
            rbi += 1
            P.dma('pool', None, None, reads=['eidx'], writes=[rn],
                  fn=lambda e, r=r, slot=slot: e.indirect_dma_start(out=r[:], out_offset=None, in_=V_d,
                                                                     in_offset=bass.IndirectOffsetOnAxis(ap=eidx[:, slot:slot + 1], axis=0)))
            if slot == 0:
                P.op('dve', lambda e, r=r: e.tensor_scalar(out=acc[:], in0=r[:], scalar1=wgt[:, 0:1], scalar2=None, op0=ALU.mult),
                     reads=[rn, 'wgt'], writes=['acc'])
            else:
                P.op('dve', lambda e, r=r, slot=slot: e.scalar_tensor_tensor(out=acc[:], in0=r[:], scalar=wgt[:, slot:slot + 1], in1=acc[:], op0=ALU.mult, op1=ALU.add),
                     reads=[rn, 'wgt', 'acc'], writes=['acc'])
        P.op('dve', lambda e: e.tensor_tensor(out=acc[:], in0=acc[:], in1=rep[("g2a", col)][:], op=ALU.mult), reads=['acc', 'rep'], writes=['acc'])
        P.op('dve', lambda e: e.tensor_tensor(out=acc[:], in0=acc[:], in1=XK[:], op=ALU.add), reads=['acc', xkn], writes=['acc'])
        for hf in range(2):
            P.op('dve', lambda e, hf=hf: e.bn_stats(out=stats[:, hf, :], in_=acc[:, hf * 512:(hf + 1) * 512]), reads=['acc'], writes=['stats'])
        P.op('dve', lambda e: e.bn_aggr(out=mv[:], in_=stats[:].rearrange("p a b -> p (a b)")), reads=['stats'], writes=['mv'])
        P.op('dve', lambda e: e.tensor_scalar(out=rs[:], in0=mv[:, 1:2], scalar1=LN_EPS / (ALPHA * ALPHA), scalar2=None, op0=ALU.add), reads=['mv'], writes=['rs'])
        P.op('act', lambda e: e.activation(out=rs[:], in_=rs[:], func=AF.Sqrt), reads=['rs'], writes=['rs'])
        P.op('dve', lambda e: e.reciprocal(out=rs[:], in_=rs[:]), reads=['rs'], writes=['rs'])
        XO = xo[b]
        xon = f"xo{b}"
        P.op('dve', lambda e: e.tensor_scalar(out=XO[:], in0=acc[:], scalar1=mv[:, 0:1], scalar2=rs[:, 0:1], op0=ALU.subtract, op1=ALU.mult),
             reads=['acc', 'mv', 'rs'], writes=[xon])
        P.op('dve', lambda e: e.tensor_tensor(out=XO[:], in0=XO[:], in1=lng[:], op=ALU.mult), reads=[xon, 'lng'], writes=[xon])
        P.op('dve', lambda e: e.tensor_tensor(out=XO[:], in0=XO[:], in1=lnb[:], op=ALU.add), reads=[xon, 'lnb'], writes=[xon])
        P.dma('sp', out_d[s], XO[:], reads=[xon], writes=['out'])
    K.close()
    return nc


CDW = 2832
RMS_EPS = 1e-6


def build_l1a(ntiles, mcols):
    K = Ctx()
    nc, P = K.nc, K.P
    xt_d = K.din("xtiles", [ntiles, D, NT])
    cos_d = K.din("cosT", [ntiles, 64, NT])
    sin_d = K.din("sinT", [ntiles, 64, NT])
    cvec_d = K.din("cvec", [128, KC, 2])
    modw_d = K.din("mod_w", [D, 6144])
    modb_d = K.din("mod_bT", [128, 48])
    win_d = K.din("w_in", [D, CDW])
    gb_d = K.din("gate_bT", [16, 1])
    qg_d = K.din("qn_gT", [64, 1])
    kg_d = K.din("kn_gT", [64, 1])
    pm_d = K.din("ropeP", [64, 64])
    qm_o = K.dout("qmT", [ntiles, 4, 128, NT], BF16)
    km_o = K.dout("kmT", [ntiles, 4, 128, NT], BF16)
    vm_o = K.dout("vmT", [ntiles, 4, 128, NT], BF16)
    om_o = K.dout("omT", [ntiles, 4, 128, NT], F32)
    qa_o = K.dout("qaT", [ntiles, 8, 64, NT], BF16)
    ka_o = K.dout("kaT", [ntiles, 2, 64, NT], BF16)
    va_o = K.dout("vaT", [ntiles, 2, 64, NT], BF16)
    gt_o = K.dout("gT", [ntiles, 16, NT], F32)

    psA = [K.ps(f"psA{i}", [128, 512]) for i in range(2)]
    psr = K.ps("psr", [128, 512])
    psw = K.ps("psw", [128, 512])
    psm = K.ps("psm", [128, 512])

    ones64 = K.sb("ones64", [64, 64])
    P.op('dve', lambda e: e.memset(ones64[:], 1.0 / 64), writes=['ones64'])
    gb = K.sb("gb", [16, 1]); qg = K.sb("qg", [64, 1]); kg = K.sb("kg", [64, 1]); pm = K.sb("pm", [64, 64])
    for t_, d_, n_ in ((gb, gb_d, 'gb'), (qg, qg_d, 'qg'), (kg, kg_d, 'kg'), (pm, pm_d, 'pm')):
        P.dma('sp', t_[:], d_, writes=[n_])
    sc1p = K.sb("sc1p", [128, KC, 2])
    win_b = K.sb("win_b", [128, KC, CDW], BF16)
    tmp = ExitStack()
    modT = mod_setup(K, cvec_d, modw_d, modb_d, 0, 16, psm, tmp=tmp)
    stage = [tmp.enter_context(nc.sbuf_tensor(f"stage{i}", [128, CDW], F32)) for i in range(2)]
    P.op('dve', lambda e: e.tensor_scalar(out=sc1p[:], in0=modT[:, 8:16, :], scalar1=1.0, scalar2=None, op0=ALU.add),
         reads=['modT'], writes=['sc1p'])
    for k in range(KC):
        load_cast(K, win_b[:, k, :], win_d[k * 128:(k + 1) * 128, :], stage[k % 2][:], f"stage{k % 2}", "win_b", eng='dve' if k % 2 == 0 else 'act')
    P.barrier()
    tmp.close()

    xt = [K.sb(f"xt{i}", [128, KC, NT]) for i in range(2)]
    cs = [K.sb(f"cs{i}", [64, NT]) for i in range(2)]
    sn = [K.sb(f"sn{i}", [64, NT]) for i in range(2)]
    hT = K.sb("hT", [128, KC, NT], BF16)
    ob = [K.sb(f"ob{i}", [128, NT], BF16) for i in range(4)]
    of = [K.sb(f"of{i}", [128, NT], F32) for i in range(2)]
    sq = K.sb("sq", [64, NT]); rstd = K.sb("rstd", [64, NT]); qn = K.sb("qn", [64, NT]); t1 = K.sb("t1", [64, NT]); t2 = K.sb("t2", [64, NT])
    obi = 0
    ofi = 0

    def proj(c0, M, ps, pn):
        for k in range(KC):
            P.op('pe', lambda e, k=k: e.matmul(ps[0:M, 0:NT], lhsT=win_b[:, k, c0:c0 + M], rhs=hT[:, k, :], start=(k == 0), stop=(k == KC - 1)),
                 reads=['win_b', 'hT'], writes=[pn], inc=(k == KC - 1))

    for t in range(ntiles):
        col = mcols[t]
        b = t % 2
        X, CS, SN = xt[b], cs[b], sn[b]
        xn, csn, snn = f"xt{b}", f"cs{b}", f"sn{b}"
        P.dma('sp', X[:], xt_d[t].rearrange("(c p) n -> p c n", p=128), writes=[xn])
        P.dma('sp', CS[:], cos_d[t], writes=[csn])
        P.dma('sp', SN[:], sin_d[t], writes=[snn])
        for c in range(KC):
            P.op('act', lambda e, c=c: e.activation(out=hT[:, c, :], in_=X[:, c, :], func=AF.Identity,
                                                    scale=sc1p[:, c, col:col + 1], bias=modT[:, c, col:col + 1]),
                 reads=[xn, 'sc1p', 'modT'], writes=['hT'])
        for oc in range(16):
            ps = psA[oc % 2]; pn = f"psA{oc % 2}"
            proj(oc * 128, 128, ps, pn)
            grp, hh = oc // 4, oc % 4
            if grp == 3:
                o = of[ofi % 2]; on = f"of{ofi % 2}"; ofi += 1
                P.op('act', lambda e, o=o, ps=ps: e.activation(out=o[:], in_=ps[:, 0:NT], func=AF.Copy), reads=[pn], writes=[on])
                P.dma('sp', om_o[t, hh], o[:], reads=[on], writes=['om_o'])
            else:
                o = ob[obi % 4]; on = f"ob{obi % 4}"; obi += 1
                scale = (128.0 ** -0.5) if grp == 1 else 1.0
                if oc % 2 == 0:
                    P.op('act', lambda e, o=o, ps=ps, scale=scale: e.activation(out=o[:], in_=ps[:, 0:NT], func=AF.Copy, scale=scale), reads=[pn], writes=[on])
                else:
                    P.op('dve', lambda e, o=o, ps=ps, scale=scale: e.tensor_scalar(out=o[:], in0=ps[:, 0:NT], scalar1=scale, scalar2=None, op0=ALU.mult), reads=[pn], writes=[on])
                dst = (qm_o, km_o, vm_o)[grp]
                P.dma('sp', dst[t, hh], o[:], reads=[on], writes=['qkv_o'])
        for hd in range(10):
            ps = psA[hd % 2]; pn = f"psA{hd % 2}"
            proj(2048 + hd * 64, 64, ps, pn)
            gvec, gname = (qg, 'qg') if hd < 8 else (kg, 'kg')
            P.op('act', lambda e, ps=ps: e.activation(out=sq[:], in_=ps[0:64, 0:NT], func=AF.Square), reads=[pn], writes=['sq'])
            P.op('pe', lambda e: e.matmul(psr[0:64, 0:NT], lhsT=ones64[:], rhs=sq[:], start=True, stop=True), reads=['ones64', 'sq'], writes=['psr'])
            P.op('dve', lambda e: e.tensor_scalar(out=rstd[:], in0=psr[0:64, 0:NT], scalar1=RMS_EPS, scalar2=None, op0=ALU.add), reads=['psr'], writes=['rstd'])
            P.op('act', lambda e: e.activation(out=rstd[:], in_=rstd[:], func=AF.Sqrt), reads=['rstd'], writes=['rstd'])
            P.op('dve', lambda e: e.reciprocal(out=rstd[:], in_=rstd[:]), reads=['rstd'], writes=['rstd'])
            P.op('act', lambda e, ps=ps, gvec=gvec: e.activation(out=qn[:], in_=ps[0:64, 0:NT], func=AF.Copy, scale=gvec[:, 0:1]), reads=[pn, gname], writes=['qn'])
            P.op('dve', lambda e: e.tensor_tensor(out=qn[:], in0=qn[:], in1=rstd[:], op=ALU.mult), reads=['qn', 'rstd'], writes=['qn'])
            P.op('pe', lambda e: e.matmul(psw[0:64, 0:NT], lhsT=pm[:], rhs=qn[:], start=True, stop=True), reads=['pm', 'qn'], writes=['psw'])
            P.op('dve', lambda e: e.tensor_tensor(out=t1[:], in0=qn[:], in1=CS[:], op=ALU.mult), reads=['qn', csn], writes=['t1'])
            P.op('dve', lambda e: e.tensor_tensor(out=t2[:], in0=psw[0:64, 0:NT], in1=SN[:], op=ALU.mult), reads=['psw', snn], writes=['t2'])
            o = ob[obi % 4]; on = f"ob{obi % 4}"; obi += 1
            P.op('dve', lambda e, o=o: e.tensor_tensor(out=o[0:64, :], in0=t1[:], in1=t2[:], op=ALU.add), reads=['t1', 't2'], writes=[on])
            dst = qa_o[t, hd] if hd < 8 else ka_o[t, hd - 8]
            P.dma('sp', dst, o[0:64, :], reads=[on], writes=['qk_o'])
        for hd in range(2):
            ps = psA[hd % 2]; pn = f"psA{hd % 2}"
            proj(2048 + 640 + hd * 64, 64, ps, pn)
            o = ob[obi % 4]; on = f"ob{obi % 4}"; obi += 1
            P.op('act', lambda e, o=o, ps=ps: e.activation(out=o[0:64, :], in_=ps[0:64, 0:NT], func=AF.Copy), reads=[pn], writes=[on])
            P.dma('sp', va_o[t, hd], o[0:64, :], reads=[on], writes=['va_o'])
        ps = psA[0]; pn = "psA0"
        proj(2816, 16, ps, pn)
        o = of[ofi % 2]; on = f"of{ofi % 2}"; ofi += 1
        P.op('act', lambda e, o=o, ps=ps: e.activation(out=o[0:16, :], in_=ps[0:16, 0:NT], func=AF.Identity, bias=gb[:, 0:1]), reads=[pn, 'gb'], writes=[on])
        P.dma('sp', gt_o[t], o[0:16, :], reads=[on], writes=['gt_o'])
    K.close()
    return nc


NEGM = -1.0e30


def build_attn(nunits, Tq, Tk):
    K = Ctx()
    nc, P = K.nc, K.P
    nkt = Tk // 128
    nqt = Tq // 512
    q_d = K.din("qT", [nunits, 64, Tq], BF16)
    k_d = K.din("kT", [nunits, 64, Tk], BF16)
    v_d = K.din("v", [nunits, Tk, 64], BF16)
    qg_d = K.din("qn_g", [1, 64])
    kg_d = K.din("kn_g", [1, 64])
    sel_d = K.din("sel", [65, 64])
    o_d = K.dout("oT", [nunits, 64, Tq])
    psST = [K.ps(f"psST{i}", [128, 512]) for i in range(2)]
    psO = K.ps("psO", [128, 512])
    psD = K.ps("psD", [128, 512])
    sel = K.sb("sel", [65, 64])
    P.dma('sp', sel[:], sel_d, writes=['sel'])
    gq = K.sb("gq", [128, 64]); gk = K.sb("gk", [128, 64]); mq = K.sb("mq", [128, 1]); mk = K.sb("mk", [128, 1]); negC = K.sb("negC", [128, 1])
    P.dma('sp', gq[:], bass.AP(qg_d.tensor, 0, [[0, 128], [1, 64]]), writes=['gq'])
    P.dma('sp', gk[:], bass.AP(kg_d.tensor, 0, [[0, 128], [1, 64]]), writes=['gk'])
    P.op('dve', lambda e: e.tensor_reduce(out=mq[:], in_=gq[:], axis=AX.X, op=ALU.max, apply_absolute_value=True), reads=['gq'], writes=['mq'])
    P.op('dve', lambda e: e.tensor_reduce(out=mk[:], in_=gk[:], axis=AX.X, op=ALU.max, apply_absolute_value=True), reads=['gk'], writes=['mk'])
    P.op('dve', lambda e: e.scalar_tensor_tensor(out=negC[:], in0=mq[:], scalar=-8.0, in1=mk[:], op0=ALU.mult, op1=ALU.mult), reads=['mq', 'mk'], writes=['negC'])
    qT = K.sb("qT", [64, Tq], BF16)
    kT = K.sb("kT", [64, Tk], BF16)
    vaug = K.sb("vaug", [128, nkt, 65], BF16)
    PT = [K.sb(f"PT{i}", [128, 512], BF16) for i in range(2)]
    OTs = K.sb("OTs", [65, 512])
    rden = K.sb("rden", [64, 512])
    ob = [K.sb(f"ob{i}", [64, 512]) for i in range(2)]
    P.op('dve', lambda e: e.memset(vaug[:, :, 64:65], 1.0), writes=['vaug1'])
    it = 0
    for u in range(nunits):
        P.dma('sp', qT[:], q_d[u], writes=['qT'])
        P.dma('sp', kT[:], k_d[u], writes=['kT'])
        P.dma('sp', vaug[:, :, 0:64], v_d[u].rearrange("(kt p) d -> p kt d", p=128), writes=['vaug'])
        for qt in range(nqt):
            for kt in range(nkt):
                ps = psST[kt % 2]; pn = f"psST{kt % 2}"
                pt = PT[kt % 2]; ptn = f"PT{kt % 2}"
                P.op('pe', lambda e, ps=ps, kt=kt, qt=qt: e.matmul(ps[:, :], lhsT=kT[:, kt * 128:(kt + 1) * 128], rhs=qT[:, qt * 512:(qt + 1) * 512], start=True, stop=True),
                     reads=['kT', 'qT'], writes=[pn])
                P.op('act', lambda e, ps=ps, pt=pt: e.activation(out=pt[:], in_=ps[:, :], func=AF.Exp, scale=0.125, bias=negC[:, 0:1]),
                     reads=[pn, 'negC'], writes=[ptn])
                P.op('pe', lambda e, pt=pt, kt=kt: e.matmul(psO[0:65, :], lhsT=vaug[:, kt, :], rhs=pt[:], start=(kt == 0), stop=(kt == nkt - 1)),
                     reads=['vaug', 'vaug1', ptn], writes=['psO'])
            P.op('act', lambda e: e.activation(out=OTs[:], in_=psO[0:65, :], func=AF.Copy), reads=['psO'], writes=['OTs'])
            P.op('pe', lambda e: e.matmul(psD[0:64, :], lhsT=sel[:], rhs=OTs[:], start=True, stop=True), reads=['sel', 'OTs'], writes=['psD'])
            P.op('dve', lambda e: e.reciprocal(out=rden[:], in_=psD[0:64, :]), reads=['psD'], writes=['rden'])
            o = ob[it % 2]; on = f"ob{it % 2}"; it += 1
            P.op('dve', lambda e, o=o: e.tensor_tensor(out=o[:], in0=OTs[0:64, :], in1=rden[:], op=ALU.mult), reads=['OTs', 'rden'], writes=[on])
            P.dma('sp', o_d[u][:, qt * 512:(qt + 1) * 512], o[:], reads=[on], writes=['o_d'])
    K.close()
    return nc


def build_scan(nunits, nchl, ncc=2):
    K = Ctx()
    nc, P = K.nc, K.P
    NCH = ncc + nchl
    TT = NCH * 128
    q_d = K.din("qT", [nunits, 128, TT], BF16)
    k_d = K.din("kT", [nunits, 128, TT], BF16)
    kt_d = K.din("ktok", [nunits, TT, 128], BF16)
    vt_d = K.din("vtok", [nunits, TT, 128], BF16)
    g_d = K.din("G", [nunits, 4, 128, NCH])
    cst_d = K.din("consts", [5, 128, 128])
    h_o = K.dout("h", [nunits, 2, nchl * 128, 128])

    psR = K.ps("psR", [128, 512]); psQK = K.ps("psQK", [128, 512]); psT = K.ps("psT", [128, 512], BF16)
    psN = K.ps("psN", [128, 512]); psC = K.ps("psC", [128, 512]); psX = K.ps("psX", [128, 512]); psB = K.ps("psB", [128, 512])
    cst = K.sb("cst", [128, 5, 128])
    P.dma('sp', cst[:], cst_d.rearrange("c p n -> p c n"), writes=['cst'])
    ident, triL, triU, mnegL, mnegU = [cst[:, i, :] for i in range(5)]
    identb = K.sb("identb", [128, 128], BF16)
    P.op('dve', lambda e: e.tensor_copy(out=identb[:], in_=ident), reads=['cst'], writes=['identb'])
    ones = K.sb("ones", [128, 128])
    P.op('dve', lambda e: e.memset(ones[:], 1.0), writes=['ones'])
    qT = K.sb("qT", [128, TT], BF16); kT = K.sb("kT", [128, TT], BF16)
    ktok = K.sb("ktok", [128, NCH, 128], BF16); vaug = K.sb("vaug", [128, NCH, 129], BF16)
    P.op('dve', lambda e: e.memset(vaug[:, :, 128:129], 1.0), writes=['vaug1'])
    G = K.sb("G", [128, 4, NCH])
    ee = K.sb("ee", [128, NCH]); LF = K.sb("LF", [128, NCH]); Bc = K.sb("Bc", [128, NCH]); BL = K.sb("BL", [128, NCH]); IGmB = K.sb("IGmB", [128, NCH])
    Caug = K.sb("Caug", [128, 129]); Cb = K.sb("Cb", [128, 129], BF16); m = K.sb("m", [128, 1])
    igrep = K.sb("igrep", [128, 128]); dm = K.sb("dm", [128, 128]); sexp = K.sb("sexp", [128, 128]); s_b = K.sb("s_b", [128, 128], BF16)
    sT = K.sb("sT", [128, 128], BF16); numA = K.sb("numA", [128, 129]); tot = K.sb("tot", [128, 129]); kw = K.sb("kw", [128, 128], BF16)
    col = {n: K.sb(n, [128, 1]) for n in ("rowmax", "glmax", "mnew", "nmnew", "inter", "mt", "nmt", "winter", "emt", "dd", "t2", "wk", "t3", "decay")}
    hb = [K.sb(f"hb{i}", [128, 128]) for i in range(2)]
    hi = 0

    def c1(name, fn, reads, eng='dve'):
        P.op(eng, fn, reads=reads, writes=[name])

    for u in range(nunits):
        P.dma('sp', qT[:], q_d[u], writes=['qT'])
        P.dma('sp', kT[:], k_d[u], writes=['kT'])
        P.dma('sp', ktok[:], kt_d[u].rearrange("(c p) d -> p c d", p=128), writes=['ktok'])
        P.dma('sp', vaug[:, :, 0:128], vt_d[u].rearrange("(c p) d -> p c d", p=128), writes=['vaug'])
        P.dma('sp', G[:], g_d[u].rearrange("g p c -> p g c"), writes=['G'])
        for dr in range(2):
            tri = triL if dr == 0 else triU
            mneg = mnegL if dr == 0 else mnegU
            gi, gf = 2 * dr, 2 * dr + 1
            P.op('act', lambda e: e.activation(out=ee[:], in_=G[:, gf, :], func=AF.Exp, scale=-1.0), reads=['G'], writes=['ee'])
            P.op('dve', lambda e: e.tensor_scalar(out=ee[:], in0=ee[:], scalar1=1.0, scalar2=None, op0=ALU.add), reads=['ee'], writes=['ee'])
            P.op('act', lambda e: e.activation(out=ee[:], in_=ee[:], func=AF.Ln), reads=['ee'], writes=['ee'])
            P.op('dve', lambda e: e.tensor_scalar(out=LF[:], in0=ee[:], scalar1=-1.0, scalar2=None, op0=ALU.mult), reads=['ee'], writes=['LF'])
            P.op('pe', lambda e: e.matmul(psB[:, 0:NCH], lhsT=tri, rhs=LF[:], start=True, stop=True), reads=['cst', 'LF'], writes=['psB'])
            P.op('dve', lambda e: e.tensor_copy(out=Bc[:], in_=psB[:, 0:NCH]), reads=['psB'], writes=['Bc'])
            P.op('pe', lambda e: e.matmul(psB[:, 0:NCH], lhsT=ones[:], rhs=LF[:], start=True, stop=True), reads=['ones', 'LF'], writes=['psB'])
            P.op('dve', lambda e: e.tensor_copy(out=BL[:], in_=psB[:, 0:NCH]), reads=['psB'], writes=['BL'])
            P.op('dve', lambda e: e.tensor_tensor(out=IGmB[:], in0=G[:, gi, :], in1=Bc[:], op=ALU.subtract), reads=['G', 'Bc'], writes=['IGmB'])
            P.op('dve', lambda e: e.memset(Caug[:], 0.0), writes=['Caug'])
            P.op('dve', lambda e: e.memset(Cb[:], 0.0), writes=['Cb'])
            P.op('dve', lambda e: e.memset(m[:], 0.0), writes=['m'])
            order = list(range(ncc)) + [ncc + i for i in range(nchl)]
            if dr == 1:
                order = list(range(ncc))[::-1] + [ncc + i for i in range(nchl)][::-1]
            for c in order:
                lat = c >= ncc
                cc = slice(c, c + 1)
                P.op('dve', lambda e, c=c: e.tensor_copy(out=igrep[:], in_=fap(IGmB, c, [[0, 128]])), reads=['IGmB'], writes=['igrep'])
                P.op('pe', lambda e: e.matmul(psR[:, 0:128], lhsT=igrep[:], rhs=ident, start=True, stop=True), reads=['igrep', 'cst'], writes=['psR'])
                c1('glmax', lambda e: e.tensor_reduce(out=col['glmax'][:], in_=psR[:, 0:128], axis=AX.X, op=ALU.max), ['psR'])
                c1('mnew', lambda e, cc=cc: e.scalar_tensor_tensor(out=col['mnew'][:], in0=col['glmax'][:], scalar=m[:, 0:1], in1=BL[:, cc], op0=ALU.max, op1=ALU.add),
                   ['glmax', 'm', 'BL'])
                c1('nmnew', lambda e: e.tensor_scalar(out=col['nmnew'][:], in0=col['mnew'][:], scalar1=-1.0, scalar2=None, op0=ALU.mult), ['mnew'])
                if lat:
                    P.op('dve', lambda e, cc=cc: e.scalar_tensor_tensor(out=dm[:], in0=psR[:, 0:128], scalar=Bc[:, cc], in1=mneg, op0=ALU.add, op1=ALU.add),
                         reads=['psR', 'Bc', 'cst'], writes=['dm'])
                    c1('rowmax', lambda e: e.tensor_reduce(out=col['rowmax'][:], in_=dm[:], axis=AX.X, op=ALU.max), ['dm'])
                    c1('inter', lambda e, cc=cc: e.tensor_tensor(out=col['inter'][:], in0=Bc[:, cc], in1=m[:], op=ALU.add), ['Bc', 'm'])
                    c1('mt', lambda e: e.tensor_tensor(out=col['mt'][:], in0=col['inter'][:], in1=col['rowmax'][:], op=ALU.max), ['inter', 'rowmax'])
                    c1('nmt', lambda e: e.tensor_scalar(out=col['nmt'][:], in0=col['mt'][:], scalar1=-1.0, scalar2=None, op0=ALU.mult), ['mt'])
                    P.op('act', lambda e: e.activation(out=sexp[:], in_=dm[:], func=AF.Exp, bias=col['nmt'][:, 0:1]), reads=['dm', 'nmt'], writes=['sexp'])
                    c1('winter', lambda e: e.activation(out=col['winter'][:], in_=col['inter'][:], func=AF.Exp, bias=col['nmt'][:, 0:1]), ['inter', 'nmt'], eng='act')
                    c1('emt', lambda e: e.activation(out=col['emt'][:], in_=col['mt'][:], func=AF.Exp, scale=-1.0), ['mt'], eng='act')
                    P.op('pe', lambda e, c=c: e.matmul(psQK[:, 0:128], lhsT=qT[:, c * 128:(c + 1) * 128], rhs=kT[:, c * 128:(c + 1) * 128], start=True, stop=True),
                         reads=['qT', 'kT'], writes=['psQK'])
                    P.op('dve', lambda e: e.tensor_tensor(out=s_b[:], in0=psQK[:, 0:128], in1=sexp[:], op=ALU.mult), reads=['psQK', 'sexp'], writes=['s_b'])
                    P.op('pe', lambda e: e.transpose(out=psT[:, 0:128], in_=s_b[:], identity=identb[:]), reads=['s_b', 'identb'], writes=['psT'])
                    P.op('act', lambda e: e.activation(out=sT[:], in_=psT[:, 0:128], func=AF.Copy), reads=['psT'], writes=['sT'])
                    P.op('pe', lambda e, c=c: e.matmul(psN[:, 0:129], lhsT=sT[:], rhs=vaug[:, c, :], start=True, stop=True), reads=['sT', 'vaug', 'vaug1'], writes=['psN'])
                    P.op('pe', lambda e, c=c: e.matmul(psX[:, 0:129], lhsT=qT[:, c * 128:(c + 1) * 128], rhs=Cb[:], start=True, stop=True), reads=['qT', 'Cb'], writes=['psX'])
                    P.op('act', lambda e: e.activation(out=numA[:], in_=psN[:, 0:129], func=AF.Copy), reads=['psN'], writes=['numA'])
                    P.op('dve', lambda e: e.scalar_tensor_tensor(out=tot[:], in0=psX[:, 0:129], scalar=col['winter'][:, 0:1], in1=numA[:], op0=ALU.mult, op1=ALU.add),
                         reads=['psX', 'winter', 'numA'], writes=['tot'])
                    c1('dd', lambda e: e.activation(out=col['dd'][:], in_=tot[:, 128:129], func=AF.Abs), ['tot'], eng='act')
                    c1('dd', lambda e: e.tensor_tensor(out=col['dd'][:], in0=col['dd'][:], in1=col['emt'][:], op=ALU.max), ['dd', 'emt'])
                    c1('dd', lambda e: e.reciprocal(out=col['dd'][:], in_=col['dd'][:]), ['dd'])
                    H = hb[hi % 2]; hn = f"hb{hi % 2}"; hi += 1
                    P.op('dve', lambda e, H=H: e.tensor_scalar(out=H[:], in0=tot[:, 0:128], scalar1=col['dd'][:, 0:1], scalar2=None, op0=ALU.mult), reads=['tot', 'dd'], writes=[hn])
                    lc = c - ncc
                    P.dma('sp', h_o[u, dr, lc * 128:(lc + 1) * 128, :], H[:], reads=[hn], writes=['h_o'])
                c1('t2', lambda e, cc=cc: e.tensor_tensor(out=col['t2'][:], in0=IGmB[:, cc], in1=BL[:, cc], op=ALU.add), ['IGmB', 'BL'])
                c1('wk', lambda e: e.activation(out=col['wk'][:], in_=col['t2'][:], func=AF.Exp, bias=col['nmnew'][:, 0:1]), ['t2', 'nmnew'], eng='act')
                P.op('dve', lambda e, c=c: e.tensor_scalar(out=kw[:], in0=ktok[:, c, :], scalar1=col['wk'][:, 0:1], scalar2=None, op0=ALU.mult), reads=['ktok', 'wk'], writes=['kw'])
                P.op('pe', lambda e, c=c: e.matmul(psC[:, 0:129], lhsT=kw[:], rhs=vaug[:, c, :], start=True, stop=True), reads=['kw', 'vaug', 'vaug1'], writes=['psC'])
                c1('t3', lambda e, cc=cc: e.tensor_tensor(out=col['t3'][:], in0=BL[:, cc], in1=m[:], op=ALU.add), ['BL', 'm'])
                c1('decay', lambda e: e.activation(out=col['decay'][:], in_=col['t3'][:], func=AF.Exp, bias=col['nmnew'][:, 0:1]), ['t3', 'nmnew'], eng='act')
                P.op('dve', lambda e: e.scalar_tensor_tensor(out=Caug[:], in0=Caug[:], scalar=col['decay'][:, 0:1], in1=psC[:, 0:129], op0=ALU.mult, op1=ALU.add),
                     reads=['Caug', 'decay', 'psC'], writes=['Caug'])
                P.op('act', lambda e: e.activation(out=Cb[:], in_=Caug[:], func=AF.Copy), reads=['Caug'], writes=['Cb'])
                P.op('dve', lambda e: e.tensor_copy(out=m[:], in_=col['mnew'][:]), reads=['mnew', 'inter', 't3', 'glmax'], writes=['m'])
    K.close()
    return nc


def build_l1c(ntiles):
    K = Ctx()
    nc, P = K.nc, K.P
    xt_d = K.din("xtiles", [ntiles, D, NT])
    hf_d = K.din("hfT", [ntiles, 4, 128, NT])
    hb_d = K.din("hbT", [ntiles, 4, 128, NT])
    om_d = K.din("omT", [ntiles, 4, 128, NT])
    ya_d = K.din("yaT", [ntiles, 4, 128, NT])
    cvec_d = K.din("cvec", [128, KC, 2])
    modw_d = K.din("mod_w", [D, 6144])
    modb_d = K.din("mod_bT", [128, 48])
    ng_d = K.din("norm_gT", [128, 4])
    wout_d = K.din("w_out", [D, D])
    lng_d = K.din("ln_gT", [128, KC])
    lnb_d = K.din("ln_bT", [128, KC])
    out_d = K.dout("x1T", [ntiles, D, NT])

    psA = [K.ps(f"psA{i}", [128, 512]) for i in range(2)]
    psmean = K.ps("psmean", [128, 512]); pssq = K.ps("pssq", [128, 512]); psm = K.ps("psm", [128, 512])
    ones128 = K.sb("ones128", [128, 128]); ones1024 = K.sb("ones1024", [128, 128])
    P.op('dve', lambda e: e.memset(ones128[:], 1.0 / 128), writes=['ones128'])
    P.op('dve', lambda e: e.memset(ones1024[:], 1.0 / 1024), writes=['ones1024'])
    ng = K.sb("ng", [128, 4]); lng = K.sb("lng", [128, KC]); lnb = K.sb("lnb", [128, KC])
    for t_, d_, n_ in ((ng, ng_d, 'ng'), (lng, lng_d, 'lng'), (lnb, lnb_d, 'lnb')):
        P.dma('sp', t_[:], d_, writes=[n_])
    g1a = K.sb("g1a", [128, KC, 2])
    wout_b = K.sb("wout_b", [128, KC, D], BF16)
    tmp = ExitStack()
    modT = mod_setup(K, cvec_d, modw_d, modb_d, 16, 24, psm, tmp=tmp)
    stage = [tmp.enter_context(nc.sbuf_tensor(f"stage{i}", [128, D], F32)) for i in range(2)]
    P.op('dve', lambda e: e.tensor_scalar(out=g1a[:], in0=modT[:, 0:8, :], scalar1=1.0 / ALPHA, scalar2=None, op0=ALU.mult), reads=['modT'], writes=['g1a'])
    for k in range(KC):
        load_cast(K, wout_b[:, k, :], wout_d[k * 128:(k + 1) * 128, :], stage[k % 2][:], f"stage{k % 2}", "wout_b", eng='dve' if k % 2 == 0 else 'act')
    P.barrier()
    tmp.close()

    xt = [K.sb(f"xt{i}", [128, KC, NT]) for i in range(2)]
    hf = [K.sb(f"hf{i}", [128, 4, NT]) for i in range(2)]
    hbk = [K.sb(f"hbk{i}", [128, 4, NT]) for i in range(2)]
    om = [K.sb(f"om{i}", [128, 4, NT]) for i in range(2)]
    ya = [K.sb(f"ya{i}", [128, 4, NT]) for i in range(2)]
    hs = K.sb("hs", [128, 1, NT]); sgm = K.sb("sgm", [128, NT])
    sqt = [K.sb(f"sqt{i}", [128, NT]) for i in range(2)]
    mean_sb = K.sb("mean_sb", [128, NT]); rstd = K.sb("rstd", [128, NT])
    ycat = K.sb("ycat", [128, KC, NT], BF16)
    vt = K.sb("vt", [128, KC, NT])
    xo = [K.sb(f"xo{i}", [128, KC, NT]) for i in range(2)]

    def ln_stats(src, nchunk, ones, onm, srcname, eps):
        for c in range(nchunk):
            P.op('pe', lambda e, c=c: e.matmul(psmean[:, 0:NT], lhsT=ones[:], rhs=src[:, c, :], start=(c == 0), stop=(c == nchunk - 1)),
                 reads=[onm, srcname], writes=['psmean'], inc=(c == nchunk - 1))
        for c in range(nchunk):
            s = sqt[c % 2]; sn = f"sqt{c % 2}"
            P.op('act', lambda e, c=c, s=s: e.activation(out=s[:], in_=src[:, c, :], func=AF.Square), reads=[srcname], writes=[sn])
            P.op('pe', lambda e, c=c, s=s: e.matmul(pssq[:, 0:NT], lhsT=ones[:], rhs=s[:], start=(c == 0), stop=(c == nchunk - 1)),
                 reads=[onm, sn], writes=['pssq'])
        P.op('act', lambda e: e.activation(out=mean_sb[:], in_=psmean[:, 0:NT], func=AF.Copy), reads=['psmean'], writes=['mean_sb'])
        P.op('dve', lambda e: e.tensor_tensor(out=rstd[:], in0=mean_sb[:], in1=mean_sb[:], op=ALU.mult), reads=['mean_sb'], writes=['rstd'])
        P.op('dve', lambda e: e.tensor_tensor(out=rstd[:], in0=pssq[:, 0:NT], in1=rstd[:], op=ALU.subtract), reads=['pssq', 'rstd'], writes=['rstd'])
        P.op('dve', lambda e: e.tensor_scalar(out=rstd[:], in0=rstd[:], scalar1=eps, scalar2=None, op0=ALU.add), reads=['rstd'], writes=['rstd'])
        P.op('act', lambda e: e.activation(out=rstd[:], in_=rstd[:], func=AF.Sqrt), reads=['rstd'], writes=['rstd'])
        P.op('dve', lambda e: e.reciprocal(out=rstd[:], in_=rstd[:]), reads=['rstd'], writes=['rstd'])

    for t in range(ntiles):
        b = t % 2
        X, HF, HB, OM, YA = xt[b], hf[b], hbk[b], om[b], ya[b]
        xn, hfn, hbn, omn, yan = f"xt{b}", f"hf{b}", f"hbk{b}", f"om{b}", f"ya{b}"
        P.dma('sp', X[:], xt_d[t].rearrange("(c p) n -> p c n", p=128), writes=[xn])
        P.dma('sp', HF[:], hf_d[t].rearrange("c p n -> p c n"), writes=[hfn])
        P.dma('sp', HB[:], hb_d[t].rearrange("c p n -> p c n"), writes=[hbn])
        P.dma('sp', OM[:], om_d[t].rearrange("c p n -> p c n"), writes=[omn])
        P.dma('sp', YA[:], ya_d[t].rearrange("c p n -> p c n"), writes=[yan])
        for hd in range(4):
            P.op('dve', lambda e, hd=hd: e.tensor_tensor(out=hs[:, 0, :], in0=HF[:, hd, :], in1=HB[:, hd, :], op=ALU.add), reads=[hfn, hbn], writes=['hs'])
            ln_stats(hs, 1, ones128, 'ones128', 'hs', LN_EPS)
            P.op('dve', lambda e: e.tensor_tensor(out=hs[:, 0, :], in0=hs[:, 0, :], in1=mean_sb[:], op=ALU.subtract), reads=['hs', 'mean_sb'], writes=['hs'])
            P.op('dve', lambda e: e.tensor_tensor(out=hs[:, 0, :], in0=hs[:, 0, :], in1=rstd[:], op=ALU.mult), reads=['hs', 'rstd'], writes=['hs'])
            P.op('act', lambda e, hd=hd: e.activation(out=sgm[:], in_=OM[:, hd, :], func=AF.Sigmoid), reads=[omn], writes=['sgm'])
            P.op('dve', lambda e, hd=hd: e.scalar_tensor_tensor(out=ycat[:, hd, :], in0=hs[:, 0, :], scalar=ng[:, hd:hd + 1], in1=sgm[:], op0=ALU.mult, op1=ALU.mult),
                 reads=['hs', 'ng', 'sgm'], writes=['ycat'])
        for c in range(4):
            P.op('act', lambda e, c=c: e.activation(out=ycat[:, 4 + c, :], in_=YA[:, c, :], func=AF.Copy), reads=[yan], writes=['ycat'])
        for oc in range(KC):
            ps = psA[oc % 2]; pn = f"psA{oc % 2}"
            for k in range(KC):
                P.op('pe', lambda e, k=k, oc=oc, ps=ps: e.matmul(ps[:, 0:NT], lhsT=wout_b[:, k, oc * 128:(oc + 1) * 128], rhs=ycat[:, k, :],
                                                                 start=(k == 0), stop=(k == KC - 1)),
                     reads=['wout_b', 'ycat'], writes=[pn], inc=(k == KC - 1))
            P.op('dve', lambda e, oc=oc, ps=ps: e.scalar_tensor_tensor(out=vt[:, oc, :], in0=ps[:, 0:NT], scalar=g1a[:, oc, 0:1],
                                                                      in1=X[:, oc, :], op0=ALU.mult, op1=ALU.add),
                 reads=[pn, 'g1a', xn], writes=['vt'])
        ln_stats(vt, KC, ones1024, 'ones1024', 'vt', LN_EPS / (ALPHA * ALPHA))
        XO = xo[b]; xon = f"xo{b}"
        for oc in range(KC):
            P.op('dve', lambda e, oc=oc: e.tensor_tensor(out=vt[:, oc, :], in0=vt[:, oc, :], in1=mean_sb[:], op=ALU.subtract), reads=['vt', 'mean_sb'], writes=['vt'])
            P.op('dve', lambda e, oc=oc: e.tensor_tensor(out=vt[:, oc, :], in0=vt[:, oc, :], in1=rstd[:], op=ALU.mult), reads=['vt', 'rstd'], writes=['vt'])
            P.op('act', lambda e, oc=oc: e.activation(out=XO[:, oc, :], in_=vt[:, oc, :], func=AF.Identity, scale=lng[:, oc:oc + 1], bias=lnb[:, oc:oc + 1]),
                 reads=['vt', 'lng', 'lnb'], writes=[xon])
        P.dma('sp', out_d[t].rearrange("(c p) n -> p c n", p=128), XO[:], reads=[xon], writes=['out'])
    K.close()
    return nc


def seg_tiles(seq, start, ntile):
    L = seq.shape[0]
    xt = np.zeros((ntile, D, WT), np.float32)
    cm = np.zeros((ntile, 128, WT), np.float32)
    rc = np.ones((ntile, 128, 4, NT), np.float32)
    for t in range(ntile):
        s0 = start + t * NT - HALO
        lo, hi = max(s0, 0), min(s0 + WT, L)
        if hi > lo:
            xt[t, :, lo - s0:hi - s0] = seq[lo:hi].T
            cm[t, :, lo - s0:hi - s0] = 1.0
        tok = start + t * NT + np.arange(NT)
        for g, w in enumerate(POOL_WINDOWS):
            cnt = np.minimum(tok + w // 2, L) - np.maximum(tok - w // 2, 0)
            cnt = np.where(tok < L, cnt, 1).astype(np.float32)
            rc[t, :, g, :] = (1.0 / cnt)[None, :]
    return xt, cm, rc

def fm(v, nchunk):
    return np.ascontiguousarray(np.asarray(v, np.float32).reshape(nchunk, 128).T)

def cvec_of(cb, c_ctx):
    return np.ascontiguousarray(np.stack([fm(cb, KC), fm(c_ctx, KC)], axis=-1))

def plain_tiles(seq, start, ntile):
    L, Dm = seq.shape
    out = np.zeros((ntile, Dm, NT), seq.dtype)
    for t in range(ntile):
        lo = start + t * NT
        hi = min(lo + NT, L)
        if hi > lo:
            out[t, :, :hi - lo] = seq[lo:hi].T
    return out

def rope_tiles(start, ntile, latent=True):
    cs = np.ones((ntile, 64, NT), np.float32); sn = np.zeros((ntile, 64, NT), np.float32)
    if latent:
        f = (10000.0 ** (-np.arange(16, dtype=np.float32) / 16)).astype(np.float32)
        for t in range(ntile):
            tok = start + t * NT + np.arange(NT)
            row = (tok // 64).astype(np.float32); col = (tok % 64).astype(np.float32)
            ar = (row[None, :] * f[:, None]).astype(np.float32); ac = (col[None, :] * f[:, None]).astype(np.float32)
            cs[t] = np.concatenate([np.cos(ar), np.cos(ar), np.cos(ac), np.cos(ac)], 0)
            sn[t] = np.concatenate([np.sin(ar), np.sin(ar), np.sin(ac), np.sin(ac)], 0)
    return cs, sn

def rope_perm():
    Pm = np.zeros((64, 64), np.float32)
    for base in (0, 32):
        for d in range(16):
            Pm[base + d + 16, base + d] = -1.0
            Pm[base + d, base + d + 16] = 1.0
    return Pm

CD_ORDER = None
def cd_perm_cols(w):
    return np.ascontiguousarray(np.concatenate([w[:, 0:2048], w[:, 2064:2832], w[:, 2048:2064]], axis=1))

def scan_consts():
    i = np.arange(128)
    ident = np.eye(128, dtype=np.float32)
    triL = (i[:, None] <= i[None, :]).astype(np.float32)
    triU = (i[:, None] >= i[None, :]).astype(np.float32)
    mnegL = np.where(i[None, :] <= i[:, None], 0.0, -1.0e30).astype(np.float32)
    mnegU = np.where(i[None, :] >= i[:, None], 0.0, -1.0e30).astype(np.float32)
    return np.stack([ident, triL, triU, mnegL, mnegU])

def attn_sel():
    s = np.zeros((65, 64), np.float32); s[64, :] = 1.0
    return s


_PROGS = {}


def _prog(key, fn):
    if key not in _PROGS:
        _PROGS[key] = fn()
    return _PROGS[key]


def _run(nc, in_maps):
    res = run_bass_kernel_spmd(nc, in_maps, core_ids=list(range(len(in_maps))))
    return res.results


def _untile(a, n):
    a = np.asarray(a[:n])
    return np.ascontiguousarray(a.transpose(1, 2, 0, 3).reshape(a.shape[1], a.shape[2], n * NT))


def kernel(x, c, ctx, c_ctx, mod_w, mod_b, ln_g, ln_b, ab_w_in, pool_w, pool_ls, conv_w, conv_b, conv_ln_g, conv_ln_b, ab_w_out,
           cd_w_in, mlstm_gate_b, mlstm_norm_g, q_norm_g, k_norm_g, cd_w_out, peer_w_q, peer_keys, peer_u, peer_v):
    f32 = lambda a: np.ascontiguousarray(np.asarray(a, np.float32))
    x, c, ctx, c_ctx, mod_w, mod_b, ln_g, ln_b = map(f32, (x, c, ctx, c_ctx, mod_w, mod_b, ln_g, ln_b))
    peer_w_q, peer_keys, peer_u, peer_v = map(f32, (peer_w_q, peer_keys, peer_u, peer_v))
    cd_w_in, cd_w_out, ab_w_in, ab_w_out = map(f32, (cd_w_in, cd_w_out, ab_w_in, ab_w_out))
    B, T, _ = x.shape
    Lc = ctx.shape[1]
    NH = 8 // B
    ncores = B * NH
    Th = T // NH
    Lh = Lc // NH
    assert Lh == 128
    ntl = Th // NT
    cores = [(i // NH, i % NH) for i in range(ncores)]
    ident = np.eye(128, dtype=np.float32)
    iota16 = np.tile(np.arange(16, dtype=np.float32), (128, 1))

    nc = _prog(("mixa", ntl), lambda: build_mixa(ntl + 1, [0] * ntl + [1]))
    maps = []
    for (b, hf_) in cores:
        xt, cm, rc = seg_tiles(x[b], hf_ * Th, ntl)
        xt2, cm2, rc2 = seg_tiles(ctx[b], hf_ * Lh, 1)
        maps.append(dict(
            xtiles=np.concatenate([xt, xt2]), cmask=np.concatenate([cm, cm2]), rcorr=np.concatenate([rc, rc2]),
            cvec=cvec_of(c[b], c_ctx), mod_w=mod_w[0], mod_bT=fm(mod_b[0], 48),
            w_in=ab_w_in[0], pool_w=f32(pool_w[0]), pool_lsT=fm(pool_ls[0], 4),
            conv_wT=np.ascontiguousarray(f32(conv_w[0]).T.reshape(4, 128, 31).transpose(1, 0, 2)),
            conv_bT=fm(conv_b[0], 4), cln_gT=fm(conv_ln_g[0], 4), cln_bT=fm(conv_ln_b[0], 4),
            w_out=ab_w_out[0], ln_gT=fm(ln_g[0, 0], KC), ln_bT=fm(ln_b[0, 0], KC)))
    r1 = _run(nc, maps)

    def peer_launch(layer, x1T_list, mcols):
        nsub = len(mcols)
        ncp = _prog(("peer", nsub, tuple(mcols)), lambda: build_peer(nsub, mcols))
        mp = []
        for i, (b, hf_) in enumerate(cores):
            x1T = x1T_list[i]
            mp.append(dict(x1T=x1T, x1=np.ascontiguousarray(x1T.transpose(0, 2, 1)),
                           cvec=cvec_of(c[b], c_ctx), mod_w=mod_w[layer], mod_bT=fm(mod_b[layer], 48),
                           w_q=peer_w_q[layer], keysT=np.ascontiguousarray(peer_keys[layer].transpose(2, 0, 1)),
                           peer_u=peer_u[layer], peer_v=peer_v[layer], ln_g=ln_g[layer, 1][None].copy(), ln_b=ln_b[layer, 1][None].copy(),
                           ident=ident, iota16=iota16))
        return _run(ncp, mp)

    def subtiles(x1T_tiles, nt, extra=None):
        a = x1T_tiles[:nt].reshape(nt, D, 2, 128).transpose(0, 2, 1, 3).reshape(2 * nt, D, 128)
        if extra is not None:
            a = np.concatenate([a, extra[None]])
        return np.ascontiguousarray(a)

    x1T_list = [subtiles(r1[i]["x1T"], ntl, r1[i]["x1T"][ntl][:, :128]) for i in range(ncores)]
    r2 = peer_launch(0, x1T_list, [0] * (2 * ntl) + [1])
    xl0 = np.zeros((B, T, D), np.float32)
    xc0 = np.zeros((B, Lc, D), np.float32)
    for i, (b, hf_) in enumerate(cores):
        o = r2[i]["x2"]
        xl0[b, hf_ * Th:(hf_ + 1) * Th] = o[:2 * ntl].reshape(Th, D)
        xc0[b, hf_ * Lh:(hf_ + 1) * Lh] = o[2 * ntl]

    nc = _prog(("l1a", ntl), lambda: build_l1a(ntl + 1, [0] * ntl + [1]))
    maps = []
    w_in_p = cd_perm_cols(cd_w_in[0])
    for (b, hf_) in cores:
        cs, sn = rope_tiles(hf_ * Th, ntl)
        cs2, sn2 = rope_tiles(0, 1, latent=False)
        maps.append(dict(xtiles=np.concatenate([plain_tiles(xl0[b], hf_ * Th, ntl), plain_tiles(xc0[b], hf_ * Lh, 1)]),
                         cosT=np.concatenate([cs, cs2]), sinT=np.concatenate([sn, sn2]),
                         cvec=cvec_of(c[b], c_ctx), mod_w=mod_w[1], mod_bT=fm(mod_b[1], 48), w_in=w_in_p,
                         gate_bT=f32(mlstm_gate_b[0]).reshape(16, 1).copy(), qn_gT=f32(q_norm_g[0]).reshape(64, 1).copy(),
                         kn_gT=f32(k_norm_g[0]).reshape(64, 1).copy(), ropeP=rope_perm()))
    r3 = _run(nc, maps)
    full = {}
    for nm in ("qmT", "kmT", "vmT", "qaT", "kaT", "vaT"):
        H, dd = r3[0][nm].shape[1], r3[0][nm].shape[2]
        lat = np.zeros((B, H, dd, T), r3[0][nm].dtype)
        cx = np.zeros((B, H, dd, Lc), r3[0][nm].dtype)
        for i, (b, hf_) in enumerate(cores):
            lat[b, :, :, hf_ * Th:(hf_ + 1) * Th] = _untile(r3[i][nm], ntl)
            cx[b, :, :, hf_ * Lh:(hf_ + 1) * Lh] = np.asarray(r3[i][nm][ntl])[:, :, :Lh]
        full[nm] = (lat, cx)
    gl = np.zeros((B, 16, T), np.float32)
    gc = np.zeros((B, 16, Lc), np.float32)
    for i, (b, hf_) in enumerate(cores):
        g = np.asarray(r3[i]["gT"])
        gl[b, :, hf_ * Th:(hf_ + 1) * Th] = g[:ntl].transpose(1, 0, 2).reshape(16, Th)
        gc[b, :, hf_ * Lh:(hf_ + 1) * Lh] = g[ntl][:, :Lh]

    nchl = T // 128
    ncc = Lc // 128
    NCH = nchl + ncc
    units = [(b, h) for b in range(B) for h in range(4)]
    upc = len(units) // ncores
    nc = _prog(("scan", upc, nchl), lambda: build_scan(upc, nchl, ncc))
    consts = scan_consts()
    maps = []
    for i in range(ncores):
        us = units[i * upc:(i + 1) * upc]
        cat = lambda nm, b, h: np.concatenate([full[nm][1][b, h], full[nm][0][b, h]], axis=1)
        qT = np.stack([cat("qmT", b, h) for (b, h) in us])
        kT = np.stack([cat("kmT", b, h) for (b, h) in us])
        vT = np.stack([cat("vmT", b, h) for (b, h) in us])
        G = np.stack([np.stack([np.concatenate([gc[b, gi * 4 + h], gl[b, gi * 4 + h]]).reshape(NCH, 128).T for gi in range(4)]) for (b, h) in us])
        maps.append(dict(qT=np.ascontiguousarray(qT), kT=np.ascontiguousarray(kT), ktok=np.ascontiguousarray(kT.transpose(0, 2, 1)),
                         vtok=np.ascontiguousarray(vT.transpose(0, 2, 1)), G=np.ascontiguousarray(G.astype(np.float32)), consts=consts))
    r4 = _run(nc, maps)
    hdir = np.zeros((B, 4, 2, T, 128), np.float32)
    for i in range(ncores):
        for j, (b, h) in enumerate(units[i * upc:(i + 1) * upc]):
            hdir[b, h] = r4[i]["h"][j]

    aunits = [(b, h) for b in range(B) for h in range(8)]
    apc = len(aunits) // ncores
    nc = _prog(("attn", apc, T, T + Lc), lambda: build_attn(apc, T, T + Lc))
    maps = []
    for i in range(ncores):
        us = aunits[i * apc:(i + 1) * apc]
        qT = np.stack([full["qaT"][0][b, h] for (b, h) in us])
        kT = np.stack([np.concatenate([full["kaT"][1][b, h // 4], full["kaT"][0][b, h // 4]], axis=1) for (b, h) in us])
        vT = np.stack([np.concatenate([full["vaT"][1][b, h // 4], full["vaT"][0][b, h // 4]], axis=1) for (b, h) in us])
        maps.append(dict(qT=np.ascontiguousarray(qT), kT=np.ascontiguousarray(kT), v=np.ascontiguousarray(vT.transpose(0, 2, 1)),
                         qn_g=f32(q_norm_g[0])[None].copy(), kn_g=f32(k_norm_g[0])[None].copy(), sel=attn_sel()))
    r5 = _run(nc, maps)
    ya = np.zeros((B, 8, 64, T), np.float32)
    for i in range(ncores):
        for j, (b, h) in enumerate(aunits[i * apc:(i + 1) * apc]):
            ya[b, h] = r5[i]["oT"][j]

    nc = _prog(("l1c", ntl), lambda: build_l1c(ntl))
    maps = []
    for i, (b, hf_) in enumerate(cores):
        sl = slice(hf_ * Th, (hf_ + 1) * Th)
        def ttile(a):
            return np.ascontiguousarray(a.reshape(4, ntl, NT, 128).transpose(1, 0, 3, 2))
        yat = ya[b][:, :, sl].reshape(4, 128, ntl, NT).transpose(2, 0, 1, 3)
        maps.append(dict(xtiles=plain_tiles(xl0[b], hf_ * Th, ntl), hfT=ttile(hdir[b, :, 0, sl]), hbT=ttile(hdir[b, :, 1, sl]),
                         omT=np.ascontiguousarray(np.asarray(r3[i]["omT"][:ntl], np.float32)), yaT=np.ascontiguousarray(yat),
                         cvec=cvec_of(c[b], c_ctx), mod_w=mod_w[1], mod_bT=fm(mod_b[1], 48), norm_gT=fm(mlstm_norm_g[0], 4),
                         w_out=cd_w_out[0], ln_gT=fm(ln_g[1, 0], KC), ln_bT=fm(ln_b[1, 0], KC)))
    r6 = _run(nc, maps)

    x1T_list = [subtiles(r6[i]["x1T"], ntl) for i in range(ncores)]
    r7 = peer_launch(1, x1T_list, [0] * (2 * ntl))
    out = np.zeros((B, T, D), np.float32)
    for i, (b, hf_) in enumerate(cores):
        out[b, hf_ * Th:(hf_ + 1) * Th] = r7[i]["x2"].reshape(Th, D)
    return out
```

```python
import numpy as np
import ml_dtypes
from contextlib import ExitStack
import concourse.bass as bass
import concourse.mybir as mybir
from concourse.bass_utils import run_bass_kernel_spmd

F32 = mybir.dt.float32
BF16 = mybir.dt.bfloat16
I32 = mybir.dt.int32
U32 = mybir.dt.uint32
AF = mybir.ActivationFunctionType
ALU = mybir.AluOpType
AX = mybir.AxisListType

ENG = {'pe': 'tensor', 'act': 'scalar', 'dve': 'vector', 'pool': 'gpsimd', 'sp': 'sync'}


class Prog:
    def __init__(self, nc, ndma=20):
        self.nc = nc
        self.stack = ExitStack()
        self.sem = {e: self.stack.enter_context(nc.semaphore(f"s_{e}")) for e in ENG}
        self.cnt = {e: 0 for e in ENG}
        self.ndma = ndma
        self.dsem = [self.stack.enter_context(nc.semaphore(f"d{i}")) for i in range(ndma)]
        self.dcnt = [0] * ndma
        self.dnext = 0
        self.seen = {e: {} for e in ENG}
        self.lastw = {}
        self.readers = {}
        self.pending = {e: [] for e in ENG}
        self.nops = 0

    def eng(self, e):
        return getattr(self.nc, ENG[e])

    def _wait(self, e, key, val):
        if val <= 0:
            return
        if self.seen[e].get(key, 0) >= val:
            return
        self.seen[e][key] = val
        sem = self.sem[key] if isinstance(key, str) else self.dsem[key]
        self.eng(e).wait_ge(sem, val)

    def _deps(self, e, reads, writes, sync_same):
        toks = {}
        for r in reads:
            t = self.lastw.get(r)
            if t is not None:
                toks[t[0]] = max(toks.get(t[0], 0), t[1])
        for w in writes:
            t = self.lastw.get(w)
            if t is not None:
                toks[t[0]] = max(toks.get(t[0], 0), t[1])
            for k, v in self.readers.get(w, {}).items():
                toks[k] = max(toks.get(k, 0), v)
        for k, v in toks.items():
            if k == e and not sync_same:
                continue
            self._wait(e, k, v)

    def _record(self, tok, reads, writes):
        for r in reads:
            d = self.readers.setdefault(r, {})
            d[tok[0]] = max(d.get(tok[0], 0), tok[1])
        for w in writes:
            self.lastw[w] = tok
            self.readers[w] = {}

    def op(self, e, fn, reads=(), writes=(), sync_same=None, inc=True):
        if sync_same is None:
            sync_same = (e != 'pe')
        self._deps(e, reads, writes, sync_same)
        ins = fn(self.eng(e))
        self.nops += 1
        if not inc:
            self.pending[e].append((tuple(reads), tuple(writes)))
            return ins
        self.cnt[e] += 1
        ins.then_inc(self.sem[e], 1)
        tok = (e, self.cnt[e])
        for (r, w) in self.pending[e]:
            self._record(tok, r, w)
        self.pending[e] = []
        self._record(tok, reads, writes)
        return ins

    def dma(self, e, out, in_, reads=(), writes=(), fn=None, **kw):
        s = self.dnext
        self.dnext = (s + 1) % self.ndma
        self._wait(e, s, self.dcnt[s] * 16)
        self._deps(e, reads, writes, True)
        if fn is None:
            ins = self.eng(e).dma_start(out=out, in_=in_, **kw)
        else:
            ins = fn(self.eng(e))
        self.nops += 1
        self.dcnt[s] += 1
        ins.then_inc(self.dsem[s], 16)
        tok = (s, self.dcnt[s] * 16)
        self._record(tok, reads, writes)
        return ins

    def barrier(self):
        for e in ENG:
            for s_ in range(self.ndma):
                self._wait(e, s_, self.dcnt[s_] * 16)
            for k in ENG:
                if k != e:
                    self._wait(e, k, self.cnt[k])

    def finish(self, e='sp'):
        for s in range(self.ndma):
            self._wait(e, s, self.dcnt[s] * 16)
        for k in ENG:
            if k != e:
                self._wait(e, k, self.cnt[k])
        self.stack.close()


D = 1024
KC = 8
NT = 256
HALO = 16
WT = NT + 2 * HALO
ALPHA = 4.0 ** 0.25
LN_EPS = 1e-5
POOL_WINDOWS = (2, 4, 8, 16)


class Ctx:
    def __init__(self, name="k"):
        self.nc = bass.Bass("TRN2", target_bir_lowering=False)
        self.P = Prog(self.nc)
        self.es = ExitStack()
        self.pfx = ""
        self.drams = {}

    def begin_stage(self, pfx):
        self.es = ExitStack()
        self.pfx = pfx

    def end_stage(self):
        self.P.barrier()
        self.es.close()

    def sb(self, name, shape, dt=F32):
        return self.es.enter_context(self.nc.sbuf_tensor("s_" + self.pfx + name, list(shape), dt))

    def ps(self, name, shape, dt=F32):
        return self.es.enter_context(self.nc.psum_tensor("p_" + self.pfx + name, list(shape), dt))

    def _dram(self, name, shape, dt, kind):
        if name not in self.drams:
            self.drams[name] = self.nc.dram_tensor(name, list(shape), dt, kind=kind).ap()
        return self.drams[name]

    def din(self, name, shape, dt=F32):
        return self._dram(name, shape, dt, "ExternalInput")

    def dout(self, name, shape, dt=F32):
        return self._dram(name, shape, dt, "ExternalOutput")

    def scr(self, name, shape, dt=F32):
        return self._dram(name, shape, dt, "Internal")

    def close(self):
        self.P.finish('sp')


def fap(t, off, dims):
    fs = 1
    for s in t.shape[1:]:
        fs *= s
    return bass.AP(t, off, [[fs, t.shape[0]]] + [list(d) for d in dims])


def load_cast(K, dst_bf, src_ap, stage, rname, wname, eng='dve', q='sp'):
    P = K.P
    P.dma(q, stage, src_ap, writes=[rname])
    if eng == 'dve':
        P.op('dve', lambda e: e.tensor_copy(out=dst_bf, in_=stage), reads=[rname], writes=[wname])
    else:
        P.op('act', lambda e: e.activation(out=dst_bf, in_=stage, func=AF.Copy), reads=[rname], writes=[wname])


def mod_setup(K, cvec_d, modw_d, modb_d, j0, j1, psm, tmp=None):
    nc, P = K.nc, K.P
    nj = j1 - j0
    cv = K.sb("cv", [128, KC, 2])
    sc = K.sb("sc", [128, KC, 2])
    mb = K.sb("mb", [128, 48])
    modT = K.sb("modT", [128, nj, 2])
    tmp = tmp if tmp is not None else K.es
    mwblk = [tmp.enter_context(K.nc.sbuf_tensor(f"s_{K.pfx}mwblk{i}", [128, KC, 512], F32)) for i in range(2)]
    P.dma('sp', cv[:], cvec_d, writes=['cv'])
    P.dma('sp', mb[:], modb_d, writes=['mb'])
    P.op('act', lambda e: e.activation(out=sc[:], in_=cv[:], func=AF.Silu), reads=['cv'], writes=['sc'])
    assert j0 % 4 == 0 and j1 % 4 == 0
    for bi, blk in enumerate(range(j0 // 4, j1 // 4)):
        buf = mwblk[bi % 2]
        bn = f"mwblk{bi % 2}"
        P.dma('sp', buf[:], modw_d[:, blk * 512:(blk + 1) * 512].rearrange("(k p) f -> p k f", p=128), writes=[bn])
        for jj in range(4):
            j = blk * 4 + jj - j0
            for k in range(KC):
                P.op('pe', lambda e, k=k, jj=jj, j=j: e.matmul(psm[:, 2 * j:2 * j + 2], lhsT=buf[:, k, jj * 128:(jj + 1) * 128],
                                                               rhs=sc[:, k, :], start=(k == 0), stop=(k == KC - 1)),
                     reads=[bn, 'sc'], writes=['psm'], inc=(k == KC - 1))
    P.op('dve', lambda e: e.tensor_tensor(out=modT[:], in0=psm[:, 0:2 * nj].rearrange("p (j c) -> p j c", c=2),
                                          in1=fap(mb, j0, [[1, nj], [0, 2]]), op=ALU.add),
         reads=['psm', 'mb'], writes=['modT'])
    return modT


def stage_mixa(K, ntiles, mcols, out_d):
    K.begin_stage("mx_")
    nc, P = K.nc, K.P
    xt_d = K.din("xtiles", [ntiles, D, WT])
    cm_d = K.din("cmask", [ntiles, 128, WT])
    rc_d = K.din("rcorr", [ntiles, 128, 4, NT])
    cvec_d = K.din("cvec", [128, KC, 2])
    modw_d = K.din("mod_w0", [D, 6144])
    modb_d = K.din("mod_bT0", [128, 48])
    win_d = K.din("ab_w_in", [D, 1536])
    poolw_d = K.din("pool_w", [4, 128, 128])
    poolls_d = K.din("pool_lsT", [128, 4])
    convw_d = K.din("conv_wT", [128, 4, 31])
    convb_d = K.din("conv_bT", [128, 4])
    clng_d = K.din("cln_gT", [128, 4])
    clnb_d = K.din("cln_bT", [128, 4])
    wout_d = K.din("ab_w_out", [D, D])
    lng_d = K.din("ln_gT00", [128, KC])
    lnb_d = K.din("ln_bT00", [128, KC])

    psA = [K.ps(f"psA{i}", [128, 512]) for i in range(2)]
    psp = K.ps("psp", [128, 512])
    psmean = K.ps("psmean", [128, 512])
    pssq = K.ps("pssq", [128, 512])
    psm = K.ps("psm", [128, 512])

    ones512 = K.sb("ones512", [128, 128])
    ones1024 = K.sb("ones1024", [128, 128])
    P.op('dve', lambda e: e.memset(ones512[:], 1.0 / 512), writes=['ones512'])
    P.op('dve', lambda e: e.memset(ones1024[:], 1.0 / 1024), writes=['ones1024'])
    small = {}
    for nm, d, shp in [("pls", poolls_d, [128, 4]), ("cw", convw_d, [128, 4, 31]), ("cb", convb_d, [128, 4]),
                       ("clg", clng_d, [128, 4]), ("clb", clnb_d, [128, 4]), ("lng", lng_d, [128, KC]),
                       ("lnb", lnb_d, [128, KC])]:
        t = K.sb(nm, shp)
        P.dma('sp', t[:], d, writes=[nm])
        small[nm] = t
    pls, cw, cb, clg, clb, lng, lnb = [small[n] for n in ("pls", "cw", "cb", "clg", "clb", "lng", "lnb")]

    modT = mod_setup(K, cvec_d, modw_d, modb_d, 0, 24, psm)
    sc1p = K.sb("sc1p", [128, KC, 2])
    g1a = K.sb("g1a", [128, KC, 2])
    P.op('dve', lambda e: e.tensor_scalar(out=sc1p[:], in0=modT[:, 8:16, :], scalar1=1.0, scalar2=None, op0=ALU.add),
         reads=['modT'], writes=['sc1p'])
    P.op('dve', lambda e: e.tensor_scalar(out=g1a[:], in0=modT[:, 16:24, :], scalar1=1.0 / ALPHA, scalar2=None, op0=ALU.mult),
         reads=['modT'], writes=['g1a'])

    stage = [K.sb(f"stage{i}", [128, 1536]) for i in range(2)]
    win_b = K.sb("win_b", [128, KC, 1536], BF16)
    wout_b = K.sb("wout_b", [128, KC, D], BF16)
    poolw_b = K.sb("poolw_b", [128, 4, 128], BF16)
    si = 0
    for k in range(KC):
        load_cast(K, win_b[:, k, :], win_d[k * 128:(k + 1) * 128, :], stage[si % 2][:, :], f"stage{si % 2}", "win_b",
                  eng='dve' if k % 2 == 0 else 'act')
        si += 1
    for k in range(KC):
        load_cast(K, wout_b[:, k, :], wout_d[k * 128:(k + 1) * 128, :], stage[si % 2][:, 0:D], f"stage{si % 2}", "wout_b",
                  eng='dve' if k % 2 == 0 else 'act')
        si += 1
    for g in range(4):
        load_cast(K, poolw_b[:, g, :], poolw_d[g], stage[si % 2][:, 0:128], f"stage{si % 2}", "poolw_b")
        si += 1

    xt = [K.sb(f"xt{i}", [128, KC, WT]) for i in range(2)]
    cm = [K.sb(f"cm{i}", [128, WT]) for i in range(2)]
    rc = [K.sb(f"rc{i}", [128, 4, NT]) for i in range(2)]
    hT = K.sb("hT", [128, KC, WT], BF16)
    zp = K.sb("zp", [128, 4, WT])
    ua = K.sb("ua", [128, 4, WT])
    sg = K.sb("sg", [128, WT])
    u = K.sb("u", [128, 4, WT])
    pa = K.sb("pa", [128, WT])
    pb = K.sb("pb", [128, WT])
    dif = K.sb("dif", [128, NT], BF16)
    yc = K.sb("yc", [128, 4, NT])
    sqt = [K.sb(f"sqt{i}", [128, NT]) for i in range(2)]
    mean_sb = K.sb("mean_sb", [128, NT])
    rstd = K.sb("rstd", [128, NT])
    yn = K.sb("yn", [128, NT])
    ycat = K.sb("ycat", [128, KC, NT], BF16)
    vt = K.sb("vt", [128, KC, NT])
    xo = [K.sb(f"xo{i}", [128, KC, NT]) for i in range(2)]

    def ln_stats(src, nchunk, ones, onm, srcname, eps):
        for c in range(nchunk):
            P.op('pe', lambda e, c=c: e.matmul(psmean[:, 0:NT], lhsT=ones[:], rhs=src[:, c, :], start=(c == 0), stop=(c == nchunk - 1)),
                 reads=[onm, srcname], writes=['psmean'], inc=(c == nchunk - 1))
        for c in range(nchunk):
            s = sqt[c % 2]
            sn = f"sqt{c % 2}"
            P.op('act', lambda e, c=c, s=s: e.activation(out=s[:], in_=src[:, c, :], func=AF.Square), reads=[srcname], writes=[sn])
            P.op('pe', lambda e, c=c, s=s: e.matmul(pssq[:, 0:NT], lhsT=ones[:], rhs=s[:], start=(c == 0), stop=(c == nchunk - 1)),
                 reads=[onm, sn], writes=['pssq'])
        P.op('act', lambda e: e.activation(out=mean_sb[:], in_=psmean[:, 0:NT], func=AF.Copy), reads=['psmean'], writes=['mean_sb'])
        P.op('dve', lambda e: e.tensor_tensor(out=rstd[:], in0=mean_sb[:], in1=mean_sb[:], op=ALU.mult), reads=['mean_sb'], writes=['rstd'])
        P.op('dve', lambda e: e.tensor_tensor(out=rstd[:], in0=pssq[:, 0:NT], in1=rstd[:], op=ALU.subtract), reads=['pssq', 'rstd'], writes=['rstd'])
        P.op('dve', lambda e: e.tensor_scalar(out=rstd[:], in0=rstd[:], scalar1=eps, scalar2=None, op0=ALU.add),
             reads=['rstd'], writes=['rstd'])
        P.op('act', lambda e: e.activation(out=rstd[:], in_=rstd[:], func=AF.Sqrt), reads=['rstd'], writes=['rstd'])
        P.op('dve', lambda e: e.reciprocal(out=rstd[:], in_=rstd[:]), reads=['rstd'], writes=['rstd'])

    for t in range(ntiles):
        col = mcols[t]
        b = t % 2
        X, CM, RC = xt[b], cm[b], rc[b]
        xn, cmn, rcn = f"xt{b}", f"cm{b}", f"rc{b}"
        P.dma('sp', X[:], xt_d[t].rearrange("(c p) n -> p c n", p=128), writes=[xn])
        P.dma('sp', CM[:], cm_d[t], writes=[cmn])
        P.dma('sp', RC[:], rc_d[t], writes=[rcn])
        for c in range(KC):
            P.op('act', lambda e, c=c: e.activation(out=hT[:, c, :], in_=X[:, c, :], func=AF.Identity,
                                                    scale=sc1p[:, c, col:col + 1], bias=modT[:, c, col:col + 1]),
                 reads=[xn, 'sc1p', 'modT'], writes=['hT'])
        for oc in range(12):
            ps = psA[oc % 2]
            pn = f"psA{oc % 2}"
            for k in range(KC):
                P.op('pe', lambda e, k=k, oc=oc, ps=ps: e.matmul(ps[:, 0:WT], lhsT=win_b[:, k, oc * 128:(oc + 1) * 128], rhs=hT[:, k, :],
                                                                 start=(k == 0), stop=(k == KC - 1)),
                     reads=['win_b', 'hT'], writes=[pn], inc=(k == KC - 1))
            if oc < 4:
                P.op('dve', lambda e, oc=oc, ps=ps: e.tensor_tensor(out=zp[:, oc, :], in0=ps[:, 0:WT], in1=CM[:], op=ALU.mult),
                     reads=[pn, cmn], writes=['zp'])
            elif oc < 8:
                P.op('act', lambda e, oc=oc, ps=ps: e.activation(out=ua[:, oc - 4, :], in_=ps[:, 0:WT], func=AF.Copy),
                     reads=[pn], writes=['ua'])
            else:
                P.op('act', lambda e, ps=ps: e.activation(out=sg[:], in_=ps[:, 0:WT], func=AF.Sigmoid), reads=[pn], writes=['sg'])
                P.op('dve', lambda e: e.tensor_tensor(out=sg[:], in0=sg[:], in1=CM[:], op=ALU.mult), reads=['sg', cmn], writes=['sg'])
                P.op('dve', lambda e, oc=oc: e.tensor_tensor(out=u[:, oc - 8, :], in0=ua[:, oc - 8, :], in1=sg[:], op=ALU.mult),
                     reads=['ua', 'sg'], writes=['u'])
        for g in range(4):
            src = zp[:, g, :]
            srcn = 'zp'
            lo, hi = 0, WT
            bufs = [(pa, 'pa'), (pb, 'pb')]
            dst, dn = bufs[0]
            P.op('dve', lambda e, dst=dst, g=g: e.tensor_tensor(out=dst[:, 1:WT], in0=zp[:, g, 0:WT - 1], in1=zp[:, g, 1:WT], op=ALU.add),
                 reads=['zp'], writes=[dn])
            cur, cn = dst, dn
            lo, hi = 1, WT
            sh = 1
            for lvl in range(g):
                dst, dn = bufs[(lvl + 1) % 2]
                nlo, nhi = lo + sh, hi - sh
                P.op('dve', lambda e, dst=dst, cur=cur, nlo=nlo, nhi=nhi, sh=sh: e.tensor_tensor(
                    out=dst[:, nlo:nhi], in0=cur[:, nlo - sh:nhi - sh], in1=cur[:, nlo + sh:nhi + sh], op=ALU.add),
                    reads=[cn], writes=[dn])
                cur, cn = dst, dn
                lo, hi = nlo, nhi
                sh *= 2
            assert lo <= HALO and hi >= HALO + NT
            oth, on = bufs[0] if cur is bufs[1][0] else bufs[1]
            P.op('dve', lambda e, cur=cur, oth=oth, g=g: e.tensor_tensor(out=oth[:, 0:NT], in0=cur[:, HALO:HALO + NT], in1=RC[:, g, :], op=ALU.mult),
                 reads=[cn, rcn], writes=[on])
            P.op('dve', lambda e, oth=oth, g=g: e.tensor_tensor(out=dif[:], in0=oth[:, 0:NT], in1=zp[:, g, HALO:HALO + NT], op=ALU.subtract),
                 reads=[on, 'zp'], writes=['dif'])
            P.op('pe', lambda e, g=g: e.matmul(psp[:, 0:NT], lhsT=poolw_b[:, g, :], rhs=dif[:], start=True, stop=True),
                 reads=['poolw_b', 'dif'], writes=['psp'])
            P.op('act', lambda e, g=g: e.activation(out=ycat[:, g, :], in_=psp[:, 0:NT], func=AF.Copy, scale=pls[:, g:g + 1]),
                 reads=['psp', 'pls'], writes=['ycat'])
        for c in range(4):
            P.op('dve', lambda e, c=c: e.tensor_scalar(out=yc[:, c, :], in0=u[:, c, 1:1 + NT], scalar1=cw[:, c, 0:1], scalar2=cb[:, c:c + 1],
                                                       op0=ALU.mult, op1=ALU.add), reads=['u', 'cw', 'cb'], writes=['yc'])
            for k in range(1, 31):
                P.op('dve', lambda e, c=c, k=k: e.scalar_tensor_tensor(out=yc[:, c, :], in0=u[:, c, k + 1:k + 1 + NT], scalar=cw[:, c, k:k + 1],
                                                                      in1=yc[:, c, :], op0=ALU.mult, op1=ALU.add),
                     reads=['u', 'cw', 'yc'], writes=['yc'])
        ln_stats(yc, 4, ones512, 'ones512', 'yc', LN_EPS)
        for c in range(4):
            P.op('dve', lambda e, c=c: e.tensor_tensor(out=yn[:], in0=yc[:, c, :], in1=mean_sb[:], op=ALU.subtract),
                 reads=['yc', 'mean_sb'], writes=['yn'])
            P.op('dve', lambda e: e.tensor_tensor(out=yn[:], in0=yn[:], in1=rstd[:], op=ALU.mult), reads=['yn', 'rstd'], writes=['yn'])
            P.op('act', lambda e, c=c: e.activation(out=ycat[:, 4 + c, :], in_=yn[:], func=AF.Silu, scale=clg[:, c:c + 1], bias=clb[:, c:c + 1]),
                 reads=['yn', 'clg', 'clb'], writes=['ycat'])
        for oc in range(KC):
            ps = psA[oc % 2]
            pn = f"psA{oc % 2}"
            for k in range(KC):
                P.op('pe', lambda e, k=k, oc=oc, ps=ps: e.matmul(ps[:, 0:NT], lhsT=wout_b[:, k, oc * 128:(oc + 1) * 128], rhs=ycat[:, k, :],
                                                                 start=(k == 0), stop=(k == KC - 1)),
                     reads=['wout_b', 'ycat'], writes=[pn], inc=(k == KC - 1))
            P.op('dve', lambda e, oc=oc, ps=ps: e.scalar_tensor_tensor(out=vt[:, oc, :], in0=ps[:, 0:NT], scalar=g1a[:, oc, col:col + 1],
                                                                      in1=X[:, oc, HALO:HALO + NT], op0=ALU.mult, op1=ALU.add),
                 reads=[pn, 'g1a', xn], writes=['vt'])
        ln_stats(vt, KC, ones1024, 'ones1024', 'vt', LN_EPS / (ALPHA * ALPHA))
        XO = xo[b]
        xon = f"xo{b}"
        for oc in range(KC):
            P.op('dve', lambda e, oc=oc: e.tensor_tensor(out=vt[:, oc, :], in0=vt[:, oc, :], in1=mean_sb[:], op=ALU.subtract),
                 reads=['vt', 'mean_sb'], writes=['vt'])
            P.op('dve', lambda e, oc=oc: e.tensor_tensor(out=vt[:, oc, :], in0=vt[:, oc, :], in1=rstd[:], op=ALU.mult),
                 reads=['vt', 'rstd'], writes=['vt'])
            P.op('act', lambda e, oc=oc: e.activation(out=XO[:, oc, :], in_=vt[:, oc, :], func=AF.Identity, scale=lng[:, oc:oc + 1], bias=lnb[:, oc:oc + 1]),
                 reads=['vt', 'lng', 'lnb'], writes=[xon])
        P.dma('sp', out_d[t].rearrange("(c p) n -> p c n", p=128), XO[:], reads=[xon], writes=['x1T_scr'])
    K.end_stage()


NSLOT = 128
NEG = -1.0e30


def stage_conv(K, L, uvb):
    K.begin_stage(f"cv{L}_")
    nc, P = K.nc, K.P
    uv_d = K.din(f"peer_uv{L}", [16384, 2 * D])
    src = uv_d.rearrange("(c p j) d -> c p (j d)", p=128, j=2)
    dst = uvb.rearrange("(c p j) d -> c p (j d)", p=128, j=2)
    fb = [K.sb(f"fb{i}", [128, 4 * D]) for i in range(3)]
    bb = [K.sb(f"bb{i}", [128, 4 * D], BF16) for i in range(3)]
    for ci in range(64):
        i = ci % 3
        P.dma('sp' if ci % 2 == 0 else 'act', fb[i][:], src[ci], writes=[f"fb{i}"])
        if ci % 2 == 0:
            P.op('dve', lambda e, i=i: e.tensor_copy(out=bb[i][:], in_=fb[i][:]), reads=[f"fb{i}"], writes=[f"bb{i}"])
        else:
            P.op('pool', lambda e, i=i: e.tensor_copy(out=bb[i][:], in_=fb[i][:]), reads=[f"fb{i}"], writes=[f"bb{i}"])
        P.dma('sp' if ci % 2 == 0 else 'act', dst[ci], bb[i][:], reads=[f"bb{i}"], writes=['uvb'])
    K.end_stage()


def stage_peer(K, nsub, mcols, L, src_d, out_d, uvb):
    K.begin_stage(f"pr{L}_")
    nc, P = K.nc, K.P
    cvec_d = K.din("cvec", [128, KC, 2])
    modw_d = K.din(f"mod_w{L}", [D, 6144])
    modb_d = K.din(f"mod_bT{L}", [128, 48])
    wq_d = K.din(f"w_q{L}", [D, 2048])
    keysT_d = K.din(f"keysT{L}", [128, 2, 128])
    lng_d = K.din(f"ln_g{L}1", [1, D])
    lnb_d = K.din(f"ln_b{L}1", [1, D])
    ident_d = K.din("ident", [128, 128])
    iota_d = K.din("iota16", [128, 16])

    psq = [K.ps(f"psq{i}", [128, 512]) for i in range(2)]
    psS = K.ps("psS", [128, 2048])
    psm = K.ps("psm", [128, 512])
    psr = K.ps("psr", [128, 512])

    ident = K.sb("ident", [128, 128])
    iota16 = K.sb("iota16", [128, 16])
    ones = K.sb("ones", [128, 128])
    P.dma('sp', ident[:], ident_d, writes=['ident'])
    P.dma('sp', iota16[:], iota_d, writes=['iota16'])
    P.op('dve', lambda e: e.memset(ones[:], 1.0), writes=['ones'])
    lng = K.sb("lng", [128, D])
    lnb = K.sb("lnb", [128, D])
    P.dma('sp', lng[:], bass.AP(lng_d.tensor, 0, [[0, 128], [1, D]]), writes=['lng'])
    P.dma('sp', lnb[:], bass.AP(lnb_d.tensor, 0, [[0, 128], [1, D]]), writes=['lnb'])

    tmp = ExitStack()
    sc2p = K.sb("sc2p", [128, KC, 2])
    g2a = K.sb("g2a", [128, KC, 2])
    dg = K.sb("dg", [128, 128])
    reps = {(vn, col): K.sb(f"rep_{vn}{col}", [128, D]) for vn in ("sh2", "sc2p", "g2a") for col in range(2)}
    wq_b = K.sb("wq_b", [128, KC, 2048], BF16)
    keysT = K.sb("keysT", [128, 2, 128], BF16)
    modT = mod_setup(K, cvec_d, modw_d, modb_d, 24, 48, psm, tmp=tmp)
    stage = [tmp.enter_context(nc.sbuf_tensor(f"s_{K.pfx}stage{i}", [128, 2048], F32)) for i in range(2)]
    P.op('dve', lambda e: e.tensor_scalar(out=sc2p[:], in0=modT[:, 8:16, :], scalar1=1.0, scalar2=None, op0=ALU.add),
         reads=['modT'], writes=['sc2p'])
    P.op('dve', lambda e: e.tensor_scalar(out=g2a[:], in0=modT[:, 16:24, :], scalar1=1.0 / ALPHA, scalar2=None, op0=ALU.mult),
         reads=['modT'], writes=['g2a'])
    rep = {}
    for vn, src, j0 in (("sh2", modT, 0), ("sc2p", sc2p, 0), ("g2a", g2a, 0)):
        for col in range(2):
            r = reps[(vn, col)]
            for c in range(KC):
                P.op('dve', lambda e, c=c, src=src, col=col: e.tensor_scalar(out=dg[:], in0=ident[:], scalar1=src[:, j0 + c, col:col + 1], scalar2=None, op0=ALU.mult),
                     reads=['ident', 'modT', 'sc2p', 'g2a'], writes=['dg'])
                P.op('pe', lambda e: e.matmul(psr[:, 0:128], lhsT=ones[:], rhs=dg[:], start=True, stop=True), reads=['ones', 'dg'], writes=['psr'])
                P.op('act', lambda e, c=c, r=r: e.activation(out=r[:, c * 128:(c + 1) * 128], in_=psr[:, 0:128], func=AF.Copy),
                     reads=['psr'], writes=['rep'])
            rep[(vn, col)] = r

    for k in range(KC):
        load_cast(K, wq_b[:, k, :], wq_d[k * 128:(k + 1) * 128, :], stage[k % 2][:], f"stage{k % 2}", "wq_b", eng='dve' if k % 2 == 0 else 'act')
    load_cast(K, keysT[:], keysT_d, stage[0][:, 0:256].rearrange("p (a b) -> p a b", a=2), "stage0", "keysT")
    P.barrier()
    tmp.close()

    x1T = [K.sb(f"x1T{i}", [128, KC, 128]) for i in range(1)]
    x1t = [K.sb(f"x1t{i}", [128, D]) for i in range(1)]
    h2T = K.sb("h2T", [128, KC, 128], BF16)
    qT = K.sb("qT", [128, 16, 128], BF16)
    S = K.sb("S", [128, 16, 128])
    S2 = K.sb("S2", [128, 16, 128])
    sv = K.sb("sv", [128, 16, 16])
    si = K.sb("si", [128, 16, 16], U32)
    sif = K.sb("sif", [128, 16, 16])
    cand = K.sb("cand", [128, 8, 256])
    cand2 = S2[:].rearrange("p a b -> p (a b)").rearrange("p (h c) -> p h c", h=8)
    ts = K.sb("ts", [128, 8, 16])
    pos = K.sb("pos", [128, 8, 16], U32)
    pa = K.sb("pa_", [128, 8, 16], U32)
    pbb = K.sb("pb_", [128, 8, 16], U32)
    af = K.sb("af", [128, 8, 16])
    bf = K.sb("bf", [128, 8, 16])
    oh = K.sb("oh", [128, 8, 16, 16])
    isel = K.sb("isel", [128, 8, 16])
    jsel = K.sb("jsel", [128, 8, 16])
    eidf = K.sb("eidf", [128, 128])
    eidx = K.sb("eidx", [128, 128], I32)
    ex = K.sb("ex", [128, 8, 16])
    Z = K.sb("Z", [128, 8])
    gate = K.sb("gate", [128, 128])
    A = K.sb("A", [128, 128])
    wgt = K.sb("wgt", [128, 128])
    h2t = K.sb("h2t", [128, D], BF16)
    h2f = K.sb("h2f", [128, D])
    NB = 16
    rows = [K.sb(f"rows{i}", [128, 2 * D], BF16) for i in range(NB)]
    junk = K.sb("junk", [128, D], BF16)
    identb = K.sb("identb", [128, 128], BF16)
    P.op('dve', lambda e: e.tensor_copy(out=identb[:], in_=ident[:]), reads=['ident'], writes=['identb'])
    RG = 4
    gA = [K.sb(f"gA{i}", [128, 1]) for i in range(RG)]
    Acol = [K.sb(f"Acol{i}", [128, 1]) for i in range(RG)]
    dgs = [K.sb(f"dgs{i}", [128, 128], BF16) for i in range(RG)]
    acc = K.sb("acc", [128, D])
    stats = K.sb("stats", [128, 2, 6])
    mv = K.sb("mv", [128, 2])
    rs = K.sb("rs", [128, 1])
    xo = [K.sb(f"xo{i}", [128, D]) for i in range(1)]
    rbi = 0

    for s in range(nsub):
        col = mcols[s]
        b = 0
        XT, XK = x1T[b], x1t[b]
        xtn, xkn = f"x1T{b}", f"x1t{b}"
        P.dma('sp', XT[:], src_d[s // 2][:, (s % 2) * 128:(s % 2) * 128 + 128].rearrange("(c p) n -> p c n", p=128), reads=['x1T_scr'], writes=[xtn])
        for c in range(KC):
            pst = psm if c < 4 else psr
            P.op('pe', lambda e, c=c, pst=pst: e.transpose(out=pst[:, (c % 4) * 128:(c % 4) * 128 + 128], in_=XT[:, c, :], identity=ident[:]),
                 reads=[xtn, 'ident'], writes=['psm' if c < 4 else 'psr'])
        P.op('act', lambda e: e.activation(out=XK[:, 0:512], in_=psm[:, :], func=AF.Copy), reads=['psm'], writes=[xkn])
        P.op('dve', lambda e: e.tensor_copy(out=XK[:, 512:1024], in_=psr[:, :]), reads=['psr'], writes=[xkn])
        for c in range(KC):
            P.op('act', lambda e, c=c: e.activation(out=h2T[:, c, :], in_=XT[:, c, :], func=AF.Identity,
                                                    scale=sc2p[:, c, col:col + 1], bias=modT[:, c, col:col + 1]),
                 reads=[xtn, 'sc2p', 'modT'], writes=['h2T'])
        for hp in range(16):
            ps = psq[hp % 2]
            pn = f"psq{hp % 2}"
            for k in range(KC):
                P.op('pe', lambda e, k=k, hp=hp, ps=ps: e.matmul(ps[:, 0:128], lhsT=wq_b[:, k, hp * 128:(hp + 1) * 128], rhs=h2T[:, k, :],
                                                                 start=(k == 0), stop=(k == KC - 1)),
                     reads=['wq_b', 'h2T'], writes=[pn], inc=(k == KC - 1))
            if hp % 2 == 0:
                P.op('act', lambda e, hp=hp, ps=ps: e.activation(out=qT[:, hp, :], in_=ps[:, 0:128], func=AF.Copy), reads=[pn], writes=['qT'])
            else:
                P.op('dve', lambda e, hp=hp, ps=ps: e.tensor_copy(out=qT[:, hp, :], in_=ps[:, 0:128]), reads=[pn], writes=['qT'])
        for hp in range(16):
            P.op('pe', lambda e, hp=hp: e.matmul(psS[:, hp * 128:(hp + 1) * 128], lhsT=qT[:, hp, :], rhs=keysT[:, hp % 2, :], start=True, stop=True),
                 reads=['qT', 'keysT'], writes=['psS'], inc=(hp == 15))
        for q4 in range(4):
            P.op('act', lambda e, q4=q4: e.activation(out=S[:, 4 * q4:4 * q4 + 4, :], in_=psS[:, 512 * q4:512 * q4 + 512].rearrange("p (a b) -> p a b", a=4), func=AF.Copy),
                 reads=['psS'], writes=['S'])
        for hp in range(16):
            P.op('dve', lambda e, hp=hp: e.max(out=sv[:, hp, 0:8], in_=S[:, hp, :]), reads=['S'], writes=['sv'])
            P.op('dve', lambda e, hp=hp: e.max_index(out=si[:, hp, 0:8], in_max=sv[:, hp, 0:8], in_values=S[:, hp, :]), reads=['S', 'sv'], writes=['si'])
            P.op('dve', lambda e, hp=hp: e.match_replace(out=S2[:, hp, :], in_to_replace=sv[:, hp, 0:8], in_values=S[:, hp, :], imm_value=NEG),
                 reads=['S', 'sv'], writes=['S2'])
            P.op('dve', lambda e, hp=hp: e.max(out=sv[:, hp, 8:16], in_=S2[:, hp, :]), reads=['S2'], writes=['sv'])
            P.op('dve', lambda e, hp=hp: e.max_index(out=si[:, hp, 8:16], in_max=sv[:, hp, 8:16], in_values=S2[:, hp, :]), reads=['S2', 'sv'], writes=['si'])
        P.op('dve', lambda e: e.tensor_copy(out=sif[:], in_=si[:]), reads=['si'], writes=['sif'])
        P.op('dve', lambda e: e.tensor_tensor(out=cand[:].rearrange("p h (a b) -> p h a b", a=16),
                                              in0=fap(sv, 0, [[32, 8], [1, 16], [0, 16]]), in1=fap(sv, 16, [[32, 8], [0, 16], [1, 16]]), op=ALU.add),
             reads=['sv'], writes=['cand'])
        for h in range(8):
            P.op('dve', lambda e, h=h: e.max(out=ts[:, h, 0:8], in_=cand[:, h, :]), reads=['cand'], writes=['ts'])
            P.op('dve', lambda e, h=h: e.max_index(out=pos[:, h, 0:8], in_max=ts[:, h, 0:8], in_values=cand[:, h, :]), reads=['cand', 'ts'], writes=['pos'])
            P.op('dve', lambda e, h=h: e.match_replace(out=cand2[:, h, :], in_to_replace=ts[:, h, 0:8], in_values=cand[:, h, :], imm_value=NEG),
                 reads=['cand', 'ts'], writes=['S2'])
            P.op('dve', lambda e, h=h: e.max(out=ts[:, h, 8:16], in_=cand2[:, h, :]), reads=['S2'], writes=['ts'])
            P.op('dve', lambda e, h=h: e.max_index(out=pos[:, h, 8:16], in_max=ts[:, h, 8:16], in_values=cand2[:, h, :]), reads=['S2', 'ts'], writes=['pos'])
        P.op('dve', lambda e: e.tensor_single_scalar(out=pa[:], in_=pos[:], scalar=4, op=ALU.logical_shift_right), reads=['pos'], writes=['pa'])
        P.op('dve', lambda e: e.tensor_single_scalar(out=pbb[:], in_=pos[:], scalar=15, op=ALU.bitwise_and), reads=['pos'], writes=['pb'])
        P.op('dve', lambda e: e.tensor_copy(out=af[:], in_=pa[:]), reads=['pa'], writes=['af'])
        P.op('dve', lambda e: e.tensor_copy(out=bf[:], in_=pbb[:]), reads=['pb'], writes=['bf'])
        for (src, p_, dst, dn) in ((af, 0, isel, 'isel'), (bf, 1, jsel, 'jsel')):
            P.op('dve', lambda e, src=src: e.tensor_tensor(out=oh[:], in0=fap(src, 0, [[16, 8], [1, 16], [0, 16]]),
                                                           in1=fap(iota16, 0, [[0, 8], [0, 16], [1, 16]]), op=ALU.is_equal),
                 reads=['af', 'bf', 'iota16'], writes=['oh'])
            P.op('dve', lambda e, p_=p_: e.tensor_tensor(out=oh[:], in0=oh[:], in1=fap(sif, 16 * p_, [[32, 8], [0, 16], [1, 16]]), op=ALU.mult),
                 reads=['oh', 'sif'], writes=['oh'])
            P.op('dve', lambda e, dst=dst: e.tensor_reduce(out=dst[:], in_=oh[:], axis=AX.X, op=ALU.add), reads=['oh'], writes=[dn])
        P.op('dve', lambda e: e.scalar_tensor_tensor(out=eidf[:], in0=isel[:].rearrange("p h k -> p (h k)"), scalar=128.0,
                                                     in1=jsel[:].rearrange("p h k -> p (h k)"), op0=ALU.mult, op1=ALU.add),
             reads=['isel', 'jsel'], writes=['eidf'])
        P.op('dve', lambda e: e.tensor_copy(out=eidx[:], in_=eidf[:]), reads=['eidf'], writes=['eidx'])
        P.op('dve', lambda e: e.tensor_tensor(out=ex[:], in0=ts[:], in1=fap(ts, 0, [[16, 8], [0, 16]]), op=ALU.subtract), reads=['ts'], writes=['ex'])
        P.op('act', lambda e: e.activation(out=ex[:], in_=ex[:], func=AF.Exp), reads=['ex'], writes=['ex'])
        P.op('dve', lambda e: e.tensor_reduce(out=Z[:], in_=ex[:], axis=AX.X, op=ALU.add), reads=['ex'], writes=['Z'])
        P.op('dve', lambda e: e.reciprocal(out=Z[:], in_=Z[:]), reads=['Z'], writes=['Z'])
        P.op('dve', lambda e: e.tensor_tensor(out=gate[:].rearrange("p (h k) -> p h k", h=8), in0=ex[:], in1=fap(Z, 0, [[1, 8], [0, 16]]), op=ALU.mult),
             reads=['ex', 'Z'], writes=['gate'])
        P.op('dve', lambda e: e.tensor_tensor(out=h2f[:], in0=XK[:], in1=rep[("sc2p", col)][:], op=ALU.mult), reads=[xkn, 'rep'], writes=['h2f'])
        P.op('dve', lambda e: e.tensor_tensor(out=h2t[:], in0=h2f[:], in1=rep[("sh2", col)][:], op=ALU.add), reads=['h2f', 'rep'], writes=['h2t'])
        slot_rows = {}
        for slot in range(NSLOT + 1):
            if slot < NSLOT:
                r = rows[rbi % NB]
                rn = f"rows{rbi % NB}"
                rbi += 1
                slot_rows[slot] = (r, rn)
                g_ = slot % RG
                P.dma('pool', None, None, reads=['eidx', 'uvb'], writes=[rn],
                      fn=lambda e, r=r, slot=slot: e.indirect_dma_start(out=r[:], out_offset=None, in_=uvb,
                                                                         in_offset=bass.IndirectOffsetOnAxis(ap=eidx[:, slot:slot + 1], axis=0)))
                P.op('dve', lambda e, r=r, g_=g_: e.scalar_tensor_tensor(out=junk[:], in0=r[:, 0:D], scalar=1.0, in1=h2t[:], op0=ALU.mult, op1=ALU.mult,
                                                                         accum_out=Acol[g_][:, 0:1]),
                     reads=[rn, 'h2t'], writes=['junk', f"Acol{g_}"])
                P.op('act', lambda e, g_=g_: e.activation(out=gA[g_][:], in_=Acol[g_][:], func=AF.Gelu), reads=[f"Acol{g_}"], writes=[f"gA{g_}"])
            if slot >= 1:
                ps_ = slot - 1
                r, rn = slot_rows.pop(ps_)
                g_ = ps_ % RG
                P.op('act', lambda e, g_=g_, ps_=ps_: e.activation(out=gA[g_][:], in_=gA[g_][:], func=AF.Copy, scale=gate[:, ps_:ps_ + 1]),
                     reads=[f"gA{g_}", 'gate'], writes=[f"gA{g_}"])
                P.op('act', lambda e, g_=g_: e.activation(out=dgs[g_][:], in_=identb[:], func=AF.Copy, scale=gA[g_][:, 0:1]),
                     reads=['identb', f"gA{g_}"], writes=[f"dgs{g_}"])
                for hf_ in range(2):
                    pst = psm if hf_ == 0 else psr
                    P.op('pe', lambda e, r=r, g_=g_, hf_=hf_, pst=pst, ps_=ps_: e.matmul(pst[:, :], lhsT=dgs[g_][:], rhs=r[:, D + hf_ * 512:D + (hf_ + 1) * 512],
                                                                                       start=(ps_ == 0), stop=(ps_ == NSLOT - 1)),
                         reads=[f"dgs{g_}", rn], writes=['psm' if hf_ == 0 else 'psr'])
        P.op('dve', lambda e: e.tensor_tensor(out=acc[:, 0:512], in0=psm[:, :], in1=rep[("g2a", col)][:, 0:512], op=ALU.mult), reads=['psm', 'rep'], writes=['acc'])
        P.op('dve', lambda e: e.tensor_tensor(out=acc[:, 512:1024], in0=psr[:, :], in1=rep[("g2a", col)][:, 512:1024], op=ALU.mult), reads=['psr', 'rep'], writes=['acc'])
        P.op('dve', lambda e: e.tensor_tensor(out=acc[:], in0=acc[:], in1=XK[:], op=ALU.add), reads=['acc', xkn], writes=['acc'])
        for hf in range(2):
            P.op('dve', lambda e, hf=hf: e.bn_stats(out=stats[:, hf, :], in_=acc[:, hf * 512:(hf + 1) * 512]), reads=['acc'], writes=['stats'])
        P.op('dve', lambda e: e.bn_aggr(out=mv[:], in_=stats[:].rearrange("p a b -> p (a b)")), reads=['stats'], writes=['mv'])
        P.op('dve', lambda e: e.tensor_scalar(out=rs[:], in0=mv[:, 1:2], scalar1=LN_EPS / (ALPHA * ALPHA), scalar2=None, op0=ALU.add), reads=['mv'], writes=['rs'])
        P.op('act', lambda e: e.activation(out=rs[:], in_=rs[:], func=AF.Sqrt), reads=['rs'], writes=['rs'])
        P.op('dve', lambda e: e.reciprocal(out=rs[:], in_=rs[:]), reads=['rs'], writes=['rs'])
        XO = xo[b]
        xon = f"xo{b}"
        P.op('dve', lambda e: e.tensor_scalar(out=XO[:], in0=acc[:], scalar1=mv[:, 0:1], scalar2=rs[:, 0:1], op0=ALU.subtract, op1=ALU.mult),
             reads=['acc', 'mv', 'rs'], writes=[xon])
        P.op('dve', lambda e: e.tensor_tensor(out=XO[:], in0=XO[:], in1=lng[:], op=ALU.mult), reads=[xon, 'lng'], writes=[xon])
        P.op('dve', lambda e: e.tensor_tensor(out=XO[:], in0=XO[:], in1=lnb[:], op=ALU.add), reads=[xon, 'lnb'], writes=[xon])
        P.dma('sp', out_d[s], XO[:], reads=[xon], writes=['x2_scr'])
    K.end_stage()


CDW = 2832
RMS_EPS = 1e-6


def stage_l1a(K, ntiles, mcols, src_d, nsub, gb_):
    K.begin_stage("la_")
    nc, P = K.nc, K.P
    cos_d = K.din("cosT", [ntiles, 64, NT])
    sin_d = K.din("sinT", [ntiles, 64, NT])
    cvec_d = K.din("cvec", [128, KC, 2])
    modw_d = K.din("mod_w1", [D, 6144])
    modb_d = K.din("mod_bT1", [128, 48])
    ident_d = K.din("ident", [128, 128])
    win_d = K.din("cd_w_in", [D, CDW])
    gb_d = K.din("gate_bT", [16, 1])
    qg_d = K.din("qn_gT", [64, 1])
    kg_d = K.din("kn_gT", [64, 1])
    pm_d = K.din("ropeP", [64, 64])
    tsl = lambda t: slice(t * NT, (t + 1) * NT)
    xl_o = gb_['xl0T']

    psA = [K.ps(f"psA{i}", [128, 512]) for i in range(2)]
    psr = K.ps("psr", [128, 512])
    psw = K.ps("psw", [128, 512])
    psm = K.ps("psm", [128, 512])
    psX = [K.ps(f"psX{i}", [128, 512]) for i in range(2)]

    ident = K.sb("ident", [128, 128])
    P.dma('sp', ident[:], ident_d, writes=['ident'])
    ones64 = K.sb("ones64", [64, 64])
    P.op('dve', lambda e: e.memset(ones64[:], 1.0 / 64), writes=['ones64'])
    gb = K.sb("gb", [16, 1]); qg = K.sb("qg", [64, 1]); kg = K.sb("kg", [64, 1]); pm = K.sb("pm", [64, 64])
    for t_, d_, n_ in ((gb, gb_d, 'gb'), (qg, qg_d, 'qg'), (kg, kg_d, 'kg'), (pm, pm_d, 'pm')):
        P.dma('sp', t_[:], d_, writes=[n_])
    sc1p = K.sb("sc1p", [128, KC, 2])
    win_b = K.sb("win_b", [128, KC, CDW], BF16)
    tmp = ExitStack()
    modT = mod_setup(K, cvec_d, modw_d, modb_d, 0, 16, psm, tmp=tmp)
    stage = [tmp.enter_context(nc.sbuf_tensor(f"s_{K.pfx}stage{i}", [128, CDW], F32)) for i in range(2)]
    P.op('dve', lambda e: e.tensor_scalar(out=sc1p[:], in0=modT[:, 8:16, :], scalar1=1.0, scalar2=None, op0=ALU.add),
         reads=['modT'], writes=['sc1p'])
    for k in range(KC):
        load_cast(K, win_b[:, k, :], win_d[k * 128:(k + 1) * 128, :], stage[k % 2][:], f"stage{k % 2}", "win_b", eng='dve' if k % 2 == 0 else 'act')
    P.barrier()
    tmp.close()

    xt = [K.sb(f"xt{i}", [128, KC, NT]) for i in range(2)]
    xk = [K.sb(f"xk{i}", [128, D]) for i in range(2)]
    cs = [K.sb(f"cs{i}", [64, NT]) for i in range(2)]
    sn = [K.sb(f"sn{i}", [64, NT]) for i in range(2)]
    hT = K.sb("hT", [128, KC, NT], BF16)
    ob = [K.sb(f"ob{i}", [128, NT], BF16) for i in range(4)]
    of = [K.sb(f"of{i}", [128, NT], F32) for i in range(2)]
    sq = K.sb("sq", [64, NT]); rstd = K.sb("rstd", [64, NT]); qn = K.sb("qn", [64, NT]); t1 = K.sb("t1", [64, NT]); t2 = K.sb("t2", [64, NT])
    obi = 0
    ofi = 0

    def proj(c0, M, ps, pn):
        for k in range(KC):
            P.op('pe', lambda e, k=k: e.matmul(ps[0:M, 0:NT], lhsT=win_b[:, k, c0:c0 + M], rhs=hT[:, k, :], start=(k == 0), stop=(k == KC - 1)),
                 reads=['win_b', 'hT'], writes=[pn], inc=(k == KC - 1))

    for t in range(ntiles):
        col = mcols[t]
        b = t % 2
        X, CS, SN = xt[b], cs[b], sn[b]
        xn, csn, snn = f"xt{b}", f"cs{b}", f"sn{b}"
        for hh in range(2):
            s_ = 2 * t + hh
            if s_ >= nsub:
                P.op('dve', lambda e, hh=hh: e.memset(X[:, :, hh * 128:(hh + 1) * 128], 0.0), writes=[xn])
                continue
            XK = xk[hh]; xkn = f"xk{hh}"
            P.dma('sp', XK[:], src_d[s_], reads=['x2_scr'], writes=[xkn])
            for c in range(KC):
                pst = psX[c // 4]
                P.op('pe', lambda e, c=c, pst=pst, XK=XK: e.transpose(out=pst[:, (c % 4) * 128:(c % 4) * 128 + 128], in_=XK[:, c * 128:(c + 1) * 128], identity=ident[:]),
                     reads=[xkn, 'ident'], writes=[f'psX{c // 4}'])
            P.op('act', lambda e, hh=hh: e.activation(out=X[:, 0:4, hh * 128:(hh + 1) * 128], in_=psX[0][:, :].rearrange("p (c n) -> p c n", c=4), func=AF.Copy),
                 reads=['psX0'], writes=[xn])
            P.op('dve', lambda e, hh=hh: e.tensor_copy(out=X[:, 4:8, hh * 128:(hh + 1) * 128], in_=psX[1][:, :].rearrange("p (c n) -> p c n", c=4)),
                 reads=['psX1'], writes=[xn])
        if t < ntiles - 1:
            P.dma('sp', xl_o[t].rearrange("(c p) n -> p c n", p=128), X[:], reads=[xn], writes=['xl_o'])
        P.dma('sp', CS[:], cos_d[t], writes=[csn])
        P.dma('sp', SN[:], sin_d[t], writes=[snn])
        for c in range(KC):
            P.op('act', lambda e, c=c: e.activation(out=hT[:, c, :], in_=X[:, c, :], func=AF.Identity,
                                                    scale=sc1p[:, c, col:col + 1], bias=modT[:, c, col:col + 1]),
                 reads=[xn, 'sc1p', 'modT'], writes=['hT'])
        for oc in range(16):
            ps = psA[oc % 2]; pn = f"psA{oc % 2}"
            proj(oc * 128, 128, ps, pn)
            grp, hh = oc // 4, oc % 4
            if grp == 3:
                o = of[ofi % 2]; on = f"of{ofi % 2}"; ofi += 1
                P.op('act', lambda e, o=o, ps=ps: e.activation(out=o[:], in_=ps[:, 0:NT], func=AF.Copy), reads=[pn], writes=[on])
                P.dma('sp', gb_['om'][hh * 128:(hh + 1) * 128, tsl(t)], o[:], reads=[on], writes=['om_o'])
            else:
                o = ob[obi % 4]; on = f"ob{obi % 4}"; obi += 1
                scale = (128.0 ** -0.5) if grp == 1 else 1.0
                if oc % 2 == 0:
                    P.op('act', lambda e, o=o, ps=ps, scale=scale: e.activation(out=o[:], in_=ps[:, 0:NT], func=AF.Copy, scale=scale), reads=[pn], writes=[on])
                else:
                    P.op('dve', lambda e, o=o, ps=ps, scale=scale: e.tensor_scalar(out=o[:], in0=ps[:, 0:NT], scalar1=scale, scalar2=None, op0=ALU.mult), reads=[pn], writes=[on])
                dst = (gb_['qm'], gb_['km'], gb_['vm'])[grp]
                P.dma('sp', dst[hh * 128:(hh + 1) * 128, tsl(t)], o[:], reads=[on], writes=['qkv_o'])
        for hd in range(10):
            ps = psA[hd % 2]; pn = f"psA{hd % 2}"
            proj(2048 + hd * 64, 64, ps, pn)
            gvec, gname = (qg, 'qg') if hd < 8 else (kg, 'kg')
            P.op('act', lambda e, ps=ps: e.activation(out=sq[:], in_=ps[0:64, 0:NT], func=AF.Square), reads=[pn], writes=['sq'])
            P.op('pe', lambda e: e.matmul(psr[0:64, 0:NT], lhsT=ones64[:], rhs=sq[:], start=True, stop=True), reads=['ones64', 'sq'], writes=['psr'])
            P.op('dve', lambda e: e.tensor_scalar(out=rstd[:], in0=psr[0:64, 0:NT], scalar1=RMS_EPS, scalar2=None, op0=ALU.add), reads=['psr'], writes=['rstd'])
            P.op('act', lambda e: e.activation(out=rstd[:], in_=rstd[:], func=AF.Sqrt), reads=['rstd'], writes=['rstd'])
            P.op('dve', lambda e: e.reciprocal(out=rstd[:], in_=rstd[:]), reads=['rstd'], writes=['rstd'])
            P.op('act', lambda e, ps=ps, gvec=gvec: e.activation(out=qn[:], in_=ps[0:64, 0:NT], func=AF.Copy, scale=gvec[:, 0:1]), reads=[pn, gname], writes=['qn'])
            P.op('dve', lambda e: e.tensor_tensor(out=qn[:], in0=qn[:], in1=rstd[:], op=ALU.mult), reads=['qn', 'rstd'], writes=['qn'])
            P.op('pe', lambda e: e.matmul(psw[0:64, 0:NT], lhsT=pm[:], rhs=qn[:], start=True, stop=True), reads=['pm', 'qn'], writes=['psw'])
            P.op('dve', lambda e: e.tensor_tensor(out=t1[:], in0=qn[:], in1=CS[:], op=ALU.mult), reads=['qn', csn], writes=['t1'])
            P.op('dve', lambda e: e.tensor_tensor(out=t2[:], in0=psw[0:64, 0:NT], in1=SN[:], op=ALU.mult), reads=['psw', snn], writes=['t2'])
            o = ob[obi % 4]; on = f"ob{obi % 4}"; obi += 1
            P.op('dve', lambda e, o=o: e.tensor_tensor(out=o[0:64, :], in0=t1[:], in1=t2[:], op=ALU.add), reads=['t1', 't2'], writes=[on])
            dst = gb_['qa'][hd * 64:(hd + 1) * 64, tsl(t)] if hd < 8 else gb_['ka'][(hd - 8) * 64:(hd - 7) * 64, tsl(t)]
            P.dma('sp', dst, o[0:64, :], reads=[on], writes=['qk_o'])
        for hd in range(2):
            ps = psA[hd % 2]; pn = f"psA{hd % 2}"
            proj(2048 + 640 + hd * 64, 64, ps, pn)
            o = ob[obi % 4]; on = f"ob{obi % 4}"; obi += 1
            P.op('act', lambda e, o=o, ps=ps: e.activation(out=o[0:64, :], in_=ps[0:64, 0:NT], func=AF.Copy), reads=[pn], writes=[on])
            P.dma('sp', gb_['va'][hd * 64:(hd + 1) * 64, tsl(t)], o[0:64, :], reads=[on], writes=['va_o'])
        ps = psA[0]; pn = "psA0"
        proj(2816, 16, ps, pn)
        o = of[ofi % 2]; on = f"of{ofi % 2}"; ofi += 1
        P.op('act', lambda e, o=o, ps=ps: e.activation(out=o[0:16, :], in_=ps[0:16, 0:NT], func=AF.Identity, bias=gb[:, 0:1]), reads=[pn, 'gb'], writes=[on])
        P.dma('sp', gb_['g'][:, tsl(t)], o[0:16, :], reads=[on], writes=['gt_o'])
    K.end_stage()


NEGM = -1.0e30


def stage_attn(K, Th, TOK, gb_):
    K.begin_stage("at_")
    nc, P = K.nc, K.P
    Tq = Th
    Tk = 2 * Th + 256
    nkt = Tk // 128
    nqt = Tq // 512
    qg_d = K.din("qn_g", [1, 64])
    kg_d = K.din("kn_g", [1, 64])
    sel_d = K.din("sel", [65, 64])
    ident_d = K.din("ident", [128, 128])
    psST = [K.ps(f"psST{i}", [128, 512]) for i in range(2)]
    psO = K.ps("psO", [128, 512])
    psD = K.ps("psD", [128, 512])
    psT = K.ps("psT", [128, 512], BF16)
    sel = K.sb("sel", [65, 64])
    P.dma('sp', sel[:], sel_d, writes=['sel'])
    identf = K.sb("identf", [128, 128]); identb = K.sb("identb", [128, 128], BF16)
    P.dma('sp', identf[:], ident_d, writes=['identf'])
    P.op('dve', lambda e: e.tensor_copy(out=identb[:], in_=identf[:]), reads=['identf'], writes=['identb'])
    gq = K.sb("gq", [128, 64]); gk = K.sb("gk", [128, 64]); mq = K.sb("mq", [128, 1]); mk = K.sb("mk", [128, 1]); negC = K.sb("negC", [128, 1])
    P.dma('sp', gq[:], bass.AP(qg_d.tensor, 0, [[0, 128], [1, 64]]), writes=['gq'])
    P.dma('sp', gk[:], bass.AP(kg_d.tensor, 0, [[0, 128], [1, 64]]), writes=['gk'])
    P.op('dve', lambda e: e.tensor_reduce(out=mq[:], in_=gq[:], axis=AX.X, op=ALU.max, apply_absolute_value=True), reads=['gq'], writes=['mq'])
    P.op('dve', lambda e: e.tensor_reduce(out=mk[:], in_=gk[:], axis=AX.X, op=ALU.max, apply_absolute_value=True), reads=['gk'], writes=['mk'])
    P.op('dve', lambda e: e.scalar_tensor_tensor(out=negC[:], in0=mq[:], scalar=-8.0, in1=mk[:], op0=ALU.mult, op1=ALU.mult), reads=['mq', 'mk'], writes=['negC'])
    qT = K.sb("qT", [64, Tq], BF16)
    kT = K.sb("kT", [64, Tk], BF16)
    vT = K.sb("vT", [64, Tk], BF16)
    vaug = K.sb("vaug", [128, nkt, 65], BF16)
    PT = [K.sb(f"PT{i}", [128, 512], BF16) for i in range(2)]
    OTs = K.sb("OTs", [65, 512])
    rden = K.sb("rden", [64, 512])
    ob = [K.sb(f"ob{i}", [64, 512]) for i in range(2)]
    P.op('dve', lambda e: e.memset(vaug[:, :, 64:65], 1.0), writes=['vaug1'])
    it = 0
    for u in range(8):
        kvh = u // 4
        P.dma('sp', qT[:], gb_['qa'][u * 64:(u + 1) * 64, 0:Th], reads=['qk_o'], writes=['qT'])
        if u % 4 == 0:
            for (dstT, srcn, dn) in ((kT, 'ka_all', 'kT'), (vT, 'va_all', 'vT')):
                for m_ in range(2):
                    rows = slice(m_ * 128 + kvh * 64, m_ * 128 + kvh * 64 + 64)
                    P.dma('sp', dstT[:, m_ * 128:(m_ + 1) * 128], gb_[srcn][rows, Th:Th + 128], reads=[srcn], writes=[dn])
                    P.dma('sp', dstT[:, 256 + m_ * Th:256 + (m_ + 1) * Th], gb_[srcn][rows, 0:Th], reads=[srcn], writes=[dn])
            for kt in range(nkt):
                P.op('pe', lambda e, kt=kt: e.transpose(out=psT[:, 0:64], in_=vT[:, kt * 128:(kt + 1) * 128], identity=identb[0:64, 0:64]),
                     reads=['vT', 'identb'], writes=['psT'])
                P.op('act', lambda e, kt=kt: e.activation(out=vaug[:, kt, 0:64], in_=psT[:, 0:64], func=AF.Copy), reads=['psT'], writes=['vaug'])
        for qt in range(nqt):
            def score(kt, qt=qt):
                ps = psST[kt % 2]
                P.op('pe', lambda e, ps=ps, kt=kt, qt=qt: e.matmul(ps[:, :], lhsT=kT[:, kt * 128:(kt + 1) * 128], rhs=qT[:, qt * 512:(qt + 1) * 512], start=True, stop=True),
                     reads=['kT', 'qT'], writes=[f"psST{kt % 2}"])
            score(0)
            for kt in range(nkt):
                ps = psST[kt % 2]; pn = f"psST{kt % 2}"
                pt = PT[kt % 2]; ptn = f"PT{kt % 2}"
                if kt + 1 < nkt:
                    score(kt + 1)
                P.op('act', lambda e, ps=ps, pt=pt: e.activation(out=pt[:], in_=ps[:, :], func=AF.Exp, scale=0.125, bias=negC[:, 0:1]),
                     reads=[pn, 'negC'], writes=[ptn])
                P.op('pe', lambda e, pt=pt, kt=kt: e.matmul(psO[0:65, :], lhsT=vaug[:, kt, :], rhs=pt[:], start=(kt == 0), stop=(kt == nkt - 1)),
                     reads=['vaug', 'vaug1', ptn], writes=['psO'])
            P.op('act', lambda e: e.activation(out=OTs[:], in_=psO[0:65, :], func=AF.Copy), reads=['psO'], writes=['OTs'])
            P.op('pe', lambda e: e.matmul(psD[0:64, :], lhsT=sel[:], rhs=OTs[:], start=True, stop=True), reads=['sel', 'OTs'], writes=['psD'])
            P.op('dve', lambda e: e.reciprocal(out=rden[:], in_=psD[0:64, :]), reads=['psD'], writes=['rden'])
            o = ob[it % 2]; on = f"ob{it % 2}"; it += 1
            P.op('dve', lambda e, o=o: e.tensor_tensor(out=o[:], in0=OTs[0:64, :], in1=rden[:], op=ALU.mult), reads=['OTs', 'rden'], writes=[on])
            P.dma('sp', gb_['ya'][u * 64:(u + 1) * 64, qt * 512:(qt + 1) * 512], o[:], reads=[on], writes=['ya_l'])
    K.end_stage()


def stage_scan(K, nunits, Th, TOK, gb_):
    K.begin_stage("sc_")
    nc, P = K.nc, K.P
    ncc = 2
    nchl = 2 * Th // 128
    NCH = ncc + nchl
    cpm = Th // 128
    sidx_d = K.din("scan_idx", [128, nunits * 2], I32)
    gidx_d = K.din("scan_gidx", [128, nunits * 2], I32)
    cst_d = K.din("consts", [5, 128, 128])
    h_o = gb_['h']
    def ccol(c):
        if c < ncc:
            return c, Th
        lc = c - ncc
        return lc // cpm, (lc % cpm) * 128

    psR = K.ps("psR", [128, 512]); psQK = K.ps("psQK", [128, 512]); psT = K.ps("psT", [128, 512], BF16)
    psN = K.ps("psN", [128, 512]); psC = K.ps("psC", [128, 512]); psX = K.ps("psX", [128, 512]); psB = K.ps("psB", [128, 512])
    cst = K.sb("cst", [128, 5, 128])
    P.dma('sp', cst[:], cst_d.rearrange("c p n -> p c n"), writes=['cst'])
    ident, triL, triU, mnegL, mnegU = [cst[:, i, :] for i in range(5)]
    identb = K.sb("identb", [128, 128], BF16)
    P.op('dve', lambda e: e.tensor_copy(out=identb[:], in_=ident), reads=['cst'], writes=['identb'])
    ones = K.sb("ones", [128, 128])
    P.op('dve', lambda e: e.memset(ones[:], 1.0), writes=['ones'])
    qTm = [K.sb(f"qTm{i}", [128, TOK], BF16) for i in range(2)]
    kTm = [K.sb(f"kTm{i}", [128, TOK], BF16) for i in range(2)]
    vTm = [K.sb(f"vTm{i}", [128, TOK], BF16) for i in range(2)]
    gsel = [K.sb(f"gsel{i}", [4, TOK]) for i in range(2)]
    sidx = K.sb("sidx", [128, nunits * 2], I32); gidx = K.sb("gidx", [128, nunits * 2], I32)
    P.dma('sp', sidx[:], sidx_d, writes=['sidx'])
    P.dma('sp', gidx[:], gidx_d, writes=['gidx'])
    def qsl(c):
        m_, c0 = ccol(c)
        return qTm[m_][:, c0:c0 + 128]
    def ksl(c):
        m_, c0 = ccol(c)
        return kTm[m_][:, c0:c0 + 128]
    ktok = K.sb("ktok", [128, NCH, 128], BF16); vaug = K.sb("vaug", [128, NCH, 129], BF16)
    P.op('dve', lambda e: e.memset(vaug[:, :, 128:129], 1.0), writes=['vaug1'])
    G = K.sb("G", [128, 4, NCH])
    ee = K.sb("ee", [128, NCH]); LF = K.sb("LF", [128, NCH]); Bc = K.sb("Bc", [128, NCH]); BL = K.sb("BL", [128, NCH]); IGmB = K.sb("IGmB", [128, NCH])
    Caug = K.sb("Caug", [128, 129]); Cb = K.sb("Cb", [128, 129], BF16); m = K.sb("m", [128, 1])
    igrep = K.sb("igrep", [128, 128]); dm = K.sb("dm", [128, 128]); sexp = K.sb("sexp", [128, 128]); s_b = K.sb("s_b", [128, 128], BF16)
    sT = K.sb("sT", [128, 128], BF16); numA = K.sb("numA", [128, 129]); tot = K.sb("tot", [128, 129]); kw = K.sb("kw", [128, 128], BF16)
    col = {n: K.sb(n, [128, 1]) for n in ("rowmax", "glmax", "mnew", "nmnew", "inter", "mt", "nmt", "winter", "emt", "dd", "t2", "wk", "t3", "decay")}
    hb = [K.sb(f"hb{i}", [128, 128]) for i in range(2)]
    hi = 0

    def c1(name, fn, reads, eng='dve'):
        P.op(eng, fn, reads=reads, writes=[name])

    for u in range(nunits):
        for m_ in range(2):
            ic = u * 2 + m_
            for (dst_, srcn, dn) in ((qTm[m_], 'qm_all', 'qT'), (kTm[m_], 'km_all', 'kT'), (vTm[m_], 'vm_all', 'vT')):
                P.dma('pool', None, None, reads=[srcn, 'sidx'], writes=[dn],
                      fn=lambda e, dst_=dst_, srcn=srcn, ic=ic: e.indirect_dma_start(out=dst_[:], out_offset=None, in_=gb_[srcn],
                                                                                   in_offset=bass.IndirectOffsetOnAxis(ap=sidx[:, ic:ic + 1], axis=0)))
            P.dma('pool', None, None, reads=['g_all', 'gidx'], writes=['gsel'],
                  fn=lambda e, m_=m_, ic=ic: e.indirect_dma_start(out=gsel[m_][:], out_offset=None, in_=gb_['g_all'],
                                                                   in_offset=bass.IndirectOffsetOnAxis(ap=gidx[0:4, ic:ic + 1], axis=0)))
        for c in range(NCH):
            m_, c0 = ccol(c)
            P.op('pe', lambda e, m_=m_, c0=c0: e.transpose(out=psT[:, 0:128], in_=kTm[m_][:, c0:c0 + 128], identity=identb[:]), reads=['kT', 'identb'], writes=['psT'])
            P.op('act', lambda e, c=c: e.activation(out=ktok[:, c, :], in_=psT[:, 0:128], func=AF.Copy), reads=['psT'], writes=['ktok'])
            P.op('pe', lambda e, m_=m_, c0=c0: e.transpose(out=psT[:, 128:256], in_=vTm[m_][:, c0:c0 + 128], identity=identb[:]), reads=['vT', 'identb'], writes=['psT'])
            P.op('dve', lambda e, c=c: e.tensor_copy(out=vaug[:, c, 0:128], in_=psT[:, 128:256]), reads=['psT'], writes=['vaug'])
            P.op('pe', lambda e, m_=m_, c0=c0: e.transpose(out=psB[:, 0:4], in_=gsel[m_][0:4, c0:c0 + 128], identity=ident[0:4, 0:4]), reads=['gsel', 'cst'], writes=['psB'])
            P.op('dve', lambda e, c=c: e.tensor_copy(out=G[:, :, c], in_=psB[:, 0:4]), reads=['psB'], writes=['G'])
        for dr in range(2):
            tri = triL if dr == 0 else triU
            mneg = mnegL if dr == 0 else mnegU
            gi, gf = 2 * dr, 2 * dr + 1
            P.op('act', lambda e: e.activation(out=ee[:], in_=G[:, gf, :], func=AF.Exp, scale=-1.0), reads=['G'], writes=['ee'])
            P.op('dve', lambda e: e.tensor_scalar(out=ee[:], in0=ee[:], scalar1=1.0, scalar2=None, op0=ALU.add), reads=['ee'], writes=['ee'])
            P.op('act', lambda e: e.activation(out=ee[:], in_=ee[:], func=AF.Ln), reads=['ee'], writes=['ee'])
            P.op('dve', lambda e: e.tensor_scalar(out=LF[:], in0=ee[:], scalar1=-1.0, scalar2=None, op0=ALU.mult), reads=['ee'], writes=['LF'])
            P.op('pe', lambda e: e.matmul(psB[:, 0:NCH], lhsT=tri, rhs=LF[:], start=True, stop=True), reads=['cst', 'LF'], writes=['psB'])
            P.op('dve', lambda e: e.tensor_copy(out=Bc[:], in_=psB[:, 0:NCH]), reads=['psB'], writes=['Bc'])
            P.op('pe', lambda e: e.matmul(psB[:, 0:NCH], lhsT=ones[:], rhs=LF[:], start=True, stop=True), reads=['ones', 'LF'], writes=['psB'])
            P.op('dve', lambda e: e.tensor_copy(out=BL[:], in_=psB[:, 0:NCH]), reads=['psB'], writes=['BL'])
            P.op('dve', lambda e: e.tensor_tensor(out=IGmB[:], in0=G[:, gi, :], in1=Bc[:], op=ALU.subtract), reads=['G', 'Bc'], writes=['IGmB'])
            P.op('dve', lambda e: e.memset(Caug[:], 0.0), writes=['Caug'])
            P.op('dve', lambda e: e.memset(Cb[:], 0.0), writes=['Cb'])
            P.op('dve', lambda e: e.memset(m[:], 0.0), writes=['m'])
            order = list(range(ncc)) + [ncc + i for i in range(nchl)]
            if dr == 1:
                order = list(range(ncc))[::-1] + [ncc + i for i in range(nchl)][::-1]
            for c in order:
                lat = c >= ncc
                cc = slice(c, c + 1)
                P.op('dve', lambda e, c=c: e.tensor_copy(out=igrep[:], in_=fap(IGmB, c, [[0, 128]])), reads=['IGmB'], writes=['igrep'])
                P.op('pe', lambda e: e.matmul(psR[:, 0:128], lhsT=igrep[:], rhs=ident, start=True, stop=True), reads=['igrep', 'cst'], writes=['psR'])
                c1('glmax', lambda e: e.tensor_reduce(out=col['glmax'][:], in_=psR[:, 0:128], axis=AX.X, op=ALU.max), ['psR'])
                c1('mnew', lambda e, cc=cc: e.scalar_tensor_tensor(out=col['mnew'][:], in0=col['glmax'][:], scalar=m[:, 0:1], in1=BL[:, cc], op0=ALU.max, op1=ALU.add),
                   ['glmax', 'm', 'BL'])
                c1('nmnew', lambda e: e.tensor_scalar(out=col['nmnew'][:], in0=col['mnew'][:], scalar1=-1.0, scalar2=None, op0=ALU.mult), ['mnew'])
                if lat:
                    P.op('dve', lambda e, cc=cc: e.scalar_tensor_tensor(out=dm[:], in0=psR[:, 0:128], scalar=Bc[:, cc], in1=mneg, op0=ALU.add, op1=ALU.add),
                         reads=['psR', 'Bc', 'cst'], writes=['dm'])
                    c1('rowmax', lambda e: e.tensor_reduce(out=col['rowmax'][:], in_=dm[:], axis=AX.X, op=ALU.max), ['dm'])
                    c1('inter', lambda e, cc=cc: e.tensor_tensor(out=col['inter'][:], in0=Bc[:, cc], in1=m[:], op=ALU.add), ['Bc', 'm'])
                    c1('mt', lambda e: e.tensor_tensor(out=col['mt'][:], in0=col['inter'][:], in1=col['rowmax'][:], op=ALU.max), ['inter', 'rowmax'])
                    c1('nmt', lambda e: e.tensor_scalar(out=col['nmt'][:], in0=col['mt'][:], scalar1=-1.0, scalar2=None, op0=ALU.mult), ['mt'])
                    P.op('act', lambda e: e.activation(out=sexp[:], in_=dm[:], func=AF.Exp, bias=col['nmt'][:, 0:1]), reads=['dm', 'nmt'], writes=['sexp'])
                    c1('winter', lambda e: e.activation(out=col['winter'][:], in_=col['inter'][:], func=AF.Exp, bias=col['nmt'][:, 0:1]), ['inter', 'nmt'], eng='act')
                    c1('emt', lambda e: e.activation(out=col['emt'][:], in_=col['mt'][:], func=AF.Exp, scale=-1.0), ['mt'], eng='act')
                    P.op('pe', lambda e, c=c: e.matmul(psQK[:, 0:128], lhsT=qsl(c), rhs=ksl(c), start=True, stop=True),
                         reads=['qT', 'kT'], writes=['psQK'])
                    P.op('dve', lambda e: e.tensor_tensor(out=s_b[:], in0=psQK[:, 0:128], in1=sexp[:], op=ALU.mult), reads=['psQK', 'sexp'], writes=['s_b'])
                    P.op('pe', lambda e: e.transpose(out=psT[:, 0:128], in_=s_b[:], identity=identb[:]), reads=['s_b', 'identb'], writes=['psT'])
                    P.op('act', lambda e: e.activation(out=sT[:], in_=psT[:, 0:128], func=AF.Copy), reads=['psT'], writes=['sT'])
                    P.op('pe', lambda e, c=c: e.matmul(psN[:, 0:129], lhsT=sT[:], rhs=vaug[:, c, :], start=True, stop=True), reads=['sT', 'vaug', 'vaug1'], writes=['psN'])
                    P.op('pe', lambda e, c=c: e.matmul(psX[:, 0:129], lhsT=qsl(c), rhs=Cb[:], start=True, stop=True), reads=['qT', 'Cb'], writes=['psX'])
                    P.op('act', lambda e: e.activation(out=numA[:], in_=psN[:, 0:129], func=AF.Copy), reads=['psN'], writes=['numA'])
                    P.op('dve', lambda e: e.scalar_tensor_tensor(out=tot[:], in0=psX[:, 0:129], scalar=col['winter'][:, 0:1], in1=numA[:], op0=ALU.mult, op1=ALU.add),
                         reads=['psX', 'winter', 'numA'], writes=['tot'])
                    c1('dd', lambda e: e.activation(out=col['dd'][:], in_=tot[:, 128:129], func=AF.Abs), ['tot'], eng='act')
                    c1('dd', lambda e: e.tensor_tensor(out=col['dd'][:], in0=col['dd'][:], in1=col['emt'][:], op=ALU.max), ['dd', 'emt'])
                    c1('dd', lambda e: e.reciprocal(out=col['dd'][:], in_=col['dd'][:]), ['dd'])
                    H = hb[hi % 2]; hn = f"hb{hi % 2}"; hi += 1
                    P.op('dve', lambda e, H=H: e.tensor_scalar(out=H[:], in0=tot[:, 0:128], scalar1=col['dd'][:, 0:1], scalar2=None, op0=ALU.mult), reads=['tot', 'dd'], writes=[hn])
                    lc = c - ncc
                    r0 = ((u * 2 + dr) * nchl + lc) * 128
                    P.dma('sp', h_o[r0:r0 + 128, :], H[:], reads=[hn], writes=['h_o'])
                c1('t2', lambda e, cc=cc: e.tensor_tensor(out=col['t2'][:], in0=IGmB[:, cc], in1=BL[:, cc], op=ALU.add), ['IGmB', 'BL'])
                c1('wk', lambda e: e.activation(out=col['wk'][:], in_=col['t2'][:], func=AF.Exp, bias=col['nmnew'][:, 0:1]), ['t2', 'nmnew'], eng='act')
                P.op('dve', lambda e, c=c: e.tensor_scalar(out=kw[:], in0=ktok[:, c, :], scalar1=col['wk'][:, 0:1], scalar2=None, op0=ALU.mult), reads=['ktok', 'wk'], writes=['kw'])
                P.op('pe', lambda e, c=c: e.matmul(psC[:, 0:129], lhsT=kw[:], rhs=vaug[:, c, :], start=True, stop=True), reads=['kw', 'vaug', 'vaug1'], writes=['psC'])
                c1('t3', lambda e, cc=cc: e.tensor_tensor(out=col['t3'][:], in0=BL[:, cc], in1=m[:], op=ALU.add), ['BL', 'm'])
                c1('decay', lambda e: e.activation(out=col['decay'][:], in_=col['t3'][:], func=AF.Exp, bias=col['nmnew'][:, 0:1]), ['t3', 'nmnew'], eng='act')
                P.op('dve', lambda e: e.scalar_tensor_tensor(out=Caug[:], in0=Caug[:], scalar=col['decay'][:, 0:1], in1=psC[:, 0:129], op0=ALU.mult, op1=ALU.add),
                     reads=['Caug', 'decay', 'psC'], writes=['Caug'])
                P.op('act', lambda e: e.activation(out=Cb[:], in_=Caug[:], func=AF.Copy), reads=['Caug'], writes=['Cb'])
                P.op('dve', lambda e: e.tensor_copy(out=m[:], in_=col['mnew'][:]), reads=['mnew', 'inter', 't3', 'glmax'], writes=['m'])
    K.end_stage()


def stage_l1c(K, ntiles, out_d, gb_, half_tok):
    K.begin_stage("lc_")
    nc, P = K.nc, K.P
    xt_d = gb_['xl0T']
    hidx_d = K.din("h_idx", [128, ntiles * 2 * 8], I32)
    ident_d = K.din("ident", [128, 128])
    cvec_d = K.din("cvec", [128, KC, 2])
    modw_d = K.din("mod_w1", [D, 6144])
    modb_d = K.din("mod_bT1", [128, 48])
    ng_d = K.din("norm_gT", [128, 4])
    wout_d = K.din("cd_w_out", [D, D])
    lng_d = K.din("ln_gT10", [128, KC])
    lnb_d = K.din("ln_bT10", [128, KC])

    psA = [K.ps(f"psA{i}", [128, 512]) for i in range(2)]
    psmean = K.ps("psmean", [128, 512]); pssq = K.ps("pssq", [128, 512]); psm = K.ps("psm", [128, 512])
    psX = K.ps("psX", [128, 512])
    ident = K.sb("ident", [128, 128])
    P.dma('sp', ident[:], ident_d, writes=['ident'])
    hidx = K.sb("hidx", [128, ntiles * 2 * 8], I32)
    P.dma('sp', hidx[:], hidx_d, writes=['hidx'])
    ones128 = K.sb("ones128", [128, 128]); ones1024 = K.sb("ones1024", [128, 128])
    P.op('dve', lambda e: e.memset(ones128[:], 1.0 / 128), writes=['ones128'])
    P.op('dve', lambda e: e.memset(ones1024[:], 1.0 / 1024), writes=['ones1024'])
    ng = K.sb("ng", [128, 4]); lng = K.sb("lng", [128, KC]); lnb = K.sb("lnb", [128, KC])
    for t_, d_, n_ in ((ng, ng_d, 'ng'), (lng, lng_d, 'lng'), (lnb, lnb_d, 'lnb')):
        P.dma('sp', t_[:], d_, writes=[n_])
    g1a = K.sb("g1a", [128, KC, 2])
    wout_b = K.sb("wout_b", [128, KC, D], BF16)
    tmp = ExitStack()
    modT = mod_setup(K, cvec_d, modw_d, modb_d, 16, 24, psm, tmp=tmp)
    stage = [tmp.enter_context(nc.sbuf_tensor(f"s_{K.pfx}stage{i}", [128, D], F32)) for i in range(2)]
    P.op('dve', lambda e: e.tensor_scalar(out=g1a[:], in0=modT[:, 0:8, :], scalar1=1.0 / ALPHA, scalar2=None, op0=ALU.mult), reads=['modT'], writes=['g1a'])
    for k in range(KC):
        load_cast(K, wout_b[:, k, :], wout_d[k * 128:(k + 1) * 128, :], stage[k % 2][:], f"stage{k % 2}", "wout_b", eng='dve' if k % 2 == 0 else 'act')
    P.barrier()
    tmp.close()

    xt = [K.sb(f"xt{i}", [128, KC, NT]) for i in range(2)]
    htk = [K.sb(f"htk{i}", [128, 4, 128]) for i in range(2)]
    HS = K.sb("HS", [128, 4, NT])
    om = [K.sb(f"om{i}", [128, 4, NT]) for i in range(2)]
    ya = [K.sb(f"ya{i}", [128, 4, NT]) for i in range(2)]
    hs = K.sb("hs", [128, 1, NT]); sgm = K.sb("sgm", [128, NT])
    sqt = [K.sb(f"sqt{i}", [128, NT]) for i in range(2)]
    mean_sb = K.sb("mean_sb", [128, NT]); rstd = K.sb("rstd", [128, NT])
    ycat = K.sb("ycat", [128, KC, NT], BF16)
    vt = K.sb("vt", [128, KC, NT])
    xo = [K.sb(f"xo{i}", [128, KC, NT]) for i in range(2)]

    def ln_stats(src, nchunk, ones, onm, srcname, eps):
        for c in range(nchunk):
            P.op('pe', lambda e, c=c: e.matmul(psmean[:, 0:NT], lhsT=ones[:], rhs=src[:, c, :], start=(c == 0), stop=(c == nchunk - 1)),
                 reads=[onm, srcname], writes=['psmean'], inc=(c == nchunk - 1))
        for c in range(nchunk):
            s = sqt[c % 2]; sn = f"sqt{c % 2}"
            P.op('act', lambda e, c=c, s=s: e.activation(out=s[:], in_=src[:, c, :], func=AF.Square), reads=[srcname], writes=[sn])
            P.op('pe', lambda e, c=c, s=s: e.matmul(pssq[:, 0:NT], lhsT=ones[:], rhs=s[:], start=(c == 0), stop=(c == nchunk - 1)),
                 reads=[onm, sn], writes=['pssq'])
        P.op('act', lambda e: e.activation(out=mean_sb[:], in_=psmean[:, 0:NT], func=AF.Copy), reads=['psmean'], writes=['mean_sb'])
        P.op('dve', lambda e: e.tensor_tensor(out=rstd[:], in0=mean_sb[:], in1=mean_sb[:], op=ALU.mult), reads=['mean_sb'], writes=['rstd'])
        P.op('dve', lambda e: e.tensor_tensor(out=rstd[:], in0=pssq[:, 0:NT], in1=rstd[:], op=ALU.subtract), reads=['pssq', 'rstd'], writes=['rstd'])
        P.op('dve', lambda e: e.tensor_scalar(out=rstd[:], in0=rstd[:], scalar1=eps, scalar2=None, op0=ALU.add), reads=['rstd'], writes=['rstd'])
        P.op('act', lambda e: e.activation(out=rstd[:], in_=rstd[:], func=AF.Sqrt), reads=['rstd'], writes=['rstd'])
        P.op('dve', lambda e: e.reciprocal(out=rstd[:], in_=rstd[:]), reads=['rstd'], writes=['rstd'])

    for t in range(ntiles):
        b = t % 2
        X, OM, YA = xt[b], om[b], ya[b]
        xn, omn, yan = f"xt{b}", f"om{b}", f"ya{b}"
        P.dma('sp', X[:], xt_d[t].rearrange("(c p) n -> p c n", p=128), reads=['xl_o'], writes=[xn])
        P.dma('sp', OM[:], gb_['om'][:, t * NT:(t + 1) * NT].rearrange("(c p) n -> p c n", p=128), reads=['om_o'], writes=[omn])
        for c in range(4):
            for par in range(2):
                hd_ = 2 * c + par
                P.dma('sp', YA[par * 64:(par + 1) * 64, c, :], gb_['ya'][hd_ * 64:(hd_ + 1) * 64, t * NT:(t + 1) * NT], reads=['ya_l'], writes=[yan])
        for sub in range(2):
            s_ = 2 * t + sub
            for dr in range(2):
                for hd in range(4):
                    ic = s_ * 8 + hd * 2 + dr
                    P.dma('pool', None, None, reads=['h_all', 'hidx'], writes=[f"htk{dr}"],
                          fn=lambda e, dr=dr, hd=hd, ic=ic: e.indirect_dma_start(out=htk[dr][:, hd, :], out_offset=None, in_=gb_['h_all'],
                                                                                 in_offset=bass.IndirectOffsetOnAxis(ap=hidx[:, ic:ic + 1], axis=0)))
            P.op('dve', lambda e: e.tensor_tensor(out=htk[0][:], in0=htk[0][:], in1=htk[1][:], op=ALU.add), reads=['htk0', 'htk1'], writes=['htk0'])
            for hd in range(4):
                P.op('pe', lambda e, hd=hd: e.transpose(out=psX[:, hd * 128:(hd + 1) * 128], in_=htk[0][:, hd, :], identity=ident[:]), reads=['htk0', 'ident'], writes=['psX'])
            P.op('act', lambda e, sub=sub: e.activation(out=HS[:, :, sub * 128:(sub + 1) * 128], in_=psX[:, :].rearrange("p (c n) -> p c n", c=4), func=AF.Copy),
                 reads=['psX'], writes=['HS'])
        for hd in range(4):
            P.op('dve', lambda e, hd=hd: e.tensor_copy(out=hs[:, 0, :], in_=HS[:, hd, :]), reads=['HS'], writes=['hs'])
            ln_stats(hs, 1, ones128, 'ones128', 'hs', LN_EPS)
            P.op('dve', lambda e: e.tensor_tensor(out=hs[:, 0, :], in0=hs[:, 0, :], in1=mean_sb[:], op=ALU.subtract), reads=['hs', 'mean_sb'], writes=['hs'])
            P.op('dve', lambda e: e.tensor_tensor(out=hs[:, 0, :], in0=hs[:, 0, :], in1=rstd[:], op=ALU.mult), reads=['hs', 'rstd'], writes=['hs'])
            P.op('act', lambda e, hd=hd: e.activation(out=sgm[:], in_=OM[:, hd, :], func=AF.Sigmoid), reads=[omn], writes=['sgm'])
            P.op('dve', lambda e, hd=hd: e.scalar_tensor_tensor(out=ycat[:, hd, :], in0=hs[:, 0, :], scalar=ng[:, hd:hd + 1], in1=sgm[:], op0=ALU.mult, op1=ALU.mult),
                 reads=['hs', 'ng', 'sgm'], writes=['ycat'])
        for c in range(4):
            P.op('act', lambda e, c=c: e.activation(out=ycat[:, 4 + c, :], in_=YA[:, c, :], func=AF.Copy), reads=[yan], writes=['ycat'])
        for oc in range(KC):
            ps = psA[oc % 2]; pn = f"psA{oc % 2}"
            for k in range(KC):
                P.op('pe', lambda e, k=k, oc=oc, ps=ps: e.matmul(ps[:, 0:NT], lhsT=wout_b[:, k, oc * 128:(oc + 1) * 128], rhs=ycat[:, k, :],
                                                                 start=(k == 0), stop=(k == KC - 1)),
                     reads=['wout_b', 'ycat'], writes=[pn], inc=(k == KC - 1))
            P.op('dve', lambda e, oc=oc, ps=ps: e.scalar_tensor_tensor(out=vt[:, oc, :], in0=ps[:, 0:NT], scalar=g1a[:, oc, 0:1],
                                                                      in1=X[:, oc, :], op0=ALU.mult, op1=ALU.add),
                 reads=[pn, 'g1a', xn], writes=['vt'])
        ln_stats(vt, KC, ones1024, 'ones1024', 'vt', LN_EPS / (ALPHA * ALPHA))
        XO = xo[b]; xon = f"xo{b}"
        for oc in range(KC):
            P.op('dve', lambda e, oc=oc: e.tensor_tensor(out=vt[:, oc, :], in0=vt[:, oc, :], in1=mean_sb[:], op=ALU.subtract), reads=['vt', 'mean_sb'], writes=['vt'])
            P.op('dve', lambda e, oc=oc: e.tensor_tensor(out=vt[:, oc, :], in0=vt[:, oc, :], in1=rstd[:], op=ALU.mult), reads=['vt', 'rstd'], writes=['vt'])
            P.op('act', lambda e, oc=oc: e.activation(out=XO[:, oc, :], in_=vt[:, oc, :], func=AF.Identity, scale=lng[:, oc:oc + 1], bias=lnb[:, oc:oc + 1]),
                 reads=['vt', 'lng', 'lnb'], writes=[xon])
        P.dma('sp', out_d[t].rearrange("(c p) n -> p c n", p=128), XO[:], reads=[xon], writes=['x1T_scr'])
    K.end_stage()


def seg_tiles(seq, start, ntile):
    L = seq.shape[0]
    xt = np.zeros((ntile, D, WT), np.float32)
    cm = np.zeros((ntile, 128, WT), np.float32)
    rc = np.ones((ntile, 128, 4, NT), np.float32)
    for t in range(ntile):
        s0 = start + t * NT - HALO
        lo, hi = max(s0, 0), min(s0 + WT, L)
        if hi > lo:
            xt[t, :, lo - s0:hi - s0] = seq[lo:hi].T
            cm[t, :, lo - s0:hi - s0] = 1.0
        tok = start + t * NT + np.arange(NT)
        for g, w in enumerate(POOL_WINDOWS):
            cnt = np.minimum(tok + w // 2, L) - np.maximum(tok - w // 2, 0)
            cnt = np.where(tok < L, cnt, 1).astype(np.float32)
            rc[t, :, g, :] = (1.0 / cnt)[None, :]
    return xt, cm, rc

def fm(v, nchunk):
    return np.ascontiguousarray(np.asarray(v, np.float32).reshape(nchunk, 128).T)

def cvec_of(cb, c_ctx):
    return np.ascontiguousarray(np.stack([fm(cb, KC), fm(c_ctx, KC)], axis=-1))

def plain_tiles(seq, start, ntile):
    L, Dm = seq.shape
    out = np.zeros((ntile, Dm, NT), seq.dtype)
    for t in range(ntile):
        lo = start + t * NT
        hi = min(lo + NT, L)
        if hi > lo:
            out[t, :, :hi - lo] = seq[lo:hi].T
    return out

def rope_tiles(start, ntile, latent=True):
    cs = np.ones((ntile, 64, NT), np.float32); sn = np.zeros((ntile, 64, NT), np.float32)
    if latent:
        f = (10000.0 ** (-np.arange(16, dtype=np.float32) / 16)).astype(np.float32)
        for t in range(ntile):
            tok = start + t * NT + np.arange(NT)
            row = (tok // 64).astype(np.float32); col = (tok % 64).astype(np.float32)
            ar = (row[None, :] * f[:, None]).astype(np.float32); ac = (col[None, :] * f[:, None]).astype(np.float32)
            cs[t] = np.concatenate([np.cos(ar), np.cos(ar), np.cos(ac), np.cos(ac)], 0)
            sn[t] = np.concatenate([np.sin(ar), np.sin(ar), np.sin(ac), np.sin(ac)], 0)
    return cs, sn

def rope_perm():
    Pm = np.zeros((64, 64), np.float32)
    for base in (0, 32):
        for d in range(16):
            Pm[base + d + 16, base + d] = -1.0
            Pm[base + d, base + d + 16] = 1.0
    return Pm

CD_ORDER = None
def cd_perm_cols(w):
    return np.ascontiguousarray(np.concatenate([w[:, 0:2048], w[:, 2064:2832], w[:, 2048:2064]], axis=1))

def scan_consts():
    i = np.arange(128)
    ident = np.eye(128, dtype=np.float32)
    triL = (i[:, None] <= i[None, :]).astype(np.float32)
    triU = (i[:, None] >= i[None, :]).astype(np.float32)
    mnegL = np.where(i[None, :] <= i[:, None], 0.0, -1.0e30).astype(np.float32)
    mnegU = np.where(i[None, :] >= i[:, None], 0.0, -1.0e30).astype(np.float32)
    return np.stack([ident, triL, triU, mnegL, mnegU])

def attn_sel():
    s = np.zeros((65, 64), np.float32); s[64, :] = 1.0
    return s


_PROGS = {}
PAIRS = [[0, 1], [2, 3], [4, 5], [6, 7]]


def _prog(key, fn):
    if key not in _PROGS:
        _PROGS[key] = fn()
    return _PROGS[key]


def _allgather(K, src, dst, rn_src, rn_dst):
    P = K.P
    P._deps('pool', [rn_src], [rn_dst], True)
    ins = K.nc.gpsimd.collective_compute("AllGather", ALU.bypass, replica_groups=PAIRS, ins=[src.opt()], outs=[dst.opt()])
    P.cnt['pool'] += 1
    ins.then_inc(P.sem['pool'], 1)
    P._record(('pool', P.cnt['pool']), [rn_src], [rn_dst])


def _build_fused(ntl):
    K = Ctx()
    Th = ntl * NT
    T = 2 * Th
    TOK = (ntl + 1) * NT
    nsub = 2 * ntl + 1
    x1T_scr0 = K.scr("x1T_scr0", [ntl + 1, D, NT])
    x2_scr0 = K.scr("x2_scr0", [nsub, 128, D])
    x1T_scr1 = K.scr("x1T_scr1", [ntl, D, NT])
    gb = dict(qm=K.scr("g_qm", [512, TOK], BF16), km=K.scr("g_km", [512, TOK], BF16), vm=K.scr("g_vm", [512, TOK], BF16),
              ka=K.scr("g_ka", [128, TOK], BF16), va=K.scr("g_va", [128, TOK], BF16), g=K.scr("g_g", [16, TOK], F32),
              qa=K.scr("l_qa", [512, TOK], BF16), om=K.scr("l_om", [512, TOK], F32), xl0T=K.scr("l_xl0T", [ntl, D, NT]),
              ya=K.scr("l_ya", [512, Th]), h=K.scr("g_h", [2 * 2 * T, 128]),
              qm_all=K.scr("a_qm", [1024, TOK], BF16), km_all=K.scr("a_km", [1024, TOK], BF16), vm_all=K.scr("a_vm", [1024, TOK], BF16),
              ka_all=K.scr("a_ka", [256, TOK], BF16), va_all=K.scr("a_va", [256, TOK], BF16), g_all=K.scr("a_g", [32, TOK], F32),
              h_all=K.scr("a_h", [2 * 2 * 2 * T, 128]))
    out_d = K.dout("x2", [2 * ntl, 128, D])
    uvb = [K.scr(f"uvb{L}", [16384, 2 * D], BF16) for L in range(2)]
    stage_conv(K, 0, uvb[0])
    stage_conv(K, 1, uvb[1])
    stage_mixa(K, ntl + 1, [0] * ntl + [1], x1T_scr0)
    stage_peer(K, nsub, [0] * (2 * ntl) + [1], 0, x1T_scr0, x2_scr0, uvb[0])
    stage_l1a(K, ntl + 1, [0] * ntl + [1], x2_scr0, nsub, gb)
    for nm in ("qm", "km", "vm"):
        for hd in range(4):
            _allgather(K, gb[nm][hd * 128:(hd + 1) * 128, :], gb[nm + "_all"][hd * 256:(hd + 1) * 256, :], nm + "_src", nm + "_all")
    for nm in ("ka", "va", "g"):
        _allgather(K, gb[nm], gb[nm + "_all"], nm + "_src", nm + "_all")
    K.P.barrier()
    stage_scan(K, 2, Th, TOK, gb)
    stage_attn(K, Th, TOK, gb)
    hrows = 2 * 2 * T
    PR = min(4096, hrows)
    for k in range(hrows // PR):
        _allgather(K, gb["h"][k * PR:(k + 1) * PR, :], gb["h_all"][k * 2 * PR:(k + 1) * 2 * PR, :], "h_src", "h_all")
    K.P.barrier()
    stage_l1c(K, ntl, x1T_scr1, gb, Th)
    stage_peer(K, 2 * ntl, [0] * (2 * ntl), 1, x1T_scr1, out_d, uvb[1])
    K.close()
    return K.nc


def kernel(x, c, ctx, c_ctx, mod_w, mod_b, ln_g, ln_b, ab_w_in, pool_w, pool_ls, conv_w, conv_b, conv_ln_g, conv_ln_b, ab_w_out,
           cd_w_in, mlstm_gate_b, mlstm_norm_g, q_norm_g, k_norm_g, cd_w_out, peer_w_q, peer_keys, peer_u, peer_v):
    f32 = lambda a: np.ascontiguousarray(np.asarray(a, np.float32))
    x, c, ctx, c_ctx, mod_w, mod_b, ln_g, ln_b = map(f32, (x, c, ctx, c_ctx, mod_w, mod_b, ln_g, ln_b))
    peer_w_q, peer_keys, peer_u, peer_v = map(f32, (peer_w_q, peer_keys, peer_u, peer_v))
    cd_w_in, cd_w_out, ab_w_in, ab_w_out = map(f32, (cd_w_in, cd_w_out, ab_w_in, ab_w_out))
    B, T, _ = x.shape
    Lc = ctx.shape[1]
    NH = 2
    ncores = B * NH
    assert ncores == 8 and Lc == 256
    Th = T // NH
    Lh = Lc // NH
    ntl = Th // NT
    cores = [(i // NH, i % NH) for i in range(ncores)]
    ident = np.eye(128, dtype=np.float32)
    iota16 = np.tile(np.arange(16, dtype=np.float32), (128, 1))
    nc = _prog(("fused", ntl), lambda: _build_fused(ntl))
    uv_tabs = [np.ascontiguousarray(np.concatenate([peer_u[L], peer_v[L]], axis=1)) for L in range(2)]
    w_in_p = cd_perm_cols(cd_w_in[0])
    consts = scan_consts()
    p = np.arange(128)
    maps = []
    for (b, hf_) in cores:
        xt, cm, rc = seg_tiles(x[b], hf_ * Th, ntl)
        xt2, cm2, rc2 = seg_tiles(ctx[b], hf_ * Lh, 1)
        cs, sn = rope_tiles(hf_ * Th, ntl)
        cs2, sn2 = rope_tiles(0, 1, latent=False)
        sidx = np.zeros((128, 4), np.int32)
        gidx = np.zeros((128, 4), np.int32)
        for j in range(2):
            h = 2 * hf_ + j
            for m_ in range(2):
                sidx[:, j * 2 + m_] = (h * 2 + m_) * 128 + p
                gidx[:4, j * 2 + m_] = m_ * 16 + np.arange(4) * 4 + h
        hidx = np.zeros((128, 2 * ntl * 8), np.int32)
        for s_ in range(2 * ntl):
            for hd in range(4):
                for dr in range(2):
                    m_, u_ = hd // 2, hd % 2
                    grow = (u_ * 2 + dr) * T + hf_ * Th + s_ * 128 + p
                    PR = min(4096, 4 * T)
                    hidx[:, s_ * 8 + hd * 2 + dr] = ((grow // PR) * 2 + m_) * PR + (grow % PR)
        m = dict(
            xtiles=np.concatenate([xt, xt2]), cmask=np.concatenate([cm, cm2]), rcorr=np.concatenate([rc, rc2]),
            cvec=cvec_of(c[b], c_ctx), ab_w_in=ab_w_in[0], pool_w=f32(pool_w[0]), pool_lsT=fm(pool_ls[0], 4),
            conv_wT=np.ascontiguousarray(f32(conv_w[0]).T.reshape(4, 128, 31).transpose(1, 0, 2)),
            conv_bT=fm(conv_b[0], 4), cln_gT=fm(conv_ln_g[0], 4), cln_bT=fm(conv_ln_b[0], 4),
            ab_w_out=ab_w_out[0], ln_gT00=fm(ln_g[0, 0], KC), ln_bT00=fm(ln_b[0, 0], KC),
            cosT=np.concatenate([cs, cs2]), sinT=np.concatenate([sn, sn2]), cd_w_in=w_in_p,
            gate_bT=f32(mlstm_gate_b[0]).reshape(16, 1).copy(), qn_gT=f32(q_norm_g[0]).reshape(64, 1).copy(),
            kn_gT=f32(k_norm_g[0]).reshape(64, 1).copy(), ropeP=rope_perm(),
            scan_idx=sidx, scan_gidx=gidx, consts=consts, qn_g=f32(q_norm_g[0])[None].copy(), kn_g=f32(k_norm_g[0])[None].copy(), sel=attn_sel(),
            h_idx=hidx, norm_gT=fm(mlstm_norm_g[0], 4), cd_w_out=cd_w_out[0], ln_gT10=fm(ln_g[1, 0], KC), ln_bT10=fm(ln_b[1, 0], KC),
            ident=ident, iota16=iota16)
        for L in range(2):
            m.update({f"mod_w{L}": mod_w[L], f"mod_bT{L}": fm(mod_b[L], 48), f"w_q{L}": peer_w_q[L],
                      f"keysT{L}": np.ascontiguousarray(peer_keys[L].transpose(2, 0, 1)), f"peer_uv{L}": uv_tabs[L],
                      f"ln_g{L}1": ln_g[L, 1][None].copy(), f"ln_b{L}1": ln_b[L, 1][None].copy()})
        maps.append(m)
    res = run_bass_kernel_spmd(nc, maps, core_ids=list(range(ncores))).results
    out = np.zeros((B, T, D), np.float32)
    for i, (b, hf_) in enumerate(cores):
        out[b, hf_ * Th:(hf_ + 1) * Th] = res[i]["x2"].reshape(Th, D)
    return out
```

```python
import numpy as np
import ml_dtypes
from contextlib import ExitStack
import concourse.bass as bass
import concourse.mybir as mybir
from concourse.bass_utils import run_bass_kernel_spmd

F32 = mybir.dt.float32
BF16 = mybir.dt.bfloat16
I32 = mybir.dt.int32
U32 = mybir.dt.uint32
AF = mybir.ActivationFunctionType
ALU = mybir.AluOpType
AX = mybir.AxisListType

ENG = {'pe': 'tensor', 'act': 'scalar', 'dve': 'vector', 'pool': 'gpsimd', 'sp': 'sync'}


class Prog:
    def __init__(self, nc, ndma=20):
        self.nc = nc
        self.stack = ExitStack()
        self.sem = {e: self.stack.enter_context(nc.semaphore(f"s_{e}")) for e in ENG}
        self.cnt = {e: 0 for e in ENG}
        self.ndma = ndma
        self.dsem = [self.stack.enter_context(nc.semaphore(f"d{i}")) for i in range(ndma)]
        self.dcnt = [0] * ndma
        self.dnext = 0
        self.seen = {e: {} for e in ENG}
        self.lastw = {}
        self.readers = {}
        self.pending = {e: [] for e in ENG}
        self.nops = 0

    def eng(self, e):
        return getattr(self.nc, ENG[e])

    def _wait(self, e, key, val):
        if val <= 0:
            return
        if self.seen[e].get(key, 0) >= val:
            return
        self.seen[e][key] = val
        sem = self.sem[key] if isinstance(key, str) else self.dsem[key]
        self.eng(e).wait_ge(sem, val)

    def _deps(self, e, reads, writes, sync_same):
        toks = {}
        for r in reads:
            t = self.lastw.get(r)
            if t is not None:
                toks[t[0]] = max(toks.get(t[0], 0), t[1])
        for w in writes:
            t = self.lastw.get(w)
            if t is not None:
                toks[t[0]] = max(toks.get(t[0], 0), t[1])
            for k, v in self.readers.get(w, {}).items():
                toks[k] = max(toks.get(k, 0), v)
        for k, v in toks.items():
            if k == e and not sync_same:
                continue
            self._wait(e, k, v)

    def _record(self, tok, reads, writes):
        for r in reads:
            d = self.readers.setdefault(r, {})
            d[tok[0]] = max(d.get(tok[0], 0), tok[1])
        for w in writes:
            self.lastw[w] = tok
            self.readers[w] = {}

    def replay(self, recs, n):
        for _ in range(min(n, len(recs))):
            kind, args, kw = recs.pop(0)
            (self.op if kind == 'op' else self.dma)(*args, **kw)

    def op(self, e, fn, reads=(), writes=(), sync_same=None, inc=True):
        if getattr(self, 'rec', None) is not None:
            self.rec.append(('op', (e, fn), dict(reads=reads, writes=writes, sync_same=sync_same, inc=inc)))
            return None
        if sync_same is None:
            sync_same = (e != 'pe')
        self._deps(e, reads, writes, sync_same)
        ins = fn(self.eng(e))
        self.nops += 1
        if not inc:
            self.pending[e].append((tuple(reads), tuple(writes)))
            return ins
        self.cnt[e] += 1
        ins.then_inc(self.sem[e], 1)
        tok = (e, self.cnt[e])
        for (r, w) in self.pending[e]:
            self._record(tok, r, w)
        self.pending[e] = []
        self._record(tok, reads, writes)
        return ins

    def dma(self, e, out, in_, reads=(), writes=(), fn=None, **kw):
        if getattr(self, 'rec', None) is not None:
            self.rec.append(('dma', (e, out, in_), dict(reads=reads, writes=writes, fn=fn, **kw)))
            return None
        s = self.dnext
        self.dnext = (s + 1) % self.ndma
        self._wait(e, s, self.dcnt[s] * 16)
        self._deps(e, reads, writes, True)
        if fn is None:
            ins = self.eng(e).dma_start(out=out, in_=in_, **kw)
        else:
            ins = fn(self.eng(e))
        self.nops += 1
        self.dcnt[s] += 1
        ins.then_inc(self.dsem[s], 16)
        tok = (s, self.dcnt[s] * 16)
        self._record(tok, reads, writes)
        return ins

    def barrier(self):
        for e in ENG:
            for s_ in range(self.ndma):
                self._wait(e, s_, self.dcnt[s_] * 16)
            for k in ENG:
                if k != e:
                    self._wait(e, k, self.cnt[k])

    def finish(self, e='sp'):
        for s in range(self.ndma):
            self._wait(e, s, self.dcnt[s] * 16)
        for k in ENG:
            if k != e:
                self._wait(e, k, self.cnt[k])
        self.stack.close()


D = 1024
KC = 8
NT = 256
HALO = 16
WT = NT + 2 * HALO
ALPHA = 4.0 ** 0.25
LN_EPS = 1e-5
POOL_WINDOWS = (2, 4, 8, 16)


class Ctx:
    def __init__(self, name="k"):
        self.nc = bass.Bass("TRN2", target_bir_lowering=False)
        self.P = Prog(self.nc)
        self.es = ExitStack()
        self.pfx = ""
        self.drams = {}

    def begin_stage(self, pfx):
        self.es = ExitStack()
        self.pfx = pfx

    def end_stage(self):
        self.P.barrier()
        self.es.close()

    def sb(self, name, shape, dt=F32):
        return self.es.enter_context(self.nc.sbuf_tensor("s_" + self.pfx + name, list(shape), dt))

    def ps(self, name, shape, dt=F32):
        return self.es.enter_context(self.nc.psum_tensor("p_" + self.pfx + name, list(shape), dt))

    def _dram(self, name, shape, dt, kind):
        if name not in self.drams:
            self.drams[name] = self.nc.dram_tensor(name, list(shape), dt, kind=kind).ap()
        return self.drams[name]

    def din(self, name, shape, dt=F32):
        return self._dram(name, shape, dt, "ExternalInput")

    def dout(self, name, shape, dt=F32):
        return self._dram(name, shape, dt, "ExternalOutput")

    def scr(self, name, shape, dt=F32):
        return self._dram(name, shape, dt, "Internal")

    def close(self):
        self.P.finish('sp')


def fap(t, off, dims):
    fs = 1
    for s in t.shape[1:]:
        fs *= s
    return bass.AP(t, off, [[fs, t.shape[0]]] + [list(d) for d in dims])


def load_cast(K, dst_bf, src_ap, stage, rname, wname, eng='dve', q='sp'):
    P = K.P
    P.dma(q, stage, src_ap, writes=[rname])
    if eng == 'dve':
        P.op('dve', lambda e: e.tensor_copy(out=dst_bf, in_=stage), reads=[rname], writes=[wname])
    else:
        P.op('act', lambda e: e.activation(out=dst_bf, in_=stage, func=AF.Copy), reads=[rname], writes=[wname])


def mod_setup(K, cvec_d, modw_d, modb_d, j0, j1, psm, tmp=None):
    nc, P = K.nc, K.P
    nj = j1 - j0
    cv = K.sb("cv", [128, KC, 2])
    sc = K.sb("sc", [128, KC, 2])
    mb = K.sb("mb", [128, 48])
    modT = K.sb("modT", [128, nj, 2])
    tmp = tmp if tmp is not None else K.es
    mwblk = [tmp.enter_context(K.nc.sbuf_tensor(f"s_{K.pfx}mwblk{i}", [128, KC, 512], F32)) for i in range(2)]
    P.dma('sp', cv[:], cvec_d, writes=['cv'])
    P.dma('sp', mb[:], modb_d, writes=['mb'])
    P.op('act', lambda e: e.activation(out=sc[:], in_=cv[:], func=AF.Silu), reads=['cv'], writes=['sc'])
    assert j0 % 4 == 0 and j1 % 4 == 0
    for bi, blk in enumerate(range(j0 // 4, j1 // 4)):
        buf = mwblk[bi % 2]
        bn = f"mwblk{bi % 2}"
        P.dma('sp', buf[:], modw_d[:, blk * 512:(blk + 1) * 512].rearrange("(k p) f -> p k f", p=128), writes=[bn])
        for jj in range(4):
            j = blk * 4 + jj - j0
            for k in range(KC):
                P.op('pe', lambda e, k=k, jj=jj, j=j: e.matmul(psm[:, 2 * j:2 * j + 2], lhsT=buf[:, k, jj * 128:(jj + 1) * 128],
                                                               rhs=sc[:, k, :], start=(k == 0), stop=(k == KC - 1)),
                     reads=[bn, 'sc'], writes=['psm'], inc=(k == KC - 1))
    P.op('dve', lambda e: e.tensor_tensor(out=modT[:], in0=psm[:, 0:2 * nj].rearrange("p (j c) -> p j c", c=2),
                                          in1=fap(mb, j0, [[1, nj], [0, 2]]), op=ALU.add),
         reads=['psm', 'mb'], writes=['modT'])
    return modT


def stage_mixa(K, ntiles, mcols, out_d):
    K.begin_stage("mx_")
    nc, P = K.nc, K.P
    xt_d = K.din("xtiles", [ntiles, D, WT])
    cm_d = K.din("cmask", [ntiles, 128, WT])
    rc_d = K.din("rcorr", [ntiles, 128, 4, NT])
    cvec_d = K.din("cvec", [128, KC, 2])
    modw_d = K.din("mod_w0", [D, 6144])
    modb_d = K.din("mod_bT0", [128, 48])
    win_d = K.din("ab_w_in", [D, 1536])
    poolw_d = K.din("pool_w", [4, 128, 128])
    poolls_d = K.din("pool_lsT", [128, 4])
    convw_d = K.din("conv_wT", [128, 4, 31])
    convb_d = K.din("conv_bT", [128, 4])
    clng_d = K.din("cln_gT", [128, 4])
    clnb_d = K.din("cln_bT", [128, 4])
    wout_d = K.din("ab_w_out", [D, D])
    lng_d = K.din("ln_gT00", [128, KC])
    lnb_d = K.din("ln_bT00", [128, KC])

    psA = [K.ps(f"psA{i}", [128, 512]) for i in range(2)]
    psp = K.ps("psp", [128, 512])
    psmean = K.ps("psmean", [128, 512])
    pssq = K.ps("pssq", [128, 512])
    psm = K.ps("psm", [128, 512])

    ones512 = K.sb("ones512", [128, 128])
    ones1024 = K.sb("ones1024", [128, 128])
    P.op('dve', lambda e: e.memset(ones512[:], 1.0 / 512), writes=['ones512'])
    P.op('dve', lambda e: e.memset(ones1024[:], 1.0 / 1024), writes=['ones1024'])
    small = {}
    for nm, d, shp in [("pls", poolls_d, [128, 4]), ("cw", convw_d, [128, 4, 31]), ("cb", convb_d, [128, 4]),
                       ("clg", clng_d, [128, 4]), ("clb", clnb_d, [128, 4]), ("lng", lng_d, [128, KC]),
                       ("lnb", lnb_d, [128, KC])]:
        t = K.sb(nm, shp)
        P.dma('sp', t[:], d, writes=[nm])
        small[nm] = t
    pls, cw, cb, clg, clb, lng, lnb = [small[n] for n in ("pls", "cw", "cb", "clg", "clb", "lng", "lnb")]

    modT = mod_setup(K, cvec_d, modw_d, modb_d, 0, 24, psm)
    sc1p = K.sb("sc1p", [128, KC, 2])
    g1a = K.sb("g1a", [128, KC, 2])
    P.op('dve', lambda e: e.tensor_scalar(out=sc1p[:], in0=modT[:, 8:16, :], scalar1=1.0, scalar2=None, op0=ALU.add),
         reads=['modT'], writes=['sc1p'])
    P.op('dve', lambda e: e.tensor_scalar(out=g1a[:], in0=modT[:, 16:24, :], scalar1=1.0 / ALPHA, scalar2=None, op0=ALU.mult),
         reads=['modT'], writes=['g1a'])

    stage = [K.sb(f"stage{i}", [128, 1536]) for i in range(2)]
    win_b = K.sb("win_b", [128, KC, 1536], BF16)
    wout_b = K.sb("wout_b", [128, KC, D], BF16)
    poolw_b = K.sb("poolw_b", [128, 4, 128], BF16)
    si = 0
    for k in range(KC):
        load_cast(K, win_b[:, k, :], win_d[k * 128:(k + 1) * 128, :], stage[si % 2][:, :], f"stage{si % 2}", "win_b",
                  eng='dve' if k % 2 == 0 else 'act')
        si += 1
    for k in range(KC):
        load_cast(K, wout_b[:, k, :], wout_d[k * 128:(k + 1) * 128, :], stage[si % 2][:, 0:D], f"stage{si % 2}", "wout_b",
                  eng='dve' if k % 2 == 0 else 'act')
        si += 1
    for g in range(4):
        load_cast(K, poolw_b[:, g, :], poolw_d[g], stage[si % 2][:, 0:128], f"stage{si % 2}", "poolw_b")
        si += 1

    xt = [K.sb(f"xt{i}", [128, KC, WT]) for i in range(2)]
    cm = [K.sb(f"cm{i}", [128, WT]) for i in range(2)]
    rc = [K.sb(f"rc{i}", [128, 4, NT]) for i in range(2)]
    hT = K.sb("hT", [128, KC, WT], BF16)
    zp = K.sb("zp", [128, 4, WT])
    ua = K.sb("ua", [128, 4, WT])
    sg = K.sb("sg", [128, WT])
    u = K.sb("u", [128, 4, WT])
    pa = K.sb("pa", [128, WT])
    pb = K.sb("pb", [128, WT])
    dif = K.sb("dif", [128, NT], BF16)
    yc = K.sb("yc", [128, 4, NT])
    sqt = [K.sb(f"sqt{i}", [128, NT]) for i in range(2)]
    mean_sb = K.sb("mean_sb", [128, NT])
    rstd = K.sb("rstd", [128, NT])
    yn = K.sb("yn", [128, NT])
    ycat = K.sb("ycat", [128, KC, NT], BF16)
    vt = K.sb("vt", [128, KC, NT])
    xo = [K.sb(f"xo{i}", [128, KC, NT]) for i in range(2)]

    def ln_stats(src, nchunk, ones, onm, srcname, eps):
        for c in range(nchunk):
            P.op('pe', lambda e, c=c: e.matmul(psmean[:, 0:NT], lhsT=ones[:], rhs=src[:, c, :], start=(c == 0), stop=(c == nchunk - 1)),
                 reads=[onm, srcname], writes=['psmean'], inc=(c == nchunk - 1))
        for c in range(nchunk):
            s = sqt[c % 2]
            sn = f"sqt{c % 2}"
            P.op('act', lambda e, c=c, s=s: e.activation(out=s[:], in_=src[:, c, :], func=AF.Square), reads=[srcname], writes=[sn])
            P.op('pe', lambda e, c=c, s=s: e.matmul(pssq[:, 0:NT], lhsT=ones[:], rhs=s[:], start=(c == 0), stop=(c == nchunk - 1)),
                 reads=[onm, sn], writes=['pssq'])
        P.op('act', lambda e: e.activation(out=mean_sb[:], in_=psmean[:, 0:NT], func=AF.Copy), reads=['psmean'], writes=['mean_sb'])
        P.op('dve', lambda e: e.tensor_tensor(out=rstd[:], in0=mean_sb[:], in1=mean_sb[:], op=ALU.mult), reads=['mean_sb'], writes=['rstd'])
        P.op('dve', lambda e: e.tensor_tensor(out=rstd[:], in0=pssq[:, 0:NT], in1=rstd[:], op=ALU.subtract), reads=['pssq', 'rstd'], writes=['rstd'])
        P.op('dve', lambda e: e.tensor_scalar(out=rstd[:], in0=rstd[:], scalar1=eps, scalar2=None, op0=ALU.add),
             reads=['rstd'], writes=['rstd'])
        P.op('act', lambda e: e.activation(out=rstd[:], in_=rstd[:], func=AF.Sqrt), reads=['rstd'], writes=['rstd'])
        P.op('dve', lambda e: e.reciprocal(out=rstd[:], in_=rstd[:]), reads=['rstd'], writes=['rstd'])

    for t in range(ntiles):
        col = mcols[t]
        b = t % 2
        X, CM, RC = xt[b], cm[b], rc[b]
        xn, cmn, rcn = f"xt{b}", f"cm{b}", f"rc{b}"
        P.dma('sp', X[:], xt_d[t].rearrange("(c p) n -> p c n", p=128), writes=[xn])
        P.dma('sp', CM[:], cm_d[t], writes=[cmn])
        P.dma('sp', RC[:], rc_d[t], writes=[rcn])
        for c in range(KC):
            P.op('act', lambda e, c=c: e.activation(out=hT[:, c, :], in_=X[:, c, :], func=AF.Identity,
                                                    scale=sc1p[:, c, col:col + 1], bias=modT[:, c, col:col + 1]),
                 reads=[xn, 'sc1p', 'modT'], writes=['hT'])
        for oc in range(12):
            ps = psA[oc % 2]
            pn = f"psA{oc % 2}"
            for k in range(KC):
                P.op('pe', lambda e, k=k, oc=oc, ps=ps: e.matmul(ps[:, 0:WT], lhsT=win_b[:, k, oc * 128:(oc + 1) * 128], rhs=hT[:, k, :],
                                                                 start=(k == 0), stop=(k == KC - 1)),
                     reads=['win_b', 'hT'], writes=[pn], inc=(k == KC - 1))
            if oc < 4:
                P.op('dve', lambda e, oc=oc, ps=ps: e.tensor_tensor(out=zp[:, oc, :], in0=ps[:, 0:WT], in1=CM[:], op=ALU.mult),
                     reads=[pn, cmn], writes=['zp'])
            elif oc < 8:
                P.op('act', lambda e, oc=oc, ps=ps: e.activation(out=ua[:, oc - 4, :], in_=ps[:, 0:WT], func=AF.Copy),
                     reads=[pn], writes=['ua'])
            else:
                P.op('act', lambda e, ps=ps: e.activation(out=sg[:], in_=ps[:, 0:WT], func=AF.Sigmoid), reads=[pn], writes=['sg'])
                P.op('dve', lambda e: e.tensor_tensor(out=sg[:], in0=sg[:], in1=CM[:], op=ALU.mult), reads=['sg', cmn], writes=['sg'])
                P.op('dve', lambda e, oc=oc: e.tensor_tensor(out=u[:, oc - 8, :], in0=ua[:, oc - 8, :], in1=sg[:], op=ALU.mult),
                     reads=['ua', 'sg'], writes=['u'])
        for g in range(4):
            src = zp[:, g, :]
            srcn = 'zp'
            lo, hi = 0, WT
            bufs = [(pa, 'pa'), (pb, 'pb')]
            dst, dn = bufs[0]
            P.op('dve', lambda e, dst=dst, g=g: e.tensor_tensor(out=dst[:, 1:WT], in0=zp[:, g, 0:WT - 1], in1=zp[:, g, 1:WT], op=ALU.add),
                 reads=['zp'], writes=[dn])
            cur, cn = dst, dn
            lo, hi = 1, WT
            sh = 1
            for lvl in range(g):
                dst, dn = bufs[(lvl + 1) % 2]
                nlo, nhi = lo + sh, hi - sh
                P.op('dve', lambda e, dst=dst, cur=cur, nlo=nlo, nhi=nhi, sh=sh: e.tensor_tensor(
                    out=dst[:, nlo:nhi], in0=cur[:, nlo - sh:nhi - sh], in1=cur[:, nlo + sh:nhi + sh], op=ALU.add),
                    reads=[cn], writes=[dn])
                cur, cn = dst, dn
                lo, hi = nlo, nhi
                sh *= 2
            assert lo <= HALO and hi >= HALO + NT
            oth, on = bufs[0] if cur is bufs[1][0] else bufs[1]
            P.op('dve', lambda e, cur=cur, oth=oth, g=g: e.tensor_tensor(out=oth[:, 0:NT], in0=cur[:, HALO:HALO + NT], in1=RC[:, g, :], op=ALU.mult),
                 reads=[cn, rcn], writes=[on])
            P.op('dve', lambda e, oth=oth, g=g: e.tensor_tensor(out=dif[:], in0=oth[:, 0:NT], in1=zp[:, g, HALO:HALO + NT], op=ALU.subtract),
                 reads=[on, 'zp'], writes=['dif'])
            P.op('pe', lambda e, g=g: e.matmul(psp[:, 0:NT], lhsT=poolw_b[:, g, :], rhs=dif[:], start=True, stop=True),
                 reads=['poolw_b', 'dif'], writes=['psp'])
            P.op('act', lambda e, g=g: e.activation(out=ycat[:, g, :], in_=psp[:, 0:NT], func=AF.Copy, scale=pls[:, g:g + 1]),
                 reads=['psp', 'pls'], writes=['ycat'])
        for c in range(4):
            P.op('dve', lambda e, c=c: e.tensor_scalar(out=yc[:, c, :], in0=u[:, c, 1:1 + NT], scalar1=cw[:, c, 0:1], scalar2=cb[:, c:c + 1],
                                                       op0=ALU.mult, op1=ALU.add), reads=['u', 'cw', 'cb'], writes=['yc'])
            for k in range(1, 31):
                P.op('dve', lambda e, c=c, k=k: e.scalar_tensor_tensor(out=yc[:, c, :], in0=u[:, c, k + 1:k + 1 + NT], scalar=cw[:, c, k:k + 1],
                                                                      in1=yc[:, c, :], op0=ALU.mult, op1=ALU.add),
                     reads=['u', 'cw', 'yc'], writes=['yc'])
        ln_stats(yc, 4, ones512, 'ones512', 'yc', LN_EPS)
        for c in range(4):
            P.op('dve', lambda e, c=c: e.tensor_tensor(out=yn[:], in0=yc[:, c, :], in1=mean_sb[:], op=ALU.subtract),
                 reads=['yc', 'mean_sb'], writes=['yn'])
            P.op('dve', lambda e: e.tensor_tensor(out=yn[:], in0=yn[:], in1=rstd[:], op=ALU.mult), reads=['yn', 'rstd'], writes=['yn'])
            P.op('act', lambda e, c=c: e.activation(out=ycat[:, 4 + c, :], in_=yn[:], func=AF.Silu, scale=clg[:, c:c + 1], bias=clb[:, c:c + 1]),
                 reads=['yn', 'clg', 'clb'], writes=['ycat'])
        for oc in range(KC):
            ps = psA[oc % 2]
            pn = f"psA{oc % 2}"
            for k in range(KC):
                P.op('pe', lambda e, k=k, oc=oc, ps=ps: e.matmul(ps[:, 0:NT], lhsT=wout_b[:, k, oc * 128:(oc + 1) * 128], rhs=ycat[:, k, :],
                                                                 start=(k == 0), stop=(k == KC - 1)),
                     reads=['wout_b', 'ycat'], writes=[pn], inc=(k == KC - 1))
            P.op('dve', lambda e, oc=oc, ps=ps: e.scalar_tensor_tensor(out=vt[:, oc, :], in0=ps[:, 0:NT], scalar=g1a[:, oc, col:col + 1],
                                                                      in1=X[:, oc, HALO:HALO + NT], op0=ALU.mult, op1=ALU.add),
                 reads=[pn, 'g1a', xn], writes=['vt'])
        ln_stats(vt, KC, ones1024, 'ones1024', 'vt', LN_EPS / (ALPHA * ALPHA))
        XO = xo[b]
        xon = f"xo{b}"
        for oc in range(KC):
            P.op('dve', lambda e, oc=oc: e.tensor_tensor(out=vt[:, oc, :], in0=vt[:, oc, :], in1=mean_sb[:], op=ALU.subtract),
                 reads=['vt', 'mean_sb'], writes=['vt'])
            P.op('dve', lambda e, oc=oc: e.tensor_tensor(out=vt[:, oc, :], in0=vt[:, oc, :], in1=rstd[:], op=ALU.mult),
                 reads=['vt', 'rstd'], writes=['vt'])
            P.op('act', lambda e, oc=oc: e.activation(out=XO[:, oc, :], in_=vt[:, oc, :], func=AF.Identity, scale=lng[:, oc:oc + 1], bias=lnb[:, oc:oc + 1]),
                 reads=['vt', 'lng', 'lnb'], writes=[xon])
        P.dma('sp', out_d[t].rearrange("(c p) n -> p c n", p=128), XO[:], reads=[xon], writes=['x1T_scr'])
    K.end_stage()


NSLOT = 128
NEG = -1.0e30


def stage_conv(K, L, uvb):
    K.begin_stage(f"cv{L}_")
    nc, P = K.nc, K.P
    uv_d = K.din(f"peer_uv{L}", [16384, 2 * D])
    src = uv_d.rearrange("(c p j) d -> c p (j d)", p=128, j=2)
    dst = uvb.rearrange("(c p j) d -> c p (j d)", p=128, j=2)
    fb = [K.sb(f"fb{i}", [128, 4 * D]) for i in range(3)]
    bb = [K.sb(f"bb{i}", [128, 4 * D], BF16) for i in range(3)]
    for ci in range(64):
        i = ci % 3
        P.dma('sp' if ci % 2 == 0 else 'act', fb[i][:], src[ci], writes=[f"fb{i}"])
        if ci % 2 == 0:
            P.op('dve', lambda e, i=i: e.tensor_copy(out=bb[i][:], in_=fb[i][:]), reads=[f"fb{i}"], writes=[f"bb{i}"])
        else:
            P.op('pool', lambda e, i=i: e.tensor_copy(out=bb[i][:], in_=fb[i][:]), reads=[f"fb{i}"], writes=[f"bb{i}"])
        P.dma('sp' if ci % 2 == 0 else 'act', dst[ci], bb[i][:], reads=[f"bb{i}"], writes=['uvb'])
    K.end_stage()


def stage_peer(K, nsub, mcols, L, src_d, out_d, uvb):
    K.begin_stage(f"pr{L}_")
    nc, P = K.nc, K.P
    cvec_d = K.din("cvec", [128, KC, 2])
    modw_d = K.din(f"mod_w{L}", [D, 6144])
    modb_d = K.din(f"mod_bT{L}", [128, 48])
    wq_d = K.din(f"w_q{L}", [D, 2048])
    keysT_d = K.din(f"keysT{L}", [128, 2, 128])
    lng_d = K.din(f"ln_g{L}1", [1, D])
    lnb_d = K.din(f"ln_b{L}1", [1, D])
    ident_d = K.din("ident", [128, 128])
    iota_d = K.din("iota16", [128, 16])

    psq = [K.ps(f"psq{i}", [128, 512]) for i in range(2)]
    psS = K.ps("psS", [128, 1024])
    psX = [K.ps(f"psX{i}", [128, 512]) for i in range(2)]
    psm = K.ps("psm", [128, 512])
    psr = K.ps("psr", [128, 512])

    ident = K.sb("ident", [128, 128])
    iota16 = K.sb("iota16", [128, 16])
    ones = K.sb("ones", [128, 128])
    P.dma('sp', ident[:], ident_d, writes=['ident'])
    P.dma('sp', iota16[:], iota_d, writes=['iota16'])
    P.op('dve', lambda e: e.memset(ones[:], 1.0), writes=['ones'])
    lng = K.sb("lng", [128, D])
    lnb = K.sb("lnb", [128, D])
    P.dma('sp', lng[:], bass.AP(lng_d.tensor, 0, [[0, 128], [1, D]]), writes=['lng'])
    P.dma('sp', lnb[:], bass.AP(lnb_d.tensor, 0, [[0, 128], [1, D]]), writes=['lnb'])

    tmp = ExitStack()
    sc2p = K.sb("sc2p", [128, KC, 2])
    g2a = K.sb("g2a", [128, KC, 2])
    dg = K.sb("dg", [128, 128])
    reps = {(vn, col): K.sb(f"rep_{vn}{col}", [128, D]) for vn in ("sh2", "sc2p", "g2a") for col in range(2)}
    wq_b = K.sb("wq_b", [128, KC, 2048], BF16)
    keysT = K.sb("keysT", [128, 2, 128], BF16)
    modT = mod_setup(K, cvec_d, modw_d, modb_d, 24, 48, psm, tmp=tmp)
    stage = [tmp.enter_context(nc.sbuf_tensor(f"s_{K.pfx}stage{i}", [128, 2048], F32)) for i in range(2)]
    P.op('dve', lambda e: e.tensor_scalar(out=sc2p[:], in0=modT[:, 8:16, :], scalar1=1.0, scalar2=None, op0=ALU.add),
         reads=['modT'], writes=['sc2p'])
    P.op('dve', lambda e: e.tensor_scalar(out=g2a[:], in0=modT[:, 16:24, :], scalar1=1.0 / ALPHA, scalar2=None, op0=ALU.mult),
         reads=['modT'], writes=['g2a'])
    rep = {}
    for vn, src, j0 in (("sh2", modT, 0), ("sc2p", sc2p, 0), ("g2a", g2a, 0)):
        for col in range(2):
            r = reps[(vn, col)]
            for c in range(KC):
                P.op('dve', lambda e, c=c, src=src, col=col: e.tensor_scalar(out=dg[:], in0=ident[:], scalar1=src[:, j0 + c, col:col + 1], scalar2=None, op0=ALU.mult),
                     reads=['ident', 'modT', 'sc2p', 'g2a'], writes=['dg'])
                P.op('pe', lambda e: e.matmul(psr[:, 0:128], lhsT=ones[:], rhs=dg[:], start=True, stop=True), reads=['ones', 'dg'], writes=['psr'])
                P.op('act', lambda e, c=c, r=r: e.activation(out=r[:, c * 128:(c + 1) * 128], in_=psr[:, 0:128], func=AF.Copy),
                     reads=['psr'], writes=['rep'])
            rep[(vn, col)] = r

    for k in range(KC):
        load_cast(K, wq_b[:, k, :], wq_d[k * 128:(k + 1) * 128, :], stage[k % 2][:], f"stage{k % 2}", "wq_b", eng='dve' if k % 2 == 0 else 'act')
    load_cast(K, keysT[:], keysT_d, stage[0][:, 0:256].rearrange("p (a b) -> p a b", a=2), "stage0", "keysT")
    P.barrier()
    tmp.close()

    x1T = [K.sb(f"x1T{i}", [128, KC, 128]) for i in range(1)]
    x1t = [K.sb(f"x1t{i}", [128, D]) for i in range(2)]
    h2T = K.sb("h2T", [128, KC, 128], BF16)
    qT = K.sb("qT", [128, 16, 128], BF16)
    S = K.sb("S", [128, 16, 128])
    S2 = K.sb("S2", [128, 16, 128])
    sv = K.sb("sv", [128, 16, 16])
    si = K.sb("si", [128, 16, 16], U32)
    sif = K.sb("sif", [128, 16, 16])
    cand = K.sb("cand", [128, 8, 256])
    cand2 = S2[:].rearrange("p a b -> p (a b)").rearrange("p (h c) -> p h c", h=8)
    ts = K.sb("ts", [128, 8, 16])
    pos = K.sb("pos", [128, 8, 16], U32)
    pa = K.sb("pa_", [128, 8, 16], U32)
    pbb = K.sb("pb_", [128, 8, 16], U32)
    af = K.sb("af", [128, 8, 16])
    bf = K.sb("bf", [128, 8, 16])
    oh = K.sb("oh", [128, 8, 16, 16])
    isel = K.sb("isel", [128, 8, 16])
    jsel = K.sb("jsel", [128, 8, 16])
    eidf = K.sb("eidf", [128, 128])
    eidx2 = [K.sb(f"eidx{i}", [128, 128], I32) for i in range(2)]
    ex = K.sb("ex", [128, 8, 16])
    Z = K.sb("Z", [128, 8])
    gate2 = [K.sb(f"gate{i}", [128, 128]) for i in range(2)]
    A = K.sb("A", [128, 128])
    wgt = K.sb("wgt", [128, 128])
    h2t2 = [K.sb(f"h2t{i}", [128, D], BF16) for i in range(2)]
    h2f = K.sb("h2f", [128, D])
    NB = 12
    rows = [K.sb(f"rows{i}", [128, 2 * D], BF16) for i in range(NB)]
    junk = K.sb("junk", [128, D], BF16)
    identb = K.sb("identb", [128, 128], BF16)
    P.op('dve', lambda e: e.tensor_copy(out=identb[:], in_=ident[:]), reads=['ident'], writes=['identb'])
    RG = 4
    gA = [K.sb(f"gA{i}", [128, 1]) for i in range(RG)]
    Acol = [K.sb(f"Acol{i}", [128, 1]) for i in range(RG)]
    dgs = [K.sb(f"dgs{i}", [128, 128], BF16) for i in range(RG)]
    acc = K.sb("acc", [128, D])
    stats = K.sb("stats", [128, 2, 6])
    mv = K.sb("mv", [128, 2])
    rs = K.sb("rs", [128, 1])
    xo = [K.sb(f"xo{i}", [128, D]) for i in range(1)]
    rbi = 0

    def front(s):
        col = mcols[s]
        par = s % 2
        XT, XK = x1T[0], x1t[par]
        xtn, xkn = "x1T0", f"x1t{par}"
        eidx, gate, h2t = eidx2[par], gate2[par], h2t2[par]
        en, gn, hn_ = f"eidx{par}", f"gate{par}", f"h2t{par}"
        P.dma('sp', XT[:], src_d[s // 2][:, (s % 2) * 128:(s % 2) * 128 + 128].rearrange("(c p) n -> p c n", p=128), reads=['x1T_scr'], writes=[xtn])
        for c in range(KC):
            pst = psX[c // 4]
            P.op('pe', lambda e, c=c, pst=pst: e.transpose(out=pst[:, (c % 4) * 128:(c % 4) * 128 + 128], in_=XT[:, c, :], identity=ident[:]),
                 reads=[xtn, 'ident'], writes=[f'psX{c // 4}'])
        P.op('act', lambda e: e.activation(out=XK[:, 0:512], in_=psX[0][:, :], func=AF.Copy), reads=['psX0'], writes=[xkn])
        P.op('act', lambda e: e.activation(out=XK[:, 512:1024], in_=psX[1][:, :], func=AF.Copy), reads=['psX1'], writes=[xkn])
        for c in range(KC):
            P.op('act', lambda e, c=c: e.activation(out=h2T[:, c, :], in_=XT[:, c, :], func=AF.Identity,
                                                    scale=sc2p[:, c, col:col + 1], bias=modT[:, c, col:col + 1]),
                 reads=[xtn, 'sc2p', 'modT'], writes=['h2T'])
        for hp in range(16):
            ps = psq[hp % 2]
            pn = f"psq{hp % 2}"
            for k in range(KC):
                P.op('pe', lambda e, k=k, hp=hp, ps=ps: e.matmul(ps[:, 0:128], lhsT=wq_b[:, k, hp * 128:(hp + 1) * 128], rhs=h2T[:, k, :],
                                                                 start=(k == 0), stop=(k == KC - 1)),
                     reads=['wq_b', 'h2T'], writes=[pn], inc=(k == KC - 1))
            if hp % 2 == 0:
                P.op('act', lambda e, hp=hp, ps=ps: e.activation(out=qT[:, hp, :], in_=ps[:, 0:128], func=AF.Copy), reads=[pn], writes=['qT'])
            else:
                P.op('dve', lambda e, hp=hp, ps=ps: e.tensor_copy(out=qT[:, hp, :], in_=ps[:, 0:128]), reads=[pn], writes=['qT'])
        for rnd in range(2):
            for j in range(8):
                hp = rnd * 8 + j
                P.op('pe', lambda e, hp=hp, j=j: e.matmul(psS[:, j * 128:(j + 1) * 128], lhsT=qT[:, hp, :], rhs=keysT[:, hp % 2, :], start=True, stop=True),
                     reads=['qT', 'keysT'], writes=['psS'], inc=(j == 7))
            for q4 in range(2):
                P.op('act', lambda e, q4=q4, rnd=rnd: e.activation(out=S[:, rnd * 8 + 4 * q4:rnd * 8 + 4 * q4 + 4, :],
                                                                  in_=psS[:, 512 * q4:512 * q4 + 512].rearrange("p (a b) -> p a b", a=4), func=AF.Copy),
                     reads=['psS'], writes=['S'])
        for hp in range(16):
            P.op('dve', lambda e, hp=hp: e.max(out=sv[:, hp, 0:8], in_=S[:, hp, :]), reads=['S'], writes=['sv'])
            P.op('dve', lambda e, hp=hp: e.max_index(out=si[:, hp, 0:8], in_max=sv[:, hp, 0:8], in_values=S[:, hp, :]), reads=['S', 'sv'], writes=['si'])
            P.op('dve', lambda e, hp=hp: e.match_replace(out=S2[:, hp, :], in_to_replace=sv[:, hp, 0:8], in_values=S[:, hp, :], imm_value=NEG),
                 reads=['S', 'sv'], writes=['S2'])
            P.op('dve', lambda e, hp=hp: e.max(out=sv[:, hp, 8:16], in_=S2[:, hp, :]), reads=['S2'], writes=['sv'])
            P.op('dve', lambda e, hp=hp: e.max_index(out=si[:, hp, 8:16], in_max=sv[:, hp, 8:16], in_values=S2[:, hp, :]), reads=['S2', 'sv'], writes=['si'])
        P.op('dve', lambda e: e.tensor_copy(out=sif[:], in_=si[:]), reads=['si'], writes=['sif'])
        P.op('dve', lambda e: e.tensor_tensor(out=cand[:].rearrange("p h (a b) -> p h a b", a=16),
                                              in0=fap(sv, 0, [[32, 8], [1, 16], [0, 16]]), in1=fap(sv, 16, [[32, 8], [0, 16], [1, 16]]), op=ALU.add),
             reads=['sv'], writes=['cand'])
        for h in range(8):
            P.op('dve', lambda e, h=h: e.max(out=ts[:, h, 0:8], in_=cand[:, h, :]), reads=['cand'], writes=['ts'])
            P.op('dve', lambda e, h=h: e.max_index(out=pos[:, h, 0:8], in_max=ts[:, h, 0:8], in_values=cand[:, h, :]), reads=['cand', 'ts'], writes=['pos'])
            P.op('dve', lambda e, h=h: e.match_replace(out=cand2[:, h, :], in_to_replace=ts[:, h, 0:8], in_values=cand[:, h, :], imm_value=NEG),
                 reads=['cand', 'ts'], writes=['S2'])
            P.op('dve', lambda e, h=h: e.max(out=ts[:, h, 8:16], in_=cand2[:, h, :]), reads=['S2'], writes=['ts'])
            P.op('dve', lambda e, h=h: e.max_index(out=pos[:, h, 8:16], in_max=ts[:, h, 8:16], in_values=cand2[:, h, :]), reads=['S2', 'ts'], writes=['pos'])
        P.op('dve', lambda e: e.tensor_single_scalar(out=pa[:], in_=pos[:], scalar=4, op=ALU.logical_shift_right), reads=['pos'], writes=['pa'])
        P.op('dve', lambda e: e.tensor_single_scalar(out=pbb[:], in_=pos[:], scalar=15, op=ALU.bitwise_and), reads=['pos'], writes=['pb'])
        P.op('dve', lambda e: e.tensor_copy(out=af[:], in_=pa[:]), reads=['pa'], writes=['af'])
        P.op('dve', lambda e: e.tensor_copy(out=bf[:], in_=pbb[:]), reads=['pb'], writes=['bf'])
        for (src, p_, dst, dn) in ((af, 0, isel, 'isel'), (bf, 1, jsel, 'jsel')):
            P.op('dve', lambda e, src=src: e.tensor_tensor(out=oh[:], in0=fap(src, 0, [[16, 8], [1, 16], [0, 16]]),
                                                           in1=fap(iota16, 0, [[0, 8], [0, 16], [1, 16]]), op=ALU.is_equal),
                 reads=['af', 'bf', 'iota16'], writes=['oh'])
            P.op('dve', lambda e, p_=p_: e.tensor_tensor(out=oh[:], in0=oh[:], in1=fap(sif, 16 * p_, [[32, 8], [0, 16], [1, 16]]), op=ALU.mult),
                 reads=['oh', 'sif'], writes=['oh'])
            P.op('dve', lambda e, dst=dst: e.tensor_reduce(out=dst[:], in_=oh[:], axis=AX.X, op=ALU.add), reads=['oh'], writes=[dn])
        P.op('dve', lambda e: e.scalar_tensor_tensor(out=eidf[:], in0=isel[:].rearrange("p h k -> p (h k)"), scalar=128.0,
                                                     in1=jsel[:].rearrange("p h k -> p (h k)"), op0=ALU.mult, op1=ALU.add),
             reads=['isel', 'jsel'], writes=['eidf'])
        P.op('dve', lambda e: e.tensor_copy(out=eidx[:], in_=eidf[:]), reads=['eidf'], writes=[en])
        P.op('dve', lambda e: e.tensor_tensor(out=ex[:], in0=ts[:], in1=fap(ts, 0, [[16, 8], [0, 16]]), op=ALU.subtract), reads=['ts'], writes=['ex'])
        P.op('act', lambda e: e.activation(out=ex[:], in_=ex[:], func=AF.Exp), reads=['ex'], writes=['ex'])
        P.op('dve', lambda e: e.tensor_reduce(out=Z[:], in_=ex[:], axis=AX.X, op=ALU.add), reads=['ex'], writes=['Z'])
        P.op('dve', lambda e: e.reciprocal(out=Z[:], in_=Z[:]), reads=['Z'], writes=['Z'])
        P.op('dve', lambda e: e.tensor_tensor(out=gate[:].rearrange("p (h k) -> p h k", h=8), in0=ex[:], in1=fap(Z, 0, [[1, 8], [0, 16]]), op=ALU.mult),
             reads=['ex', 'Z'], writes=[gn])
        P.op('dve', lambda e: e.tensor_tensor(out=h2f[:], in0=XK[:], in1=rep[("sc2p", col)][:], op=ALU.mult), reads=[xkn, 'rep'], writes=['h2f'])
        P.op('dve', lambda e: e.tensor_tensor(out=h2t[:], in0=h2f[:], in1=rep[("sh2", col)][:], op=ALU.add), reads=['h2f', 'rep'], writes=[hn_])
    def back(s, recs):
        nonlocal rbi
        col = mcols[s]
        par = s % 2
        XK, xkn = x1t[par], f"x1t{par}"
        eidx, gate, h2t = eidx2[par], gate2[par], h2t2[par]
        en, gn, hn_ = f"eidx{par}", f"gate{par}", f"h2t{par}"
        b = 0
        per = (len(recs) + NSLOT - 1) // NSLOT
        slot_rows = {}
        for slot in range(NSLOT + 1):
            if slot < NSLOT:
                r = rows[rbi % NB]
                rn = f"rows{rbi % NB}"
                rbi += 1
                slot_rows[slot] = (r, rn)
                g_ = slot % RG
                P.dma('pool', None, None, reads=[en, 'uvb'], writes=[rn],
                      fn=lambda e, r=r, slot=slot: e.indirect_dma_start(out=r[:], out_offset=None, in_=uvb,
                                                                         in_offset=bass.IndirectOffsetOnAxis(ap=eidx[:, slot:slot + 1], axis=0)))
                P.op('dve', lambda e, r=r, g_=g_: e.scalar_tensor_tensor(out=junk[:], in0=r[:, 0:D], scalar=1.0, in1=h2t[:], op0=ALU.mult, op1=ALU.mult,
                                                                         accum_out=Acol[g_][:, 0:1]),
                     reads=[rn, hn_], writes=['junk', f"Acol{g_}"])
                P.op('act', lambda e, g_=g_: e.activation(out=gA[g_][:], in_=Acol[g_][:], func=AF.Gelu), reads=[f"Acol{g_}"], writes=[f"gA{g_}"])
            if slot >= 1:
                ps_ = slot - 1
                r, rn = slot_rows.pop(ps_)
                g_ = ps_ % RG
                P.op('act', lambda e, g_=g_, ps_=ps_: e.activation(out=gA[g_][:], in_=gA[g_][:], func=AF.Copy, scale=gate[:, ps_:ps_ + 1]),
                     reads=[f"gA{g_}", gn], writes=[f"gA{g_}"])
                P.op('act', lambda e, g_=g_: e.activation(out=dgs[g_][:], in_=identb[:], func=AF.Copy, scale=gA[g_][:, 0:1]),
                     reads=['identb', f"gA{g_}"], writes=[f"dgs{g_}"])
                for hf_ in range(2):
                    pst = psm if hf_ == 0 else psr
                    P.op('pe', lambda e, r=r, g_=g_, hf_=hf_, pst=pst, ps_=ps_: e.matmul(pst[:, :], lhsT=dgs[g_][:], rhs=r[:, D + hf_ * 512:D + (hf_ + 1) * 512],
                                                                                       start=(ps_ == 0), stop=(ps_ == NSLOT - 1)),
                         reads=[f"dgs{g_}", rn], writes=['psm' if hf_ == 0 else 'psr'])
            P.replay(recs, per)
        P.replay(recs, len(recs))
        P.op('dve', lambda e: e.tensor_tensor(out=acc[:, 0:512], in0=psm[:, :], in1=rep[("g2a", col)][:, 0:512], op=ALU.mult), reads=['psm', 'rep'], writes=['acc'])
        P.op('dve', lambda e: e.tensor_tensor(out=acc[:, 512:1024], in0=psr[:, :], in1=rep[("g2a", col)][:, 512:1024], op=ALU.mult), reads=['psr', 'rep'], writes=['acc'])
        P.op('dve', lambda e: e.tensor_tensor(out=acc[:], in0=acc[:], in1=XK[:], op=ALU.add), reads=['acc', xkn], writes=['acc'])
        for hf in range(2):
            P.op('dve', lambda e, hf=hf: e.bn_stats(out=stats[:, hf, :], in_=acc[:, hf * 512:(hf + 1) * 512]), reads=['acc'], writes=['stats'])
        P.op('dve', lambda e: e.bn_aggr(out=mv[:], in_=stats[:].rearrange("p a b -> p (a b)")), reads=['stats'], writes=['mv'])
        P.op('dve', lambda e: e.tensor_scalar(out=rs[:], in0=mv[:, 1:2], scalar1=LN_EPS / (ALPHA * ALPHA), scalar2=None, op0=ALU.add), reads=['mv'], writes=['rs'])
        P.op('act', lambda e: e.activation(out=rs[:], in_=rs[:], func=AF.Sqrt), reads=['rs'], writes=['rs'])
        P.op('dve', lambda e: e.reciprocal(out=rs[:], in_=rs[:]), reads=['rs'], writes=['rs'])
        XO = xo[b]
        xon = f"xo{b}"
        P.op('dve', lambda e: e.tensor_scalar(out=XO[:], in0=acc[:], scalar1=mv[:, 0:1], scalar2=rs[:, 0:1], op0=ALU.subtract, op1=ALU.mult),
             reads=['acc', 'mv', 'rs'], writes=[xon])
        P.op('dve', lambda e: e.tensor_tensor(out=XO[:], in0=XO[:], in1=lng[:], op=ALU.mult), reads=[xon, 'lng'], writes=[xon])
        P.op('dve', lambda e: e.tensor_tensor(out=XO[:], in0=XO[:], in1=lnb[:], op=ALU.add), reads=[xon, 'lnb'], writes=[xon])
        P.dma('sp', out_d[s], XO[:], reads=[xon], writes=['x2_scr'])

    front(0)
    for s in range(nsub):
        recs = []
        if s + 1 < nsub:
            P.rec = recs
            front(s + 1)
            P.rec = None
        back(s, recs)
    K.end_stage()


CDW = 2832
RMS_EPS = 1e-6


def stage_l1a(K, ntiles, mcols, src_d, nsub, gb_):
    K.begin_stage("la_")
    nc, P = K.nc, K.P
    cos_d = K.din("cosT", [ntiles, 64, NT])
    sin_d = K.din("sinT", [ntiles, 64, NT])
    cvec_d = K.din("cvec", [128, KC, 2])
    modw_d = K.din("mod_w1", [D, 6144])
    modb_d = K.din("mod_bT1", [128, 48])
    ident_d = K.din("ident", [128, 128])
    win_d = K.din("cd_w_in", [D, CDW])
    gb_d = K.din("gate_bT", [16, 1])
    qg_d = K.din("qn_gT", [64, 1])
    kg_d = K.din("kn_gT", [64, 1])
    pm_d = K.din("ropeP", [64, 64])
    tsl = lambda t: slice(t * NT, (t + 1) * NT)
    xl_o = gb_['xl0T']

    psA = [K.ps(f"psA{i}", [128, 512]) for i in range(2)]
    psr = K.ps("psr", [128, 512])
    psw = K.ps("psw", [128, 512])
    psm = K.ps("psm", [128, 512])
    psX = [K.ps(f"psX{i}", [128, 512]) for i in range(2)]

    ident = K.sb("ident", [128, 128])
    P.dma('sp', ident[:], ident_d, writes=['ident'])
    ones64 = K.sb("ones64", [64, 64])
    P.op('dve', lambda e: e.memset(ones64[:], 1.0 / 64), writes=['ones64'])
    gb = K.sb("gb", [16, 1]); qg = K.sb("qg", [64, 1]); kg = K.sb("kg", [64, 1]); pm = K.sb("pm", [64, 64])
    for t_, d_, n_ in ((gb, gb_d, 'gb'), (qg, qg_d, 'qg'), (kg, kg_d, 'kg'), (pm, pm_d, 'pm')):
        P.dma('sp', t_[:], d_, writes=[n_])
    sc1p = K.sb("sc1p", [128, KC, 2])
    win_b = K.sb("win_b", [128, KC, CDW], BF16)
    tmp = ExitStack()
    modT = mod_setup(K, cvec_d, modw_d, modb_d, 0, 16, psm, tmp=tmp)
    stage = [tmp.enter_context(nc.sbuf_tensor(f"s_{K.pfx}stage{i}", [128, CDW], F32)) for i in range(2)]
    P.op('dve', lambda e: e.tensor_scalar(out=sc1p[:], in0=modT[:, 8:16, :], scalar1=1.0, scalar2=None, op0=ALU.add),
         reads=['modT'], writes=['sc1p'])
    for k in range(KC):
        load_cast(K, win_b[:, k, :], win_d[k * 128:(k + 1) * 128, :], stage[k % 2][:], f"stage{k % 2}", "win_b", eng='dve' if k % 2 == 0 else 'act')
    P.barrier()
    tmp.close()

    xt = [K.sb(f"xt{i}", [128, KC, NT]) for i in range(2)]
    xk = [K.sb(f"xk{i}", [128, D]) for i in range(2)]
    cs = [K.sb(f"cs{i}", [64, NT]) for i in range(2)]
    sn = [K.sb(f"sn{i}", [64, NT]) for i in range(2)]
    hT = K.sb("hT", [128, KC, NT], BF16)
    ob = [K.sb(f"ob{i}", [128, NT], BF16) for i in range(4)]
    of = [K.sb(f"of{i}", [128, NT], F32) for i in range(2)]
    sq = K.sb("sq", [64, NT]); rstd = K.sb("rstd", [64, NT]); qn = K.sb("qn", [64, NT]); t1 = K.sb("t1", [64, NT]); t2 = K.sb("t2", [64, NT])
    obi = 0
    ofi = 0

    def proj(c0, M, ps, pn):
        for k in range(KC):
            P.op('pe', lambda e, k=k: e.matmul(ps[0:M, 0:NT], lhsT=win_b[:, k, c0:c0 + M], rhs=hT[:, k, :], start=(k == 0), stop=(k == KC - 1)),
                 reads=['win_b', 'hT'], writes=[pn], inc=(k == KC - 1))

    for t in range(ntiles):
        col = mcols[t]
        b = t % 2
        X, CS, SN = xt[b], cs[b], sn[b]
        xn, csn, snn = f"xt{b}", f"cs{b}", f"sn{b}"
        for hh in range(2):
            s_ = 2 * t + hh
            if s_ >= nsub:
                P.op('dve', lambda e, hh=hh: e.memset(X[:, :, hh * 128:(hh + 1) * 128], 0.0), writes=[xn])
                continue
            XK = xk[hh]; xkn = f"xk{hh}"
            P.dma('sp', XK[:], src_d[s_], reads=['x2_scr'], writes=[xkn])
            for c in range(KC):
                pst = psX[c // 4]
                P.op('pe', lambda e, c=c, pst=pst, XK=XK: e.transpose(out=pst[:, (c % 4) * 128:(c % 4) * 128 + 128], in_=XK[:, c * 128:(c + 1) * 128], identity=ident[:]),
                     reads=[xkn, 'ident'], writes=[f'psX{c // 4}'])
            P.op('act', lambda e, hh=hh: e.activation(out=X[:, 0:4, hh * 128:(hh + 1) * 128], in_=psX[0][:, :].rearrange("p (c n) -> p c n", c=4), func=AF.Copy),
                 reads=['psX0'], writes=[xn])
            P.op('dve', lambda e, hh=hh: e.tensor_copy(out=X[:, 4:8, hh * 128:(hh + 1) * 128], in_=psX[1][:, :].rearrange("p (c n) -> p c n", c=4)),
                 reads=['psX1'], writes=[xn])
        if t < ntiles - 1:
            P.dma('sp', xl_o[t].rearrange("(c p) n -> p c n", p=128), X[:], reads=[xn], writes=['xl_o'])
        P.dma('sp', CS[:], cos_d[t], writes=[csn])
        P.dma('sp', SN[:], sin_d[t], writes=[snn])
        for c in range(KC):
            P.op('act', lambda e, c=c: e.activation(out=hT[:, c, :], in_=X[:, c, :], func=AF.Identity,
                                                    scale=sc1p[:, c, col:col + 1], bias=modT[:, c, col:col + 1]),
                 reads=[xn, 'sc1p', 'modT'], writes=['hT'])
        for oc in range(16):
            ps = psA[oc % 2]; pn = f"psA{oc % 2}"
            proj(oc * 128, 128, ps, pn)
            grp, hh = oc // 4, oc % 4
            if grp == 3:
                o = of[ofi % 2]; on = f"of{ofi % 2}"; ofi += 1
                P.op('act', lambda e, o=o, ps=ps: e.activation(out=o[:], in_=ps[:, 0:NT], func=AF.Copy), reads=[pn], writes=[on])
                P.dma('sp', gb_['om'][hh * 128:(hh + 1) * 128, tsl(t)], o[:], reads=[on], writes=['om_o'])
            else:
                o = ob[obi % 4]; on = f"ob{obi % 4}"; obi += 1
                scale = (128.0 ** -0.5) if grp == 1 else 1.0
                if oc % 2 == 0:
                    P.op('act', lambda e, o=o, ps=ps, scale=scale: e.activation(out=o[:], in_=ps[:, 0:NT], func=AF.Copy, scale=scale), reads=[pn], writes=[on])
                else:
                    P.op('dve', lambda e, o=o, ps=ps, scale=scale: e.tensor_scalar(out=o[:], in0=ps[:, 0:NT], scalar1=scale, scalar2=None, op0=ALU.mult), reads=[pn], writes=[on])
                dst = (gb_['qm'], gb_['km'], gb_['vm'])[grp]
                P.dma('sp', dst[hh * 128:(hh + 1) * 128, tsl(t)], o[:], reads=[on], writes=['qkv_o'])
        for hd in range(10):
            ps = psA[hd % 2]; pn = f"psA{hd % 2}"
            proj(2048 + hd * 64, 64, ps, pn)
            gvec, gname = (qg, 'qg') if hd < 8 else (kg, 'kg')
            P.op('act', lambda e, ps=ps: e.activation(out=sq[:], in_=ps[0:64, 0:NT], func=AF.Square), reads=[pn], writes=['sq'])
            P.op('pe', lambda e: e.matmul(psr[0:64, 0:NT], lhsT=ones64[:], rhs=sq[:], start=True, stop=True), reads=['ones64', 'sq'], writes=['psr'])
            P.op('dve', lambda e: e.tensor_scalar(out=rstd[:], in0=psr[0:64, 0:NT], scalar1=RMS_EPS, scalar2=None, op0=ALU.add), reads=['psr'], writes=['rstd'])
            P.op('act', lambda e: e.activation(out=rstd[:], in_=rstd[:], func=AF.Sqrt), reads=['rstd'], writes=['rstd'])
            P.op('dve', lambda e: e.reciprocal(out=rstd[:], in_=rstd[:]), reads=['rstd'], writes=['rstd'])
            P.op('act', lambda e, ps=ps, gvec=gvec: e.activation(out=qn[:], in_=ps[0:64, 0:NT], func=AF.Copy, scale=gvec[:, 0:1]), reads=[pn, gname], writes=['qn'])
            P.op('dve', lambda e: e.tensor_tensor(out=qn[:], in0=qn[:], in1=rstd[:], op=ALU.mult), reads=['qn', 'rstd'], writes=['qn'])
            P.op('pe', lambda e: e.matmul(psw[0:64, 0:NT], lhsT=pm[:], rhs=qn[:], start=True, stop=True), reads=['pm', 'qn'], writes=['psw'])
            P.op('dve', lambda e: e.tensor_tensor(out=t1[:], in0=qn[:], in1=CS[:], op=ALU.mult), reads=['qn', csn], writes=['t1'])
            P.op('dve', lambda e: e.tensor_tensor(out=t2[:], in0=psw[0:64, 0:NT], in1=SN[:], op=ALU.mult), reads=['psw', snn], writes=['t2'])
            o = ob[obi % 4]; on = f"ob{obi % 4}"; obi += 1
            P.op('dve', lambda e, o=o: e.tensor_tensor(out=o[0:64, :], in0=t1[:], in1=t2[:], op=ALU.add), reads=['t1', 't2'], writes=[on])
            dst = gb_['qa'][hd * 64:(hd + 1) * 64, tsl(t)] if hd < 8 else gb_['ka'][(hd - 8) * 64:(hd - 7) * 64, tsl(t)]
            P.dma('sp', dst, o[0:64, :], reads=[on], writes=['qk_o'])
        for hd in range(2):
            ps = psA[hd % 2]; pn = f"psA{hd % 2}"
            proj(2048 + 640 + hd * 64, 64, ps, pn)
            o = ob[obi % 4]; on = f"ob{obi % 4}"; obi += 1
            P.op('act', lambda e, o=o, ps=ps: e.activation(out=o[0:64, :], in_=ps[0:64, 0:NT], func=AF.Copy), reads=[pn], writes=[on])
            P.dma('sp', gb_['va'][hd * 64:(hd + 1) * 64, tsl(t)], o[0:64, :], reads=[on], writes=['va_o'])
        ps = psA[0]; pn = "psA0"
        proj(2816, 16, ps, pn)
        o = of[ofi % 2]; on = f"of{ofi % 2}"; ofi += 1
        P.op('act', lambda e, o=o, ps=ps: e.activation(out=o[0:16, :], in_=ps[0:16, 0:NT], func=AF.Identity, bias=gb[:, 0:1]), reads=[pn, 'gb'], writes=[on])
        P.dma('sp', gb_['g'][:, tsl(t)], o[0:16, :], reads=[on], writes=['gt_o'])
    K.end_stage()


NEGM = -1.0e30


def stage_attn(K, Th, TOK, gb_):
    K.begin_stage("at_")
    nc, P = K.nc, K.P
    Tq = Th
    Tk = 2 * Th + 256
    nkt = Tk // 128
    nqt = Tq // 512
    qg_d = K.din("qn_g", [1, 64])
    kg_d = K.din("kn_g", [1, 64])
    sel_d = K.din("sel", [65, 64])
    ident_d = K.din("ident", [128, 128])
    psST = [K.ps(f"psST{i}", [128, 512]) for i in range(2)]
    psO = K.ps("psO", [128, 512])
    psD = K.ps("psD", [128, 512])
    psT = K.ps("psT", [128, 512], BF16)
    sel = K.sb("sel", [65, 64])
    P.dma('sp', sel[:], sel_d, writes=['sel'])
    identf = K.sb("identf", [128, 128]); identb = K.sb("identb", [128, 128], BF16)
    P.dma('sp', identf[:], ident_d, writes=['identf'])
    P.op('dve', lambda e: e.tensor_copy(out=identb[:], in_=identf[:]), reads=['identf'], writes=['identb'])
    gq = K.sb("gq", [128, 64]); gk = K.sb("gk", [128, 64]); mq = K.sb("mq", [128, 1]); mk = K.sb("mk", [128, 1]); negC = K.sb("negC", [128, 1])
    P.dma('sp', gq[:], bass.AP(qg_d.tensor, 0, [[0, 128], [1, 64]]), writes=['gq'])
    P.dma('sp', gk[:], bass.AP(kg_d.tensor, 0, [[0, 128], [1, 64]]), writes=['gk'])
    P.op('dve', lambda e: e.tensor_reduce(out=mq[:], in_=gq[:], axis=AX.X, op=ALU.max, apply_absolute_value=True), reads=['gq'], writes=['mq'])
    P.op('dve', lambda e: e.tensor_reduce(out=mk[:], in_=gk[:], axis=AX.X, op=ALU.max, apply_absolute_value=True), reads=['gk'], writes=['mk'])
    P.op('dve', lambda e: e.scalar_tensor_tensor(out=negC[:], in0=mq[:], scalar=-8.0, in1=mk[:], op0=ALU.mult, op1=ALU.mult), reads=['mq', 'mk'], writes=['negC'])
    qT = K.sb("qT", [64, Tq], BF16)
    kT = K.sb("kT", [64, Tk], BF16)
    vT = K.sb("vT", [64, Tk], BF16)
    vaug = K.sb("vaug", [128, nkt, 65], BF16)
    PT = [K.sb(f"PT{i}", [128, 512], BF16) for i in range(2)]
    OTs = K.sb("OTs", [65, 512])
    rden = K.sb("rden", [64, 512])
    ob = [K.sb(f"ob{i}", [64, 512]) for i in range(2)]
    P.op('dve', lambda e: e.memset(vaug[:, :, 64:65], 1.0), writes=['vaug1'])
    it = 0
    for u in range(8):
        kvh = u // 4
        P.dma('sp', qT[:], gb_['qa'][u * 64:(u + 1) * 64, 0:Th], reads=['qk_o'], writes=['qT'])
        if u % 4 == 0:
            for (dstT, srcn, dn) in ((kT, 'ka_all', 'kT'), (vT, 'va_all', 'vT')):
                for m_ in range(2):
                    rows = slice(m_ * 128 + kvh * 64, m_ * 128 + kvh * 64 + 64)
                    P.dma('sp', dstT[:, m_ * 128:(m_ + 1) * 128], gb_[srcn][rows, Th:Th + 128], reads=[srcn], writes=[dn])
                    P.dma('sp', dstT[:, 256 + m_ * Th:256 + (m_ + 1) * Th], gb_[srcn][rows, 0:Th], reads=[srcn], writes=[dn])
            for kt in range(nkt):
                P.op('pe', lambda e, kt=kt: e.transpose(out=psT[:, 0:64], in_=vT[:, kt * 128:(kt + 1) * 128], identity=identb[0:64, 0:64]),
                     reads=['vT', 'identb'], writes=['psT'])
                P.op('act', lambda e, kt=kt: e.activation(out=vaug[:, kt, 0:64], in_=psT[:, 0:64], func=AF.Copy), reads=['psT'], writes=['vaug'])
        for qt in range(nqt):
            def score(kt, qt=qt):
                ps = psST[kt % 2]
                P.op('pe', lambda e, ps=ps, kt=kt, qt=qt: e.matmul(ps[:, :], lhsT=kT[:, kt * 128:(kt + 1) * 128], rhs=qT[:, qt * 512:(qt + 1) * 512], start=True, stop=True),
                     reads=['kT', 'qT'], writes=[f"psST{kt % 2}"])
            score(0)
            for kt in range(nkt):
                ps = psST[kt % 2]; pn = f"psST{kt % 2}"
                pt = PT[kt % 2]; ptn = f"PT{kt % 2}"
                if kt + 1 < nkt:
                    score(kt + 1)
                P.op('act', lambda e, ps=ps, pt=pt: e.activation(out=pt[:], in_=ps[:, :], func=AF.Exp, scale=0.125, bias=negC[:, 0:1]),
                     reads=[pn, 'negC'], writes=[ptn])
                P.op('pe', lambda e, pt=pt, kt=kt: e.matmul(psO[0:65, :], lhsT=vaug[:, kt, :], rhs=pt[:], start=(kt == 0), stop=(kt == nkt - 1)),
                     reads=['vaug', 'vaug1', ptn], writes=['psO'])
            P.op('act', lambda e: e.activation(out=OTs[:], in_=psO[0:65, :], func=AF.Copy), reads=['psO'], writes=['OTs'])
            P.op('pe', lambda e: e.matmul(psD[0:64, :], lhsT=sel[:], rhs=OTs[:], start=True, stop=True), reads=['sel', 'OTs'], writes=['psD'])
            P.op('dve', lambda e: e.reciprocal(out=rden[:], in_=psD[0:64, :]), reads=['psD'], writes=['rden'])
            o = ob[it % 2]; on = f"ob{it % 2}"; it += 1
            P.op('dve', lambda e, o=o: e.tensor_tensor(out=o[:], in0=OTs[0:64, :], in1=rden[:], op=ALU.mult), reads=['OTs', 'rden'], writes=[on])
            P.dma('sp', gb_['ya'][u * 64:(u + 1) * 64, qt * 512:(qt + 1) * 512], o[:], reads=[on], writes=['ya_l'])
    K.end_stage()


def stage_scan(K, nunits, Th, TOK, gb_):
    K.begin_stage("sc_")
    nc, P = K.nc, K.P
    ncc = 2
    nchl = 2 * Th // 128
    NCH = ncc + nchl
    cpm = Th // 128
    sidx_d = K.din("scan_idx", [128, nunits * 2], I32)
    gidx_d = K.din("scan_gidx", [128, nunits * 2], I32)
    cst_d = K.din("consts", [5, 128, 128])
    h_o = gb_['h']
    def ccol(c):
        if c < ncc:
            return c, Th
        lc = c - ncc
        return lc // cpm, (lc % cpm) * 128

    psR = K.ps("psR", [128, 512]); psQK = K.ps("psQK", [128, 512]); psT = K.ps("psT", [128, 512], BF16)
    psN = K.ps("psN", [128, 512]); psC = K.ps("psC", [128, 512]); psX = K.ps("psX", [128, 512]); psB = K.ps("psB", [128, 512])
    cst = K.sb("cst", [128, 5, 128])
    P.dma('sp', cst[:], cst_d.rearrange("c p n -> p c n"), writes=['cst'])
    ident, triL, triU, mnegL, mnegU = [cst[:, i, :] for i in range(5)]
    identb = K.sb("identb", [128, 128], BF16)
    P.op('dve', lambda e: e.tensor_copy(out=identb[:], in_=ident), reads=['cst'], writes=['identb'])
    ones = K.sb("ones", [128, 128])
    P.op('dve', lambda e: e.memset(ones[:], 1.0), writes=['ones'])
    qTm = [K.sb(f"qTm{i}", [128, TOK], BF16) for i in range(2)]
    kTm = [K.sb(f"kTm{i}", [128, TOK], BF16) for i in range(2)]
    vTm = [K.sb(f"vTm{i}", [128, TOK], BF16) for i in range(2)]
    gsel = [K.sb(f"gsel{i}", [4, TOK]) for i in range(2)]
    sidx = K.sb("sidx", [128, nunits * 2], I32); gidx = K.sb("gidx", [128, nunits * 2], I32)
    P.dma('sp', sidx[:], sidx_d, writes=['sidx'])
    P.dma('sp', gidx[:], gidx_d, writes=['gidx'])
    def qsl(c):
        m_, c0 = ccol(c)
        return qTm[m_][:, c0:c0 + 128]
    def ksl(c):
        m_, c0 = ccol(c)
        return kTm[m_][:, c0:c0 + 128]
    ktok = K.sb("ktok", [128, NCH, 128], BF16); vaug = K.sb("vaug", [128, NCH, 129], BF16)
    P.op('dve', lambda e: e.memset(vaug[:, :, 128:129], 1.0), writes=['vaug1'])
    G = K.sb("G", [128, 4, NCH])
    ee = K.sb("ee", [128, NCH]); LF = K.sb("LF", [128, NCH]); Bc = K.sb("Bc", [128, NCH]); BL = K.sb("BL", [128, NCH]); IGmB = K.sb("IGmB", [128, NCH])
    Caug = K.sb("Caug", [128, 129]); Cb = K.sb("Cb", [128, 129], BF16); m = K.sb("m", [128, 1])
    igrep = K.sb("igrep", [128, 128]); dm = K.sb("dm", [128, 128]); sexp = K.sb("sexp", [128, 128]); s_b = K.sb("s_b", [128, 128], BF16)
    sT = K.sb("sT", [128, 128], BF16); numA = K.sb("numA", [128, 129]); tot = K.sb("tot", [128, 129]); kw = K.sb("kw", [128, 128], BF16)
    col = {n: K.sb(n, [128, 1]) for n in ("rowmax", "glmax", "mnew", "nmnew", "inter", "mt", "nmt", "winter", "emt", "dd", "t2", "wk", "t3", "decay")}
    hb = [K.sb(f"hb{i}", [128, 128]) for i in range(2)]
    hi = 0

    def c1(name, fn, reads, eng='dve'):
        P.op(eng, fn, reads=reads, writes=[name])

    for u in range(nunits):
        for m_ in range(2):
            ic = u * 2 + m_
            for (dst_, srcn, dn) in ((qTm[m_], 'qm_all', 'qT'), (kTm[m_], 'km_all', 'kT'), (vTm[m_], 'vm_all', 'vT')):
                P.dma('pool', None, None, reads=[srcn, 'sidx'], writes=[dn],
                      fn=lambda e, dst_=dst_, srcn=srcn, ic=ic: e.indirect_dma_start(out=dst_[:], out_offset=None, in_=gb_[srcn],
                                                                                   in_offset=bass.IndirectOffsetOnAxis(ap=sidx[:, ic:ic + 1], axis=0)))
            P.dma('pool', None, None, reads=['g_all', 'gidx'], writes=['gsel'],
                  fn=lambda e, m_=m_, ic=ic: e.indirect_dma_start(out=gsel[m_][:], out_offset=None, in_=gb_['g_all'],
                                                                   in_offset=bass.IndirectOffsetOnAxis(ap=gidx[0:4, ic:ic + 1], axis=0)))
        for c in range(NCH):
            m_, c0 = ccol(c)
            P.op('pe', lambda e, m_=m_, c0=c0: e.transpose(out=psT[:, 0:128], in_=kTm[m_][:, c0:c0 + 128], identity=identb[:]), reads=['kT', 'identb'], writes=['psT'])
            P.op('act', lambda e, c=c: e.activation(out=ktok[:, c, :], in_=psT[:, 0:128], func=AF.Copy), reads=['psT'], writes=['ktok'])
            P.op('pe', lambda e, m_=m_, c0=c0: e.transpose(out=psT[:, 128:256], in_=vTm[m_][:, c0:c0 + 128], identity=identb[:]), reads=['vT', 'identb'], writes=['psT'])
            P.op('dve', lambda e, c=c: e.tensor_copy(out=vaug[:, c, 0:128], in_=psT[:, 128:256]), reads=['psT'], writes=['vaug'])
            P.op('pe', lambda e, m_=m_, c0=c0: e.transpose(out=psB[:, 0:4], in_=gsel[m_][0:4, c0:c0 + 128], identity=ident[0:4, 0:4]), reads=['gsel', 'cst'], writes=['psB'])
            P.op('dve', lambda e, c=c: e.tensor_copy(out=G[:, :, c], in_=psB[:, 0:4]), reads=['psB'], writes=['G'])
        for dr in range(2):
            tri = triL if dr == 0 else triU
            mneg = mnegL if dr == 0 else mnegU
            gi, gf = 2 * dr, 2 * dr + 1
            P.op('act', lambda e: e.activation(out=ee[:], in_=G[:, gf, :], func=AF.Exp, scale=-1.0), reads=['G'], writes=['ee'])
            P.op('dve', lambda e: e.tensor_scalar(out=ee[:], in0=ee[:], scalar1=1.0, scalar2=None, op0=ALU.add), reads=['ee'], writes=['ee'])
            P.op('act', lambda e: e.activation(out=ee[:], in_=ee[:], func=AF.Ln), reads=['ee'], writes=['ee'])
            P.op('dve', lambda e: e.tensor_scalar(out=LF[:], in0=ee[:], scalar1=-1.0, scalar2=None, op0=ALU.mult), reads=['ee'], writes=['LF'])
            P.op('pe', lambda e: e.matmul(psB[:, 0:NCH], lhsT=tri, rhs=LF[:], start=True, stop=True), reads=['cst', 'LF'], writes=['psB'])
            P.op('dve', lambda e: e.tensor_copy(out=Bc[:], in_=psB[:, 0:NCH]), reads=['psB'], writes=['Bc'])
            P.op('pe', lambda e: e.matmul(psB[:, 0:NCH], lhsT=ones[:], rhs=LF[:], start=True, stop=True), reads=['ones', 'LF'], writes=['psB'])
            P.op('dve', lambda e: e.tensor_copy(out=BL[:], in_=psB[:, 0:NCH]), reads=['psB'], writes=['BL'])
            P.op('dve', lambda e: e.tensor_tensor(out=IGmB[:], in0=G[:, gi, :], in1=Bc[:], op=ALU.subtract), reads=['G', 'Bc'], writes=['IGmB'])
            P.op('dve', lambda e: e.memset(Caug[:], 0.0), writes=['Caug'])
            P.op('dve', lambda e: e.memset(Cb[:], 0.0), writes=['Cb'])
            P.op('dve', lambda e: e.memset(m[:], 0.0), writes=['m'])
            order = list(range(ncc)) + [ncc + i for i in range(nchl)]
            if dr == 1:
                order = list(range(ncc))[::-1] + [ncc + i for i in range(nchl)][::-1]
            for c in order:
                lat = c >= ncc
                cc = slice(c, c + 1)
                P.op('dve', lambda e, c=c: e.tensor_copy(out=igrep[:], in_=fap(IGmB, c, [[0, 128]])), reads=['IGmB'], writes=['igrep'])
                P.op('pe', lambda e: e.matmul(psR[:, 0:128], lhsT=igrep[:], rhs=ident, start=True, stop=True), reads=['igrep', 'cst'], writes=['psR'])
                c1('glmax', lambda e: e.tensor_reduce(out=col['glmax'][:], in_=psR[:, 0:128], axis=AX.X, op=ALU.max), ['psR'])
                c1('mnew', lambda e, cc=cc: e.scalar_tensor_tensor(out=col['mnew'][:], in0=col['glmax'][:], scalar=m[:, 0:1], in1=BL[:, cc], op0=ALU.max, op1=ALU.add),
                   ['glmax', 'm', 'BL'])
                c1('nmnew', lambda e: e.tensor_scalar(out=col['nmnew'][:], in0=col['mnew'][:], scalar1=-1.0, scalar2=None, op0=ALU.mult), ['mnew'])
                if lat:
                    P.op('dve', lambda e, cc=cc: e.scalar_tensor_tensor(out=dm[:], in0=psR[:, 0:128], scalar=Bc[:, cc], in1=mneg, op0=ALU.add, op1=ALU.add),
                         reads=['psR', 'Bc', 'cst'], writes=['dm'])
                    c1('rowmax', lambda e: e.tensor_reduce(out=col['rowmax'][:], in_=dm[:], axis=AX.X, op=ALU.max), ['dm'])
                    c1('inter', lambda e, cc=cc: e.tensor_tensor(out=col['inter'][:], in0=Bc[:, cc], in1=m[:], op=ALU.add), ['Bc', 'm'])
                    c1('mt', lambda e: e.tensor_tensor(out=col['mt'][:], in0=col['inter'][:], in1=col['rowmax'][:], op=ALU.max), ['inter', 'rowmax'])
                    c1('nmt', lambda e: e.tensor_scalar(out=col['nmt'][:], in0=col['mt'][:], scalar1=-1.0, scalar2=None, op0=ALU.mult), ['mt'])
                    P.op('act', lambda e: e.activation(out=sexp[:], in_=dm[:], func=AF.Exp, bias=col['nmt'][:, 0:1]), reads=['dm', 'nmt'], writes=['sexp'])
                    c1('winter', lambda e: e.activation(out=col['winter'][:], in_=col['inter'][:], func=AF.Exp, bias=col['nmt'][:, 0:1]), ['inter', 'nmt'], eng='act')
                    c1('emt', lambda e: e.activation(out=col['emt'][:], in_=col['mt'][:], func=AF.Exp, scale=-1.0), ['mt'], eng='act')
                    P.op('pe', lambda e, c=c: e.matmul(psQK[:, 0:128], lhsT=qsl(c), rhs=ksl(c), start=True, stop=True),
                         reads=['qT', 'kT'], writes=['psQK'])
                    P.op('dve', lambda e: e.tensor_tensor(out=s_b[:], in0=psQK[:, 0:128], in1=sexp[:], op=ALU.mult), reads=['psQK', 'sexp'], writes=['s_b'])
                    P.op('pe', lambda e: e.transpose(out=psT[:, 0:128], in_=s_b[:], identity=identb[:]), reads=['s_b', 'identb'], writes=['psT'])
                    P.op('act', lambda e: e.activation(out=sT[:], in_=psT[:, 0:128], func=AF.Copy), reads=['psT'], writes=['sT'])
                    P.op('pe', lambda e, c=c: e.matmul(psN[:, 0:129], lhsT=sT[:], rhs=vaug[:, c, :], start=True, stop=True), reads=['sT', 'vaug', 'vaug1'], writes=['psN'])
                    P.op('pe', lambda e, c=c: e.matmul(psX[:, 0:129], lhsT=qsl(c), rhs=Cb[:], start=True, stop=True), reads=['qT', 'Cb'], writes=['psX'])
                    P.op('act', lambda e: e.activation(out=numA[:], in_=psN[:, 0:129], func=AF.Copy), reads=['psN'], writes=['numA'])
                    P.op('dve', lambda e: e.scalar_tensor_tensor(out=tot[:], in0=psX[:, 0:129], scalar=col['winter'][:, 0:1], in1=numA[:], op0=ALU.mult, op1=ALU.add),
                         reads=['psX', 'winter', 'numA'], writes=['tot'])
                    c1('dd', lambda e: e.activation(out=col['dd'][:], in_=tot[:, 128:129], func=AF.Abs), ['tot'], eng='act')
                    c1('dd', lambda e: e.tensor_tensor(out=col['dd'][:], in0=col['dd'][:], in1=col['emt'][:], op=ALU.max), ['dd', 'emt'])
                    c1('dd', lambda e: e.reciprocal(out=col['dd'][:], in_=col['dd'][:]), ['dd'])
                    H = hb[hi % 2]; hn = f"hb{hi % 2}"; hi += 1
                    P.op('dve', lambda e, H=H: e.tensor_scalar(out=H[:], in0=tot[:, 0:128], scalar1=col['dd'][:, 0:1], scalar2=None, op0=ALU.mult), reads=['tot', 'dd'], writes=[hn])
                    lc = c - ncc
                    r0 = ((u * 2 + dr) * nchl + lc) * 128
                    P.dma('sp', h_o[r0:r0 + 128, :], H[:], reads=[hn], writes=['h_o'])
                c1('t2', lambda e, cc=cc: e.tensor_tensor(out=col['t2'][:], in0=IGmB[:, cc], in1=BL[:, cc], op=ALU.add), ['IGmB', 'BL'])
                c1('wk', lambda e: e.activation(out=col['wk'][:], in_=col['t2'][:], func=AF.Exp, bias=col['nmnew'][:, 0:1]), ['t2', 'nmnew'], eng='act')
                P.op('dve', lambda e, c=c: e.tensor_scalar(out=kw[:], in0=ktok[:, c, :], scalar1=col['wk'][:, 0:1], scalar2=None, op0=ALU.mult), reads=['ktok', 'wk'], writes=['kw'])
                P.op('pe', lambda e, c=c: e.matmul(psC[:, 0:129], lhsT=kw[:], rhs=vaug[:, c, :], start=True, stop=True), reads=['kw', 'vaug', 'vaug1'], writes=['psC'])
                c1('t3', lambda e, cc=cc: e.tensor_tensor(out=col['t3'][:], in0=BL[:, cc], in1=m[:], op=ALU.add), ['BL', 'm'])
                c1('decay', lambda e: e.activation(out=col['decay'][:], in_=col['t3'][:], func=AF.Exp, bias=col['nmnew'][:, 0:1]), ['t3', 'nmnew'], eng='act')
                P.op('dve', lambda e: e.scalar_tensor_tensor(out=Caug[:], in0=Caug[:], scalar=col['decay'][:, 0:1], in1=psC[:, 0:129], op0=ALU.mult, op1=ALU.add),
                     reads=['Caug', 'decay', 'psC'], writes=['Caug'])
                P.op('act', lambda e: e.activation(out=Cb[:], in_=Caug[:], func=AF.Copy), reads=['Caug'], writes=['Cb'])
                P.op('dve', lambda e: e.tensor_copy(out=m[:], in_=col['mnew'][:]), reads=['mnew', 'inter', 't3', 'glmax'], writes=['m'])
    K.end_stage()


def stage_l1c(K, ntiles, out_d, gb_, half_tok):
    K.begin_stage("lc_")
    nc, P = K.nc, K.P
    xt_d = gb_['xl0T']
    hidx_d = K.din("h_idx", [128, ntiles * 2 * 8], I32)
    ident_d = K.din("ident", [128, 128])
    cvec_d = K.din("cvec", [128, KC, 2])
    modw_d = K.din("mod_w1", [D, 6144])
    modb_d = K.din("mod_bT1", [128, 48])
    ng_d = K.din("norm_gT", [128, 4])
    wout_d = K.din("cd_w_out", [D, D])
    lng_d = K.din("ln_gT10", [128, KC])
    lnb_d = K.din("ln_bT10", [128, KC])

    psA = [K.ps(f"psA{i}", [128, 512]) for i in range(2)]
    psmean = K.ps("psmean", [128, 512]); pssq = K.ps("pssq", [128, 512]); psm = K.ps("psm", [128, 512])
    psX = K.ps("psX", [128, 512])
    ident = K.sb("ident", [128, 128])
    P.dma('sp', ident[:], ident_d, writes=['ident'])
    hidx = K.sb("hidx", [128, ntiles * 2 * 8], I32)
    P.dma('sp', hidx[:], hidx_d, writes=['hidx'])
    ones128 = K.sb("ones128", [128, 128]); ones1024 = K.sb("ones1024", [128, 128])
    P.op('dve', lambda e: e.memset(ones128[:], 1.0 / 128), writes=['ones128'])
    P.op('dve', lambda e: e.memset(ones1024[:], 1.0 / 1024), writes=['ones1024'])
    ng = K.sb("ng", [128, 4]); lng = K.sb("lng", [128, KC]); lnb = K.sb("lnb", [128, KC])
    for t_, d_, n_ in ((ng, ng_d, 'ng'), (lng, lng_d, 'lng'), (lnb, lnb_d, 'lnb')):
        P.dma('sp', t_[:], d_, writes=[n_])
    g1a = K.sb("g1a", [128, KC, 2])
    wout_b = K.sb("wout_b", [128, KC, D], BF16)
    tmp = ExitStack()
    modT = mod_setup(K, cvec_d, modw_d, modb_d, 16, 24, psm, tmp=tmp)
    stage = [tmp.enter_context(nc.sbuf_tensor(f"s_{K.pfx}stage{i}", [128, D], F32)) for i in range(2)]
    P.op('dve', lambda e: e.tensor_scalar(out=g1a[:], in0=modT[:, 0:8, :], scalar1=1.0 / ALPHA, scalar2=None, op0=ALU.mult), reads=['modT'], writes=['g1a'])
    for k in range(KC):
        load_cast(K, wout_b[:, k, :], wout_d[k * 128:(k + 1) * 128, :], stage[k % 2][:], f"stage{k % 2}", "wout_b", eng='dve' if k % 2 == 0 else 'act')
    P.barrier()
    tmp.close()

    xt = [K.sb(f"xt{i}", [128, KC, NT]) for i in range(2)]
    htk = [K.sb(f"htk{i}", [128, 4, 128]) for i in range(2)]
    HS = K.sb("HS", [128, 4, NT])
    om = [K.sb(f"om{i}", [128, 4, NT]) for i in range(2)]
    ya = [K.sb(f"ya{i}", [128, 4, NT]) for i in range(2)]
    hs = K.sb("hs", [128, 1, NT]); sgm = K.sb("sgm", [128, NT])
    sqt = [K.sb(f"sqt{i}", [128, NT]) for i in range(2)]
    mean_sb = K.sb("mean_sb", [128, NT]); rstd = K.sb("rstd", [128, NT])
    ycat = K.sb("ycat", [128, KC, NT], BF16)
    vt = K.sb("vt", [128, KC, NT])
    xo = [K.sb(f"xo{i}", [128, KC, NT]) for i in range(2)]

    def ln_stats(src, nchunk, ones, onm, srcname, eps):
        for c in range(nchunk):
            P.op('pe', lambda e, c=c: e.matmul(psmean[:, 0:NT], lhsT=ones[:], rhs=src[:, c, :], start=(c == 0), stop=(c == nchunk - 1)),
                 reads=[onm, srcname], writes=['psmean'], inc=(c == nchunk - 1))
        for c in range(nchunk):
            s = sqt[c % 2]; sn = f"sqt{c % 2}"
            P.op('act', lambda e, c=c, s=s: e.activation(out=s[:], in_=src[:, c, :], func=AF.Square), reads=[srcname], writes=[sn])
            P.op('pe', lambda e, c=c, s=s: e.matmul(pssq[:, 0:NT], lhsT=ones[:], rhs=s[:], start=(c == 0), stop=(c == nchunk - 1)),
                 reads=[onm, sn], writes=['pssq'])
        P.op('act', lambda e: e.activation(out=mean_sb[:], in_=psmean[:, 0:NT], func=AF.Copy), reads=['psmean'], writes=['mean_sb'])
        P.op('dve', lambda e: e.tensor_tensor(out=rstd[:], in0=mean_sb[:], in1=mean_sb[:], op=ALU.mult), reads=['mean_sb'], writes=['rstd'])
        P.op('dve', lambda e: e.tensor_tensor(out=rstd[:], in0=pssq[:, 0:NT], in1=rstd[:], op=ALU.subtract), reads=['pssq', 'rstd'], writes=['rstd'])
        P.op('dve', lambda e: e.tensor_scalar(out=rstd[:], in0=rstd[:], scalar1=eps, scalar2=None, op0=ALU.add), reads=['rstd'], writes=['rstd'])
        P.op('act', lambda e: e.activation(out=rstd[:], in_=rstd[:], func=AF.Sqrt), reads=['rstd'], writes=['rstd'])
        P.op('dve', lambda e: e.reciprocal(out=rstd[:], in_=rstd[:]), reads=['rstd'], writes=['rstd'])

    for t in range(ntiles):
        b = t % 2
        X, OM, YA = xt[b], om[b], ya[b]
        xn, omn, yan = f"xt{b}", f"om{b}", f"ya{b}"
        P.dma('sp', X[:], xt_d[t].rearrange("(c p) n -> p c n", p=128), reads=['xl_o'], writes=[xn])
        P.dma('sp', OM[:], gb_['om'][:, t * NT:(t + 1) * NT].rearrange("(c p) n -> p c n", p=128), reads=['om_o'], writes=[omn])
        for c in range(4):
            for par in range(2):
                hd_ = 2 * c + par
                P.dma('sp', YA[par * 64:(par + 1) * 64, c, :], gb_['ya'][hd_ * 64:(hd_ + 1) * 64, t * NT:(t + 1) * NT], reads=['ya_l'], writes=[yan])
        for sub in range(2):
            s_ = 2 * t + sub
            for dr in range(2):
                for hd in range(4):
                    ic = s_ * 8 + hd * 2 + dr
                    P.dma('pool', None, None, reads=['h_all', 'hidx'], writes=[f"htk{dr}"],
                          fn=lambda e, dr=dr, hd=hd, ic=ic: e.indirect_dma_start(out=htk[dr][:, hd, :], out_offset=None, in_=gb_['h_all'],
                                                                                 in_offset=bass.IndirectOffsetOnAxis(ap=hidx[:, ic:ic + 1], axis=0)))
            P.op('dve', lambda e: e.tensor_tensor(out=htk[0][:], in0=htk[0][:], in1=htk[1][:], op=ALU.add), reads=['htk0', 'htk1'], writes=['htk0'])
            for hd in range(4):
                P.op('pe', lambda e, hd=hd: e.transpose(out=psX[:, hd * 128:(hd + 1) * 128], in_=htk[0][:, hd, :], identity=ident[:]), reads=['htk0', 'ident'], writes=['psX'])
            P.op('act', lambda e, sub=sub: e.activation(out=HS[:, :, sub * 128:(sub + 1) * 128], in_=psX[:, :].rearrange("p (c n) -> p c n", c=4), func=AF.Copy),
                 reads=['psX'], writes=['HS'])
        for hd in range(4):
            P.op('dve', lambda e, hd=hd: e.tensor_copy(out=hs[:, 0, :], in_=HS[:, hd, :]), reads=['HS'], writes=['hs'])
            ln_stats(hs, 1, ones128, 'ones128', 'hs', LN_EPS)
            P.op('dve', lambda e: e.tensor_tensor(out=hs[:, 0, :], in0=hs[:, 0, :], in1=mean_sb[:], op=ALU.subtract), reads=['hs', 'mean_sb'], writes=['hs'])
            P.op('dve', lambda e: e.tensor_tensor(out=hs[:, 0, :], in0=hs[:, 0, :], in1=rstd[:], op=ALU.mult), reads=['hs', 'rstd'], writes=['hs'])
            P.op('act', lambda e, hd=hd: e.activation(out=sgm[:], in_=OM[:, hd, :], func=AF.Sigmoid), reads=[omn], writes=['sgm'])
            P.op('dve', lambda e, hd=hd: e.scalar_tensor_tensor(out=ycat[:, hd, :], in0=hs[:, 0, :], scalar=ng[:, hd:hd + 1], in1=sgm[:], op0=ALU.mult, op1=ALU.mult),
                 reads=['hs', 'ng', 'sgm'], writes=['ycat'])
        for c in range(4):
            P.op('act', lambda e, c=c: e.activation(out=ycat[:, 4 + c, :], in_=YA[:, c, :], func=AF.Copy), reads=[yan], writes=['ycat'])
        for oc in range(KC):
            ps = psA[oc % 2]; pn = f"psA{oc % 2}"
            for k in range(KC):
                P.op('pe', lambda e, k=k, oc=oc, ps=ps: e.matmul(ps[:, 0:NT], lhsT=wout_b[:, k, oc * 128:(oc + 1) * 128], rhs=ycat[:, k, :],
                                                                 start=(k == 0), stop=(k == KC - 1)),
                     reads=['wout_b', 'ycat'], writes=[pn], inc=(k == KC - 1))
            P.op('dve', lambda e, oc=oc, ps=ps: e.scalar_tensor_tensor(out=vt[:, oc, :], in0=ps[:, 0:NT], scalar=g1a[:, oc, 0:1],
                                                                      in1=X[:, oc, :], op0=ALU.mult, op1=ALU.add),
                 reads=[pn, 'g1a', xn], writes=['vt'])
        ln_stats(vt, KC, ones1024, 'ones1024', 'vt', LN_EPS / (ALPHA * ALPHA))
        XO = xo[b]; xon = f"xo{b}"
        for oc in range(KC):
            P.op('dve', lambda e, oc=oc: e.tensor_tensor(out=vt[:, oc, :], in0=vt[:, oc, :], in1=mean_sb[:], op=ALU.subtract), reads=['vt', 'mean_sb'], writes=['vt'])
            P.op('dve', lambda e, oc=oc: e.tensor_tensor(out=vt[:, oc, :], in0=vt[:, oc, :], in1=rstd[:], op=ALU.mult), reads=['vt', 'rstd'], writes=['vt'])
            P.op('act', lambda e, oc=oc: e.activation(out=XO[:, oc, :], in_=vt[:, oc, :], func=AF.Identity, scale=lng[:, oc:oc + 1], bias=lnb[:, oc:oc + 1]),
                 reads=['vt', 'lng', 'lnb'], writes=[xon])
        P.dma('sp', out_d[t].rearrange("(c p) n -> p c n", p=128), XO[:], reads=[xon], writes=['x1T_scr'])
    K.end_stage()


def seg_tiles(seq, start, ntile):
    L = seq.shape[0]
    xt = np.zeros((ntile, D, WT), np.float32)
    cm = np.zeros((ntile, 128, WT), np.float32)
    rc = np.ones((ntile, 128, 4, NT), np.float32)
    for t in range(ntile):
        s0 = start + t * NT - HALO
        lo, hi = max(s0, 0), min(s0 + WT, L)
        if hi > lo:
            xt[t, :, lo - s0:hi - s0] = seq[lo:hi].T
            cm[t, :, lo - s0:hi - s0] = 1.0
        tok = start + t * NT + np.arange(NT)
        for g, w in enumerate(POOL_WINDOWS):
            cnt = np.minimum(tok + w // 2, L) - np.maximum(tok - w // 2, 0)
            cnt = np.where(tok < L, cnt, 1).astype(np.float32)
            rc[t, :, g, :] = (1.0 / cnt)[None, :]
    return xt, cm, rc

def fm(v, nchunk):
    return np.ascontiguousarray(np.asarray(v, np.float32).reshape(nchunk, 128).T)

def cvec_of(cb, c_ctx):
    return np.ascontiguousarray(np.stack([fm(cb, KC), fm(c_ctx, KC)], axis=-1))

def plain_tiles(seq, start, ntile):
    L, Dm = seq.shape
    out = np.zeros((ntile, Dm, NT), seq.dtype)
    for t in range(ntile):
        lo = start + t * NT
        hi = min(lo + NT, L)
        if hi > lo:
            out[t, :, :hi - lo] = seq[lo:hi].T
    return out

def rope_tiles(start, ntile, latent=True):
    cs = np.ones((ntile, 64, NT), np.float32); sn = np.zeros((ntile, 64, NT), np.float32)
    if latent:
        f = (10000.0 ** (-np.arange(16, dtype=np.float32) / 16)).astype(np.float32)
        for t in range(ntile):
            tok = start + t * NT + np.arange(NT)
            row = (tok // 64).astype(np.float32); col = (tok % 64).astype(np.float32)
            ar = (row[None, :] * f[:, None]).astype(np.float32); ac = (col[None, :] * f[:, None]).astype(np.float32)
            cs[t] = np.concatenate([np.cos(ar), np.cos(ar), np.cos(ac), np.cos(ac)], 0)
            sn[t] = np.concatenate([np.sin(ar), np.sin(ar), np.sin(ac), np.sin(ac)], 0)
    return cs, sn

def rope_perm():
    Pm = np.zeros((64, 64), np.float32)
    for base in (0, 32):
        for d in range(16):
            Pm[base + d + 16, base + d] = -1.0
            Pm[base + d, base + d + 16] = 1.0
    return Pm

CD_ORDER = None
def cd_perm_cols(w):
    return np.ascontiguousarray(np.concatenate([w[:, 0:2048], w[:, 2064:2832], w[:, 2048:2064]], axis=1))

def scan_consts():
    i = np.arange(128)
    ident = np.eye(128, dtype=np.float32)
    triL = (i[:, None] <= i[None, :]).astype(np.float32)
    triU = (i[:, None] >= i[None, :]).astype(np.float32)
    mnegL = np.where(i[None, :] <= i[:, None], 0.0, -1.0e30).astype(np.float32)
    mnegU = np.where(i[None, :] >= i[:, None], 0.0, -1.0e30).astype(np.float32)
    return np.stack([ident, triL, triU, mnegL, mnegU])

def attn_sel():
    s = np.zeros((65, 64), np.float32); s[64, :] = 1.0
    return s


_PROGS = {}
PAIRS = [[0, 1], [2, 3], [4, 5], [6, 7]]


def _prog(key, fn):
    if key not in _PROGS:
        _PROGS[key] = fn()
    return _PROGS[key]


def _allgather(K, src, dst, rn_src, rn_dst):
    P = K.P
    P._deps('pool', [rn_src], [rn_dst], True)
    ins = K.nc.gpsimd.collective_compute("AllGather", ALU.bypass, replica_groups=PAIRS, ins=[src.opt()], outs=[dst.opt()])
    P.cnt['pool'] += 1
    ins.then_inc(P.sem['pool'], 1)
    P._record(('pool', P.cnt['pool']), [rn_src], [rn_dst])


def _build_fused(ntl):
    K = Ctx()
    Th = ntl * NT
    T = 2 * Th
    TOK = (ntl + 1) * NT
    nsub = 2 * ntl + 1
    x1T_scr0 = K.scr("x1T_scr0", [ntl + 1, D, NT])
    x2_scr0 = K.scr("x2_scr0", [nsub, 128, D])
    x1T_scr1 = K.scr("x1T_scr1", [ntl, D, NT])
    gb = dict(qm=K.scr("g_qm", [512, TOK], BF16), km=K.scr("g_km", [512, TOK], BF16), vm=K.scr("g_vm", [512, TOK], BF16),
              ka=K.scr("g_ka", [128, TOK], BF16), va=K.scr("g_va", [128, TOK], BF16), g=K.scr("g_g", [16, TOK], F32),
              qa=K.scr("l_qa", [512, TOK], BF16), om=K.scr("l_om", [512, TOK], F32), xl0T=K.scr("l_xl0T", [ntl, D, NT]),
              ya=K.scr("l_ya", [512, Th]), h=K.scr("g_h", [2 * 2 * T, 128]),
              qm_all=K.scr("a_qm", [1024, TOK], BF16), km_all=K.scr("a_km", [1024, TOK], BF16), vm_all=K.scr("a_vm", [1024, TOK], BF16),
              ka_all=K.scr("a_ka", [256, TOK], BF16), va_all=K.scr("a_va", [256, TOK], BF16), g_all=K.scr("a_g", [32, TOK], F32),
              h_all=K.scr("a_h", [2 * 2 * 2 * T, 128]))
    out_d = K.dout("x2", [2 * ntl, 128, D])
    uvb = [K.scr(f"uvb{L}", [16384, 2 * D], BF16) for L in range(2)]
    stage_conv(K, 0, uvb[0])
    stage_conv(K, 1, uvb[1])
    stage_mixa(K, ntl + 1, [0] * ntl + [1], x1T_scr0)
    stage_peer(K, nsub, [0] * (2 * ntl) + [1], 0, x1T_scr0, x2_scr0, uvb[0])
    stage_l1a(K, ntl + 1, [0] * ntl + [1], x2_scr0, nsub, gb)
    for nm in ("qm", "km", "vm"):
        for hd in range(4):
            _allgather(K, gb[nm][hd * 128:(hd + 1) * 128, :], gb[nm + "_all"][hd * 256:(hd + 1) * 256, :], nm + "_src", nm + "_all")
    for nm in ("ka", "va", "g"):
        _allgather(K, gb[nm], gb[nm + "_all"], nm + "_src", nm + "_all")
    K.P.barrier()
    stage_scan(K, 2, Th, TOK, gb)
    stage_attn(K, Th, TOK, gb)
    hrows = 2 * 2 * T
    PR = min(4096, hrows)
    for k in range(hrows // PR):
        _allgather(K, gb["h"][k * PR:(k + 1) * PR, :], gb["h_all"][k * 2 * PR:(k + 1) * 2 * PR, :], "h_src", "h_all")
    K.P.barrier()
    stage_l1c(K, ntl, x1T_scr1, gb, Th)
    stage_peer(K, 2 * ntl, [0] * (2 * ntl), 1, x1T_scr1, out_d, uvb[1])
    K.close()
    return K.nc


def kernel(x, c, ctx, c_ctx, mod_w, mod_b, ln_g, ln_b, ab_w_in, pool_w, pool_ls, conv_w, conv_b, conv_ln_g, conv_ln_b, ab_w_out,
           cd_w_in, mlstm_gate_b, mlstm_norm_g, q_norm_g, k_norm_g, cd_w_out, peer_w_q, peer_keys, peer_u, peer_v):
    f32 = lambda a: np.ascontiguousarray(np.asarray(a, np.float32))
    x, c, ctx, c_ctx, mod_w, mod_b, ln_g, ln_b = map(f32, (x, c, ctx, c_ctx, mod_w, mod_b, ln_g, ln_b))
    peer_w_q, peer_keys, peer_u, peer_v = map(f32, (peer_w_q, peer_keys, peer_u, peer_v))
    cd_w_in, cd_w_out, ab_w_in, ab_w_out = map(f32, (cd_w_in, cd_w_out, ab_w_in, ab_w_out))
    B, T, _ = x.shape
    Lc = ctx.shape[1]
    NH = 2
    ncores = B * NH
    assert ncores == 8 and Lc == 256
    Th = T // NH
    Lh = Lc // NH
    ntl = Th // NT
    cores = [(i // NH, i % NH) for i in range(ncores)]
    ident = np.eye(128, dtype=np.float32)
    iota16 = np.tile(np.arange(16, dtype=np.float32), (128, 1))
    nc = _prog(("fused", ntl), lambda: _build_fused(ntl))
    uv_tabs = [np.ascontiguousarray(np.concatenate([peer_u[L], peer_v[L]], axis=1)) for L in range(2)]
    w_in_p = cd_perm_cols(cd_w_in[0])
    consts = scan_consts()
    p = np.arange(128)
    maps = []
    for (b, hf_) in cores:
        xt, cm, rc = seg_tiles(x[b], hf_ * Th, ntl)
        xt2, cm2, rc2 = seg_tiles(ctx[b], hf_ * Lh, 1)
        cs, sn = rope_tiles(hf_ * Th, ntl)
        cs2, sn2 = rope_tiles(0, 1, latent=False)
        sidx = np.zeros((128, 4), np.int32)
        gidx = np.zeros((128, 4), np.int32)
        for j in range(2):
            h = 2 * hf_ + j
            for m_ in range(2):
                sidx[:, j * 2 + m_] = (h * 2 + m_) * 128 + p
                gidx[:4, j * 2 + m_] = m_ * 16 + np.arange(4) * 4 + h
        hidx = np.zeros((128, 2 * ntl * 8), np.int32)
        for s_ in range(2 * ntl):
            for hd in range(4):
                for dr in range(2):
                    m_, u_ = hd // 2, hd % 2
                    grow = (u_ * 2 + dr) * T + hf_ * Th + s_ * 128 + p
                    PR = min(4096, 4 * T)
                    hidx[:, s_ * 8 + hd * 2 + dr] = ((grow // PR) * 2 + m_) * PR + (grow % PR)
        m = dict(
            xtiles=np.concatenate([xt, xt2]), cmask=np.concatenate([cm, cm2]), rcorr=np.concatenate([rc, rc2]),
            cvec=cvec_of(c[b], c_ctx), ab_w_in=ab_w_in[0], pool_w=f32(pool_w[0]), pool_lsT=fm(pool_ls[0], 4),
            conv_wT=np.ascontiguousarray(f32(conv_w[0]).T.reshape(4, 128, 31).transpose(1, 0, 2)),
            conv_bT=fm(conv_b[0], 4), cln_gT=fm(conv_ln_g[0], 4), cln_bT=fm(conv_ln_b[0], 4),
            ab_w_out=ab_w_out[0], ln_gT00=fm(ln_g[0, 0], KC), ln_bT00=fm(ln_b[0, 0], KC),
            cosT=np.concatenate([cs, cs2]), sinT=np.concatenate([sn, sn2]), cd_w_in=w_in_p,
            gate_bT=f32(mlstm_gate_b[0]).reshape(16, 1).copy(), qn_gT=f32(q_norm_g[0]).reshape(64, 1).copy(),
            kn_gT=f32(k_norm_g[0]).reshape(64, 1).copy(), ropeP=rope_perm(),
            scan_idx=sidx, scan_gidx=gidx, consts=consts, qn_g=f32(q_norm_g[0])[None].copy(), kn_g=f32(k_norm_g[0])[None].copy(), sel=attn_sel(),
            h_idx=hidx, norm_gT=fm(mlstm_norm_g[0], 4), cd_w_out=cd_w_out[0], ln_gT10=fm(ln_g[1, 0], KC), ln_bT10=fm(ln_b[1, 0], KC),
            ident=ident, iota16=iota16)
        for L in range(2):
            m.update({f"mod_w{L}": mod_w[L], f"mod_bT{L}": fm(mod_b[L], 48), f"w_q{L}": peer_w_q[L],
                      f"keysT{L}": np.ascontiguousarray(peer_keys[L].transpose(2, 0, 1)), f"peer_uv{L}": uv_tabs[L],
                      f"ln_g{L}1": ln_g[L, 1][None].copy(), f"ln_b{L}1": ln_b[L, 1][None].copy()})
        maps.append(m)
    res = run_bass_kernel_spmd(nc, maps, core_ids=list(range(ncores))).results
    out = np.zeros((B, T, D), np.float32)
    for i, (b, hf_) in enumerate(cores):
        out[b, hf_ * Th:(hf_ + 1) * Th] = res[i]["x2"].reshape(Th, D)
    return out
```

```python
import numpy as np
import ml_dtypes
from contextlib import ExitStack
import concourse.bass as bass
import concourse.mybir as mybir
from concourse.bass_utils import run_bass_kernel_spmd

F32 = mybir.dt.float32
BF16 = mybir.dt.bfloat16
I32 = mybir.dt.int32
U32 = mybir.dt.uint32
AF = mybir.ActivationFunctionType
ALU = mybir.AluOpType
AX = mybir.AxisListType

ENG = {'pe': 'tensor', 'act': 'scalar', 'dve': 'vector', 'pool': 'gpsimd', 'sp': 'sync'}


class Prog:
    def __init__(self, nc, ndma=20):
        self.nc = nc
        self.stack = ExitStack()
        self.sem = {e: self.stack.enter_context(nc.semaphore(f"s_{e}")) for e in ENG}
        self.cnt = {e: 0 for e in ENG}
        self.ndma = ndma
        self.dsem = [self.stack.enter_context(nc.semaphore(f"d{i}")) for i in range(ndma)]
        self.dcnt = [0] * ndma
        self.dnext = 0
        self.seen = {e: {} for e in ENG}
        self.lastw = {}
        self.readers = {}
        self.pending = {e: [] for e in ENG}
        self.nops = 0

    def eng(self, e):
        return getattr(self.nc, ENG[e])

    def _wait(self, e, key, val):
        if val <= 0:
            return
        if self.seen[e].get(key, 0) >= val:
            return
        self.seen[e][key] = val
        sem = self.sem[key] if isinstance(key, str) else self.dsem[key]
        self.eng(e).wait_ge(sem, val)

    def _deps(self, e, reads, writes, sync_same):
        toks = {}
        for r in reads:
            t = self.lastw.get(r)
            if t is not None:
                toks[t[0]] = max(toks.get(t[0], 0), t[1])
        for w in writes:
            t = self.lastw.get(w)
            if t is not None:
                toks[t[0]] = max(toks.get(t[0], 0), t[1])
            for k, v in self.readers.get(w, {}).items():
                toks[k] = max(toks.get(k, 0), v)
        for k, v in toks.items():
            if k == e and not sync_same:
                continue
            self._wait(e, k, v)

    def _record(self, tok, reads, writes):
        for r in reads:
            d = self.readers.setdefault(r, {})
            d[tok[0]] = max(d.get(tok[0], 0), tok[1])
        for w in writes:
            self.lastw[w] = tok
            self.readers[w] = {}

    def replay(self, recs, n):
        for _ in range(min(n, len(recs))):
            kind, args, kw = recs.pop(0)
            (self.op if kind == 'op' else self.dma)(*args, **kw)

    def op(self, e, fn, reads=(), writes=(), sync_same=None, inc=True):
        if getattr(self, 'rec', None) is not None:
            self.rec.append(('op', (e, fn), dict(reads=reads, writes=writes, sync_same=sync_same, inc=inc)))
            return None
        if sync_same is None:
            sync_same = (e != 'pe')
        self._deps(e, reads, writes, sync_same)
        ins = fn(self.eng(e))
        self.nops += 1
        if not inc:
            self.pending[e].append((tuple(reads), tuple(writes)))
            return ins
        self.cnt[e] += 1
        ins.then_inc(self.sem[e], 1)
        tok = (e, self.cnt[e])
        for (r, w) in self.pending[e]:
            self._record(tok, r, w)
        self.pending[e] = []
        self._record(tok, reads, writes)
        return ins

    def dma(self, e, out, in_, reads=(), writes=(), fn=None, **kw):
        if getattr(self, 'rec', None) is not None:
            self.rec.append(('dma', (e, out, in_), dict(reads=reads, writes=writes, fn=fn, **kw)))
            return None
        s = self.dnext
        self.dnext = (s + 1) % self.ndma
        self._wait(e, s, self.dcnt[s] * 16)
        self._deps(e, reads, writes, True)
        if fn is None:
            ins = self.eng(e).dma_start(out=out, in_=in_, **kw)
        else:
            ins = fn(self.eng(e))
        self.nops += 1
        self.dcnt[s] += 1
        ins.then_inc(self.dsem[s], 16)
        tok = (s, self.dcnt[s] * 16)
        self._record(tok, reads, writes)
        return ins

    def barrier(self):
        for e in ENG:
            for s_ in range(self.ndma):
                self._wait(e, s_, self.dcnt[s_] * 16)
            for k in ENG:
                if k != e:
                    self._wait(e, k, self.cnt[k])

    def finish(self, e='sp'):
        for s in range(self.ndma):
            self._wait(e, s, self.dcnt[s] * 16)
        for k in ENG:
            if k != e:
                self._wait(e, k, self.cnt[k])
        self.stack.close()


D = 1024
KC = 8
NT = 256
HALO = 16
WT = NT + 2 * HALO
ALPHA = 4.0 ** 0.25
LN_EPS = 1e-5
POOL_WINDOWS = (2, 4, 8, 16)


class Ctx:
    def __init__(self, name="k"):
        self.nc = bass.Bass("TRN2", target_bir_lowering=False)
        self.P = Prog(self.nc)
        self.es = ExitStack()
        self.pfx = ""
        self.drams = {}

    def begin_stage(self, pfx):
        self.es = ExitStack()
        self.pfx = pfx

    def end_stage(self):
        self.P.barrier()
        self.es.close()

    def sb(self, name, shape, dt=F32):
        return self.es.enter_context(self.nc.sbuf_tensor("s_" + self.pfx + name, list(shape), dt))

    def ps(self, name, shape, dt=F32):
        return self.es.enter_context(self.nc.psum_tensor("p_" + self.pfx + name, list(shape), dt))

    def _dram(self, name, shape, dt, kind):
        if name not in self.drams:
            self.drams[name] = self.nc.dram_tensor(name, list(shape), dt, kind=kind).ap()
        return self.drams[name]

    def din(self, name, shape, dt=F32):
        return self._dram(name, shape, dt, "ExternalInput")

    def dout(self, name, shape, dt=F32):
        return self._dram(name, shape, dt, "ExternalOutput")

    def scr(self, name, shape, dt=F32):
        return self._dram(name, shape, dt, "Internal")

    def close(self):
        self.P.finish('sp')


def fap(t, off, dims):
    fs = 1
    for s in t.shape[1:]:
        fs *= s
    return bass.AP(t, off, [[fs, t.shape[0]]] + [list(d) for d in dims])


def load_cast(K, dst_bf, src_ap, stage, rname, wname, eng='dve', q='sp'):
    P = K.P
    P.dma(q, stage, src_ap, writes=[rname])
    if eng == 'dve':
        P.op('dve', lambda e: e.tensor_copy(out=dst_bf, in_=stage), reads=[rname], writes=[wname])
    else:
        P.op('act', lambda e: e.activation(out=dst_bf, in_=stage, func=AF.Copy), reads=[rname], writes=[wname])


def mod_setup(K, cvec_d, modw_d, modb_d, j0, j1, psm, tmp=None):
    nc, P = K.nc, K.P
    nj = j1 - j0
    cv = K.sb("cv", [128, KC, 2])
    sc = K.sb("sc", [128, KC, 2])
    mb = K.sb("mb", [128, 48])
    modT = K.sb("modT", [128, nj, 2])
    tmp = tmp if tmp is not None else K.es
    mwblk = [tmp.enter_context(K.nc.sbuf_tensor(f"s_{K.pfx}mwblk{i}", [128, KC, 512], F32)) for i in range(2)]
    P.dma('sp', cv[:], cvec_d, writes=['cv'])
    P.dma('sp', mb[:], modb_d, writes=['mb'])
    P.op('act', lambda e: e.activation(out=sc[:], in_=cv[:], func=AF.Silu), reads=['cv'], writes=['sc'])
    assert j0 % 4 == 0 and j1 % 4 == 0
    for bi, blk in enumerate(range(j0 // 4, j1 // 4)):
        buf = mwblk[bi % 2]
        bn = f"mwblk{bi % 2}"
        P.dma('sp', buf[:], modw_d[:, blk * 512:(blk + 1) * 512].rearrange("(k p) f -> p k f", p=128), writes=[bn])
        for jj in range(4):
            j = blk * 4 + jj - j0
            for k in range(KC):
                P.op('pe', lambda e, k=k, jj=jj, j=j: e.matmul(psm[:, 2 * j:2 * j + 2], lhsT=buf[:, k, jj * 128:(jj + 1) * 128],
                                                               rhs=sc[:, k, :], start=(k == 0), stop=(k == KC - 1)),
                     reads=[bn, 'sc'], writes=['psm'], inc=(k == KC - 1))
    P.op('dve', lambda e: e.tensor_tensor(out=modT[:], in0=psm[:, 0:2 * nj].rearrange("p (j c) -> p j c", c=2),
                                          in1=fap(mb, j0, [[1, nj], [0, 2]]), op=ALU.add),
         reads=['psm', 'mb'], writes=['modT'])
    return modT


def stage_mixa(K, ntiles, mcols, out_d):
    K.begin_stage("mx_")
    nc, P = K.nc, K.P
    xt_d = K.din("xtiles", [ntiles, D, WT])
    cm_d = K.din("cmask", [ntiles, 128, WT])
    rc_d = K.din("rcorr", [ntiles, 128, 4, NT])
    cvec_d = K.din("cvec", [128, KC, 2])
    modw_d = K.din("mod_w0", [D, 6144])
    modb_d = K.din("mod_bT0", [128, 48])
    win_d = K.din("ab_w_in", [D, 1536])
    poolw_d = K.din("pool_w", [4, 128, 128])
    poolls_d = K.din("pool_lsT", [128, 4])
    convw_d = K.din("conv_wT", [128, 4, 31])
    convb_d = K.din("conv_bT", [128, 4])
    clng_d = K.din("cln_gT", [128, 4])
    clnb_d = K.din("cln_bT", [128, 4])
    wout_d = K.din("ab_w_out", [D, D])
    lng_d = K.din("ln_gT00", [128, KC])
    lnb_d = K.din("ln_bT00", [128, KC])

    psA = [K.ps(f"psA{i}", [128, 512]) for i in range(2)]
    psp = K.ps("psp", [128, 512])
    psmean = K.ps("psmean", [128, 512])
    pssq = K.ps("pssq", [128, 512])
    psm = K.ps("psm", [128, 512])

    ones512 = K.sb("ones512", [128, 128])
    ones1024 = K.sb("ones1024", [128, 128])
    P.op('dve', lambda e: e.memset(ones512[:], 1.0 / 512), writes=['ones512'])
    P.op('dve', lambda e: e.memset(ones1024[:], 1.0 / 1024), writes=['ones1024'])
    small = {}
    for nm, d, shp in [("pls", poolls_d, [128, 4]), ("cw", convw_d, [128, 4, 31]), ("cb", convb_d, [128, 4]),
                       ("clg", clng_d, [128, 4]), ("clb", clnb_d, [128, 4]), ("lng", lng_d, [128, KC]),
                       ("lnb", lnb_d, [128, KC])]:
        t = K.sb(nm, shp)
        P.dma('sp', t[:], d, writes=[nm])
        small[nm] = t
    pls, cw, cb, clg, clb, lng, lnb = [small[n] for n in ("pls", "cw", "cb", "clg", "clb", "lng", "lnb")]

    modT = mod_setup(K, cvec_d, modw_d, modb_d, 0, 24, psm)
    sc1p = K.sb("sc1p", [128, KC, 2])
    g1a = K.sb("g1a", [128, KC, 2])
    P.op('dve', lambda e: e.tensor_scalar(out=sc1p[:], in0=modT[:, 8:16, :], scalar1=1.0, scalar2=None, op0=ALU.add),
         reads=['modT'], writes=['sc1p'])
    P.op('dve', lambda e: e.tensor_scalar(out=g1a[:], in0=modT[:, 16:24, :], scalar1=1.0 / ALPHA, scalar2=None, op0=ALU.mult),
         reads=['modT'], writes=['g1a'])

    stage = [K.sb(f"stage{i}", [128, 1536]) for i in range(2)]
    win_b = K.sb("win_b", [128, KC, 1536], BF16)
    wout_b = K.sb("wout_b", [128, KC, D], BF16)
    poolw_b = K.sb("poolw_b", [128, 4, 128], BF16)
    si = 0
    for k in range(KC):
        load_cast(K, win_b[:, k, :], win_d[k * 128:(k + 1) * 128, :], stage[si % 2][:, :], f"stage{si % 2}", "win_b",
                  eng='dve' if k % 2 == 0 else 'act')
        si += 1
    for k in range(KC):
        load_cast(K, wout_b[:, k, :], wout_d[k * 128:(k + 1) * 128, :], stage[si % 2][:, 0:D], f"stage{si % 2}", "wout_b",
                  eng='dve' if k % 2 == 0 else 'act')
        si += 1
    for g in range(4):
        load_cast(K, poolw_b[:, g, :], poolw_d[g], stage[si % 2][:, 0:128], f"stage{si % 2}", "poolw_b")
        si += 1

    xt = [K.sb(f"xt{i}", [128, KC, WT]) for i in range(2)]
    cm = [K.sb(f"cm{i}", [128, WT]) for i in range(2)]
    rc = [K.sb(f"rc{i}", [128, 4, NT]) for i in range(2)]
    hT = K.sb("hT", [128, KC, WT], BF16)
    zp = K.sb("zp", [128, 4, WT])
    ua = K.sb("ua", [128, 4, WT])
    sg = K.sb("sg", [128, WT])
    u = K.sb("u", [128, 4, WT])
    pa = K.sb("pa", [128, WT])
    pb = K.sb("pb", [128, WT])
    dif = K.sb("dif", [128, NT], BF16)
    yc = K.sb("yc", [128, 4, NT])
    sqt = [K.sb(f"sqt{i}", [128, NT]) for i in range(2)]
    mean_sb = K.sb("mean_sb", [128, NT])
    rstd = K.sb("rstd", [128, NT])
    yn = K.sb("yn", [128, NT])
    ycat = K.sb("ycat", [128, KC, NT], BF16)
    vt = K.sb("vt", [128, KC, NT])
    xo = [K.sb(f"xo{i}", [128, KC, NT]) for i in range(2)]

    def ln_stats(src, nchunk, ones, onm, srcname, eps):
        for c in range(nchunk):
            P.op('pe', lambda e, c=c: e.matmul(psmean[:, 0:NT], lhsT=ones[:], rhs=src[:, c, :], start=(c == 0), stop=(c == nchunk - 1)),
                 reads=[onm, srcname], writes=['psmean'], inc=(c == nchunk - 1))
        for c in range(nchunk):
            s = sqt[c % 2]
            sn = f"sqt{c % 2}"
            P.op('act', lambda e, c=c, s=s: e.activation(out=s[:], in_=src[:, c, :], func=AF.Square), reads=[srcname], writes=[sn])
            P.op('pe', lambda e, c=c, s=s: e.matmul(pssq[:, 0:NT], lhsT=ones[:], rhs=s[:], start=(c == 0), stop=(c == nchunk - 1)),
                 reads=[onm, sn], writes=['pssq'])
        P.op('act', lambda e: e.activation(out=mean_sb[:], in_=psmean[:, 0:NT], func=AF.Copy), reads=['psmean'], writes=['mean_sb'])
        P.op('dve', lambda e: e.tensor_tensor(out=rstd[:], in0=mean_sb[:], in1=mean_sb[:], op=ALU.mult), reads=['mean_sb'], writes=['rstd'])
        P.op('dve', lambda e: e.tensor_tensor(out=rstd[:], in0=pssq[:, 0:NT], in1=rstd[:], op=ALU.subtract), reads=['pssq', 'rstd'], writes=['rstd'])
        P.op('dve', lambda e: e.tensor_scalar(out=rstd[:], in0=rstd[:], scalar1=eps, scalar2=None, op0=ALU.add),
             reads=['rstd'], writes=['rstd'])
        P.op('act', lambda e: e.activation(out=rstd[:], in_=rstd[:], func=AF.Sqrt), reads=['rstd'], writes=['rstd'])
        P.op('dve', lambda e: e.reciprocal(out=rstd[:], in_=rstd[:]), reads=['rstd'], writes=['rstd'])

    for t in range(ntiles):
        col = mcols[t]
        b = t % 2
        X, CM, RC = xt[b], cm[b], rc[b]
        xn, cmn, rcn = f"xt{b}", f"cm{b}", f"rc{b}"
        P.dma('sp', X[:], xt_d[t].rearrange("(c p) n -> p c n", p=128), writes=[xn])
        P.dma('sp', CM[:], cm_d[t], writes=[cmn])
        P.dma('sp', RC[:], rc_d[t], writes=[rcn])
        for c in range(KC):
            P.op('act', lambda e, c=c: e.activation(out=hT[:, c, :], in_=X[:, c, :], func=AF.Identity,
                                                    scale=sc1p[:, c, col:col + 1], bias=modT[:, c, col:col + 1]),
                 reads=[xn, 'sc1p', 'modT'], writes=['hT'])
        for oc in range(12):
            ps = psA[oc % 2]
            pn = f"psA{oc % 2}"
            for k in range(KC):
                P.op('pe', lambda e, k=k, oc=oc, ps=ps: e.matmul(ps[:, 0:WT], lhsT=win_b[:, k, oc * 128:(oc + 1) * 128], rhs=hT[:, k, :],
                                                                 start=(k == 0), stop=(k == KC - 1)),
                     reads=['win_b', 'hT'], writes=[pn], inc=(k == KC - 1))
            if oc < 4:
                P.op('dve', lambda e, oc=oc, ps=ps: e.tensor_tensor(out=zp[:, oc, :], in0=ps[:, 0:WT], in1=CM[:], op=ALU.mult),
                     reads=[pn, cmn], writes=['zp'])
            elif oc < 8:
                P.op('act', lambda e, oc=oc, ps=ps: e.activation(out=ua[:, oc - 4, :], in_=ps[:, 0:WT], func=AF.Copy),
                     reads=[pn], writes=['ua'])
            else:
                P.op('act', lambda e, ps=ps: e.activation(out=sg[:], in_=ps[:, 0:WT], func=AF.Sigmoid), reads=[pn], writes=['sg'])
                P.op('dve', lambda e: e.tensor_tensor(out=sg[:], in0=sg[:], in1=CM[:], op=ALU.mult), reads=['sg', cmn], writes=['sg'])
                P.op('dve', lambda e, oc=oc: e.tensor_tensor(out=u[:, oc - 8, :], in0=ua[:, oc - 8, :], in1=sg[:], op=ALU.mult),
                     reads=['ua', 'sg'], writes=['u'])
        for g in range(4):
            src = zp[:, g, :]
            srcn = 'zp'
            lo, hi = 0, WT
            bufs = [(pa, 'pa'), (pb, 'pb')]
            dst, dn = bufs[0]
            P.op('dve', lambda e, dst=dst, g=g: e.tensor_tensor(out=dst[:, 1:WT], in0=zp[:, g, 0:WT - 1], in1=zp[:, g, 1:WT], op=ALU.add),
                 reads=['zp'], writes=[dn])
            cur, cn = dst, dn
            lo, hi = 1, WT
            sh = 1
            for lvl in range(g):
                dst, dn = bufs[(lvl + 1) % 2]
                nlo, nhi = lo + sh, hi - sh
                P.op('dve', lambda e, dst=dst, cur=cur, nlo=nlo, nhi=nhi, sh=sh: e.tensor_tensor(
                    out=dst[:, nlo:nhi], in0=cur[:, nlo - sh:nhi - sh], in1=cur[:, nlo + sh:nhi + sh], op=ALU.add),
                    reads=[cn], writes=[dn])
                cur, cn = dst, dn
                lo, hi = nlo, nhi
                sh *= 2
            assert lo <= HALO and hi >= HALO + NT
            oth, on = bufs[0] if cur is bufs[1][0] else bufs[1]
            P.op('dve', lambda e, cur=cur, oth=oth, g=g: e.tensor_tensor(out=oth[:, 0:NT], in0=cur[:, HALO:HALO + NT], in1=RC[:, g, :], op=ALU.mult),
                 reads=[cn, rcn], writes=[on])
            P.op('dve', lambda e, oth=oth, g=g: e.tensor_tensor(out=dif[:], in0=oth[:, 0:NT], in1=zp[:, g, HALO:HALO + NT], op=ALU.subtract),
                 reads=[on, 'zp'], writes=['dif'])
            P.op('pe', lambda e, g=g: e.matmul(psp[:, 0:NT], lhsT=poolw_b[:, g, :], rhs=dif[:], start=True, stop=True),
                 reads=['poolw_b', 'dif'], writes=['psp'])
            P.op('act', lambda e, g=g: e.activation(out=ycat[:, g, :], in_=psp[:, 0:NT], func=AF.Copy, scale=pls[:, g:g + 1]),
                 reads=['psp', 'pls'], writes=['ycat'])
        for c in range(4):
            P.op('dve', lambda e, c=c: e.tensor_scalar(out=yc[:, c, :], in0=u[:, c, 1:1 + NT], scalar1=cw[:, c, 0:1], scalar2=cb[:, c:c + 1],
                                                       op0=ALU.mult, op1=ALU.add), reads=['u', 'cw', 'cb'], writes=['yc'])
            for k in range(1, 31):
                P.op('dve', lambda e, c=c, k=k: e.scalar_tensor_tensor(out=yc[:, c, :], in0=u[:, c, k + 1:k + 1 + NT], scalar=cw[:, c, k:k + 1],
                                                                      in1=yc[:, c, :], op0=ALU.mult, op1=ALU.add),
                     reads=['u', 'cw', 'yc'], writes=['yc'])
        ln_stats(yc, 4, ones512, 'ones512', 'yc', LN_EPS)
        for c in range(4):
            P.op('dve', lambda e, c=c: e.tensor_tensor(out=yn[:], in0=yc[:, c, :], in1=mean_sb[:], op=ALU.subtract),
                 reads=['yc', 'mean_sb'], writes=['yn'])
            P.op('dve', lambda e: e.tensor_tensor(out=yn[:], in0=yn[:], in1=rstd[:], op=ALU.mult), reads=['yn', 'rstd'], writes=['yn'])
            P.op('act', lambda e, c=c: e.activation(out=ycat[:, 4 + c, :], in_=yn[:], func=AF.Silu, scale=clg[:, c:c + 1], bias=clb[:, c:c + 1]),
                 reads=['yn', 'clg', 'clb'], writes=['ycat'])
        for oc in range(KC):
            ps = psA[oc % 2]
            pn = f"psA{oc % 2}"
            for k in range(KC):
                P.op('pe', lambda e, k=k, oc=oc, ps=ps: e.matmul(ps[:, 0:NT], lhsT=wout_b[:, k, oc * 128:(oc + 1) * 128], rhs=ycat[:, k, :],
                                                                 start=(k == 0), stop=(k == KC - 1)),
                     reads=['wout_b', 'ycat'], writes=[pn], inc=(k == KC - 1))
            P.op('dve', lambda e, oc=oc, ps=ps: e.scalar_tensor_tensor(out=vt[:, oc, :], in0=ps[:, 0:NT], scalar=g1a[:, oc, col:col + 1],
                                                                      in1=X[:, oc, HALO:HALO + NT], op0=ALU.mult, op1=ALU.add),
                 reads=[pn, 'g1a', xn], writes=['vt'])
        ln_stats(vt, KC, ones1024, 'ones1024', 'vt', LN_EPS / (ALPHA * ALPHA))
        XO = xo[b]
        xon = f"xo{b}"
        for oc in range(KC):
            P.op('dve', lambda e, oc=oc: e.tensor_tensor(out=vt[:, oc, :], in0=vt[:, oc, :], in1=mean_sb[:], op=ALU.subtract),
                 reads=['vt', 'mean_sb'], writes=['vt'])
            P.op('dve', lambda e, oc=oc: e.tensor_tensor(out=vt[:, oc, :], in0=vt[:, oc, :], in1=rstd[:], op=ALU.mult),
                 reads=['vt', 'rstd'], writes=['vt'])
            P.op('act', lambda e, oc=oc: e.activation(out=XO[:, oc, :], in_=vt[:, oc, :], func=AF.Identity, scale=lng[:, oc:oc + 1], bias=lnb[:, oc:oc + 1]),
                 reads=['vt', 'lng', 'lnb'], writes=[xon])
        P.dma('sp', out_d[t].rearrange("(c p) n -> p c n", p=128), XO[:], reads=[xon], writes=['x1T_scr'])
    K.end_stage()


NSLOT = 128
NEG = -1.0e30


def stage_conv(K, L, uvb):
    K.begin_stage(f"cv{L}_")
    nc, P = K.nc, K.P
    uv_d = K.din(f"peer_uv{L}", [16384, 2 * D])
    src = uv_d.rearrange("(c p j) d -> c p (j d)", p=128, j=2)
    dst = uvb.rearrange("(c p j) d -> c p (j d)", p=128, j=2)
    fb = [K.sb(f"fb{i}", [128, 4 * D]) for i in range(3)]
    bb = [K.sb(f"bb{i}", [128, 4 * D], BF16) for i in range(3)]
    for ci in range(64):
        i = ci % 3
        P.dma('sp' if ci % 2 == 0 else 'act', fb[i][:], src[ci], writes=[f"fb{i}"])
        if ci % 2 == 0:
            P.op('dve', lambda e, i=i: e.tensor_copy(out=bb[i][:], in_=fb[i][:]), reads=[f"fb{i}"], writes=[f"bb{i}"])
        else:
            P.op('pool', lambda e, i=i: e.tensor_copy(out=bb[i][:], in_=fb[i][:]), reads=[f"fb{i}"], writes=[f"bb{i}"])
        P.dma('sp' if ci % 2 == 0 else 'act', dst[ci], bb[i][:], reads=[f"bb{i}"], writes=['uvb'])
    K.end_stage()


def stage_peer(K, nsub, mcols, L, src_d, out_d, uvb):
    K.begin_stage(f"pr{L}_")
    nc, P = K.nc, K.P
    cvec_d = K.din("cvec", [128, KC, 2])
    modw_d = K.din(f"mod_w{L}", [D, 6144])
    modb_d = K.din(f"mod_bT{L}", [128, 48])
    wq_d = K.din(f"w_q{L}", [D, 2048])
    keysT_d = K.din(f"keysT{L}", [128, 2, 128])
    lng_d = K.din(f"ln_g{L}1", [1, D])
    lnb_d = K.din(f"ln_b{L}1", [1, D])
    ident_d = K.din("ident", [128, 128])
    iota_d = K.din("iota16", [128, 16])

    psq = [K.ps(f"psq{i}", [128, 512]) for i in range(2)]
    psS = K.ps("psS", [128, 1024])
    psX = [K.ps(f"psX{i}", [128, 512]) for i in range(2)]
    psm = K.ps("psm", [128, 512])
    psr = K.ps("psr", [128, 512])

    ident = K.sb("ident", [128, 128])
    iota16 = K.sb("iota16", [128, 16])
    ones = K.sb("ones", [128, 128])
    P.dma('sp', ident[:], ident_d, writes=['ident'])
    P.dma('sp', iota16[:], iota_d, writes=['iota16'])
    P.op('dve', lambda e: e.memset(ones[:], 1.0), writes=['ones'])
    lng = K.sb("lng", [128, D])
    lnb = K.sb("lnb", [128, D])
    P.dma('sp', lng[:], bass.AP(lng_d.tensor, 0, [[0, 128], [1, D]]), writes=['lng'])
    P.dma('sp', lnb[:], bass.AP(lnb_d.tensor, 0, [[0, 128], [1, D]]), writes=['lnb'])

    tmp = ExitStack()
    sc2p = K.sb("sc2p", [128, KC, 2])
    g2a = K.sb("g2a", [128, KC, 2])
    dg = K.sb("dg", [128, 128])
    reps = {(vn, col): K.sb(f"rep_{vn}{col}", [128, D]) for vn in ("sh2", "sc2p", "g2a") for col in range(2)}
    wq_b = K.sb("wq_b", [128, KC, 2048], BF16)
    keysT = K.sb("keysT", [128, 2, 128], BF16)
    modT = mod_setup(K, cvec_d, modw_d, modb_d, 24, 48, psm, tmp=tmp)
    stage = [tmp.enter_context(nc.sbuf_tensor(f"s_{K.pfx}stage{i}", [128, 2048], F32)) for i in range(2)]
    P.op('dve', lambda e: e.tensor_scalar(out=sc2p[:], in0=modT[:, 8:16, :], scalar1=1.0, scalar2=None, op0=ALU.add),
         reads=['modT'], writes=['sc2p'])
    P.op('dve', lambda e: e.tensor_scalar(out=g2a[:], in0=modT[:, 16:24, :], scalar1=1.0 / ALPHA, scalar2=None, op0=ALU.mult),
         reads=['modT'], writes=['g2a'])
    rep = {}
    for vn, src, j0 in (("sh2", modT, 0), ("sc2p", sc2p, 0), ("g2a", g2a, 0)):
        for col in range(2):
            r = reps[(vn, col)]
            for c in range(KC):
                P.op('dve', lambda e, c=c, src=src, col=col: e.tensor_scalar(out=dg[:], in0=ident[:], scalar1=src[:, j0 + c, col:col + 1], scalar2=None, op0=ALU.mult),
                     reads=['ident', 'modT', 'sc2p', 'g2a'], writes=['dg'])
                P.op('pe', lambda e: e.matmul(psr[:, 0:128], lhsT=ones[:], rhs=dg[:], start=True, stop=True), reads=['ones', 'dg'], writes=['psr'])
                P.op('act', lambda e, c=c, r=r: e.activation(out=r[:, c * 128:(c + 1) * 128], in_=psr[:, 0:128], func=AF.Copy),
                     reads=['psr'], writes=['rep'])
            rep[(vn, col)] = r

    for k in range(KC):
        load_cast(K, wq_b[:, k, :], wq_d[k * 128:(k + 1) * 128, :], stage[k % 2][:], f"stage{k % 2}", "wq_b", eng='dve' if k % 2 == 0 else 'act')
    load_cast(K, keysT[:], keysT_d, stage[0][:, 0:256].rearrange("p (a b) -> p a b", a=2), "stage0", "keysT")
    P.barrier()
    tmp.close()

    x1T = [K.sb(f"x1T{i}", [128, KC, 128]) for i in range(1)]
    x1t = [K.sb(f"x1t{i}", [128, D]) for i in range(2)]
    h2T = K.sb("h2T", [128, KC, 128], BF16)
    qT = K.sb("qT", [128, 16, 128], BF16)
    S = K.sb("S", [128, 16, 128])
    S2 = K.sb("S2", [128, 16, 128])
    sv = K.sb("sv", [128, 16, 16])
    si = K.sb("si", [128, 16, 16], U32)
    sif = K.sb("sif", [128, 16, 16])
    cand = K.sb("cand", [128, 8, 256])
    cand2 = S2[:].rearrange("p a b -> p (a b)").rearrange("p (h c) -> p h c", h=8)
    ts = K.sb("ts", [128, 8, 16])
    pos = K.sb("pos", [128, 8, 16], U32)
    pa = K.sb("pa_", [128, 8, 16], U32)
    pbb = K.sb("pb_", [128, 8, 16], U32)
    af = K.sb("af", [128, 8, 16])
    bf = K.sb("bf", [128, 8, 16])
    oh = K.sb("oh", [128, 8, 16, 16])
    isel = K.sb("isel", [128, 8, 16])
    jsel = K.sb("jsel", [128, 8, 16])
    eidf = K.sb("eidf", [128, 128])
    eidx2 = [K.sb(f"eidx{i}", [128, 128], I32) for i in range(2)]
    ex = K.sb("ex", [128, 8, 16])
    Z = K.sb("Z", [128, 8])
    gate2 = [K.sb(f"gate{i}", [128, 128]) for i in range(2)]
    A = K.sb("A", [128, 128])
    wgt = K.sb("wgt", [128, 128])
    h2t2 = [K.sb(f"h2t{i}", [128, D], BF16) for i in range(2)]
    h2f = K.sb("h2f", [128, D])
    NB = 12
    rows = [K.sb(f"rows{i}", [128, 2 * D], BF16) for i in range(NB)]
    junk = K.sb("junk", [128, D], BF16)
    identb = K.sb("identb", [128, 128], BF16)
    P.op('dve', lambda e: e.tensor_copy(out=identb[:], in_=ident[:]), reads=['ident'], writes=['identb'])
    RG = 4
    gA = [K.sb(f"gA{i}", [128, 1]) for i in range(RG)]
    Acol = [K.sb(f"Acol{i}", [128, 1]) for i in range(RG)]
    dgs = [K.sb(f"dgs{i}", [128, 128], BF16) for i in range(RG)]
    acc = K.sb("acc", [128, D])
    stats = K.sb("stats", [128, 2, 6])
    mv = K.sb("mv", [128, 2])
    rs = K.sb("rs", [128, 1])
    xo = [K.sb(f"xo{i}", [128, D]) for i in range(1)]
    rbi = 0

    def front(s):
        col = mcols[s]
        par = s % 2
        XT, XK = x1T[0], x1t[par]
        xtn, xkn = "x1T0", f"x1t{par}"
        eidx, gate, h2t = eidx2[par], gate2[par], h2t2[par]
        en, gn, hn_ = f"eidx{par}", f"gate{par}", f"h2t{par}"
        P.dma('sp', XT[:], src_d[s // 2][:, (s % 2) * 128:(s % 2) * 128 + 128].rearrange("(c p) n -> p c n", p=128), reads=['x1T_scr'], writes=[xtn])
        for c in range(KC):
            pst = psX[c // 4]
            P.op('pe', lambda e, c=c, pst=pst: e.transpose(out=pst[:, (c % 4) * 128:(c % 4) * 128 + 128], in_=XT[:, c, :], identity=ident[:]),
                 reads=[xtn, 'ident'], writes=[f'psX{c // 4}'])
        P.op('act', lambda e: e.activation(out=XK[:, 0:512], in_=psX[0][:, :], func=AF.Copy), reads=['psX0'], writes=[xkn])
        P.op('act', lambda e: e.activation(out=XK[:, 512:1024], in_=psX[1][:, :], func=AF.Copy), reads=['psX1'], writes=[xkn])
        for c in range(KC):
            P.op('act', lambda e, c=c: e.activation(out=h2T[:, c, :], in_=XT[:, c, :], func=AF.Identity,
                                                    scale=sc2p[:, c, col:col + 1], bias=modT[:, c, col:col + 1]),
                 reads=[xtn, 'sc2p', 'modT'], writes=['h2T'])
        for hp in range(16):
            ps = psq[hp % 2]
            pn = f"psq{hp % 2}"
            for k in range(KC):
                P.op('pe', lambda e, k=k, hp=hp, ps=ps: e.matmul(ps[:, 0:128], lhsT=wq_b[:, k, hp * 128:(hp + 1) * 128], rhs=h2T[:, k, :],
                                                                 start=(k == 0), stop=(k == KC - 1)),
                     reads=['wq_b', 'h2T'], writes=[pn], inc=(k == KC - 1))
            if hp % 2 == 0:
                P.op('act', lambda e, hp=hp, ps=ps: e.activation(out=qT[:, hp, :], in_=ps[:, 0:128], func=AF.Copy), reads=[pn], writes=['qT'])
            else:
                P.op('dve', lambda e, hp=hp, ps=ps: e.tensor_copy(out=qT[:, hp, :], in_=ps[:, 0:128]), reads=[pn], writes=['qT'])
        for rnd in range(2):
            for j in range(8):
                hp = rnd * 8 + j
                P.op('pe', lambda e, hp=hp, j=j: e.matmul(psS[:, j * 128:(j + 1) * 128], lhsT=qT[:, hp, :], rhs=keysT[:, hp % 2, :], start=True, stop=True),
                     reads=['qT', 'keysT'], writes=['psS'], inc=(j == 7))
            for q4 in range(2):
                P.op('act', lambda e, q4=q4, rnd=rnd: e.activation(out=S[:, rnd * 8 + 4 * q4:rnd * 8 + 4 * q4 + 4, :],
                                                                  in_=psS[:, 512 * q4:512 * q4 + 512].rearrange("p (a b) -> p a b", a=4), func=AF.Copy),
                     reads=['psS'], writes=['S'])
        for hp in range(16):
            P.op('dve', lambda e, hp=hp: e.max(out=sv[:, hp, 0:8], in_=S[:, hp, :]), reads=['S'], writes=['sv'])
            P.op('dve', lambda e, hp=hp: e.max_index(out=si[:, hp, 0:8], in_max=sv[:, hp, 0:8], in_values=S[:, hp, :]), reads=['S', 'sv'], writes=['si'])
            P.op('dve', lambda e, hp=hp: e.match_replace(out=S2[:, hp, :], in_to_replace=sv[:, hp, 0:8], in_values=S[:, hp, :], imm_value=NEG),
                 reads=['S', 'sv'], writes=['S2'])
            P.op('dve', lambda e, hp=hp: e.max(out=sv[:, hp, 8:16], in_=S2[:, hp, :]), reads=['S2'], writes=['sv'])
            P.op('dve', lambda e, hp=hp: e.max_index(out=si[:, hp, 8:16], in_max=sv[:, hp, 8:16], in_values=S2[:, hp, :]), reads=['S2', 'sv'], writes=['si'])
        P.op('dve', lambda e: e.tensor_copy(out=sif[:], in_=si[:]), reads=['si'], writes=['sif'])
        P.op('dve', lambda e: e.tensor_tensor(out=cand[:].rearrange("p h (a b) -> p h a b", a=16),
                                              in0=fap(sv, 0, [[32, 8], [1, 16], [0, 16]]), in1=fap(sv, 16, [[32, 8], [0, 16], [1, 16]]), op=ALU.add),
             reads=['sv'], writes=['cand'])
        for h in range(8):
            P.op('dve', lambda e, h=h: e.max(out=ts[:, h, 0:8], in_=cand[:, h, :]), reads=['cand'], writes=['ts'])
            P.op('dve', lambda e, h=h: e.max_index(out=pos[:, h, 0:8], in_max=ts[:, h, 0:8], in_values=cand[:, h, :]), reads=['cand', 'ts'], writes=['pos'])
            P.op('dve', lambda e, h=h: e.match_replace(out=cand2[:, h, :], in_to_replace=ts[:, h, 0:8], in_values=cand[:, h, :], imm_value=NEG),
                 reads=['cand', 'ts'], writes=['S2'])
            P.op('dve', lambda e, h=h: e.max(out=ts[:, h, 8:16], in_=cand2[:, h, :]), reads=['S2'], writes=['ts'])
            P.op('dve', lambda e, h=h: e.max_index(out=pos[:, h, 8:16], in_max=ts[:, h, 8:16], in_values=cand2[:, h, :]), reads=['S2', 'ts'], writes=['pos'])
        P.op('dve', lambda e: e.tensor_single_scalar(out=pa[:], in_=pos[:], scalar=4, op=ALU.logical_shift_right), reads=['pos'], writes=['pa'])
        P.op('dve', lambda e: e.tensor_single_scalar(out=pbb[:], in_=pos[:], scalar=15, op=ALU.bitwise_and), reads=['pos'], writes=['pb'])
        P.op('dve', lambda e: e.tensor_copy(out=af[:], in_=pa[:]), reads=['pa'], writes=['af'])
        P.op('dve', lambda e: e.tensor_copy(out=bf[:], in_=pbb[:]), reads=['pb'], writes=['bf'])
        for (src, p_, dst, dn) in ((af, 0, isel, 'isel'), (bf, 1, jsel, 'jsel')):
            P.op('dve', lambda e, src=src: e.tensor_tensor(out=oh[:], in0=fap(src, 0, [[16, 8], [1, 16], [0, 16]]),
                                                           in1=fap(iota16, 0, [[0, 8], [0, 16], [1, 16]]), op=ALU.is_equal),
                 reads=['af', 'bf', 'iota16'], writes=['oh'])
            P.op('dve', lambda e, p_=p_: e.tensor_tensor(out=oh[:], in0=oh[:], in1=fap(sif, 16 * p_, [[32, 8], [0, 16], [1, 16]]), op=ALU.mult),
                 reads=['oh', 'sif'], writes=['oh'])
            P.op('dve', lambda e, dst=dst: e.tensor_reduce(out=dst[:], in_=oh[:], axis=AX.X, op=ALU.add), reads=['oh'], writes=[dn])
        P.op('dve', lambda e: e.scalar_tensor_tensor(out=eidf[:], in0=isel[:].rearrange("p h k -> p (h k)"), scalar=128.0,
                                                     in1=jsel[:].rearrange("p h k -> p (h k)"), op0=ALU.mult, op1=ALU.add),
             reads=['isel', 'jsel'], writes=['eidf'])
        P.op('dve', lambda e: e.tensor_copy(out=eidx[:], in_=eidf[:]), reads=['eidf'], writes=[en])
        P.op('dve', lambda e: e.tensor_tensor(out=ex[:], in0=ts[:], in1=fap(ts, 0, [[16, 8], [0, 16]]), op=ALU.subtract), reads=['ts'], writes=['ex'])
        P.op('act', lambda e: e.activation(out=ex[:], in_=ex[:], func=AF.Exp), reads=['ex'], writes=['ex'])
        P.op('dve', lambda e: e.tensor_reduce(out=Z[:], in_=ex[:], axis=AX.X, op=ALU.add), reads=['ex'], writes=['Z'])
        P.op('dve', lambda e: e.reciprocal(out=Z[:], in_=Z[:]), reads=['Z'], writes=['Z'])
        P.op('dve', lambda e: e.tensor_tensor(out=gate[:].rearrange("p (h k) -> p h k", h=8), in0=ex[:], in1=fap(Z, 0, [[1, 8], [0, 16]]), op=ALU.mult),
             reads=['ex', 'Z'], writes=[gn])
        P.op('dve', lambda e: e.tensor_tensor(out=h2f[:], in0=XK[:], in1=rep[("sc2p", col)][:], op=ALU.mult), reads=[xkn, 'rep'], writes=['h2f'])
        P.op('dve', lambda e: e.tensor_tensor(out=h2t[:], in0=h2f[:], in1=rep[("sh2", col)][:], op=ALU.add), reads=['h2f', 'rep'], writes=[hn_])
    def back(s, recs):
        nonlocal rbi
        col = mcols[s]
        par = s % 2
        XK, xkn = x1t[par], f"x1t{par}"
        eidx, gate, h2t = eidx2[par], gate2[par], h2t2[par]
        en, gn, hn_ = f"eidx{par}", f"gate{par}", f"h2t{par}"
        b = 0
        per = (len(recs) + NSLOT - 1) // NSLOT
        slot_rows = {}
        for slot in range(NSLOT + 1):
            if slot < NSLOT:
                r = rows[rbi % NB]
                rn = f"rows{rbi % NB}"
                rbi += 1
                slot_rows[slot] = (r, rn)
                g_ = slot % RG
                P.dma('pool', None, None, reads=[en, 'uvb'], writes=[rn],
                      fn=lambda e, r=r, slot=slot: e.indirect_dma_start(out=r[:], out_offset=None, in_=uvb,
                                                                         in_offset=bass.IndirectOffsetOnAxis(ap=eidx[:, slot:slot + 1], axis=0)))
                P.op('dve', lambda e, r=r, g_=g_: e.scalar_tensor_tensor(out=junk[:], in0=r[:, 0:D], scalar=1.0, in1=h2t[:], op0=ALU.mult, op1=ALU.mult,
                                                                         accum_out=Acol[g_][:, 0:1]),
                     reads=[rn, hn_], writes=['junk', f"Acol{g_}"])
                P.op('act', lambda e, g_=g_: e.activation(out=gA[g_][:], in_=Acol[g_][:], func=AF.Gelu), reads=[f"Acol{g_}"], writes=[f"gA{g_}"])
            if slot >= 1:
                ps_ = slot - 1
                r, rn = slot_rows.pop(ps_)
                g_ = ps_ % RG
                P.op('act', lambda e, g_=g_, ps_=ps_: e.activation(out=gA[g_][:], in_=gA[g_][:], func=AF.Copy, scale=gate[:, ps_:ps_ + 1]),
                     reads=[f"gA{g_}", gn], writes=[f"gA{g_}"])
                P.op('act', lambda e, g_=g_: e.activation(out=dgs[g_][:], in_=identb[:], func=AF.Copy, scale=gA[g_][:, 0:1]),
                     reads=['identb', f"gA{g_}"], writes=[f"dgs{g_}"])
                for hf_ in range(2):
                    pst = psm if hf_ == 0 else psr
                    P.op('pe', lambda e, r=r, g_=g_, hf_=hf_, pst=pst, ps_=ps_: e.matmul(pst[:, :], lhsT=dgs[g_][:], rhs=r[:, D + hf_ * 512:D + (hf_ + 1) * 512],
                                                                                       start=(ps_ == 0), stop=(ps_ == NSLOT - 1)),
                         reads=[f"dgs{g_}", rn], writes=['psm' if hf_ == 0 else 'psr'])
            P.replay(recs, per)
        P.replay(recs, len(recs))
        P.op('dve', lambda e: e.tensor_tensor(out=acc[:, 0:512], in0=psm[:, :], in1=rep[("g2a", col)][:, 0:512], op=ALU.mult), reads=['psm', 'rep'], writes=['acc'])
        P.op('dve', lambda e: e.tensor_tensor(out=acc[:, 512:1024], in0=psr[:, :], in1=rep[("g2a", col)][:, 512:1024], op=ALU.mult), reads=['psr', 'rep'], writes=['acc'])
        P.op('dve', lambda e: e.tensor_tensor(out=acc[:], in0=acc[:], in1=XK[:], op=ALU.add), reads=['acc', xkn], writes=['acc'])
        for hf in range(2):
            P.op('dve', lambda e, hf=hf: e.bn_stats(out=stats[:, hf, :], in_=acc[:, hf * 512:(hf + 1) * 512]), reads=['acc'], writes=['stats'])
        P.op('dve', lambda e: e.bn_aggr(out=mv[:], in_=stats[:].rearrange("p a b -> p (a b)")), reads=['stats'], writes=['mv'])
        P.op('dve', lambda e: e.tensor_scalar(out=rs[:], in0=mv[:, 1:2], scalar1=LN_EPS / (ALPHA * ALPHA), scalar2=None, op0=ALU.add), reads=['mv'], writes=['rs'])
        P.op('act', lambda e: e.activation(out=rs[:], in_=rs[:], func=AF.Sqrt), reads=['rs'], writes=['rs'])
        P.op('dve', lambda e: e.reciprocal(out=rs[:], in_=rs[:]), reads=['rs'], writes=['rs'])
        XO = xo[b]
        xon = f"xo{b}"
        P.op('dve', lambda e: e.tensor_scalar(out=XO[:], in0=acc[:], scalar1=mv[:, 0:1], scalar2=rs[:, 0:1], op0=ALU.subtract, op1=ALU.mult),
             reads=['acc', 'mv', 'rs'], writes=[xon])
        P.op('dve', lambda e: e.tensor_tensor(out=XO[:], in0=XO[:], in1=lng[:], op=ALU.mult), reads=[xon, 'lng'], writes=[xon])
        P.op('dve', lambda e: e.tensor_tensor(out=XO[:], in0=XO[:], in1=lnb[:], op=ALU.add), reads=[xon, 'lnb'], writes=[xon])
        P.dma('sp', out_d[s], XO[:], reads=[xon], writes=['x2_scr'])

    front(0)
    for s in range(nsub):
        recs = []
        if s + 1 < nsub:
            P.rec = recs
            front(s + 1)
            P.rec = None
        back(s, recs)
    K.end_stage()


CDW = 2832
RMS_EPS = 1e-6


def stage_l1a(K, ntiles, mcols, src_d, nsub, gb_):
    K.begin_stage("la_")
    nc, P = K.nc, K.P
    cos_d = K.din("cosT", [ntiles, 64, NT])
    sin_d = K.din("sinT", [ntiles, 64, NT])
    cvec_d = K.din("cvec", [128, KC, 2])
    modw_d = K.din("mod_w1", [D, 6144])
    modb_d = K.din("mod_bT1", [128, 48])
    ident_d = K.din("ident", [128, 128])
    win_d = K.din("cd_w_in", [D, CDW])
    gb_d = K.din("gate_bT", [16, 1])
    qg_d = K.din("qn_gT", [64, 1])
    kg_d = K.din("kn_gT", [64, 1])
    pm_d = K.din("ropeP", [64, 64])
    tsl = lambda t: slice(t * NT, (t + 1) * NT)
    xl_o = gb_['xl0T']

    psA = [K.ps(f"psA{i}", [128, 512]) for i in range(2)]
    psr = K.ps("psr", [128, 512])
    psw = K.ps("psw", [128, 512])
    psm = K.ps("psm", [128, 512])
    psX = [K.ps(f"psX{i}", [128, 512]) for i in range(2)]

    ident = K.sb("ident", [128, 128])
    P.dma('sp', ident[:], ident_d, writes=['ident'])
    ones64 = K.sb("ones64", [64, 64])
    P.op('dve', lambda e: e.memset(ones64[:], 1.0 / 64), writes=['ones64'])
    gb = K.sb("gb", [16, 1]); qg = K.sb("qg", [64, 1]); kg = K.sb("kg", [64, 1]); pm = K.sb("pm", [64, 64])
    for t_, d_, n_ in ((gb, gb_d, 'gb'), (qg, qg_d, 'qg'), (kg, kg_d, 'kg'), (pm, pm_d, 'pm')):
        P.dma('sp', t_[:], d_, writes=[n_])
    sc1p = K.sb("sc1p", [128, KC, 2])
    win_b = K.sb("win_b", [128, KC, CDW], BF16)
    tmp = ExitStack()
    modT = mod_setup(K, cvec_d, modw_d, modb_d, 0, 16, psm, tmp=tmp)
    stage = [tmp.enter_context(nc.sbuf_tensor(f"s_{K.pfx}stage{i}", [128, CDW], F32)) for i in range(2)]
    P.op('dve', lambda e: e.tensor_scalar(out=sc1p[:], in0=modT[:, 8:16, :], scalar1=1.0, scalar2=None, op0=ALU.add),
         reads=['modT'], writes=['sc1p'])
    for k in range(KC):
        load_cast(K, win_b[:, k, :], win_d[k * 128:(k + 1) * 128, :], stage[k % 2][:], f"stage{k % 2}", "win_b", eng='dve' if k % 2 == 0 else 'act')
    P.barrier()
    tmp.close()

    xt = [K.sb(f"xt{i}", [128, KC, NT]) for i in range(2)]
    xk = [K.sb(f"xk{i}", [128, D]) for i in range(2)]
    cs = [K.sb(f"cs{i}", [64, NT]) for i in range(2)]
    sn = [K.sb(f"sn{i}", [64, NT]) for i in range(2)]
    hT = K.sb("hT", [128, KC, NT], BF16)
    ob = [K.sb(f"ob{i}", [128, NT], BF16) for i in range(4)]
    of = [K.sb(f"of{i}", [128, NT], F32) for i in range(2)]
    sq = K.sb("sq", [64, NT]); rstd = K.sb("rstd", [64, NT]); qn = K.sb("qn", [64, NT]); t1 = K.sb("t1", [64, NT]); t2 = K.sb("t2", [64, NT])
    obi = 0
    ofi = 0

    def proj(c0, M, ps, pn):
        for k in range(KC):
            P.op('pe', lambda e, k=k: e.matmul(ps[0:M, 0:NT], lhsT=win_b[:, k, c0:c0 + M], rhs=hT[:, k, :], start=(k == 0), stop=(k == KC - 1)),
                 reads=['win_b', 'hT'], writes=[pn], inc=(k == KC - 1))

    for t in range(ntiles):
        col = mcols[t]
        b = t % 2
        X, CS, SN = xt[b], cs[b], sn[b]
        xn, csn, snn = f"xt{b}", f"cs{b}", f"sn{b}"
        for hh in range(2):
            s_ = 2 * t + hh
            if s_ >= nsub:
                P.op('dve', lambda e, hh=hh: e.memset(X[:, :, hh * 128:(hh + 1) * 128], 0.0), writes=[xn])
                continue
            XK = xk[hh]; xkn = f"xk{hh}"
            P.dma('sp', XK[:], src_d[s_], reads=['x2_scr'], writes=[xkn])
            for c in range(KC):
                pst = psX[c // 4]
                P.op('pe', lambda e, c=c, pst=pst, XK=XK: e.transpose(out=pst[:, (c % 4) * 128:(c % 4) * 128 + 128], in_=XK[:, c * 128:(c + 1) * 128], identity=ident[:]),
                     reads=[xkn, 'ident'], writes=[f'psX{c // 4}'])
            P.op('act', lambda e, hh=hh: e.activation(out=X[:, 0:4, hh * 128:(hh + 1) * 128], in_=psX[0][:, :].rearrange("p (c n) -> p c n", c=4), func=AF.Copy),
                 reads=['psX0'], writes=[xn])
            P.op('dve', lambda e, hh=hh: e.tensor_copy(out=X[:, 4:8, hh * 128:(hh + 1) * 128], in_=psX[1][:, :].rearrange("p (c n) -> p c n", c=4)),
                 reads=['psX1'], writes=[xn])
        if t < ntiles - 1:
            P.dma('sp', xl_o[t].rearrange("(c p) n -> p c n", p=128), X[:], reads=[xn], writes=['xl_o'])
        P.dma('sp', CS[:], cos_d[t], writes=[csn])
        P.dma('sp', SN[:], sin_d[t], writes=[snn])
        for c in range(KC):
            P.op('act', lambda e, c=c: e.activation(out=hT[:, c, :], in_=X[:, c, :], func=AF.Identity,
                                                    scale=sc1p[:, c, col:col + 1], bias=modT[:, c, col:col + 1]),
                 reads=[xn, 'sc1p', 'modT'], writes=['hT'])
        for oc in range(16):
            ps = psA[oc % 2]; pn = f"psA{oc % 2}"
            proj(oc * 128, 128, ps, pn)
            grp, hh = oc // 4, oc % 4
            if grp == 3:
                o = of[ofi % 2]; on = f"of{ofi % 2}"; ofi += 1
                P.op('act', lambda e, o=o, ps=ps: e.activation(out=o[:], in_=ps[:, 0:NT], func=AF.Copy), reads=[pn], writes=[on])
                P.dma('sp', gb_['om'][hh * 128:(hh + 1) * 128, tsl(t)], o[:], reads=[on], writes=['om_o'])
            else:
                o = ob[obi % 4]; on = f"ob{obi % 4}"; obi += 1
                scale = (128.0 ** -0.5) if grp == 1 else 1.0
                if oc % 2 == 0:
                    P.op('act', lambda e, o=o, ps=ps, scale=scale: e.activation(out=o[:], in_=ps[:, 0:NT], func=AF.Copy, scale=scale), reads=[pn], writes=[on])
                else:
                    P.op('dve', lambda e, o=o, ps=ps, scale=scale: e.tensor_scalar(out=o[:], in0=ps[:, 0:NT], scalar1=scale, scalar2=None, op0=ALU.mult), reads=[pn], writes=[on])
                dst = (gb_['qm'], gb_['km'], gb_['vm'])[grp]
                P.dma('sp', dst[hh * 128:(hh + 1) * 128, tsl(t)], o[:], reads=[on], writes=['qkv_o'])
        for hd in range(10):
            ps = psA[hd % 2]; pn = f"psA{hd % 2}"
            proj(2048 + hd * 64, 64, ps, pn)
            gvec, gname = (qg, 'qg') if hd < 8 else (kg, 'kg')
            P.op('act', lambda e, ps=ps: e.activation(out=sq[:], in_=ps[0:64, 0:NT], func=AF.Square), reads=[pn], writes=['sq'])
            P.op('pe', lambda e: e.matmul(psr[0:64, 0:NT], lhsT=ones64[:], rhs=sq[:], start=True, stop=True), reads=['ones64', 'sq'], writes=['psr'])
            P.op('dve', lambda e: e.tensor_scalar(out=rstd[:], in0=psr[0:64, 0:NT], scalar1=RMS_EPS, scalar2=None, op0=ALU.add), reads=['psr'], writes=['rstd'])
            P.op('act', lambda e: e.activation(out=rstd[:], in_=rstd[:], func=AF.Sqrt), reads=['rstd'], writes=['rstd'])
            P.op('dve', lambda e: e.reciprocal(out=rstd[:], in_=rstd[:]), reads=['rstd'], writes=['rstd'])
            P.op('act', lambda e, ps=ps, gvec=gvec: e.activation(out=qn[:], in_=ps[0:64, 0:NT], func=AF.Copy, scale=gvec[:, 0:1]), reads=[pn, gname], writes=['qn'])
            P.op('dve', lambda e: e.tensor_tensor(out=qn[:], in0=qn[:], in1=rstd[:], op=ALU.mult), reads=['qn', 'rstd'], writes=['qn'])
            P.op('pe', lambda e: e.matmul(psw[0:64, 0:NT], lhsT=pm[:], rhs=qn[:], start=True, stop=True), reads=['pm', 'qn'], writes=['psw'])
            P.op('dve', lambda e: e.tensor_tensor(out=t1[:], in0=qn[:], in1=CS[:], op=ALU.mult), reads=['qn', csn], writes=['t1'])
            P.op('dve', lambda e: e.tensor_tensor(out=t2[:], in0=psw[0:64, 0:NT], in1=SN[:], op=ALU.mult), reads=['psw', snn], writes=['t2'])
            o = ob[obi % 4]; on = f"ob{obi % 4}"; obi += 1
            P.op('dve', lambda e, o=o: e.tensor_tensor(out=o[0:64, :], in0=t1[:], in1=t2[:], op=ALU.add), reads=['t1', 't2'], writes=[on])
            dst = gb_['qa'][hd * 64:(hd + 1) * 64, tsl(t)] if hd < 8 else gb_['ka'][(hd - 8) * 64:(hd - 7) * 64, tsl(t)]
            P.dma('sp', dst, o[0:64, :], reads=[on], writes=['qk_o'])
        for hd in range(2):
            ps = psA[hd % 2]; pn = f"psA{hd % 2}"
            proj(2048 + 640 + hd * 64, 64, ps, pn)
            o = ob[obi % 4]; on = f"ob{obi % 4}"; obi += 1
            P.op('act', lambda e, o=o, ps=ps: e.activation(out=o[0:64, :], in_=ps[0:64, 0:NT], func=AF.Copy), reads=[pn], writes=[on])
            P.dma('sp', gb_['va'][hd * 64:(hd + 1) * 64, tsl(t)], o[0:64, :], reads=[on], writes=['va_o'])
        ps = psA[0]; pn = "psA0"
        proj(2816, 16, ps, pn)
        o = of[ofi % 2]; on = f"of{ofi % 2}"; ofi += 1
        P.op('act', lambda e, o=o, ps=ps: e.activation(out=o[0:16, :], in_=ps[0:16, 0:NT], func=AF.Identity, bias=gb[:, 0:1]), reads=[pn, 'gb'], writes=[on])
        P.dma('sp', gb_['g'][:, tsl(t)], o[0:16, :], reads=[on], writes=['gt_o'])
    K.end_stage()


NEGM = -1.0e30


def stage_attn(K, Th, TOK, gb_):
    K.begin_stage("at_")
    nc, P = K.nc, K.P
    Tq = Th
    Tk = 2 * Th + 256
    nkt = Tk // 128
    nqt = Tq // 512
    qg_d = K.din("qn_g", [1, 64])
    kg_d = K.din("kn_g", [1, 64])
    sel_d = K.din("sel", [65, 64])
    ident_d = K.din("ident", [128, 128])
    psST = [[K.ps(f"psST{hh}{i}", [128, 512]) for i in range(2)] for hh in range(2)]
    psO = [K.ps(f"psO{hh}", [128, 512]) for hh in range(2)]
    psD = K.ps("psD", [128, 512])
    psT = K.ps("psT", [128, 512], BF16)
    sel = K.sb("sel", [65, 64])
    P.dma('sp', sel[:], sel_d, writes=['sel'])
    identf = K.sb("identf", [128, 128]); identb = K.sb("identb", [128, 128], BF16)
    P.dma('sp', identf[:], ident_d, writes=['identf'])
    P.op('dve', lambda e: e.tensor_copy(out=identb[:], in_=identf[:]), reads=['identf'], writes=['identb'])
    gq = K.sb("gq", [128, 64]); gk = K.sb("gk", [128, 64]); mq = K.sb("mq", [128, 1]); mk = K.sb("mk", [128, 1]); negC = K.sb("negC", [128, 1])
    P.dma('sp', gq[:], bass.AP(qg_d.tensor, 0, [[0, 128], [1, 64]]), writes=['gq'])
    P.dma('sp', gk[:], bass.AP(kg_d.tensor, 0, [[0, 128], [1, 64]]), writes=['gk'])
    P.op('dve', lambda e: e.tensor_reduce(out=mq[:], in_=gq[:], axis=AX.X, op=ALU.max, apply_absolute_value=True), reads=['gq'], writes=['mq'])
    P.op('dve', lambda e: e.tensor_reduce(out=mk[:], in_=gk[:], axis=AX.X, op=ALU.max, apply_absolute_value=True), reads=['gk'], writes=['mk'])
    P.op('dve', lambda e: e.scalar_tensor_tensor(out=negC[:], in0=mq[:], scalar=-8.0, in1=mk[:], op0=ALU.mult, op1=ALU.mult), reads=['mq', 'mk'], writes=['negC'])
    qT = K.sb("qT", [128, Tq], BF16)
    kT = K.sb("kT", [128, Tk], BF16)
    vT = K.sb("vT", [64, Tk], BF16)
    vaug = K.sb("vaug", [128, nkt, 65], BF16)
    PT = [[K.sb(f"PT{hh}{i}", [128, 512], BF16) for i in range(2)] for hh in range(2)]
    OTs = K.sb("OTs", [65, 512])
    rden = K.sb("rden", [64, 512])
    ob = [K.sb(f"ob{i}", [64, 512]) for i in range(2)]
    P.op('dve', lambda e: e.memset(vaug[:, :, 64:65], 1.0), writes=['vaug1'])
    it = 0
    for up in range(4):
        kvh = up // 2
        P.dma('sp', qT[:], gb_['qa'][up * 128:(up + 1) * 128, 0:Th], reads=['qk_o'], writes=['qT'])
        if up % 2 == 0:
            for m_ in range(2):
                rows = slice(m_ * 128 + kvh * 64, m_ * 128 + kvh * 64 + 64)
                for hh in range(2):
                    P.dma('sp', kT[hh * 64:(hh + 1) * 64, m_ * 128:(m_ + 1) * 128], gb_['ka_all'][rows, Th:Th + 128], reads=['ka_all'], writes=['kT'])
                    P.dma('sp', kT[hh * 64:(hh + 1) * 64, 256 + m_ * Th:256 + (m_ + 1) * Th], gb_['ka_all'][rows, 0:Th], reads=['ka_all'], writes=['kT'])
                P.dma('sp', vT[:, m_ * 128:(m_ + 1) * 128], gb_['va_all'][rows, Th:Th + 128], reads=['va_all'], writes=['vT'])
                P.dma('sp', vT[:, 256 + m_ * Th:256 + (m_ + 1) * Th], gb_['va_all'][rows, 0:Th], reads=['va_all'], writes=['vT'])
            for kt in range(nkt):
                P.op('pe', lambda e, kt=kt: e.transpose(out=psT[:, 0:64], in_=vT[:, kt * 128:(kt + 1) * 128], identity=identb[0:64, 0:64]),
                     reads=['vT', 'identb'], writes=['psT'])
                P.op('act', lambda e, kt=kt: e.activation(out=vaug[:, kt, 0:64], in_=psT[:, 0:64], func=AF.Copy), reads=['psT'], writes=['vaug'])
        for qt in range(nqt):
            def score(kt, qt=qt):
                for hh in range(2):
                    ps = psST[hh][kt % 2]
                    P.op('pe', lambda e, ps=ps, kt=kt, qt=qt, hh=hh: e.matmul(ps[:, :], lhsT=kT[hh * 64:(hh + 1) * 64, kt * 128:(kt + 1) * 128],
                                                                             rhs=qT[hh * 64:(hh + 1) * 64, qt * 512:(qt + 1) * 512], start=True, stop=True),
                         reads=['kT', 'qT'], writes=[f"psST{hh}{kt % 2}"])
            score(0)
            for kt in range(nkt):
                if kt + 1 < nkt:
                    score(kt + 1)
                for hh in range(2):
                    ps = psST[hh][kt % 2]; pn = f"psST{hh}{kt % 2}"
                    pt = PT[hh][kt % 2]; ptn = f"PT{hh}{kt % 2}"
                    P.op('act', lambda e, ps=ps, pt=pt: e.activation(out=pt[:], in_=ps[:, :], func=AF.Exp, scale=0.125, bias=negC[:, 0:1]),
                         reads=[pn, 'negC'], writes=[ptn])
                    P.op('pe', lambda e, pt=pt, kt=kt, hh=hh: e.matmul(psO[hh][0:65, :], lhsT=vaug[:, kt, :], rhs=pt[:], start=(kt == 0), stop=(kt == nkt - 1)),
                         reads=['vaug', 'vaug1', ptn], writes=[f'psO{hh}'])
            for hh in range(2):
                u = up * 2 + hh
                P.op('act', lambda e, hh=hh: e.activation(out=OTs[:], in_=psO[hh][0:65, :], func=AF.Copy), reads=[f'psO{hh}'], writes=['OTs'])
                P.op('pe', lambda e: e.matmul(psD[0:64, :], lhsT=sel[:], rhs=OTs[:], start=True, stop=True), reads=['sel', 'OTs'], writes=['psD'])
                P.op('dve', lambda e: e.reciprocal(out=rden[:], in_=psD[0:64, :]), reads=['psD'], writes=['rden'])
                o = ob[it % 2]; on = f"ob{it % 2}"; it += 1
                P.op('dve', lambda e, o=o: e.tensor_tensor(out=o[:], in0=OTs[0:64, :], in1=rden[:], op=ALU.mult), reads=['OTs', 'rden'], writes=[on])
                P.dma('sp', gb_['ya'][u * 64:(u + 1) * 64, qt * 512:(qt + 1) * 512], o[:], reads=[on], writes=['ya_l'])
    K.end_stage()


def stage_scan(K, nunits, Th, TOK, gb_):
    K.begin_stage("sc_")
    nc, P = K.nc, K.P
    ncc = 2
    nchl = 2 * Th // 128
    NCH = ncc + nchl
    cpm = Th // 128
    sidx_d = K.din("scan_idx", [128, nunits * 2], I32)
    gidx_d = K.din("scan_gidx", [128, nunits * 2], I32)
    cst_d = K.din("consts", [5, 128, 128])
    h_o = gb_['h']
    def ccol(c):
        if c < ncc:
            return c, Th
        lc = c - ncc
        return lc // cpm, (lc % cpm) * 128

    psR = K.ps("psR", [128, 512]); psQK = K.ps("psQK", [128, 512]); psT = K.ps("psT", [128, 512], BF16)
    psN = K.ps("psN", [128, 512]); psC = K.ps("psC", [128, 512]); psX = K.ps("psX", [128, 512]); psB = K.ps("psB", [128, 512])
    cst = K.sb("cst", [128, 5, 128])
    P.dma('sp', cst[:], cst_d.rearrange("c p n -> p c n"), writes=['cst'])
    ident, triL, triU, mnegL, mnegU = [cst[:, i, :] for i in range(5)]
    identb = K.sb("identb", [128, 128], BF16)
    P.op('dve', lambda e: e.tensor_copy(out=identb[:], in_=ident), reads=['cst'], writes=['identb'])
    ones = K.sb("ones", [128, 128])
    P.op('dve', lambda e: e.memset(ones[:], 1.0), writes=['ones'])
    qTm = [K.sb(f"qTm{i}", [128, TOK], BF16) for i in range(2)]
    kTm = [K.sb(f"kTm{i}", [128, TOK], BF16) for i in range(2)]
    vTm = [K.sb(f"vTm{i}", [128, TOK], BF16) for i in range(2)]
    gsel = [K.sb(f"gsel{i}", [4, TOK]) for i in range(2)]
    sidx = K.sb("sidx", [128, nunits * 2], I32); gidx = K.sb("gidx", [128, nunits * 2], I32)
    P.dma('sp', sidx[:], sidx_d, writes=['sidx'])
    P.dma('sp', gidx[:], gidx_d, writes=['gidx'])
    def qsl(c):
        m_, c0 = ccol(c)
        return qTm[m_][:, c0:c0 + 128]
    def ksl(c):
        m_, c0 = ccol(c)
        return kTm[m_][:, c0:c0 + 128]
    ktok = K.sb("ktok", [128, NCH, 128], BF16); vaug = K.sb("vaug", [128, NCH, 129], BF16)
    P.op('dve', lambda e: e.memset(vaug[:, :, 128:129], 1.0), writes=['vaug1'])
    G = K.sb("G", [128, 4, NCH])
    ee = K.sb("ee", [128, NCH]); LF = K.sb("LF", [128, NCH]); Bc = K.sb("Bc", [128, NCH]); BL = K.sb("BL", [128, NCH]); IGmB = K.sb("IGmB", [128, NCH])
    Caug = K.sb("Caug", [128, 129]); Cb = K.sb("Cb", [128, 129], BF16); m = K.sb("m", [128, 1])
    igrep = K.sb("igrep", [128, 128]); dm = K.sb("dm", [128, 128]); sexp = K.sb("sexp", [128, 128]); s_b = K.sb("s_b", [128, 128], BF16)
    sT = K.sb("sT", [128, 128], BF16); numA = K.sb("numA", [128, 129]); tot = K.sb("tot", [128, 129]); kw = K.sb("kw", [128, 128], BF16)
    col = {n: K.sb(n, [128, 1]) for n in ("rowmax", "glmax", "mnew", "nmnew", "inter", "mt", "nmt", "winter", "emt", "dd", "t2", "wk", "t3", "decay")}
    hb = [K.sb(f"hb{i}", [128, 128]) for i in range(2)]
    hi = 0

    def c1(name, fn, reads, eng='dve'):
        P.op(eng, fn, reads=reads, writes=[name])

    for u in range(nunits):
        for m_ in range(2):
            ic = u * 2 + m_
            for (dst_, srcn, dn) in ((qTm[m_], 'qm_all', 'qT'), (kTm[m_], 'km_all', 'kT'), (vTm[m_], 'vm_all', 'vT')):
                P.dma('pool', None, None, reads=[srcn, 'sidx'], writes=[dn],
                      fn=lambda e, dst_=dst_, srcn=srcn, ic=ic: e.indirect_dma_start(out=dst_[:], out_offset=None, in_=gb_[srcn],
                                                                                   in_offset=bass.IndirectOffsetOnAxis(ap=sidx[:, ic:ic + 1], axis=0)))
            P.dma('pool', None, None, reads=['g_all', 'gidx'], writes=['gsel'],
                  fn=lambda e, m_=m_, ic=ic: e.indirect_dma_start(out=gsel[m_][:], out_offset=None, in_=gb_['g_all'],
                                                                   in_offset=bass.IndirectOffsetOnAxis(ap=gidx[0:4, ic:ic + 1], axis=0)))
        for c in range(NCH):
            m_, c0 = ccol(c)
            P.op('pe', lambda e, m_=m_, c0=c0: e.transpose(out=psT[:, 0:128], in_=kTm[m_][:, c0:c0 + 128], identity=identb[:]), reads=['kT', 'identb'], writes=['psT'])
            P.op('act', lambda e, c=c: e.activation(out=ktok[:, c, :], in_=psT[:, 0:128], func=AF.Copy), reads=['psT'], writes=['ktok'])
            P.op('pe', lambda e, m_=m_, c0=c0: e.transpose(out=psT[:, 128:256], in_=vTm[m_][:, c0:c0 + 128], identity=identb[:]), reads=['vT', 'identb'], writes=['psT'])
            P.op('dve', lambda e, c=c: e.tensor_copy(out=vaug[:, c, 0:128], in_=psT[:, 128:256]), reads=['psT'], writes=['vaug'])
            P.op('pe', lambda e, m_=m_, c0=c0: e.transpose(out=psB[:, 0:4], in_=gsel[m_][0:4, c0:c0 + 128], identity=ident[0:4, 0:4]), reads=['gsel', 'cst'], writes=['psB'])
            P.op('dve', lambda e, c=c: e.tensor_copy(out=G[:, :, c], in_=psB[:, 0:4]), reads=['psB'], writes=['G'])
        for dr in range(2):
            tri = triL if dr == 0 else triU
            mneg = mnegL if dr == 0 else mnegU
            gi, gf = 2 * dr, 2 * dr + 1
            P.op('act', lambda e: e.activation(out=ee[:], in_=G[:, gf, :], func=AF.Exp, scale=-1.0), reads=['G'], writes=['ee'])
            P.op('dve', lambda e: e.tensor_scalar(out=ee[:], in0=ee[:], scalar1=1.0, scalar2=None, op0=ALU.add), reads=['ee'], writes=['ee'])
            P.op('act', lambda e: e.activation(out=ee[:], in_=ee[:], func=AF.Ln), reads=['ee'], writes=['ee'])
            P.op('dve', lambda e: e.tensor_scalar(out=LF[:], in0=ee[:], scalar1=-1.0, scalar2=None, op0=ALU.mult), reads=['ee'], writes=['LF'])
            P.op('pe', lambda e: e.matmul(psB[:, 0:NCH], lhsT=tri, rhs=LF[:], start=True, stop=True), reads=['cst', 'LF'], writes=['psB'])
            P.op('dve', lambda e: e.tensor_copy(out=Bc[:], in_=psB[:, 0:NCH]), reads=['psB'], writes=['Bc'])
            P.op('pe', lambda e: e.matmul(psB[:, 0:NCH], lhsT=ones[:], rhs=LF[:], start=True, stop=True), reads=['ones', 'LF'], writes=['psB'])
            P.op('dve', lambda e: e.tensor_copy(out=BL[:], in_=psB[:, 0:NCH]), reads=['psB'], writes=['BL'])
            P.op('dve', lambda e: e.tensor_tensor(out=IGmB[:], in0=G[:, gi, :], in1=Bc[:], op=ALU.subtract), reads=['G', 'Bc'], writes=['IGmB'])
            P.op('dve', lambda e: e.memset(Caug[:], 0.0), writes=['Caug'])
            P.op('dve', lambda e: e.memset(Cb[:], 0.0), writes=['Cb'])
            P.op('dve', lambda e: e.memset(m[:], 0.0), writes=['m'])
            order = list(range(ncc)) + [ncc + i for i in range(nchl)]
            if dr == 1:
                order = list(range(ncc))[::-1] + [ncc + i for i in range(nchl)][::-1]
            for c in order:
                lat = c >= ncc
                cc = slice(c, c + 1)
                P.op('dve', lambda e, c=c: e.tensor_copy(out=igrep[:], in_=fap(IGmB, c, [[0, 128]])), reads=['IGmB'], writes=['igrep'])
                P.op('pe', lambda e: e.matmul(psR[:, 0:128], lhsT=igrep[:], rhs=ident, start=True, stop=True), reads=['igrep', 'cst'], writes=['psR'])
                c1('glmax', lambda e: e.tensor_reduce(out=col['glmax'][:], in_=psR[:, 0:128], axis=AX.X, op=ALU.max), ['psR'])
                c1('mnew', lambda e, cc=cc: e.scalar_tensor_tensor(out=col['mnew'][:], in0=col['glmax'][:], scalar=m[:, 0:1], in1=BL[:, cc], op0=ALU.max, op1=ALU.add),
                   ['glmax', 'm', 'BL'])
                c1('nmnew', lambda e: e.tensor_scalar(out=col['nmnew'][:], in0=col['mnew'][:], scalar1=-1.0, scalar2=None, op0=ALU.mult), ['mnew'])
                if lat:
                    P.op('dve', lambda e, cc=cc: e.scalar_tensor_tensor(out=dm[:], in0=psR[:, 0:128], scalar=Bc[:, cc], in1=mneg, op0=ALU.add, op1=ALU.add),
                         reads=['psR', 'Bc', 'cst'], writes=['dm'])
                    c1('rowmax', lambda e: e.tensor_reduce(out=col['rowmax'][:], in_=dm[:], axis=AX.X, op=ALU.max), ['dm'])
                    c1('inter', lambda e, cc=cc: e.tensor_tensor(out=col['inter'][:], in0=Bc[:, cc], in1=m[:], op=ALU.add), ['Bc', 'm'])
                    c1('mt', lambda e: e.tensor_tensor(out=col['mt'][:], in0=col['inter'][:], in1=col['rowmax'][:], op=ALU.max), ['inter', 'rowmax'])
                    c1('nmt', lambda e: e.tensor_scalar(out=col['nmt'][:], in0=col['mt'][:], scalar1=-1.0, scalar2=None, op0=ALU.mult), ['mt'])
                    P.op('act', lambda e: e.activation(out=sexp[:], in_=dm[:], func=AF.Exp, bias=col['nmt'][:, 0:1]), reads=['dm', 'nmt'], writes=['sexp'])
                    c1('winter', lambda e: e.activation(out=col['winter'][:], in_=col['inter'][:], func=AF.Exp, bias=col['nmt'][:, 0:1]), ['inter', 'nmt'], eng='act')
                    c1('emt', lambda e: e.activation(out=col['emt'][:], in_=col['mt'][:], func=AF.Exp, scale=-1.0), ['mt'], eng='act')
                    P.op('pe', lambda e, c=c: e.matmul(psQK[:, 0:128], lhsT=qsl(c), rhs=ksl(c), start=True, stop=True),
                         reads=['qT', 'kT'], writes=['psQK'])
                    P.op('dve', lambda e: e.tensor_tensor(out=s_b[:], in0=psQK[:, 0:128], in1=sexp[:], op=ALU.mult), reads=['psQK', 'sexp'], writes=['s_b'])
                    P.op('pe', lambda e: e.transpose(out=psT[:, 0:128], in_=s_b[:], identity=identb[:]), reads=['s_b', 'identb'], writes=['psT'])
                    P.op('act', lambda e: e.activation(out=sT[:], in_=psT[:, 0:128], func=AF.Copy), reads=['psT'], writes=['sT'])
                    P.op('pe', lambda e, c=c: e.matmul(psN[:, 0:129], lhsT=sT[:], rhs=vaug[:, c, :], start=True, stop=True), reads=['sT', 'vaug', 'vaug1'], writes=['psN'])
                    P.op('pe', lambda e, c=c: e.matmul(psX[:, 0:129], lhsT=qsl(c), rhs=Cb[:], start=True, stop=True), reads=['qT', 'Cb'], writes=['psX'])
                    P.op('act', lambda e: e.activation(out=numA[:], in_=psN[:, 0:129], func=AF.Copy), reads=['psN'], writes=['numA'])
                    P.op('dve', lambda e: e.scalar_tensor_tensor(out=tot[:], in0=psX[:, 0:129], scalar=col['winter'][:, 0:1], in1=numA[:], op0=ALU.mult, op1=ALU.add),
                         reads=['psX', 'winter', 'numA'], writes=['tot'])
                    c1('dd', lambda e: e.activation(out=col['dd'][:], in_=tot[:, 128:129], func=AF.Abs), ['tot'], eng='act')
                    c1('dd', lambda e: e.tensor_tensor(out=col['dd'][:], in0=col['dd'][:], in1=col['emt'][:], op=ALU.max), ['dd', 'emt'])
                    c1('dd', lambda e: e.reciprocal(out=col['dd'][:], in_=col['dd'][:]), ['dd'])
                    H = hb[hi % 2]; hn = f"hb{hi % 2}"; hi += 1
                    P.op('dve', lambda e, H=H: e.tensor_scalar(out=H[:], in0=tot[:, 0:128], scalar1=col['dd'][:, 0:1], scalar2=None, op0=ALU.mult), reads=['tot', 'dd'], writes=[hn])
                    lc = c - ncc
                    r0 = ((u * 2 + dr) * nchl + lc) * 128
                    P.dma('sp', h_o[r0:r0 + 128, :], H[:], reads=[hn], writes=['h_o'])
                c1('t2', lambda e, cc=cc: e.tensor_tensor(out=col['t2'][:], in0=IGmB[:, cc], in1=BL[:, cc], op=ALU.add), ['IGmB', 'BL'])
                c1('wk', lambda e: e.activation(out=col['wk'][:], in_=col['t2'][:], func=AF.Exp, bias=col['nmnew'][:, 0:1]), ['t2', 'nmnew'], eng='act')
                P.op('dve', lambda e, c=c: e.tensor_scalar(out=kw[:], in0=ktok[:, c, :], scalar1=col['wk'][:, 0:1], scalar2=None, op0=ALU.mult), reads=['ktok', 'wk'], writes=['kw'])
                P.op('pe', lambda e, c=c: e.matmul(psC[:, 0:129], lhsT=kw[:], rhs=vaug[:, c, :], start=True, stop=True), reads=['kw', 'vaug', 'vaug1'], writes=['psC'])
                c1('t3', lambda e, cc=cc: e.tensor_tensor(out=col['t3'][:], in0=BL[:, cc], in1=m[:], op=ALU.add), ['BL', 'm'])
                c1('decay', lambda e: e.activation(out=col['decay'][:], in_=col['t3'][:], func=AF.Exp, bias=col['nmnew'][:, 0:1]), ['t3', 'nmnew'], eng='act')
                P.op('dve', lambda e: e.scalar_tensor_tensor(out=Caug[:], in0=Caug[:], scalar=col['decay'][:, 0:1], in1=psC[:, 0:129], op0=ALU.mult, op1=ALU.add),
                     reads=['Caug', 'decay', 'psC'], writes=['Caug'])
                P.op('act', lambda e: e.activation(out=Cb[:], in_=Caug[:], func=AF.Copy), reads=['Caug'], writes=['Cb'])
                P.op('dve', lambda e: e.tensor_copy(out=m[:], in_=col['mnew'][:]), reads=['mnew', 'inter', 't3', 'glmax'], writes=['m'])
    K.end_stage()


def stage_l1c(K, ntiles, out_d, gb_, half_tok):
    K.begin_stage("lc_")
    nc, P = K.nc, K.P
    xt_d = gb_['xl0T']
    hidx_d = K.din("h_idx", [128, ntiles * 2 * 8], I32)
    ident_d = K.din("ident", [128, 128])
    cvec_d = K.din("cvec", [128, KC, 2])
    modw_d = K.din("mod_w1", [D, 6144])
    modb_d = K.din("mod_bT1", [128, 48])
    ng_d = K.din("norm_gT", [128, 4])
    wout_d = K.din("cd_w_out", [D, D])
    lng_d = K.din("ln_gT10", [128, KC])
    lnb_d = K.din("ln_bT10", [128, KC])

    psA = [K.ps(f"psA{i}", [128, 512]) for i in range(2)]
    psmean = K.ps("psmean", [128, 512]); pssq = K.ps("pssq", [128, 512]); psm = K.ps("psm", [128, 512])
    psX = K.ps("psX", [128, 512])
    ident = K.sb("ident", [128, 128])
    P.dma('sp', ident[:], ident_d, writes=['ident'])
    hidx = K.sb("hidx", [128, ntiles * 2 * 8], I32)
    P.dma('sp', hidx[:], hidx_d, writes=['hidx'])
    ones128 = K.sb("ones128", [128, 128]); ones1024 = K.sb("ones1024", [128, 128])
    P.op('dve', lambda e: e.memset(ones128[:], 1.0 / 128), writes=['ones128'])
    P.op('dve', lambda e: e.memset(ones1024[:], 1.0 / 1024), writes=['ones1024'])
    ng = K.sb("ng", [128, 4]); lng = K.sb("lng", [128, KC]); lnb = K.sb("lnb", [128, KC])
    for t_, d_, n_ in ((ng, ng_d, 'ng'), (lng, lng_d, 'lng'), (lnb, lnb_d, 'lnb')):
        P.dma('sp', t_[:], d_, writes=[n_])
    g1a = K.sb("g1a", [128, KC, 2])
    wout_b = K.sb("wout_b", [128, KC, D], BF16)
    tmp = ExitStack()
    modT = mod_setup(K, cvec_d, modw_d, modb_d, 16, 24, psm, tmp=tmp)
    stage = [tmp.enter_context(nc.sbuf_tensor(f"s_{K.pfx}stage{i}", [128, D], F32)) for i in range(2)]
    P.op('dve', lambda e: e.tensor_scalar(out=g1a[:], in0=modT[:, 0:8, :], scalar1=1.0 / ALPHA, scalar2=None, op0=ALU.mult), reads=['modT'], writes=['g1a'])
    for k in range(KC):
        load_cast(K, wout_b[:, k, :], wout_d[k * 128:(k + 1) * 128, :], stage[k % 2][:], f"stage{k % 2}", "wout_b", eng='dve' if k % 2 == 0 else 'act')
    P.barrier()
    tmp.close()

    xt = [K.sb(f"xt{i}", [128, KC, NT]) for i in range(2)]
    htk = [K.sb(f"htk{i}", [128, 4, 128]) for i in range(2)]
    HS = K.sb("HS", [128, 4, NT])
    om = [K.sb(f"om{i}", [128, 4, NT]) for i in range(2)]
    ya = [K.sb(f"ya{i}", [128, 4, NT]) for i in range(2)]
    hs = K.sb("hs", [128, 1, NT]); sgm = K.sb("sgm", [128, NT])
    sqt = [K.sb(f"sqt{i}", [128, NT]) for i in range(2)]
    mean_sb = K.sb("mean_sb", [128, NT]); rstd = K.sb("rstd", [128, NT])
    ycat = K.sb("ycat", [128, KC, NT], BF16)
    vt = K.sb("vt", [128, KC, NT])
    xo = [K.sb(f"xo{i}", [128, KC, NT]) for i in range(2)]

    def ln_stats(src, nchunk, ones, onm, srcname, eps):
        for c in range(nchunk):
            P.op('pe', lambda e, c=c: e.matmul(psmean[:, 0:NT], lhsT=ones[:], rhs=src[:, c, :], start=(c == 0), stop=(c == nchunk - 1)),
                 reads=[onm, srcname], writes=['psmean'], inc=(c == nchunk - 1))
        for c in range(nchunk):
            s = sqt[c % 2]; sn = f"sqt{c % 2}"
            P.op('act', lambda e, c=c, s=s: e.activation(out=s[:], in_=src[:, c, :], func=AF.Square), reads=[srcname], writes=[sn])
            P.op('pe', lambda e, c=c, s=s: e.matmul(pssq[:, 0:NT], lhsT=ones[:], rhs=s[:], start=(c == 0), stop=(c == nchunk - 1)),
                 reads=[onm, sn], writes=['pssq'])
        P.op('act', lambda e: e.activation(out=mean_sb[:], in_=psmean[:, 0:NT], func=AF.Copy), reads=['psmean'], writes=['mean_sb'])
        P.op('dve', lambda e: e.tensor_tensor(out=rstd[:], in0=mean_sb[:], in1=mean_sb[:], op=ALU.mult), reads=['mean_sb'], writes=['rstd'])
        P.op('dve', lambda e: e.tensor_tensor(out=rstd[:], in0=pssq[:, 0:NT], in1=rstd[:], op=ALU.subtract), reads=['pssq', 'rstd'], writes=['rstd'])
        P.op('dve', lambda e: e.tensor_scalar(out=rstd[:], in0=rstd[:], scalar1=eps, scalar2=None, op0=ALU.add), reads=['rstd'], writes=['rstd'])
        P.op('act', lambda e: e.activation(out=rstd[:], in_=rstd[:], func=AF.Sqrt), reads=['rstd'], writes=['rstd'])
        P.op('dve', lambda e: e.reciprocal(out=rstd[:], in_=rstd[:]), reads=['rstd'], writes=['rstd'])

    for t in range(ntiles):
        b = t % 2
        X, OM, YA = xt[b], om[b], ya[b]
        xn, omn, yan = f"xt{b}", f"om{b}", f"ya{b}"
        P.dma('sp', X[:], xt_d[t].rearrange("(c p) n -> p c n", p=128), reads=['xl_o'], writes=[xn])
        P.dma('sp', OM[:], gb_['om'][:, t * NT:(t + 1) * NT].rearrange("(c p) n -> p c n", p=128), reads=['om_o'], writes=[omn])
        for c in range(4):
            for par in range(2):
                hd_ = 2 * c + par
                P.dma('sp', YA[par * 64:(par + 1) * 64, c, :], gb_['ya'][hd_ * 64:(hd_ + 1) * 64, t * NT:(t + 1) * NT], reads=['ya_l'], writes=[yan])
        for sub in range(2):
            s_ = 2 * t + sub
            for dr in range(2):
                for hd in range(4):
                    ic = s_ * 8 + hd * 2 + dr
                    P.dma('pool', None, None, reads=['h_all', 'hidx'], writes=[f"htk{dr}"],
                          fn=lambda e, dr=dr, hd=hd, ic=ic: e.indirect_dma_start(out=htk[dr][:, hd, :], out_offset=None, in_=gb_['h_all'],
                                                                                 in_offset=bass.IndirectOffsetOnAxis(ap=hidx[:, ic:ic + 1], axis=0)))
            P.op('dve', lambda e: e.tensor_tensor(out=htk[0][:], in0=htk[0][:], in1=htk[1][:], op=ALU.add), reads=['htk0', 'htk1'], writes=['htk0'])
            for hd in range(4):
                P.op('pe', lambda e, hd=hd: e.transpose(out=psX[:, hd * 128:(hd + 1) * 128], in_=htk[0][:, hd, :], identity=ident[:]), reads=['htk0', 'ident'], writes=['psX'])
            P.op('act', lambda e, sub=sub: e.activation(out=HS[:, :, sub * 128:(sub + 1) * 128], in_=psX[:, :].rearrange("p (c n) -> p c n", c=4), func=AF.Copy),
                 reads=['psX'], writes=['HS'])
        for hd in range(4):
            P.op('dve', lambda e, hd=hd: e.tensor_copy(out=hs[:, 0, :], in_=HS[:, hd, :]), reads=['HS'], writes=['hs'])
            ln_stats(hs, 1, ones128, 'ones128', 'hs', LN_EPS)
            P.op('dve', lambda e: e.tensor_tensor(out=hs[:, 0, :], in0=hs[:, 0, :], in1=mean_sb[:], op=ALU.subtract), reads=['hs', 'mean_sb'], writes=['hs'])
            P.op('dve', lambda e: e.tensor_tensor(out=hs[:, 0, :], in0=hs[:, 0, :], in1=rstd[:], op=ALU.mult), reads=['hs', 'rstd'], writes=['hs'])
            P.op('act', lambda e, hd=hd: e.activation(out=sgm[:], in_=OM[:, hd, :], func=AF.Sigmoid), reads=[omn], writes=['sgm'])
            P.op('dve', lambda e, hd=hd: e.scalar_tensor_tensor(out=ycat[:, hd, :], in0=hs[:, 0, :], scalar=ng[:, hd:hd + 1], in1=sgm[:], op0=ALU.mult, op1=ALU.mult),
                 reads=['hs', 'ng', 'sgm'], writes=['ycat'])
        for c in range(4):
            P.op('act', lambda e, c=c: e.activation(out=ycat[:, 4 + c, :], in_=YA[:, c, :], func=AF.Copy), reads=[yan], writes=['ycat'])
        for oc in range(KC):
            ps = psA[oc % 2]; pn = f"psA{oc % 2}"
            for k in range(KC):
                P.op('pe', lambda e, k=k, oc=oc, ps=ps: e.matmul(ps[:, 0:NT], lhsT=wout_b[:, k, oc * 128:(oc + 1) * 128], rhs=ycat[:, k, :],
                                                                 start=(k == 0), stop=(k == KC - 1)),
                     reads=['wout_b', 'ycat'], writes=[pn], inc=(k == KC - 1))
            P.op('dve', lambda e, oc=oc, ps=ps: e.scalar_tensor_tensor(out=vt[:, oc, :], in0=ps[:, 0:NT], scalar=g1a[:, oc, 0:1],
                                                                      in1=X[:, oc, :], op0=ALU.mult, op1=ALU.add),
                 reads=[pn, 'g1a', xn], writes=['vt'])
        ln_stats(vt, KC, ones1024, 'ones1024', 'vt', LN_EPS / (ALPHA * ALPHA))
        XO = xo[b]; xon = f"xo{b}"
        for oc in range(KC):
            P.op('dve', lambda e, oc=oc: e.tensor_tensor(out=vt[:, oc, :], in0=vt[:, oc, :], in1=mean_sb[:], op=ALU.subtract), reads=['vt', 'mean_sb'], writes=['vt'])
            P.op('dve', lambda e, oc=oc: e.tensor_tensor(out=vt[:, oc, :], in0=vt[:, oc, :], in1=rstd[:], op=ALU.mult), reads=['vt', 'rstd'], writes=['vt'])
            P.op('act', lambda e, oc=oc: e.activation(out=XO[:, oc, :], in_=vt[:, oc, :], func=AF.Identity, scale=lng[:, oc:oc + 1], bias=lnb[:, oc:oc + 1]),
                 reads=['vt', 'lng', 'lnb'], writes=[xon])
        P.dma('sp', out_d[t].rearrange("(c p) n -> p c n", p=128), XO[:], reads=[xon], writes=['x1T_scr'])
    K.end_stage()


def seg_tiles(seq, start, ntile):
    L = seq.shape[0]
    xt = np.zeros((ntile, D, WT), np.float32)
    cm = np.zeros((ntile, 128, WT), np.float32)
    rc = np.ones((ntile, 128, 4, NT), np.float32)
    for t in range(ntile):
        s0 = start + t * NT - HALO
        lo, hi = max(s0, 0), min(s0 + WT, L)
        if hi > lo:
            xt[t, :, lo - s0:hi - s0] = seq[lo:hi].T
            cm[t, :, lo - s0:hi - s0] = 1.0
        tok = start + t * NT + np.arange(NT)
        for g, w in enumerate(POOL_WINDOWS):
            cnt = np.minimum(tok + w // 2, L) - np.maximum(tok - w // 2, 0)
            cnt = np.where(tok < L, cnt, 1).astype(np.float32)
            rc[t, :, g, :] = (1.0 / cnt)[None, :]
    return xt, cm, rc

def fm(v, nchunk):
    return np.ascontiguousarray(np.asarray(v, np.float32).reshape(nchunk, 128).T)

def cvec_of(cb, c_ctx):
    return np.ascontiguousarray(np.stack([fm(cb, KC), fm(c_ctx, KC)], axis=-1))

def plain_tiles(seq, start, ntile):
    L, Dm = seq.shape
    out = np.zeros((ntile, Dm, NT), seq.dtype)
    for t in range(ntile):
        lo = start + t * NT
        hi = min(lo + NT, L)
        if hi > lo:
            out[t, :, :hi - lo] = seq[lo:hi].T
    return out

def rope_tiles(start, ntile, latent=True):
    cs = np.ones((ntile, 64, NT), np.float32); sn = np.zeros((ntile, 64, NT), np.float32)
    if latent:
        f = (10000.0 ** (-np.arange(16, dtype=np.float32) / 16)).astype(np.float32)
        for t in range(ntile):
            tok = start + t * NT + np.arange(NT)
            row = (tok // 64).astype(np.float32); col = (tok % 64).astype(np.float32)
            ar = (row[None, :] * f[:, None]).astype(np.float32); ac = (col[None, :] * f[:, None]).astype(np.float32)
            cs[t] = np.concatenate([np.cos(ar), np.cos(ar), np.cos(ac), np.cos(ac)], 0)
            sn[t] = np.concatenate([np.sin(ar), np.sin(ar), np.sin(ac), np.sin(ac)], 0)
    return cs, sn

def rope_perm():
    Pm = np.zeros((64, 64), np.float32)
    for base in (0, 32):
        for d in range(16):
            Pm[base + d + 16, base + d] = -1.0
            Pm[base + d, base + d + 16] = 1.0
    return Pm

CD_ORDER = None
def cd_perm_cols(w):
    return np.ascontiguousarray(np.concatenate([w[:, 0:2048], w[:, 2064:2832], w[:, 2048:2064]], axis=1))

def scan_consts():
    i = np.arange(128)
    ident = np.eye(128, dtype=np.float32)
    triL = (i[:, None] <= i[None, :]).astype(np.float32)
    triU = (i[:, None] >= i[None, :]).astype(np.float32)
    mnegL = np.where(i[None, :] <= i[:, None], 0.0, -1.0e30).astype(np.float32)
    mnegU = np.where(i[None, :] >= i[:, None], 0.0, -1.0e30).astype(np.float32)
    return np.stack([ident, triL, triU, mnegL, mnegU])

def attn_sel():
    s = np.zeros((65, 64), np.float32); s[64, :] = 1.0
    return s


_PROGS = {}
PAIRS = [[0, 1], [2, 3], [4, 5], [6, 7]]


def _prog(key, fn):
    if key not in _PROGS:
        _PROGS[key] = fn()
    return _PROGS[key]


def _allgather(K, src, dst, rn_src, rn_dst):
    P = K.P
    P._deps('pool', [rn_src], [rn_dst], True)
    ins = K.nc.gpsimd.collective_compute("AllGather", ALU.bypass, replica_groups=PAIRS, ins=[src.opt()], outs=[dst.opt()])
    P.cnt['pool'] += 1
    ins.then_inc(P.sem['pool'], 1)
    P._record(('pool', P.cnt['pool']), [rn_src], [rn_dst])


def _build_fused(ntl):
    K = Ctx()
    Th = ntl * NT
    T = 2 * Th
    TOK = (ntl + 1) * NT
    nsub = 2 * ntl + 1
    x1T_scr0 = K.scr("x1T_scr0", [ntl + 1, D, NT])
    x2_scr0 = K.scr("x2_scr0", [nsub, 128, D])
    x1T_scr1 = K.scr("x1T_scr1", [ntl, D, NT])
    gb = dict(qm=K.scr("g_qm", [512, TOK], BF16), km=K.scr("g_km", [512, TOK], BF16), vm=K.scr("g_vm", [512, TOK], BF16),
              ka=K.scr("g_ka", [128, TOK], BF16), va=K.scr("g_va", [128, TOK], BF16), g=K.scr("g_g", [16, TOK], F32),
              qa=K.scr("l_qa", [512, TOK], BF16), om=K.scr("l_om", [512, TOK], F32), xl0T=K.scr("l_xl0T", [ntl, D, NT]),
              ya=K.scr("l_ya", [512, Th]), h=K.scr("g_h", [2 * 2 * T, 128]),
              qm_all=K.scr("a_qm", [1024, TOK], BF16), km_all=K.scr("a_km", [1024, TOK], BF16), vm_all=K.scr("a_vm", [1024, TOK], BF16),
              ka_all=K.scr("a_ka", [256, TOK], BF16), va_all=K.scr("a_va", [256, TOK], BF16), g_all=K.scr("a_g", [32, TOK], F32),
              h_all=K.scr("a_h", [2 * 2 * 2 * T, 128]))
    out_d = K.dout("x2", [2 * ntl, 128, D])
    uvb = [K.scr(f"uvb{L}", [16384, 2 * D], BF16) for L in range(2)]
    stage_conv(K, 0, uvb[0])
    stage_conv(K, 1, uvb[1])
    stage_mixa(K, ntl + 1, [0] * ntl + [1], x1T_scr0)
    stage_peer(K, nsub, [0] * (2 * ntl) + [1], 0, x1T_scr0, x2_scr0, uvb[0])
    stage_l1a(K, ntl + 1, [0] * ntl + [1], x2_scr0, nsub, gb)
    for nm in ("qm", "km", "vm"):
        for hd in range(4):
            _allgather(K, gb[nm][hd * 128:(hd + 1) * 128, :], gb[nm + "_all"][hd * 256:(hd + 1) * 256, :], nm + "_src", nm + "_all")
    for nm in ("ka", "va", "g"):
        _allgather(K, gb[nm], gb[nm + "_all"], nm + "_src", nm + "_all")
    K.P.barrier()
    stage_scan(K, 2, Th, TOK, gb)
    stage_attn(K, Th, TOK, gb)
    hrows = 2 * 2 * T
    PR = min(4096, hrows)
    for k in range(hrows // PR):
        _allgather(K, gb["h"][k * PR:(k + 1) * PR, :], gb["h_all"][k * 2 * PR:(k + 1) * 2 * PR, :], "h_src", "h_all")
    K.P.barrier()
    stage_l1c(K, ntl, x1T_scr1, gb, Th)
    stage_peer(K, 2 * ntl, [0] * (2 * ntl), 1, x1T_scr1, out_d, uvb[1])
    K.close()
    return K.nc


def kernel(x, c, ctx, c_ctx, mod_w, mod_b, ln_g, ln_b, ab_w_in, pool_w, pool_ls, conv_w, conv_b, conv_ln_g, conv_ln_b, ab_w_out,
           cd_w_in, mlstm_gate_b, mlstm_norm_g, q_norm_g, k_norm_g, cd_w_out, peer_w_q, peer_keys, peer_u, peer_v):
    f32 = lambda a: np.ascontiguousarray(np.asarray(a, np.float32))
    x, c, ctx, c_ctx, mod_w, mod_b, ln_g, ln_b = map(f32, (x, c, ctx, c_ctx, mod_w, mod_b, ln_g, ln_b))
    peer_w_q, peer_keys, peer_u, peer_v = map(f32, (peer_w_q, peer_keys, peer_u, peer_v))
    cd_w_in, cd_w_out, ab_w_in, ab_w_out = map(f32, (cd_w_in, cd_w_out, ab_w_in, ab_w_out))
    B, T, _ = x.shape
    Lc = ctx.shape[1]
    NH = 2
    ncores = B * NH
    assert ncores == 8 and Lc == 256
    Th = T // NH
    Lh = Lc // NH
    ntl = Th // NT
    cores = [(i // NH, i % NH) for i in range(ncores)]
    ident = np.eye(128, dtype=np.float32)
    iota16 = np.tile(np.arange(16, dtype=np.float32), (128, 1))
    nc = _prog(("fused", ntl), lambda: _build_fused(ntl))
    uv_tabs = [np.ascontiguousarray(np.concatenate([peer_u[L], peer_v[L]], axis=1)) for L in range(2)]
    w_in_p = cd_perm_cols(cd_w_in[0])
    consts = scan_consts()
    p = np.arange(128)
    maps = []
    for (b, hf_) in cores:
        xt, cm, rc = seg_tiles(x[b], hf_ * Th, ntl)
        xt2, cm2, rc2 = seg_tiles(ctx[b], hf_ * Lh, 1)
        cs, sn = rope_tiles(hf_ * Th, ntl)
        cs2, sn2 = rope_tiles(0, 1, latent=False)
        sidx = np.zeros((128, 4), np.int32)
        gidx = np.zeros((128, 4), np.int32)
        for j in range(2):
            h = 2 * hf_ + j
            for m_ in range(2):
                sidx[:, j * 2 + m_] = (h * 2 + m_) * 128 + p
                gidx[:4, j * 2 + m_] = m_ * 16 + np.arange(4) * 4 + h
        hidx = np.zeros((128, 2 * ntl * 8), np.int32)
        for s_ in range(2 * ntl):
            for hd in range(4):
                for dr in range(2):
                    m_, u_ = hd // 2, hd % 2
                    grow = (u_ * 2 + dr) * T + hf_ * Th + s_ * 128 + p
                    PR = min(4096, 4 * T)
                    hidx[:, s_ * 8 + hd * 2 + dr] = ((grow // PR) * 2 + m_) * PR + (grow % PR)
        m = dict(
            xtiles=np.concatenate([xt, xt2]), cmask=np.concatenate([cm, cm2]), rcorr=np.concatenate([rc, rc2]),
            cvec=cvec_of(c[b], c_ctx), ab_w_in=ab_w_in[0], pool_w=f32(pool_w[0]), pool_lsT=fm(pool_ls[0], 4),
            conv_wT=np.ascontiguousarray(f32(conv_w[0]).T.reshape(4, 128, 31).transpose(1, 0, 2)),
            conv_bT=fm(conv_b[0], 4), cln_gT=fm(conv_ln_g[0], 4), cln_bT=fm(conv_ln_b[0], 4),
            ab_w_out=ab_w_out[0], ln_gT00=fm(ln_g[0, 0], KC), ln_bT00=fm(ln_b[0, 0], KC),
            cosT=np.concatenate([cs, cs2]), sinT=np.concatenate([sn, sn2]), cd_w_in=w_in_p,
            gate_bT=f32(mlstm_gate_b[0]).reshape(16, 1).copy(), qn_gT=f32(q_norm_g[0]).reshape(64, 1).copy(),
            kn_gT=f32(k_norm_g[0]).reshape(64, 1).copy(), ropeP=rope_perm(),
            scan_idx=sidx, scan_gidx=gidx, consts=consts, qn_g=f32(q_norm_g[0])[None].copy(), kn_g=f32(k_norm_g[0])[None].copy(), sel=attn_sel(),
            h_idx=hidx, norm_gT=fm(mlstm_norm_g[0], 4), cd_w_out=cd_w_out[0], ln_gT10=fm(ln_g[1, 0], KC), ln_bT10=fm(ln_b[1, 0], KC),
            ident=ident, iota16=iota16)
        for L in range(2):
            m.update({f"mod_w{L}": mod_w[L], f"mod_bT{L}": fm(mod_b[L], 48), f"w_q{L}": peer_w_q[L],
                      f"keysT{L}": np.ascontiguousarray(peer_keys[L].transpose(2, 0, 1)), f"peer_uv{L}": uv_tabs[L],
                      f"ln_g{L}1": ln_g[L, 1][None].copy(), f"ln_b{L}1": ln_b[L, 1][None].copy()})
        maps.append(m)
    res = run_bass_kernel_spmd(nc, maps, core_ids=list(range(ncores))).results
    out = np.zeros((B, T, D), np.float32)
    for i, (b, hf_) in enumerate(cores):
        out[b, hf_ * Th:(hf_ + 1) * Th] = res[i]["x2"].reshape(Th, D)
    return out
```

```python
import numpy as np
import ml_dtypes
from contextlib import ExitStack
import concourse.bass as bass
import concourse.mybir as mybir
from concourse.bass_utils import run_bass_kernel_spmd

F32 = mybir.dt.float32
BF16 = mybir.dt.bfloat16
I32 = mybir.dt.int32
U32 = mybir.dt.uint32
AF = mybir.ActivationFunctionType
ALU = mybir.AluOpType
AX = mybir.AxisListType

ENG = {'pe': 'tensor', 'act': 'scalar', 'dve': 'vector', 'pool': 'gpsimd', 'sp': 'sync'}


class Prog:
    def __init__(self, nc, ndma=20):
        self.nc = nc
        self.stack = ExitStack()
        self.sem = {e: self.stack.enter_context(nc.semaphore(f"s_{e}")) for e in ENG}
        self.cnt = {e: 0 for e in ENG}
        self.ndma = ndma
        self.dsem = [self.stack.enter_context(nc.semaphore(f"d{i}")) for i in range(ndma)]
        self.dcnt = [0] * ndma
        self.dnext = 0
        self.seen = {e: {} for e in ENG}
        self.lastw = {}
        self.readers = {}
        self.pending = {e: [] for e in ENG}
        self.nops = 0

    def eng(self, e):
        return getattr(self.nc, ENG[e])

    def _wait(self, e, key, val):
        if val <= 0:
            return
        if self.seen[e].get(key, 0) >= val:
            return
        self.seen[e][key] = val
        sem = self.sem[key] if isinstance(key, str) else self.dsem[key]
        self.eng(e).wait_ge(sem, val)

    def _deps(self, e, reads, writes, sync_same):
        toks = {}
        for r in reads:
            t = self.lastw.get(r)
            if t is not None:
                toks[t[0]] = max(toks.get(t[0], 0), t[1])
        for w in writes:
            t = self.lastw.get(w)
            if t is not None:
                toks[t[0]] = max(toks.get(t[0], 0), t[1])
            for k, v in self.readers.get(w, {}).items():
                toks[k] = max(toks.get(k, 0), v)
        for k, v in toks.items():
            if k == e and not sync_same:
                continue
            self._wait(e, k, v)

    def _record(self, tok, reads, writes):
        for r in reads:
            d = self.readers.setdefault(r, {})
            d[tok[0]] = max(d.get(tok[0], 0), tok[1])
        for w in writes:
            self.lastw[w] = tok
            self.readers[w] = {}

    def replay(self, recs, n):
        for _ in range(min(n, len(recs))):
            kind, args, kw = recs.pop(0)
            (self.op if kind == 'op' else self.dma)(*args, **kw)

    def op(self, e, fn, reads=(), writes=(), sync_same=None, inc=True):
        if getattr(self, 'rec', None) is not None:
            self.rec.append(('op', (e, fn), dict(reads=reads, writes=writes, sync_same=sync_same, inc=inc)))
            return None
        if sync_same is None:
            sync_same = (e != 'pe')
        self._deps(e, reads, writes, sync_same)
        ins = fn(self.eng(e))
        self.nops += 1
        if not inc:
            self.pending[e].append((tuple(reads), tuple(writes)))
            return ins
        self.cnt[e] += 1
        ins.then_inc(self.sem[e], 1)
        tok = (e, self.cnt[e])
        for (r, w) in self.pending[e]:
            self._record(tok, r, w)
        self.pending[e] = []
        self._record(tok, reads, writes)
        return ins

    def dma(self, e, out, in_, reads=(), writes=(), fn=None, **kw):
        if getattr(self, 'rec', None) is not None:
            self.rec.append(('dma', (e, out, in_), dict(reads=reads, writes=writes, fn=fn, **kw)))
            return None
        s = self.dnext
        self.dnext = (s + 1) % self.ndma
        self._wait(e, s, self.dcnt[s] * 16)
        self._deps(e, reads, writes, True)
        if fn is None:
            ins = self.eng(e).dma_start(out=out, in_=in_, **kw)
        else:
            ins = fn(self.eng(e))
        self.nops += 1
        self.dcnt[s] += 1
        ins.then_inc(self.dsem[s], 16)
        tok = (s, self.dcnt[s] * 16)
        self._record(tok, reads, writes)
        return ins

    def barrier(self):
        for e in ENG:
            for s_ in range(self.ndma):
                self._wait(e, s_, self.dcnt[s_] * 16)
            for k in ENG:
                if k != e:
                    self._wait(e, k, self.cnt[k])

    def finish(self, e='sp'):
        for s in range(self.ndma):
            self._wait(e, s, self.dcnt[s] * 16)
        for k in ENG:
            if k != e:
                self._wait(e, k, self.cnt[k])
        self.stack.close()


D = 1024
KC = 8
NT = 256
HALO = 16
WT = NT + 2 * HALO
ALPHA = 4.0 ** 0.25
LN_EPS = 1e-5
POOL_WINDOWS = (2, 4, 8, 16)


class Ctx:
    def __init__(self, name="k"):
        self.nc = bass.Bass("TRN2", target_bir_lowering=False)
        self.P = Prog(self.nc)
        self.es = ExitStack()
        self.pfx = ""
        self.drams = {}

    def begin_stage(self, pfx):
        self.es = ExitStack()
        self.pfx = pfx

    def end_stage(self):
        self.P.barrier()
        self.es.close()

    def sb(self, name, shape, dt=F32):
        return self.es.enter_context(self.nc.sbuf_tensor("s_" + self.pfx + name, list(shape), dt))

    def ps(self, name, shape, dt=F32):
        return self.es.enter_context(self.nc.psum_tensor("p_" + self.pfx + name, list(shape), dt))

    def _dram(self, name, shape, dt, kind):
        if name not in self.drams:
            self.drams[name] = self.nc.dram_tensor(name, list(shape), dt, kind=kind).ap()
        return self.drams[name]

    def din(self, name, shape, dt=F32):
        return self._dram(name, shape, dt, "ExternalInput")

    def dout(self, name, shape, dt=F32):
        return self._dram(name, shape, dt, "ExternalOutput")

    def scr(self, name, shape, dt=F32):
        return self._dram(name, shape, dt, "Internal")

    def close(self):
        self.P.finish('sp')


def fap(t, off, dims):
    fs = 1
    for s in t.shape[1:]:
        fs *= s
    return bass.AP(t, off, [[fs, t.shape[0]]] + [list(d) for d in dims])


def load_cast(K, dst_bf, src_ap, stage, rname, wname, eng='dve', q='sp'):
    P = K.P
    P.dma(q, stage, src_ap, writes=[rname])
    if eng == 'dve':
        P.op('dve', lambda e: e.tensor_copy(out=dst_bf, in_=stage), reads=[rname], writes=[wname])
    else:
        P.op('act', lambda e: e.activation(out=dst_bf, in_=stage, func=AF.Copy), reads=[rname], writes=[wname])


def mod_setup(K, cvec_d, modw_d, modb_d, j0, j1, psm, tmp=None):
    nc, P = K.nc, K.P
    nj = j1 - j0
    cv = K.sb("cv", [128, KC, 2])
    sc = K.sb("sc", [128, KC, 2])
    mb = K.sb("mb", [128, 48])
    modT = K.sb("modT", [128, nj, 2])
    tmp = tmp if tmp is not None else K.es
    mwblk = [tmp.enter_context(K.nc.sbuf_tensor(f"s_{K.pfx}mwblk{i}", [128, KC, 512], F32)) for i in range(2)]
    P.dma('sp', cv[:], cvec_d, writes=['cv'])
    P.dma('sp', mb[:], modb_d, writes=['mb'])
    P.op('act', lambda e: e.activation(out=sc[:], in_=cv[:], func=AF.Silu), reads=['cv'], writes=['sc'])
    assert j0 % 4 == 0 and j1 % 4 == 0
    for bi, blk in enumerate(range(j0 // 4, j1 // 4)):
        buf = mwblk[bi % 2]
        bn = f"mwblk{bi % 2}"
        P.dma('sp', buf[:], modw_d[:, blk * 512:(blk + 1) * 512].rearrange("(k p) f -> p k f", p=128), writes=[bn])
        for jj in range(4):
            j = blk * 4 + jj - j0
            for k in range(KC):
                P.op('pe', lambda e, k=k, jj=jj, j=j: e.matmul(psm[:, 2 * j:2 * j + 2], lhsT=buf[:, k, jj * 128:(jj + 1) * 128],
                                                               rhs=sc[:, k, :], start=(k == 0), stop=(k == KC - 1)),
                     reads=[bn, 'sc'], writes=['psm'], inc=(k == KC - 1))
    P.op('dve', lambda e: e.tensor_tensor(out=modT[:], in0=psm[:, 0:2 * nj].rearrange("p (j c) -> p j c", c=2),
                                          in1=fap(mb, j0, [[1, nj], [0, 2]]), op=ALU.add),
         reads=['psm', 'mb'], writes=['modT'])
    return modT


def stage_mixa(K, ntiles, mcols, out_d):
    K.begin_stage("mx_")
    nc, P = K.nc, K.P
    xt_d = K.din("xtiles", [ntiles, D, WT])
    cm_d = K.din("cmask", [ntiles, 128, WT])
    rc_d = K.din("rcorr", [ntiles, 128, 4, NT])
    cvec_d = K.din("cvec", [128, KC, 2])
    modw_d = K.din("mod_w0", [D, 6144])
    modb_d = K.din("mod_bT0", [128, 48])
    win_d = K.din("ab_w_in", [D, 1536])
    poolw_d = K.din("pool_w", [4, 128, 128])
    poolls_d = K.din("pool_lsT", [128, 4])
    convw_d = K.din("conv_wT", [128, 4, 31])
    convb_d = K.din("conv_bT", [128, 4])
    clng_d = K.din("cln_gT", [128, 4])
    clnb_d = K.din("cln_bT", [128, 4])
    wout_d = K.din("ab_w_out", [D, D])
    lng_d = K.din("ln_gT00", [128, KC])
    lnb_d = K.din("ln_bT00", [128, KC])

    psA = [K.ps(f"psA{i}", [128, 512]) for i in range(2)]
    psp = K.ps("psp", [128, 512])
    psmean = K.ps("psmean", [128, 512])
    pssq = K.ps("pssq", [128, 512])
    psm = K.ps("psm", [128, 512])

    ones512 = K.sb("ones512", [128, 128])
    ones1024 = K.sb("ones1024", [128, 128])
    P.op('dve', lambda e: e.memset(ones512[:], 1.0 / 512), writes=['ones512'])
    P.op('dve', lambda e: e.memset(ones1024[:], 1.0 / 1024), writes=['ones1024'])
    small = {}
    for nm, d, shp in [("pls", poolls_d, [128, 4]), ("cw", convw_d, [128, 4, 31]), ("cb", convb_d, [128, 4]),
                       ("clg", clng_d, [128, 4]), ("clb", clnb_d, [128, 4]), ("lng", lng_d, [128, KC]),
                       ("lnb", lnb_d, [128, KC])]:
        t = K.sb(nm, shp)
        P.dma('sp', t[:], d, writes=[nm])
        small[nm] = t
    pls, cw, cb, clg, clb, lng, lnb = [small[n] for n in ("pls", "cw", "cb", "clg", "clb", "lng", "lnb")]

    modT = mod_setup(K, cvec_d, modw_d, modb_d, 0, 24, psm)
    sc1p = K.sb("sc1p", [128, KC, 2])
    g1a = K.sb("g1a", [128, KC, 2])
    P.op('dve', lambda e: e.tensor_scalar(out=sc1p[:], in0=modT[:, 8:16, :], scalar1=1.0, scalar2=None, op0=ALU.add),
         reads=['modT'], writes=['sc1p'])
    P.op('dve', lambda e: e.tensor_scalar(out=g1a[:], in0=modT[:, 16:24, :], scalar1=1.0 / ALPHA, scalar2=None, op0=ALU.mult),
         reads=['modT'], writes=['g1a'])

    stage = [K.sb(f"stage{i}", [128, 1536]) for i in range(2)]
    win_b = K.sb("win_b", [128, KC, 1536], BF16)
    wout_b = K.sb("wout_b", [128, KC, D], BF16)
    poolw_b = K.sb("poolw_b", [128, 4, 128], BF16)
    si = 0
    for k in range(KC):
        load_cast(K, win_b[:, k, :], win_d[k * 128:(k + 1) * 128, :], stage[si % 2][:, :], f"stage{si % 2}", "win_b",
                  eng='dve' if k % 2 == 0 else 'act')
        si += 1
    for k in range(KC):
        load_cast(K, wout_b[:, k, :], wout_d[k * 128:(k + 1) * 128, :], stage[si % 2][:, 0:D], f"stage{si % 2}", "wout_b",
                  eng='dve' if k % 2 == 0 else 'act')
        si += 1
    for g in range(4):
        load_cast(K, poolw_b[:, g, :], poolw_d[g], stage[si % 2][:, 0:128], f"stage{si % 2}", "poolw_b")
        si += 1

    xt = [K.sb(f"xt{i}", [128, KC, WT]) for i in range(2)]
    cm = [K.sb(f"cm{i}", [128, WT]) for i in range(2)]
    rc = [K.sb(f"rc{i}", [128, 4, NT]) for i in range(2)]
    hT = K.sb("hT", [128, KC, WT], BF16)
    zp = K.sb("zp", [128, 4, WT])
    ua = K.sb("ua", [128, 4, WT])
    sg = K.sb("sg", [128, WT])
    u = K.sb("u", [128, 4, WT])
    pa = K.sb("pa", [128, WT])
    pb = K.sb("pb", [128, WT])
    dif = K.sb("dif", [128, NT], BF16)
    yc = K.sb("yc", [128, 4, NT])
    sqt = [K.sb(f"sqt{i}", [128, NT]) for i in range(2)]
    mean_sb = K.sb("mean_sb", [128, NT])
    rstd = K.sb("rstd", [128, NT])
    yn = K.sb("yn", [128, NT])
    ycat = K.sb("ycat", [128, KC, NT], BF16)
    vt = K.sb("vt", [128, KC, NT])
    xo = [K.sb(f"xo{i}", [128, KC, NT]) for i in range(2)]

    def ln_stats(src, nchunk, ones, onm, srcname, eps):
        for c in range(nchunk):
            P.op('pe', lambda e, c=c: e.matmul(psmean[:, 0:NT], lhsT=ones[:], rhs=src[:, c, :], start=(c == 0), stop=(c == nchunk - 1)),
                 reads=[onm, srcname], writes=['psmean'], inc=(c == nchunk - 1))
        for c in range(nchunk):
            s = sqt[c % 2]
            sn = f"sqt{c % 2}"
            P.op('act', lambda e, c=c, s=s: e.activation(out=s[:], in_=src[:, c, :], func=AF.Square), reads=[srcname], writes=[sn])
            P.op('pe', lambda e, c=c, s=s: e.matmul(pssq[:, 0:NT], lhsT=ones[:], rhs=s[:], start=(c == 0), stop=(c == nchunk - 1)),
                 reads=[onm, sn], writes=['pssq'])
        P.op('act', lambda e: e.activation(out=mean_sb[:], in_=psmean[:, 0:NT], func=AF.Copy), reads=['psmean'], writes=['mean_sb'])
        P.op('dve', lambda e: e.tensor_tensor(out=rstd[:], in0=mean_sb[:], in1=mean_sb[:], op=ALU.mult), reads=['mean_sb'], writes=['rstd'])
        P.op('dve', lambda e: e.tensor_tensor(out=rstd[:], in0=pssq[:, 0:NT], in1=rstd[:], op=ALU.subtract), reads=['pssq', 'rstd'], writes=['rstd'])
        P.op('dve', lambda e: e.tensor_scalar(out=rstd[:], in0=rstd[:], scalar1=eps, scalar2=None, op0=ALU.add),
             reads=['rstd'], writes=['rstd'])
        P.op('act', lambda e: e.activation(out=rstd[:], in_=rstd[:], func=AF.Sqrt), reads=['rstd'], writes=['rstd'])
        P.op('dve', lambda e: e.reciprocal(out=rstd[:], in_=rstd[:]), reads=['rstd'], writes=['rstd'])

    for t in range(ntiles):
        col = mcols[t]
        b = t % 2
        X, CM, RC = xt[b], cm[b], rc[b]
        xn, cmn, rcn = f"xt{b}", f"cm{b}", f"rc{b}"
        P.dma('sp', X[:], xt_d[t].rearrange("(c p) n -> p c n", p=128), writes=[xn])
        P.dma('sp', CM[:], cm_d[t], writes=[cmn])
        P.dma('sp', RC[:], rc_d[t], writes=[rcn])
        for c in range(KC):
            P.op('act', lambda e, c=c: e.activation(out=hT[:, c, :], in_=X[:, c, :], func=AF.Identity,
                                                    scale=sc1p[:, c, col:col + 1], bias=modT[:, c, col:col + 1]),
                 reads=[xn, 'sc1p', 'modT'], writes=['hT'])
        for oc in range(12):
            ps = psA[oc % 2]
            pn = f"psA{oc % 2}"
            for k in range(KC):
                P.op('pe', lambda e, k=k, oc=oc, ps=ps: e.matmul(ps[:, 0:WT], lhsT=win_b[:, k, oc * 128:(oc + 1) * 128], rhs=hT[:, k, :],
                                                                 start=(k == 0), stop=(k == KC - 1)),
                     reads=['win_b', 'hT'], writes=[pn], inc=(k == KC - 1))
            if oc < 4:
                P.op('dve', lambda e, oc=oc, ps=ps: e.tensor_tensor(out=zp[:, oc, :], in0=ps[:, 0:WT], in1=CM[:], op=ALU.mult),
                     reads=[pn, cmn], writes=['zp'])
            elif oc < 8:
                P.op('act', lambda e, oc=oc, ps=ps: e.activation(out=ua[:, oc - 4, :], in_=ps[:, 0:WT], func=AF.Copy),
                     reads=[pn], writes=['ua'])
            else:
                P.op('act', lambda e, ps=ps: e.activation(out=sg[:], in_=ps[:, 0:WT], func=AF.Sigmoid), reads=[pn], writes=['sg'])
                P.op('dve', lambda e: e.tensor_tensor(out=sg[:], in0=sg[:], in1=CM[:], op=ALU.mult), reads=['sg', cmn], writes=['sg'])
                P.op('dve', lambda e, oc=oc: e.tensor_tensor(out=u[:, oc - 8, :], in0=ua[:, oc - 8, :], in1=sg[:], op=ALU.mult),
                     reads=['ua', 'sg'], writes=['u'])
        for g in range(4):
            src = zp[:, g, :]
            srcn = 'zp'
            lo, hi = 0, WT
            bufs = [(pa, 'pa'), (pb, 'pb')]
            dst, dn = bufs[0]
            P.op('dve', lambda e, dst=dst, g=g: e.tensor_tensor(out=dst[:, 1:WT], in0=zp[:, g, 0:WT - 1], in1=zp[:, g, 1:WT], op=ALU.add),
                 reads=['zp'], writes=[dn])
            cur, cn = dst, dn
            lo, hi = 1, WT
            sh = 1
            for lvl in range(g):
                dst, dn = bufs[(lvl + 1) % 2]
                nlo, nhi = lo + sh, hi - sh
                P.op('dve', lambda e, dst=dst, cur=cur, nlo=nlo, nhi=nhi, sh=sh: e.tensor_tensor(
                    out=dst[:, nlo:nhi], in0=cur[:, nlo - sh:nhi - sh], in1=cur[:, nlo + sh:nhi + sh], op=ALU.add),
                    reads=[cn], writes=[dn])
                cur, cn = dst, dn
                lo, hi = nlo, nhi
                sh *= 2
            assert lo <= HALO and hi >= HALO + NT
            oth, on = bufs[0] if cur is bufs[1][0] else bufs[1]
            P.op('dve', lambda e, cur=cur, oth=oth, g=g: e.tensor_tensor(out=oth[:, 0:NT], in0=cur[:, HALO:HALO + NT], in1=RC[:, g, :], op=ALU.mult),
                 reads=[cn, rcn], writes=[on])
            P.op('dve', lambda e, oth=oth, g=g: e.tensor_tensor(out=dif[:], in0=oth[:, 0:NT], in1=zp[:, g, HALO:HALO + NT], op=ALU.subtract),
                 reads=[on, 'zp'], writes=['dif'])
            P.op('pe', lambda e, g=g: e.matmul(psp[:, 0:NT], lhsT=poolw_b[:, g, :], rhs=dif[:], start=True, stop=True),
                 reads=['poolw_b', 'dif'], writes=['psp'])
            P.op('act', lambda e, g=g: e.activation(out=ycat[:, g, :], in_=psp[:, 0:NT], func=AF.Copy, scale=pls[:, g:g + 1]),
                 reads=['psp', 'pls'], writes=['ycat'])
        for c in range(4):
            P.op('dve', lambda e, c=c: e.tensor_scalar(out=yc[:, c, :], in0=u[:, c, 1:1 + NT], scalar1=cw[:, c, 0:1], scalar2=cb[:, c:c + 1],
                                                       op0=ALU.mult, op1=ALU.add), reads=['u', 'cw', 'cb'], writes=['yc'])
            for k in range(1, 31):
                P.op('dve', lambda e, c=c, k=k: e.scalar_tensor_tensor(out=yc[:, c, :], in0=u[:, c, k + 1:k + 1 + NT], scalar=cw[:, c, k:k + 1],
                                                                      in1=yc[:, c, :], op0=ALU.mult, op1=ALU.add),
                     reads=['u', 'cw', 'yc'], writes=['yc'])
        ln_stats(yc, 4, ones512, 'ones512', 'yc', LN_EPS)
        for c in range(4):
            P.op('dve', lambda e, c=c: e.tensor_tensor(out=yn[:], in0=yc[:, c, :], in1=mean_sb[:], op=ALU.subtract),
                 reads=['yc', 'mean_sb'], writes=['yn'])
            P.op('dve', lambda e: e.tensor_tensor(out=yn[:], in0=yn[:], in1=rstd[:], op=ALU.mult), reads=['yn', 'rstd'], writes=['yn'])
            P.op('act', lambda e, c=c: e.activation(out=ycat[:, 4 + c, :], in_=yn[:], func=AF.Silu, scale=clg[:, c:c + 1], bias=clb[:, c:c + 1]),
                 reads=['yn', 'clg', 'clb'], writes=['ycat'])
        for oc in range(KC):
            ps = psA[oc % 2]
            pn = f"psA{oc % 2}"
            for k in range(KC):
                P.op('pe', lambda e, k=k, oc=oc, ps=ps: e.matmul(ps[:, 0:NT], lhsT=wout_b[:, k, oc * 128:(oc + 1) * 128], rhs=ycat[:, k, :],
                                                                 start=(k == 0), stop=(k == KC - 1)),
                     reads=['wout_b', 'ycat'], writes=[pn], inc=(k == KC - 1))
            P.op('dve', lambda e, oc=oc, ps=ps: e.scalar_tensor_tensor(out=vt[:, oc, :], in0=ps[:, 0:NT], scalar=g1a[:, oc, col:col + 1],
                                                                      in1=X[:, oc, HALO:HALO + NT], op0=ALU.mult, op1=ALU.add),
                 reads=[pn, 'g1a', xn], writes=['vt'])
        ln_stats(vt, KC, ones1024, 'ones1024', 'vt', LN_EPS / (ALPHA * ALPHA))
        XO = xo[b]
        xon = f"xo{b}"
        for oc in range(KC):
            P.op('dve', lambda e, oc=oc: e.tensor_tensor(out=vt[:, oc, :], in0=vt[:, oc, :], in1=mean_sb[:], op=ALU.subtract),
                 reads=['vt', 'mean_sb'], writes=['vt'])
            P.op('dve', lambda e, oc=oc: e.tensor_tensor(out=vt[:, oc, :], in0=vt[:, oc, :], in1=rstd[:], op=ALU.mult),
                 reads=['vt', 'rstd'], writes=['vt'])
            P.op('act', lambda e, oc=oc: e.activation(out=XO[:, oc, :], in_=vt[:, oc, :], func=AF.Identity, scale=lng[:, oc:oc + 1], bias=lnb[:, oc:oc + 1]),
                 reads=['vt', 'lng', 'lnb'], writes=[xon])
        P.dma('sp', out_d[t].rearrange("(c p) n -> p c n", p=128), XO[:], reads=[xon], writes=['x1T_scr'])
    K.end_stage()


NSLOT = 128
NEG = -1.0e30


def stage_conv(K, L, uvb):
    K.begin_stage(f"cv{L}_")
    nc, P = K.nc, K.P
    uv_d = K.din(f"peer_uv{L}", [16384, 2 * D])
    src = uv_d.rearrange("(c p j) d -> c p (j d)", p=128, j=2)
    dst = uvb.rearrange("(c p j) d -> c p (j d)", p=128, j=2)
    fb = [K.sb(f"fb{i}", [128, 4 * D]) for i in range(3)]
    bb = [K.sb(f"bb{i}", [128, 4 * D], BF16) for i in range(3)]
    conv_ops(K, src, dst, fb, bb, ('sp', 'act'))
    K.end_stage()


def conv_ops(K, src, dst, fb, bb, queues):
    P = K.P
    for ci in range(64):
        i = ci % 3
        q = queues[ci % len(queues)]
        P.dma(q, fb[i][:], src[ci], writes=[f"fb{i}"])
        if ci % 2 == 0:
            P.op('dve', lambda e, i=i: e.tensor_copy(out=bb[i][:], in_=fb[i][:]), reads=[f"fb{i}"], writes=[f"bb{i}"])
        else:
            P.op('pool', lambda e, i=i: e.tensor_copy(out=bb[i][:], in_=fb[i][:]), reads=[f"fb{i}"], writes=[f"bb{i}"])
        P.dma(q, dst[ci], bb[i][:], reads=[f"bb{i}"], writes=['uvb'])


def stage_peer(K, nsub, mcols, L, src_d, out_d, uvb):
    K.begin_stage(f"pr{L}_")
    nc, P = K.nc, K.P
    cvec_d = K.din("cvec", [128, KC, 2])
    modw_d = K.din(f"mod_w{L}", [D, 6144])
    modb_d = K.din(f"mod_bT{L}", [128, 48])
    wq_d = K.din(f"w_q{L}", [D, 2048])
    keysT_d = K.din(f"keysT{L}", [128, 2, 128])
    lng_d = K.din(f"ln_g{L}1", [1, D])
    lnb_d = K.din(f"ln_b{L}1", [1, D])
    ident_d = K.din("ident", [128, 128])
    iota_d = K.din("iota16", [128, 16])

    psq = [K.ps(f"psq{i}", [128, 512]) for i in range(2)]
    psS = K.ps("psS", [128, 1024])
    psX = [K.ps(f"psX{i}", [128, 512]) for i in range(2)]
    psm = K.ps("psm", [128, 512])
    psr = K.ps("psr", [128, 512])

    ident = K.sb("ident", [128, 128])
    iota16 = K.sb("iota16", [128, 16])
    ones = K.sb("ones", [128, 128])
    P.dma('sp', ident[:], ident_d, writes=['ident'])
    P.dma('sp', iota16[:], iota_d, writes=['iota16'])
    P.op('dve', lambda e: e.memset(ones[:], 1.0), writes=['ones'])
    lng = K.sb("lng", [128, D])
    lnb = K.sb("lnb", [128, D])
    P.dma('sp', lng[:], bass.AP(lng_d.tensor, 0, [[0, 128], [1, D]]), writes=['lng'])
    P.dma('sp', lnb[:], bass.AP(lnb_d.tensor, 0, [[0, 128], [1, D]]), writes=['lnb'])

    tmp = ExitStack()
    sc2p = K.sb("sc2p", [128, KC, 2])
    g2a = K.sb("g2a", [128, KC, 2])
    dg = K.sb("dg", [128, 128])
    reps = {(vn, col): K.sb(f"rep_{vn}{col}", [128, D]) for vn in ("sh2", "sc2p", "g2a") for col in range(2)}
    wq_b = K.sb("wq_b", [128, KC, 2048], BF16)
    keysT = K.sb("keysT", [128, 2, 128], BF16)
    modT = mod_setup(K, cvec_d, modw_d, modb_d, 24, 48, psm, tmp=tmp)
    stage = [tmp.enter_context(nc.sbuf_tensor(f"s_{K.pfx}stage{i}", [128, 2048], F32)) for i in range(2)]
    P.op('dve', lambda e: e.tensor_scalar(out=sc2p[:], in0=modT[:, 8:16, :], scalar1=1.0, scalar2=None, op0=ALU.add),
         reads=['modT'], writes=['sc2p'])
    P.op('dve', lambda e: e.tensor_scalar(out=g2a[:], in0=modT[:, 16:24, :], scalar1=1.0 / ALPHA, scalar2=None, op0=ALU.mult),
         reads=['modT'], writes=['g2a'])
    rep = {}
    for vn, src, j0 in (("sh2", modT, 0), ("sc2p", sc2p, 0), ("g2a", g2a, 0)):
        for col in range(2):
            r = reps[(vn, col)]
            for c in range(KC):
                P.op('dve', lambda e, c=c, src=src, col=col: e.tensor_scalar(out=dg[:], in0=ident[:], scalar1=src[:, j0 + c, col:col + 1], scalar2=None, op0=ALU.mult),
                     reads=['ident', 'modT', 'sc2p', 'g2a'], writes=['dg'])
                P.op('pe', lambda e: e.matmul(psr[:, 0:128], lhsT=ones[:], rhs=dg[:], start=True, stop=True), reads=['ones', 'dg'], writes=['psr'])
                P.op('act', lambda e, c=c, r=r: e.activation(out=r[:, c * 128:(c + 1) * 128], in_=psr[:, 0:128], func=AF.Copy),
                     reads=['psr'], writes=['rep'])
            rep[(vn, col)] = r

    for k in range(KC):
        load_cast(K, wq_b[:, k, :], wq_d[k * 128:(k + 1) * 128, :], stage[k % 2][:], f"stage{k % 2}", "wq_b", eng='dve' if k % 2 == 0 else 'act')
    load_cast(K, keysT[:], keysT_d, stage[0][:, 0:256].rearrange("p (a b) -> p a b", a=2), "stage0", "keysT")
    P.barrier()
    tmp.close()

    x1T = [K.sb(f"x1T{i}", [128, KC, 128]) for i in range(1)]
    x1t = [K.sb(f"x1t{i}", [128, D]) for i in range(2)]
    h2T = K.sb("h2T", [128, KC, 128], BF16)
    qT = K.sb("qT", [128, 16, 128], BF16)
    S = K.sb("S", [128, 16, 128])
    S2 = K.sb("S2", [128, 16, 128])
    sv = K.sb("sv", [128, 16, 16])
    si = K.sb("si", [128, 16, 16], U32)
    sif = K.sb("sif", [128, 16, 16])
    cand = K.sb("cand", [128, 8, 256])
    cand2 = S2[:].rearrange("p a b -> p (a b)").rearrange("p (h c) -> p h c", h=8)
    ts = K.sb("ts", [128, 8, 16])
    pos = K.sb("pos", [128, 8, 16], U32)
    pa = K.sb("pa_", [128, 8, 16], U32)
    pbb = K.sb("pb_", [128, 8, 16], U32)
    af = K.sb("af", [128, 8, 16])
    bf = K.sb("bf", [128, 8, 16])
    oh = K.sb("oh", [128, 8, 16, 16])
    isel = K.sb("isel", [128, 8, 16])
    jsel = K.sb("jsel", [128, 8, 16])
    eidf = K.sb("eidf", [128, 128])
    eidx2 = [K.sb(f"eidx{i}", [128, 128], I32) for i in range(2)]
    ex = K.sb("ex", [128, 8, 16])
    Z = K.sb("Z", [128, 8])
    gate2 = [K.sb(f"gate{i}", [128, 128]) for i in range(2)]
    A = K.sb("A", [128, 128])
    wgt = K.sb("wgt", [128, 128])
    h2t2 = [K.sb(f"h2t{i}", [128, D], BF16) for i in range(2)]
    h2f = K.sb("h2f", [128, D])
    NB = 12
    rows = [K.sb(f"rows{i}", [128, 2 * D], BF16) for i in range(NB)]
    junk = K.sb("junk", [128, D], BF16)
    identb = K.sb("identb", [128, 128], BF16)
    P.op('dve', lambda e: e.tensor_copy(out=identb[:], in_=ident[:]), reads=['ident'], writes=['identb'])
    RG = 4
    gA = [K.sb(f"gA{i}", [128, 1]) for i in range(RG)]
    Acol = [K.sb(f"Acol{i}", [128, 1]) for i in range(RG)]
    dgs = [K.sb(f"dgs{i}", [128, 128], BF16) for i in range(RG)]
    acc = K.sb("acc", [128, D])
    stats = K.sb("stats", [128, 2, 6])
    mv = K.sb("mv", [128, 2])
    rs = K.sb("rs", [128, 1])
    xo = [K.sb(f"xo{i}", [128, D]) for i in range(1)]
    rbi = 0

    def front(s):
        col = mcols[s]
        par = s % 2
        XT, XK = x1T[0], x1t[par]
        xtn, xkn = "x1T0", f"x1t{par}"
        eidx, gate, h2t = eidx2[par], gate2[par], h2t2[par]
        en, gn, hn_ = f"eidx{par}", f"gate{par}", f"h2t{par}"
        P.dma('sp', XT[:], src_d[s // 2][:, (s % 2) * 128:(s % 2) * 128 + 128].rearrange("(c p) n -> p c n", p=128), reads=['x1T_scr'], writes=[xtn])
        for c in range(KC):
            pst = psX[c // 4]
            P.op('pe', lambda e, c=c, pst=pst: e.transpose(out=pst[:, (c % 4) * 128:(c % 4) * 128 + 128], in_=XT[:, c, :], identity=ident[:]),
                 reads=[xtn, 'ident'], writes=[f'psX{c // 4}'])
        P.op('act', lambda e: e.activation(out=XK[:, 0:512], in_=psX[0][:, :], func=AF.Copy), reads=['psX0'], writes=[xkn])
        P.op('act', lambda e: e.activation(out=XK[:, 512:1024], in_=psX[1][:, :], func=AF.Copy), reads=['psX1'], writes=[xkn])
        for c in range(KC):
            P.op('act', lambda e, c=c: e.activation(out=h2T[:, c, :], in_=XT[:, c, :], func=AF.Identity,
                                                    scale=sc2p[:, c, col:col + 1], bias=modT[:, c, col:col + 1]),
                 reads=[xtn, 'sc2p', 'modT'], writes=['h2T'])
        for hp in range(16):
            ps = psq[hp % 2]
            pn = f"psq{hp % 2}"
            for k in range(KC):
                P.op('pe', lambda e, k=k, hp=hp, ps=ps: e.matmul(ps[:, 0:128], lhsT=wq_b[:, k, hp * 128:(hp + 1) * 128], rhs=h2T[:, k, :],
                                                                 start=(k == 0), stop=(k == KC - 1)),
                     reads=['wq_b', 'h2T'], writes=[pn], inc=(k == KC - 1))
            if hp % 2 == 0:
                P.op('act', lambda e, hp=hp, ps=ps: e.activation(out=qT[:, hp, :], in_=ps[:, 0:128], func=AF.Copy), reads=[pn], writes=['qT'])
            else:
                P.op('dve', lambda e, hp=hp, ps=ps: e.tensor_copy(out=qT[:, hp, :], in_=ps[:, 0:128]), reads=[pn], writes=['qT'])
        for rnd in range(2):
            for j in range(8):
                hp = rnd * 8 + j
                P.op('pe', lambda e, hp=hp, j=j: e.matmul(psS[:, j * 128:(j + 1) * 128], lhsT=qT[:, hp, :], rhs=keysT[:, hp % 2, :], start=True, stop=True),
                     reads=['qT', 'keysT'], writes=['psS'], inc=(j == 7))
            for q4 in range(2):
                P.op('act', lambda e, q4=q4, rnd=rnd: e.activation(out=S[:, rnd * 8 + 4 * q4:rnd * 8 + 4 * q4 + 4, :],
                                                                  in_=psS[:, 512 * q4:512 * q4 + 512].rearrange("p (a b) -> p a b", a=4), func=AF.Copy),
                     reads=['psS'], writes=['S'])
        for hp in range(16):
            P.op('dve', lambda e, hp=hp: e.max(out=sv[:, hp, 0:8], in_=S[:, hp, :]), reads=['S'], writes=['sv'])
            P.op('dve', lambda e, hp=hp: e.max_index(out=si[:, hp, 0:8], in_max=sv[:, hp, 0:8], in_values=S[:, hp, :]), reads=['S', 'sv'], writes=['si'])
            P.op('dve', lambda e, hp=hp: e.match_replace(out=S2[:, hp, :], in_to_replace=sv[:, hp, 0:8], in_values=S[:, hp, :], imm_value=NEG),
                 reads=['S', 'sv'], writes=['S2'])
            P.op('dve', lambda e, hp=hp: e.max(out=sv[:, hp, 8:16], in_=S2[:, hp, :]), reads=['S2'], writes=['sv'])
            P.op('dve', lambda e, hp=hp: e.max_index(out=si[:, hp, 8:16], in_max=sv[:, hp, 8:16], in_values=S2[:, hp, :]), reads=['S2', 'sv'], writes=['si'])
        P.op('dve', lambda e: e.tensor_copy(out=sif[:], in_=si[:]), reads=['si'], writes=['sif'])
        P.op('dve', lambda e: e.tensor_tensor(out=cand[:].rearrange("p h (a b) -> p h a b", a=16),
                                              in0=fap(sv, 0, [[32, 8], [1, 16], [0, 16]]), in1=fap(sv, 16, [[32, 8], [0, 16], [1, 16]]), op=ALU.add),
             reads=['sv'], writes=['cand'])
        for h in range(8):
            P.op('dve', lambda e, h=h: e.max(out=ts[:, h, 0:8], in_=cand[:, h, :]), reads=['cand'], writes=['ts'])
            P.op('dve', lambda e, h=h: e.max_index(out=pos[:, h, 0:8], in_max=ts[:, h, 0:8], in_values=cand[:, h, :]), reads=['cand', 'ts'], writes=['pos'])
            P.op('dve', lambda e, h=h: e.match_replace(out=cand2[:, h, :], in_to_replace=ts[:, h, 0:8], in_values=cand[:, h, :], imm_value=NEG),
                 reads=['cand', 'ts'], writes=['S2'])
            P.op('dve', lambda e, h=h: e.max(out=ts[:, h, 8:16], in_=cand2[:, h, :]), reads=['S2'], writes=['ts'])
            P.op('dve', lambda e, h=h: e.max_index(out=pos[:, h, 8:16], in_max=ts[:, h, 8:16], in_values=cand2[:, h, :]), reads=['S2', 'ts'], writes=['pos'])
        P.op('dve', lambda e: e.tensor_single_scalar(out=pa[:], in_=pos[:], scalar=4, op=ALU.logical_shift_right), reads=['pos'], writes=['pa'])
        P.op('dve', lambda e: e.tensor_single_scalar(out=pbb[:], in_=pos[:], scalar=15, op=ALU.bitwise_and), reads=['pos'], writes=['pb'])
        P.op('dve', lambda e: e.tensor_copy(out=af[:], in_=pa[:]), reads=['pa'], writes=['af'])
        P.op('dve', lambda e: e.tensor_copy(out=bf[:], in_=pbb[:]), reads=['pb'], writes=['bf'])
        for (src, p_, dst, dn) in ((af, 0, isel, 'isel'), (bf, 1, jsel, 'jsel')):
            P.op('dve', lambda e, src=src: e.tensor_tensor(out=oh[:], in0=fap(src, 0, [[16, 8], [1, 16], [0, 16]]),
                                                           in1=fap(iota16, 0, [[0, 8], [0, 16], [1, 16]]), op=ALU.is_equal),
                 reads=['af', 'bf', 'iota16'], writes=['oh'])
            P.op('dve', lambda e, p_=p_: e.tensor_tensor(out=oh[:], in0=oh[:], in1=fap(sif, 16 * p_, [[32, 8], [0, 16], [1, 16]]), op=ALU.mult),
                 reads=['oh', 'sif'], writes=['oh'])
            P.op('dve', lambda e, dst=dst: e.tensor_reduce(out=dst[:], in_=oh[:], axis=AX.X, op=ALU.add), reads=['oh'], writes=[dn])
        P.op('dve', lambda e: e.scalar_tensor_tensor(out=eidf[:], in0=isel[:].rearrange("p h k -> p (h k)"), scalar=128.0,
                                                     in1=jsel[:].rearrange("p h k -> p (h k)"), op0=ALU.mult, op1=ALU.add),
             reads=['isel', 'jsel'], writes=['eidf'])
        P.op('dve', lambda e: e.tensor_copy(out=eidx[:], in_=eidf[:]), reads=['eidf'], writes=[en])
        P.op('dve', lambda e: e.tensor_tensor(out=ex[:], in0=ts[:], in1=fap(ts, 0, [[16, 8], [0, 16]]), op=ALU.subtract), reads=['ts'], writes=['ex'])
        P.op('act', lambda e: e.activation(out=ex[:], in_=ex[:], func=AF.Exp), reads=['ex'], writes=['ex'])
        P.op('dve', lambda e: e.tensor_reduce(out=Z[:], in_=ex[:], axis=AX.X, op=ALU.add), reads=['ex'], writes=['Z'])
        P.op('dve', lambda e: e.reciprocal(out=Z[:], in_=Z[:]), reads=['Z'], writes=['Z'])
        P.op('dve', lambda e: e.tensor_tensor(out=gate[:].rearrange("p (h k) -> p h k", h=8), in0=ex[:], in1=fap(Z, 0, [[1, 8], [0, 16]]), op=ALU.mult),
             reads=['ex', 'Z'], writes=[gn])
        P.op('dve', lambda e: e.tensor_tensor(out=h2f[:], in0=XK[:], in1=rep[("sc2p", col)][:], op=ALU.mult), reads=[xkn, 'rep'], writes=['h2f'])
        P.op('dve', lambda e: e.tensor_tensor(out=h2t[:], in0=h2f[:], in1=rep[("sh2", col)][:], op=ALU.add), reads=['h2f', 'rep'], writes=[hn_])
    def back(s, recs):
        nonlocal rbi
        col = mcols[s]
        par = s % 2
        XK, xkn = x1t[par], f"x1t{par}"
        eidx, gate, h2t = eidx2[par], gate2[par], h2t2[par]
        en, gn, hn_ = f"eidx{par}", f"gate{par}", f"h2t{par}"
        b = 0
        per = (len(recs) + NSLOT - 1) // NSLOT
        slot_rows = {}
        for slot in range(NSLOT + 1):
            if slot < NSLOT:
                r = rows[rbi % NB]
                rn = f"rows{rbi % NB}"
                rbi += 1
                slot_rows[slot] = (r, rn)
                g_ = slot % RG
                P.dma('pool', None, None, reads=[en, 'uvb'], writes=[rn],
                      fn=lambda e, r=r, slot=slot: e.indirect_dma_start(out=r[:], out_offset=None, in_=uvb,
                                                                         in_offset=bass.IndirectOffsetOnAxis(ap=eidx[:, slot:slot + 1], axis=0)))
                P.op('dve', lambda e, r=r, g_=g_: e.scalar_tensor_tensor(out=junk[:], in0=r[:, 0:D], scalar=1.0, in1=h2t[:], op0=ALU.mult, op1=ALU.mult,
                                                                         accum_out=Acol[g_][:, 0:1]),
                     reads=[rn, hn_], writes=['junk', f"Acol{g_}"])
                P.op('act', lambda e, g_=g_: e.activation(out=gA[g_][:], in_=Acol[g_][:], func=AF.Gelu), reads=[f"Acol{g_}"], writes=[f"gA{g_}"])
            if slot >= 1:
                ps_ = slot - 1
                r, rn = slot_rows.pop(ps_)
                g_ = ps_ % RG
                P.op('act', lambda e, g_=g_, ps_=ps_: e.activation(out=gA[g_][:], in_=gA[g_][:], func=AF.Copy, scale=gate[:, ps_:ps_ + 1]),
                     reads=[f"gA{g_}", gn], writes=[f"gA{g_}"])
                P.op('act', lambda e, g_=g_: e.activation(out=dgs[g_][:], in_=identb[:], func=AF.Copy, scale=gA[g_][:, 0:1]),
                     reads=['identb', f"gA{g_}"], writes=[f"dgs{g_}"])
                for hf_ in range(2):
                    pst = psm if hf_ == 0 else psr
                    P.op('pe', lambda e, r=r, g_=g_, hf_=hf_, pst=pst, ps_=ps_: e.matmul(pst[:, :], lhsT=dgs[g_][:], rhs=r[:, D + hf_ * 512:D + (hf_ + 1) * 512],
                                                                                       start=(ps_ == 0), stop=(ps_ == NSLOT - 1)),
                         reads=[f"dgs{g_}", rn], writes=['psm' if hf_ == 0 else 'psr'])
            P.replay(recs, per)
        P.replay(recs, len(recs))
        P.op('dve', lambda e: e.tensor_tensor(out=acc[:, 0:512], in0=psm[:, :], in1=rep[("g2a", col)][:, 0:512], op=ALU.mult), reads=['psm', 'rep'], writes=['acc'])
        P.op('dve', lambda e: e.tensor_tensor(out=acc[:, 512:1024], in0=psr[:, :], in1=rep[("g2a", col)][:, 512:1024], op=ALU.mult), reads=['psr', 'rep'], writes=['acc'])
        P.op('dve', lambda e: e.tensor_tensor(out=acc[:], in0=acc[:], in1=XK[:], op=ALU.add), reads=['acc', xkn], writes=['acc'])
        for hf in range(2):
            P.op('dve', lambda e, hf=hf: e.bn_stats(out=stats[:, hf, :], in_=acc[:, hf * 512:(hf + 1) * 512]), reads=['acc'], writes=['stats'])
        P.op('dve', lambda e: e.bn_aggr(out=mv[:], in_=stats[:].rearrange("p a b -> p (a b)")), reads=['stats'], writes=['mv'])
        P.op('dve', lambda e: e.tensor_scalar(out=rs[:], in0=mv[:, 1:2], scalar1=LN_EPS / (ALPHA * ALPHA), scalar2=None, op0=ALU.add), reads=['mv'], writes=['rs'])
        P.op('act', lambda e: e.activation(out=rs[:], in_=rs[:], func=AF.Sqrt), reads=['rs'], writes=['rs'])
        P.op('dve', lambda e: e.reciprocal(out=rs[:], in_=rs[:]), reads=['rs'], writes=['rs'])
        XO = xo[b]
        xon = f"xo{b}"
        P.op('dve', lambda e: e.tensor_scalar(out=XO[:], in0=acc[:], scalar1=mv[:, 0:1], scalar2=rs[:, 0:1], op0=ALU.subtract, op1=ALU.mult),
             reads=['acc', 'mv', 'rs'], writes=[xon])
        P.op('dve', lambda e: e.tensor_tensor(out=XO[:], in0=XO[:], in1=lng[:], op=ALU.mult), reads=[xon, 'lng'], writes=[xon])
        P.op('dve', lambda e: e.tensor_tensor(out=XO[:], in0=XO[:], in1=lnb[:], op=ALU.add), reads=[xon, 'lnb'], writes=[xon])
        P.dma('sp', out_d[s], XO[:], reads=[xon], writes=['x2_scr'])

    front(0)
    for s in range(nsub):
        recs = []
        if s + 1 < nsub:
            P.rec = recs
            front(s + 1)
            P.rec = None
        back(s, recs)
    K.end_stage()


CDW = 2832
RMS_EPS = 1e-6


def stage_l1a(K, ntiles, mcols, src_d, nsub, gb_):
    K.begin_stage("la_")
    nc, P = K.nc, K.P
    cos_d = K.din("cosT", [ntiles, 64, NT])
    sin_d = K.din("sinT", [ntiles, 64, NT])
    cvec_d = K.din("cvec", [128, KC, 2])
    modw_d = K.din("mod_w1", [D, 6144])
    modb_d = K.din("mod_bT1", [128, 48])
    ident_d = K.din("ident", [128, 128])
    win_d = K.din("cd_w_in", [D, CDW])
    gb_d = K.din("gate_bT", [16, 1])
    qg_d = K.din("qn_gT", [64, 1])
    kg_d = K.din("kn_gT", [64, 1])
    pm_d = K.din("ropeP", [64, 64])
    tsl = lambda t: slice(t * NT, (t + 1) * NT)
    xl_o = gb_['xl0T']

    psA = [K.ps(f"psA{i}", [128, 512]) for i in range(2)]
    psr = K.ps("psr", [128, 512])
    psw = K.ps("psw", [128, 512])
    psm = K.ps("psm", [128, 512])
    psX = [K.ps(f"psX{i}", [128, 512]) for i in range(2)]

    ident = K.sb("ident", [128, 128])
    P.dma('sp', ident[:], ident_d, writes=['ident'])
    ones64 = K.sb("ones64", [64, 64])
    P.op('dve', lambda e: e.memset(ones64[:], 1.0 / 64), writes=['ones64'])
    gb = K.sb("gb", [16, 1]); qg = K.sb("qg", [64, 1]); kg = K.sb("kg", [64, 1]); pm = K.sb("pm", [64, 64])
    for t_, d_, n_ in ((gb, gb_d, 'gb'), (qg, qg_d, 'qg'), (kg, kg_d, 'kg'), (pm, pm_d, 'pm')):
        P.dma('sp', t_[:], d_, writes=[n_])
    sc1p = K.sb("sc1p", [128, KC, 2])
    win_b = K.sb("win_b", [128, KC, CDW], BF16)
    tmp = ExitStack()
    modT = mod_setup(K, cvec_d, modw_d, modb_d, 0, 16, psm, tmp=tmp)
    stage = [tmp.enter_context(nc.sbuf_tensor(f"s_{K.pfx}stage{i}", [128, CDW], F32)) for i in range(2)]
    P.op('dve', lambda e: e.tensor_scalar(out=sc1p[:], in0=modT[:, 8:16, :], scalar1=1.0, scalar2=None, op0=ALU.add),
         reads=['modT'], writes=['sc1p'])
    for k in range(KC):
        load_cast(K, win_b[:, k, :], win_d[k * 128:(k + 1) * 128, :], stage[k % 2][:], f"stage{k % 2}", "win_b", eng='dve' if k % 2 == 0 else 'act')
    P.barrier()
    tmp.close()

    xt = [K.sb(f"xt{i}", [128, KC, NT]) for i in range(2)]
    xk = [K.sb(f"xk{i}", [128, D]) for i in range(2)]
    cs = [K.sb(f"cs{i}", [64, NT]) for i in range(2)]
    sn = [K.sb(f"sn{i}", [64, NT]) for i in range(2)]
    hT = K.sb("hT", [128, KC, NT], BF16)
    ob = [K.sb(f"ob{i}", [128, NT], BF16) for i in range(4)]
    of = [K.sb(f"of{i}", [128, NT], F32) for i in range(2)]
    sq = K.sb("sq", [64, NT]); rstd = K.sb("rstd", [64, NT]); qn = K.sb("qn", [64, NT]); t1 = K.sb("t1", [64, NT]); t2 = K.sb("t2", [64, NT])
    obi = 0
    ofi = 0

    def proj(c0, M, ps, pn):
        for k in range(KC):
            P.op('pe', lambda e, k=k: e.matmul(ps[0:M, 0:NT], lhsT=win_b[:, k, c0:c0 + M], rhs=hT[:, k, :], start=(k == 0), stop=(k == KC - 1)),
                 reads=['win_b', 'hT'], writes=[pn], inc=(k == KC - 1))

    for t in range(ntiles):
        col = mcols[t]
        b = t % 2
        X, CS, SN = xt[b], cs[b], sn[b]
        xn, csn, snn = f"xt{b}", f"cs{b}", f"sn{b}"
        for hh in range(2):
            s_ = 2 * t + hh
            if s_ >= nsub:
                P.op('dve', lambda e, hh=hh: e.memset(X[:, :, hh * 128:(hh + 1) * 128], 0.0), writes=[xn])
                continue
            XK = xk[hh]; xkn = f"xk{hh}"
            P.dma('sp', XK[:], src_d[s_], reads=['x2_scr'], writes=[xkn])
            for c in range(KC):
                pst = psX[c // 4]
                P.op('pe', lambda e, c=c, pst=pst, XK=XK: e.transpose(out=pst[:, (c % 4) * 128:(c % 4) * 128 + 128], in_=XK[:, c * 128:(c + 1) * 128], identity=ident[:]),
                     reads=[xkn, 'ident'], writes=[f'psX{c // 4}'])
            P.op('act', lambda e, hh=hh: e.activation(out=X[:, 0:4, hh * 128:(hh + 1) * 128], in_=psX[0][:, :].rearrange("p (c n) -> p c n", c=4), func=AF.Copy),
                 reads=['psX0'], writes=[xn])
            P.op('dve', lambda e, hh=hh: e.tensor_copy(out=X[:, 4:8, hh * 128:(hh + 1) * 128], in_=psX[1][:, :].rearrange("p (c n) -> p c n", c=4)),
                 reads=['psX1'], writes=[xn])
        if t < ntiles - 1:
            P.dma('sp', xl_o[t].rearrange("(c p) n -> p c n", p=128), X[:], reads=[xn], writes=['xl_o'])
        P.dma('sp', CS[:], cos_d[t], writes=[csn])
        P.dma('sp', SN[:], sin_d[t], writes=[snn])
        for c in range(KC):
            P.op('act', lambda e, c=c: e.activation(out=hT[:, c, :], in_=X[:, c, :], func=AF.Identity,
                                                    scale=sc1p[:, c, col:col + 1], bias=modT[:, c, col:col + 1]),
                 reads=[xn, 'sc1p', 'modT'], writes=['hT'])
        for oc in range(16):
            ps = psA[oc % 2]; pn = f"psA{oc % 2}"
            proj(oc * 128, 128, ps, pn)
            grp, hh = oc // 4, oc % 4
            if grp == 3:
                o = of[ofi % 2]; on = f"of{ofi % 2}"; ofi += 1
                P.op('act', lambda e, o=o, ps=ps: e.activation(out=o[:], in_=ps[:, 0:NT], func=AF.Copy), reads=[pn], writes=[on])
                P.dma('sp', gb_['om'][hh * 128:(hh + 1) * 128, tsl(t)], o[:], reads=[on], writes=['om_o'])
            else:
                o = ob[obi % 4]; on = f"ob{obi % 4}"; obi += 1
                scale = (128.0 ** -0.5) if grp == 1 else 1.0
                if oc % 2 == 0:
                    P.op('act', lambda e, o=o, ps=ps, scale=scale: e.activation(out=o[:], in_=ps[:, 0:NT], func=AF.Copy, scale=scale), reads=[pn], writes=[on])
                else:
                    P.op('dve', lambda e, o=o, ps=ps, scale=scale: e.tensor_scalar(out=o[:], in0=ps[:, 0:NT], scalar1=scale, scalar2=None, op0=ALU.mult), reads=[pn], writes=[on])
                dst = (gb_['qm'], gb_['km'], gb_['vm'])[grp]
                P.dma('sp', dst[hh * 128:(hh + 1) * 128, tsl(t)], o[:], reads=[on], writes=['qkv_o'])
        for hd in range(10):
            ps = psA[hd % 2]; pn = f"psA{hd % 2}"
            proj(2048 + hd * 64, 64, ps, pn)
            gvec, gname = (qg, 'qg') if hd < 8 else (kg, 'kg')
            P.op('act', lambda e, ps=ps: e.activation(out=sq[:], in_=ps[0:64, 0:NT], func=AF.Square), reads=[pn], writes=['sq'])
            P.op('pe', lambda e: e.matmul(psr[0:64, 0:NT], lhsT=ones64[:], rhs=sq[:], start=True, stop=True), reads=['ones64', 'sq'], writes=['psr'])
            P.op('dve', lambda e: e.tensor_scalar(out=rstd[:], in0=psr[0:64, 0:NT], scalar1=RMS_EPS, scalar2=None, op0=ALU.add), reads=['psr'], writes=['rstd'])
            P.op('act', lambda e: e.activation(out=rstd[:], in_=rstd[:], func=AF.Sqrt), reads=['rstd'], writes=['rstd'])
            P.op('dve', lambda e: e.reciprocal(out=rstd[:], in_=rstd[:]), reads=['rstd'], writes=['rstd'])
            P.op('act', lambda e, ps=ps, gvec=gvec: e.activation(out=qn[:], in_=ps[0:64, 0:NT], func=AF.Copy, scale=gvec[:, 0:1]), reads=[pn, gname], writes=['qn'])
            P.op('dve', lambda e: e.tensor_tensor(out=qn[:], in0=qn[:], in1=rstd[:], op=ALU.mult), reads=['qn', 'rstd'], writes=['qn'])
            P.op('pe', lambda e: e.matmul(psw[0:64, 0:NT], lhsT=pm[:], rhs=qn[:], start=True, stop=True), reads=['pm', 'qn'], writes=['psw'])
            P.op('dve', lambda e: e.tensor_tensor(out=t1[:], in0=qn[:], in1=CS[:], op=ALU.mult), reads=['qn', csn], writes=['t1'])
            P.op('dve', lambda e: e.tensor_tensor(out=t2[:], in0=psw[0:64, 0:NT], in1=SN[:], op=ALU.mult), reads=['psw', snn], writes=['t2'])
            o = ob[obi % 4]; on = f"ob{obi % 4}"; obi += 1
            P.op('dve', lambda e, o=o: e.tensor_tensor(out=o[0:64, :], in0=t1[:], in1=t2[:], op=ALU.add), reads=['t1', 't2'], writes=[on])
            dst = gb_['qa'][hd * 64:(hd + 1) * 64, tsl(t)] if hd < 8 else gb_['ka'][(hd - 8) * 64:(hd - 7) * 64, tsl(t)]
            P.dma('sp', dst, o[0:64, :], reads=[on], writes=['qk_o'])
        for hd in range(2):
            ps = psA[hd % 2]; pn = f"psA{hd % 2}"
            proj(2048 + 640 + hd * 64, 64, ps, pn)
            o = ob[obi % 4]; on = f"ob{obi % 4}"; obi += 1
            P.op('act', lambda e, o=o, ps=ps: e.activation(out=o[0:64, :], in_=ps[0:64, 0:NT], func=AF.Copy), reads=[pn], writes=[on])
            P.dma('sp', gb_['va'][hd * 64:(hd + 1) * 64, tsl(t)], o[0:64, :], reads=[on], writes=['va_o'])
        ps = psA[0]; pn = "psA0"
        proj(2816, 16, ps, pn)
        o = of[ofi % 2]; on = f"of{ofi % 2}"; ofi += 1
        P.op('act', lambda e, o=o, ps=ps: e.activation(out=o[0:16, :], in_=ps[0:16, 0:NT], func=AF.Identity, bias=gb[:, 0:1]), reads=[pn, 'gb'], writes=[on])
        P.dma('sp', gb_['g'][:, tsl(t)], o[0:16, :], reads=[on], writes=['gt_o'])
    K.end_stage()


NEGM = -1.0e30


def stage_attn(K, Th, TOK, gb_, conv=None):
    K.begin_stage("at_")
    nc, P = K.nc, K.P
    Tq = Th
    Tk = 2 * Th + 256
    nkt = Tk // 128
    nqt = Tq // 512
    qg_d = K.din("qn_g", [1, 64])
    kg_d = K.din("kn_g", [1, 64])
    sel_d = K.din("sel", [65, 64])
    ident_d = K.din("ident", [128, 128])
    psST = [[K.ps(f"psST{hh}{i}", [128, 512]) for i in range(2)] for hh in range(2)]
    psO = [K.ps(f"psO{hh}", [128, 512]) for hh in range(2)]
    psD = K.ps("psD", [128, 512])
    psT = K.ps("psT", [128, 512], BF16)
    sel = K.sb("sel", [65, 64])
    P.dma('sp', sel[:], sel_d, writes=['sel'])
    identf = K.sb("identf", [128, 128]); identb = K.sb("identb", [128, 128], BF16)
    P.dma('sp', identf[:], ident_d, writes=['identf'])
    P.op('dve', lambda e: e.tensor_copy(out=identb[:], in_=identf[:]), reads=['identf'], writes=['identb'])
    gq = K.sb("gq", [128, 64]); gk = K.sb("gk", [128, 64]); mq = K.sb("mq", [128, 1]); mk = K.sb("mk", [128, 1]); negC = K.sb("negC", [128, 1])
    P.dma('sp', gq[:], bass.AP(qg_d.tensor, 0, [[0, 128], [1, 64]]), writes=['gq'])
    P.dma('sp', gk[:], bass.AP(kg_d.tensor, 0, [[0, 128], [1, 64]]), writes=['gk'])
    P.op('dve', lambda e: e.tensor_reduce(out=mq[:], in_=gq[:], axis=AX.X, op=ALU.max, apply_absolute_value=True), reads=['gq'], writes=['mq'])
    P.op('dve', lambda e: e.tensor_reduce(out=mk[:], in_=gk[:], axis=AX.X, op=ALU.max, apply_absolute_value=True), reads=['gk'], writes=['mk'])
    P.op('dve', lambda e: e.scalar_tensor_tensor(out=negC[:], in0=mq[:], scalar=-8.0, in1=mk[:], op0=ALU.mult, op1=ALU.mult), reads=['mq', 'mk'], writes=['negC'])
    qT = K.sb("qT", [128, Tq], BF16)
    kT = K.sb("kT", [128, Tk], BF16)
    vT = K.sb("vT", [64, Tk], BF16)
    vaug = K.sb("vaug", [128, nkt, 65], BF16)
    PT = [[K.sb(f"PT{hh}{i}", [128, 512], BF16) for i in range(2)] for hh in range(2)]
    OTs = K.sb("OTs", [65, 512])
    rden = K.sb("rden", [64, 512])
    ob = [K.sb(f"ob{i}", [64, 512]) for i in range(2)]
    P.op('dve', lambda e: e.memset(vaug[:, :, 64:65], 1.0), writes=['vaug1'])
    crecs = []
    if conv is not None:
        uv_d = K.din(f"peer_uv{conv[0]}", [16384, 2 * D])
        fb = [K.sb(f"fb{i}", [128, 4 * D]) for i in range(3)]
        bb = [K.sb(f"bb{i}", [128, 4 * D], BF16) for i in range(3)]
        P.rec = crecs
        conv_ops(K, uv_d.rearrange("(c p j) d -> c p (j d)", p=128, j=2), conv[1].rearrange("(c p j) d -> c p (j d)", p=128, j=2), fb, bb, ('sp',))
        P.rec = None
    it = 0
    citer = 0
    for up in range(4):
        kvh = up // 2
        P.dma('sp', qT[:], gb_['qa'][up * 128:(up + 1) * 128, 0:Th], reads=['qk_o'], writes=['qT'])
        if up % 2 == 0:
            for m_ in range(2):
                rows = slice(m_ * 128 + kvh * 64, m_ * 128 + kvh * 64 + 64)
                for hh in range(2):
                    P.dma('sp', kT[hh * 64:(hh + 1) * 64, m_ * 128:(m_ + 1) * 128], gb_['ka_all'][rows, Th:Th + 128], reads=['ka_all'], writes=['kT'])
                    P.dma('sp', kT[hh * 64:(hh + 1) * 64, 256 + m_ * Th:256 + (m_ + 1) * Th], gb_['ka_all'][rows, 0:Th], reads=['ka_all'], writes=['kT'])
                P.dma('sp', vT[:, m_ * 128:(m_ + 1) * 128], gb_['va_all'][rows, Th:Th + 128], reads=['va_all'], writes=['vT'])
                P.dma('sp', vT[:, 256 + m_ * Th:256 + (m_ + 1) * Th], gb_['va_all'][rows, 0:Th], reads=['va_all'], writes=['vT'])
            for kt in range(nkt):
                P.op('pe', lambda e, kt=kt: e.transpose(out=psT[:, 0:64], in_=vT[:, kt * 128:(kt + 1) * 128], identity=identb[0:64, 0:64]),
                     reads=['vT', 'identb'], writes=['psT'])
                P.op('act', lambda e, kt=kt: e.activation(out=vaug[:, kt, 0:64], in_=psT[:, 0:64], func=AF.Copy), reads=['psT'], writes=['vaug'])
        for qt in range(nqt):
            def score(kt, qt=qt):
                for hh in range(2):
                    ps = psST[hh][kt % 2]
                    P.op('pe', lambda e, ps=ps, kt=kt, qt=qt, hh=hh: e.matmul(ps[:, :], lhsT=kT[hh * 64:(hh + 1) * 64, kt * 128:(kt + 1) * 128],
                                                                             rhs=qT[hh * 64:(hh + 1) * 64, qt * 512:(qt + 1) * 512], start=True, stop=True),
                         reads=['kT', 'qT'], writes=[f"psST{hh}{kt % 2}"])
            score(0)
            for kt in range(nkt):
                if kt + 1 < nkt:
                    score(kt + 1)
                for hh in range(2):
                    ps = psST[hh][kt % 2]; pn = f"psST{hh}{kt % 2}"
                    pt = PT[hh][kt % 2]; ptn = f"PT{hh}{kt % 2}"
                    P.op('act', lambda e, ps=ps, pt=pt: e.activation(out=pt[:], in_=ps[:, :], func=AF.Exp, scale=0.125, bias=negC[:, 0:1]),
                         reads=[pn, 'negC'], writes=[ptn])
                    P.op('pe', lambda e, pt=pt, kt=kt, hh=hh: e.matmul(psO[hh][0:65, :], lhsT=vaug[:, kt, :], rhs=pt[:], start=(kt == 0), stop=(kt == nkt - 1)),
                         reads=['vaug', 'vaug1', ptn], writes=[f'psO{hh}'])
                citer += 1
                if citer % 8 == 0:
                    P.replay(crecs, 1)
            for hh in range(2):
                u = up * 2 + hh
                P.op('act', lambda e, hh=hh: e.activation(out=OTs[:], in_=psO[hh][0:65, :], func=AF.Copy), reads=[f'psO{hh}'], writes=['OTs'])
                P.op('pe', lambda e: e.matmul(psD[0:64, :], lhsT=sel[:], rhs=OTs[:], start=True, stop=True), reads=['sel', 'OTs'], writes=['psD'])
                P.op('dve', lambda e: e.reciprocal(out=rden[:], in_=psD[0:64, :]), reads=['psD'], writes=['rden'])
                o = ob[it % 2]; on = f"ob{it % 2}"; it += 1
                P.op('dve', lambda e, o=o: e.tensor_tensor(out=o[:], in0=OTs[0:64, :], in1=rden[:], op=ALU.mult), reads=['OTs', 'rden'], writes=[on])
                P.dma('sp', gb_['ya'][u * 64:(u + 1) * 64, qt * 512:(qt + 1) * 512], o[:], reads=[on], writes=['ya_l'])
    P.replay(crecs, len(crecs))
    K.end_stage()


def stage_scan(K, nunits, Th, TOK, gb_):
    K.begin_stage("sc_")
    nc, P = K.nc, K.P
    ncc = 2
    nchl = 2 * Th // 128
    NCH = ncc + nchl
    cpm = Th // 128
    sidx_d = K.din("scan_idx", [128, nunits * 2], I32)
    gidx_d = K.din("scan_gidx", [128, nunits * 2], I32)
    cst_d = K.din("consts", [5, 128, 128])
    h_o = gb_['h']
    def ccol(c):
        if c < ncc:
            return c, Th
        lc = c - ncc
        return lc // cpm, (lc % cpm) * 128

    psR = K.ps("psR", [128, 512]); psQK = K.ps("psQK", [128, 512]); psT = K.ps("psT", [128, 512], BF16)
    psN = K.ps("psN", [128, 512]); psC = K.ps("psC", [128, 512]); psX = K.ps("psX", [128, 512]); psB = K.ps("psB", [128, 512])
    cst = K.sb("cst", [128, 5, 128])
    P.dma('sp', cst[:], cst_d.rearrange("c p n -> p c n"), writes=['cst'])
    ident, triL, triU, mnegL, mnegU = [cst[:, i, :] for i in range(5)]
    identb = K.sb("identb", [128, 128], BF16)
    P.op('dve', lambda e: e.tensor_copy(out=identb[:], in_=ident), reads=['cst'], writes=['identb'])
    ones = K.sb("ones", [128, 128])
    P.op('dve', lambda e: e.memset(ones[:], 1.0), writes=['ones'])
    qTm = [K.sb(f"qTm{i}", [128, TOK], BF16) for i in range(2)]
    kTm = [K.sb(f"kTm{i}", [128, TOK], BF16) for i in range(2)]
    vTm = [K.sb(f"vTm{i}", [128, TOK], BF16) for i in range(2)]
    gsel = [K.sb(f"gsel{i}", [4, TOK]) for i in range(2)]
    sidx = K.sb("sidx", [128, nunits * 2], I32); gidx = K.sb("gidx", [128, nunits * 2], I32)
    P.dma('sp', sidx[:], sidx_d, writes=['sidx'])
    P.dma('sp', gidx[:], gidx_d, writes=['gidx'])
    def qsl(c):
        m_, c0 = ccol(c)
        return qTm[m_][:, c0:c0 + 128]
    def ksl(c):
        m_, c0 = ccol(c)
        return kTm[m_][:, c0:c0 + 128]
    ktok = K.sb("ktok", [128, NCH, 128], BF16); vaug = K.sb("vaug", [128, NCH, 129], BF16)
    P.op('dve', lambda e: e.memset(vaug[:, :, 128:129], 1.0), writes=['vaug1'])
    G = K.sb("G", [128, 4, NCH])
    ee = K.sb("ee", [128, NCH]); LF = K.sb("LF", [128, NCH]); Bc = K.sb("Bc", [128, NCH]); BL = K.sb("BL", [128, NCH]); IGmB = K.sb("IGmB", [128, NCH])
    Caug = K.sb("Caug", [128, 129]); Cb = K.sb("Cb", [128, 129], BF16); m = K.sb("m", [128, 1])
    igrep = K.sb("igrep", [128, 128]); dm = K.sb("dm", [128, 128]); sexp = K.sb("sexp", [128, 128]); s_b = K.sb("s_b", [128, 128], BF16)
    sT = K.sb("sT", [128, 128], BF16); numA = K.sb("numA", [128, 129]); tot = K.sb("tot", [128, 129]); kw = K.sb("kw", [128, 128], BF16)
    col = {n: K.sb(n, [128, 1]) for n in ("rowmax", "glmax", "mnew", "nmnew", "inter", "mt", "nmt", "winter", "emt", "dd", "t2", "wk", "t3", "decay")}
    hb = [K.sb(f"hb{i}", [128, 128]) for i in range(2)]
    hi = 0

    def c1(name, fn, reads, eng='dve'):
        P.op(eng, fn, reads=reads, writes=[name])

    for u in range(nunits):
        for m_ in range(2):
            ic = u * 2 + m_
            for (dst_, srcn, dn) in ((qTm[m_], 'qm_all', 'qT'), (kTm[m_], 'km_all', 'kT'), (vTm[m_], 'vm_all', 'vT')):
                P.dma('pool', None, None, reads=[srcn, 'sidx'], writes=[dn],
                      fn=lambda e, dst_=dst_, srcn=srcn, ic=ic: e.indirect_dma_start(out=dst_[:], out_offset=None, in_=gb_[srcn],
                                                                                   in_offset=bass.IndirectOffsetOnAxis(ap=sidx[:, ic:ic + 1], axis=0)))
            P.dma('pool', None, None, reads=['g_all', 'gidx'], writes=['gsel'],
                  fn=lambda e, m_=m_, ic=ic: e.indirect_dma_start(out=gsel[m_][:], out_offset=None, in_=gb_['g_all'],
                                                                   in_offset=bass.IndirectOffsetOnAxis(ap=gidx[0:4, ic:ic + 1], axis=0)))
        for c in range(NCH):
            m_, c0 = ccol(c)
            P.op('pe', lambda e, m_=m_, c0=c0: e.transpose(out=psT[:, 0:128], in_=kTm[m_][:, c0:c0 + 128], identity=identb[:]), reads=['kT', 'identb'], writes=['psT'])
            P.op('act', lambda e, c=c: e.activation(out=ktok[:, c, :], in_=psT[:, 0:128], func=AF.Copy), reads=['psT'], writes=['ktok'])
            P.op('pe', lambda e, m_=m_, c0=c0: e.transpose(out=psT[:, 128:256], in_=vTm[m_][:, c0:c0 + 128], identity=identb[:]), reads=['vT', 'identb'], writes=['psT'])
            P.op('dve', lambda e, c=c: e.tensor_copy(out=vaug[:, c, 0:128], in_=psT[:, 128:256]), reads=['psT'], writes=['vaug'])
            P.op('pe', lambda e, m_=m_, c0=c0: e.transpose(out=psB[:, 0:4], in_=gsel[m_][0:4, c0:c0 + 128], identity=ident[0:4, 0:4]), reads=['gsel', 'cst'], writes=['psB'])
            P.op('dve', lambda e, c=c: e.tensor_copy(out=G[:, :, c], in_=psB[:, 0:4]), reads=['psB'], writes=['G'])
        for dr in range(2):
            tri = triL if dr == 0 else triU
            mneg = mnegL if dr == 0 else mnegU
            gi, gf = 2 * dr, 2 * dr + 1
            P.op('act', lambda e: e.activation(out=ee[:], in_=G[:, gf, :], func=AF.Exp, scale=-1.0), reads=['G'], writes=['ee'])
            P.op('dve', lambda e: e.tensor_scalar(out=ee[:], in0=ee[:], scalar1=1.0, scalar2=None, op0=ALU.add), reads=['ee'], writes=['ee'])
            P.op('act', lambda e: e.activation(out=ee[:], in_=ee[:], func=AF.Ln), reads=['ee'], writes=['ee'])
            P.op('dve', lambda e: e.tensor_scalar(out=LF[:], in0=ee[:], scalar1=-1.0, scalar2=None, op0=ALU.mult), reads=['ee'], writes=['LF'])
            P.op('pe', lambda e: e.matmul(psB[:, 0:NCH], lhsT=tri, rhs=LF[:], start=True, stop=True), reads=['cst', 'LF'], writes=['psB'])
            P.op('dve', lambda e: e.tensor_copy(out=Bc[:], in_=psB[:, 0:NCH]), reads=['psB'], writes=['Bc'])
            P.op('pe', lambda e: e.matmul(psB[:, 0:NCH], lhsT=ones[:], rhs=LF[:], start=True, stop=True), reads=['ones', 'LF'], writes=['psB'])
            P.op('dve', lambda e: e.tensor_copy(out=BL[:], in_=psB[:, 0:NCH]), reads=['psB'], writes=['BL'])
            P.op('dve', lambda e: e.tensor_tensor(out=IGmB[:], in0=G[:, gi, :], in1=Bc[:], op=ALU.subtract), reads=['G', 'Bc'], writes=['IGmB'])
            P.op('dve', lambda e: e.memset(Caug[:], 0.0), writes=['Caug'])
            P.op('dve', lambda e: e.memset(Cb[:], 0.0), writes=['Cb'])
            P.op('dve', lambda e: e.memset(m[:], 0.0), writes=['m'])
            order = list(range(ncc)) + [ncc + i for i in range(nchl)]
            if dr == 1:
                order = list(range(ncc))[::-1] + [ncc + i for i in range(nchl)][::-1]
            for c in order:
                lat = c >= ncc
                cc = slice(c, c + 1)
                P.op('dve', lambda e, c=c: e.tensor_copy(out=igrep[:], in_=fap(IGmB, c, [[0, 128]])), reads=['IGmB'], writes=['igrep'])
                P.op('pe', lambda e: e.matmul(psR[:, 0:128], lhsT=igrep[:], rhs=ident, start=True, stop=True), reads=['igrep', 'cst'], writes=['psR'])
                c1('glmax', lambda e: e.tensor_reduce(out=col['glmax'][:], in_=psR[:, 0:128], axis=AX.X, op=ALU.max), ['psR'])
                c1('mnew', lambda e, cc=cc: e.scalar_tensor_tensor(out=col['mnew'][:], in0=col['glmax'][:], scalar=m[:, 0:1], in1=BL[:, cc], op0=ALU.max, op1=ALU.add),
                   ['glmax', 'm', 'BL'])
                c1('nmnew', lambda e: e.tensor_scalar(out=col['nmnew'][:], in0=col['mnew'][:], scalar1=-1.0, scalar2=None, op0=ALU.mult), ['mnew'])
                if lat:
                    P.op('dve', lambda e, cc=cc: e.scalar_tensor_tensor(out=dm[:], in0=psR[:, 0:128], scalar=Bc[:, cc], in1=mneg, op0=ALU.add, op1=ALU.add),
                         reads=['psR', 'Bc', 'cst'], writes=['dm'])
                    c1('rowmax', lambda e: e.tensor_reduce(out=col['rowmax'][:], in_=dm[:], axis=AX.X, op=ALU.max), ['dm'])
                    c1('inter', lambda e, cc=cc: e.tensor_tensor(out=col['inter'][:], in0=Bc[:, cc], in1=m[:], op=ALU.add), ['Bc', 'm'])
                    c1('mt', lambda e: e.tensor_tensor(out=col['mt'][:], in0=col['inter'][:], in1=col['rowmax'][:], op=ALU.max), ['inter', 'rowmax'])
                    c1('nmt', lambda e: e.tensor_scalar(out=col['nmt'][:], in0=col['mt'][:], scalar1=-1.0, scalar2=None, op0=ALU.mult), ['mt'])
                    P.op('act', lambda e: e.activation(out=sexp[:], in_=dm[:], func=AF.Exp, bias=col['nmt'][:, 0:1]), reads=['dm', 'nmt'], writes=['sexp'])
                    c1('winter', lambda e: e.activation(out=col['winter'][:], in_=col['inter'][:], func=AF.Exp, bias=col['nmt'][:, 0:1]), ['inter', 'nmt'], eng='act')
                    c1('emt', lambda e: e.activation(out=col['emt'][:], in_=col['mt'][:], func=AF.Exp, scale=-1.0), ['mt'], eng='act')
                    P.op('pe', lambda e, c=c: e.matmul(psQK[:, 0:128], lhsT=qsl(c), rhs=ksl(c), start=True, stop=True),
                         reads=['qT', 'kT'], writes=['psQK'])
                    P.op('dve', lambda e: e.tensor_tensor(out=s_b[:], in0=psQK[:, 0:128], in1=sexp[:], op=ALU.mult), reads=['psQK', 'sexp'], writes=['s_b'])
                    P.op('pe', lambda e: e.transpose(out=psT[:, 0:128], in_=s_b[:], identity=identb[:]), reads=['s_b', 'identb'], writes=['psT'])
                    P.op('act', lambda e: e.activation(out=sT[:], in_=psT[:, 0:128], func=AF.Copy), reads=['psT'], writes=['sT'])
                    P.op('pe', lambda e, c=c: e.matmul(psN[:, 0:129], lhsT=sT[:], rhs=vaug[:, c, :], start=True, stop=True), reads=['sT', 'vaug', 'vaug1'], writes=['psN'])
                    P.op('pe', lambda e, c=c: e.matmul(psX[:, 0:129], lhsT=qsl(c), rhs=Cb[:], start=True, stop=True), reads=['qT', 'Cb'], writes=['psX'])
                    P.op('act', lambda e: e.activation(out=numA[:], in_=psN[:, 0:129], func=AF.Copy), reads=['psN'], writes=['numA'])
                    P.op('dve', lambda e: e.scalar_tensor_tensor(out=tot[:], in0=psX[:, 0:129], scalar=col['winter'][:, 0:1], in1=numA[:], op0=ALU.mult, op1=ALU.add),
                         reads=['psX', 'winter', 'numA'], writes=['tot'])
                    c1('dd', lambda e: e.activation(out=col['dd'][:], in_=tot[:, 128:129], func=AF.Abs), ['tot'], eng='act')
                    c1('dd', lambda e: e.tensor_tensor(out=col['dd'][:], in0=col['dd'][:], in1=col['emt'][:], op=ALU.max), ['dd', 'emt'])
                    c1('dd', lambda e: e.reciprocal(out=col['dd'][:], in_=col['dd'][:]), ['dd'])
                    H = hb[hi % 2]; hn = f"hb{hi % 2}"; hi += 1
                    P.op('dve', lambda e, H=H: e.tensor_scalar(out=H[:], in0=tot[:, 0:128], scalar1=col['dd'][:, 0:1], scalar2=None, op0=ALU.mult), reads=['tot', 'dd'], writes=[hn])
                    lc = c - ncc
                    r0 = ((u * 2 + dr) * nchl + lc) * 128
                    P.dma('sp', h_o[r0:r0 + 128, :], H[:], reads=[hn], writes=['h_o'])
                c1('t2', lambda e, cc=cc: e.tensor_tensor(out=col['t2'][:], in0=IGmB[:, cc], in1=BL[:, cc], op=ALU.add), ['IGmB', 'BL'])
                c1('wk', lambda e: e.activation(out=col['wk'][:], in_=col['t2'][:], func=AF.Exp, bias=col['nmnew'][:, 0:1]), ['t2', 'nmnew'], eng='act')
                P.op('dve', lambda e, c=c: e.tensor_scalar(out=kw[:], in0=ktok[:, c, :], scalar1=col['wk'][:, 0:1], scalar2=None, op0=ALU.mult), reads=['ktok', 'wk'], writes=['kw'])
                P.op('pe', lambda e, c=c: e.matmul(psC[:, 0:129], lhsT=kw[:], rhs=vaug[:, c, :], start=True, stop=True), reads=['kw', 'vaug', 'vaug1'], writes=['psC'])
                c1('t3', lambda e, cc=cc: e.tensor_tensor(out=col['t3'][:], in0=BL[:, cc], in1=m[:], op=ALU.add), ['BL', 'm'])
                c1('decay', lambda e: e.activation(out=col['decay'][:], in_=col['t3'][:], func=AF.Exp, bias=col['nmnew'][:, 0:1]), ['t3', 'nmnew'], eng='act')
                P.op('dve', lambda e: e.scalar_tensor_tensor(out=Caug[:], in0=Caug[:], scalar=col['decay'][:, 0:1], in1=psC[:, 0:129], op0=ALU.mult, op1=ALU.add),
                     reads=['Caug', 'decay', 'psC'], writes=['Caug'])
                P.op('act', lambda e: e.activation(out=Cb[:], in_=Caug[:], func=AF.Copy), reads=['Caug'], writes=['Cb'])
                P.op('dve', lambda e: e.tensor_copy(out=m[:], in_=col['mnew'][:]), reads=['mnew', 'inter', 't3', 'glmax'], writes=['m'])
    K.end_stage()


def stage_l1c(K, ntiles, out_d, gb_, half_tok):
    K.begin_stage("lc_")
    nc, P = K.nc, K.P
    xt_d = gb_['xl0T']
    hidx_d = K.din("h_idx", [128, ntiles * 2 * 8], I32)
    ident_d = K.din("ident", [128, 128])
    cvec_d = K.din("cvec", [128, KC, 2])
    modw_d = K.din("mod_w1", [D, 6144])
    modb_d = K.din("mod_bT1", [128, 48])
    ng_d = K.din("norm_gT", [128, 4])
    wout_d = K.din("cd_w_out", [D, D])
    lng_d = K.din("ln_gT10", [128, KC])
    lnb_d = K.din("ln_bT10", [128, KC])

    psA = [K.ps(f"psA{i}", [128, 512]) for i in range(2)]
    psmean = K.ps("psmean", [128, 512]); pssq = K.ps("pssq", [128, 512]); psm = K.ps("psm", [128, 512])
    psX = K.ps("psX", [128, 512])
    ident = K.sb("ident", [128, 128])
    P.dma('sp', ident[:], ident_d, writes=['ident'])
    hidx = K.sb("hidx", [128, ntiles * 2 * 8], I32)
    P.dma('sp', hidx[:], hidx_d, writes=['hidx'])
    ones128 = K.sb("ones128", [128, 128]); ones1024 = K.sb("ones1024", [128, 128])
    P.op('dve', lambda e: e.memset(ones128[:], 1.0 / 128), writes=['ones128'])
    P.op('dve', lambda e: e.memset(ones1024[:], 1.0 / 1024), writes=['ones1024'])
    ng = K.sb("ng", [128, 4]); lng = K.sb("lng", [128, KC]); lnb = K.sb("lnb", [128, KC])
    for t_, d_, n_ in ((ng, ng_d, 'ng'), (lng, lng_d, 'lng'), (lnb, lnb_d, 'lnb')):
        P.dma('sp', t_[:], d_, writes=[n_])
    g1a = K.sb("g1a", [128, KC, 2])
    wout_b = K.sb("wout_b", [128, KC, D], BF16)
    tmp = ExitStack()
    modT = mod_setup(K, cvec_d, modw_d, modb_d, 16, 24, psm, tmp=tmp)
    stage = [tmp.enter_context(nc.sbuf_tensor(f"s_{K.pfx}stage{i}", [128, D], F32)) for i in range(2)]
    P.op('dve', lambda e: e.tensor_scalar(out=g1a[:], in0=modT[:, 0:8, :], scalar1=1.0 / ALPHA, scalar2=None, op0=ALU.mult), reads=['modT'], writes=['g1a'])
    for k in range(KC):
        load_cast(K, wout_b[:, k, :], wout_d[k * 128:(k + 1) * 128, :], stage[k % 2][:], f"stage{k % 2}", "wout_b", eng='dve' if k % 2 == 0 else 'act')
    P.barrier()
    tmp.close()

    xt = [K.sb(f"xt{i}", [128, KC, NT]) for i in range(2)]
    htk = [K.sb(f"htk{i}", [128, 4, 128]) for i in range(2)]
    HS = K.sb("HS", [128, 4, NT])
    om = [K.sb(f"om{i}", [128, 4, NT]) for i in range(2)]
    ya = [K.sb(f"ya{i}", [128, 4, NT]) for i in range(2)]
    hs = K.sb("hs", [128, 1, NT]); sgm = K.sb("sgm", [128, NT])
    sqt = [K.sb(f"sqt{i}", [128, NT]) for i in range(2)]
    mean_sb = K.sb("mean_sb", [128, NT]); rstd = K.sb("rstd", [128, NT])
    ycat = K.sb("ycat", [128, KC, NT], BF16)
    vt = K.sb("vt", [128, KC, NT])
    xo = [K.sb(f"xo{i}", [128, KC, NT]) for i in range(2)]

    def ln_stats(src, nchunk, ones, onm, srcname, eps):
        for c in range(nchunk):
            P.op('pe', lambda e, c=c: e.matmul(psmean[:, 0:NT], lhsT=ones[:], rhs=src[:, c, :], start=(c == 0), stop=(c == nchunk - 1)),
                 reads=[onm, srcname], writes=['psmean'], inc=(c == nchunk - 1))
        for c in range(nchunk):
            s = sqt[c % 2]; sn = f"sqt{c % 2}"
            P.op('act', lambda e, c=c, s=s: e.activation(out=s[:], in_=src[:, c, :], func=AF.Square), reads=[srcname], writes=[sn])
            P.op('pe', lambda e, c=c, s=s: e.matmul(pssq[:, 0:NT], lhsT=ones[:], rhs=s[:], start=(c == 0), stop=(c == nchunk - 1)),
                 reads=[onm, sn], writes=['pssq'])
        P.op('act', lambda e: e.activation(out=mean_sb[:], in_=psmean[:, 0:NT], func=AF.Copy), reads=['psmean'], writes=['mean_sb'])
        P.op('dve', lambda e: e.tensor_tensor(out=rstd[:], in0=mean_sb[:], in1=mean_sb[:], op=ALU.mult), reads=['mean_sb'], writes=['rstd'])
        P.op('dve', lambda e: e.tensor_tensor(out=rstd[:], in0=pssq[:, 0:NT], in1=rstd[:], op=ALU.subtract), reads=['pssq', 'rstd'], writes=['rstd'])
        P.op('dve', lambda e: e.tensor_scalar(out=rstd[:], in0=rstd[:], scalar1=eps, scalar2=None, op0=ALU.add), reads=['rstd'], writes=['rstd'])
        P.op('act', lambda e: e.activation(out=rstd[:], in_=rstd[:], func=AF.Sqrt), reads=['rstd'], writes=['rstd'])
        P.op('dve', lambda e: e.reciprocal(out=rstd[:], in_=rstd[:]), reads=['rstd'], writes=['rstd'])

    for t in range(ntiles):
        b = t % 2
        X, OM, YA = xt[b], om[b], ya[b]
        xn, omn, yan = f"xt{b}", f"om{b}", f"ya{b}"
        P.dma('sp', X[:], xt_d[t].rearrange("(c p) n -> p c n", p=128), reads=['xl_o'], writes=[xn])
        P.dma('sp', OM[:], gb_['om'][:, t * NT:(t + 1) * NT].rearrange("(c p) n -> p c n", p=128), reads=['om_o'], writes=[omn])
        for c in range(4):
            for par in range(2):
                hd_ = 2 * c + par
                P.dma('sp', YA[par * 64:(par + 1) * 64, c, :], gb_['ya'][hd_ * 64:(hd_ + 1) * 64, t * NT:(t + 1) * NT], reads=['ya_l'], writes=[yan])
        for sub in range(2):
            s_ = 2 * t + sub
            for dr in range(2):
                for hd in range(4):
                    ic = s_ * 8 + hd * 2 + dr
                    P.dma('pool', None, None, reads=['h_all', 'hidx'], writes=[f"htk{dr}"],
                          fn=lambda e, dr=dr, hd=hd, ic=ic: e.indirect_dma_start(out=htk[dr][:, hd, :], out_offset=None, in_=gb_['h_all'],
                                                                                 in_offset=bass.IndirectOffsetOnAxis(ap=hidx[:, ic:ic + 1], axis=0)))
            P.op('dve', lambda e: e.tensor_tensor(out=htk[0][:], in0=htk[0][:], in1=htk[1][:], op=ALU.add), reads=['htk0', 'htk1'], writes=['htk0'])
            for hd in range(4):
                P.op('pe', lambda e, hd=hd: e.transpose(out=psX[:, hd * 128:(hd + 1) * 128], in_=htk[0][:, hd, :], identity=ident[:]), reads=['htk0', 'ident'], writes=['psX'])
            P.op('act', lambda e, sub=sub: e.activation(out=HS[:, :, sub * 128:(sub + 1) * 128], in_=psX[:, :].rearrange("p (c n) -> p c n", c=4), func=AF.Copy),
                 reads=['psX'], writes=['HS'])
        for hd in range(4):
            P.op('dve', lambda e, hd=hd: e.tensor_copy(out=hs[:, 0, :], in_=HS[:, hd, :]), reads=['HS'], writes=['hs'])
            ln_stats(hs, 1, ones128, 'ones128', 'hs', LN_EPS)
            P.op('dve', lambda e: e.tensor_tensor(out=hs[:, 0, :], in0=hs[:, 0, :], in1=mean_sb[:], op=ALU.subtract), reads=['hs', 'mean_sb'], writes=['hs'])
            P.op('dve', lambda e: e.tensor_tensor(out=hs[:, 0, :], in0=hs[:, 0, :], in1=rstd[:], op=ALU.mult), reads=['hs', 'rstd'], writes=['hs'])
            P.op('act', lambda e, hd=hd: e.activation(out=sgm[:], in_=OM[:, hd, :], func=AF.Sigmoid), reads=[omn], writes=['sgm'])
            P.op('dve', lambda e, hd=hd: e.scalar_tensor_tensor(out=ycat[:, hd, :], in0=hs[:, 0, :], scalar=ng[:, hd:hd + 1], in1=sgm[:], op0=ALU.mult, op1=ALU.mult),
                 reads=['hs', 'ng', 'sgm'], writes=['ycat'])
        for c in range(4):
            P.op('act', lambda e, c=c: e.activation(out=ycat[:, 4 + c, :], in_=YA[:, c, :], func=AF.Copy), reads=[yan], writes=['ycat'])
        for oc in range(KC):
            ps = psA[oc % 2]; pn = f"psA{oc % 2}"
            for k in range(KC):
                P.op('pe', lambda e, k=k, oc=oc, ps=ps: e.matmul(ps[:, 0:NT], lhsT=wout_b[:, k, oc * 128:(oc + 1) * 128], rhs=ycat[:, k, :],
                                                                 start=(k == 0), stop=(k == KC - 1)),
                     reads=['wout_b', 'ycat'], writes=[pn], inc=(k == KC - 1))
            P.op('dve', lambda e, oc=oc, ps=ps: e.scalar_tensor_tensor(out=vt[:, oc, :], in0=ps[:, 0:NT], scalar=g1a[:, oc, 0:1],
                                                                      in1=X[:, oc, :], op0=ALU.mult, op1=ALU.add),
                 reads=[pn, 'g1a', xn], writes=['vt'])
        ln_stats(vt, KC, ones1024, 'ones1024', 'vt', LN_EPS / (ALPHA * ALPHA))
        XO = xo[b]; xon = f"xo{b}"
        for oc in range(KC):
            P.op('dve', lambda e, oc=oc: e.tensor_tensor(out=vt[:, oc, :], in0=vt[:, oc, :], in1=mean_sb[:], op=ALU.subtract), reads=['vt', 'mean_sb'], writes=['vt'])
            P.op('dve', lambda e, oc=oc: e.tensor_tensor(out=vt[:, oc, :], in0=vt[:, oc, :], in1=rstd[:], op=ALU.mult), reads=['vt', 'rstd'], writes=['vt'])
            P.op('act', lambda e, oc=oc: e.activation(out=XO[:, oc, :], in_=vt[:, oc, :], func=AF.Identity, scale=lng[:, oc:oc + 1], bias=lnb[:, oc:oc + 1]),
                 reads=['vt', 'lng', 'lnb'], writes=[xon])
        P.dma('sp', out_d[t].rearrange("(c p) n -> p c n", p=128), XO[:], reads=[xon], writes=['x1T_scr'])
    K.end_stage()


def seg_tiles(seq, start, ntile):
    L = seq.shape[0]
    xt = np.zeros((ntile, D, WT), np.float32)
    cm = np.zeros((ntile, 128, WT), np.float32)
    rc = np.ones((ntile, 128, 4, NT), np.float32)
    for t in range(ntile):
        s0 = start + t * NT - HALO
        lo, hi = max(s0, 0), min(s0 + WT, L)
        if hi > lo:
            xt[t, :, lo - s0:hi - s0] = seq[lo:hi].T
            cm[t, :, lo - s0:hi - s0] = 1.0
        tok = start + t * NT + np.arange(NT)
        for g, w in enumerate(POOL_WINDOWS):
            cnt = np.minimum(tok + w // 2, L) - np.maximum(tok - w // 2, 0)
            cnt = np.where(tok < L, cnt, 1).astype(np.float32)
            rc[t, :, g, :] = (1.0 / cnt)[None, :]
    return xt, cm, rc

def fm(v, nchunk):
    return np.ascontiguousarray(np.asarray(v, np.float32).reshape(nchunk, 128).T)

def cvec_of(cb, c_ctx):
    return np.ascontiguousarray(np.stack([fm(cb, KC), fm(c_ctx, KC)], axis=-1))

def plain_tiles(seq, start, ntile):
    L, Dm = seq.shape
    out = np.zeros((ntile, Dm, NT), seq.dtype)
    for t in range(ntile):
        lo = start + t * NT
        hi = min(lo + NT, L)
        if hi > lo:
            out[t, :, :hi - lo] = seq[lo:hi].T
    return out

def rope_tiles(start, ntile, latent=True):
    cs = np.ones((ntile, 64, NT), np.float32); sn = np.zeros((ntile, 64, NT), np.float32)
    if latent:
        f = (10000.0 ** (-np.arange(16, dtype=np.float32) / 16)).astype(np.float32)
        for t in range(ntile):
            tok = start + t * NT + np.arange(NT)
            row = (tok // 64).astype(np.float32); col = (tok % 64).astype(np.float32)
            ar = (row[None, :] * f[:, None]).astype(np.float32); ac = (col[None, :] * f[:, None]).astype(np.float32)
            cs[t] = np.concatenate([np.cos(ar), np.cos(ar), np.cos(ac), np.cos(ac)], 0)
            sn[t] = np.concatenate([np.sin(ar), np.sin(ar), np.sin(ac), np.sin(ac)], 0)
    return cs, sn

def rope_perm():
    Pm = np.zeros((64, 64), np.float32)
    for base in (0, 32):
        for d in range(16):
            Pm[base + d + 16, base + d] = -1.0
            Pm[base + d, base + d + 16] = 1.0
    return Pm

CD_ORDER = None
def cd_perm_cols(w):
    return np.ascontiguousarray(np.concatenate([w[:, 0:2048], w[:, 2064:2832], w[:, 2048:2064]], axis=1))

def scan_consts():
    i = np.arange(128)
    ident = np.eye(128, dtype=np.float32)
    triL = (i[:, None] <= i[None, :]).astype(np.float32)
    triU = (i[:, None] >= i[None, :]).astype(np.float32)
    mnegL = np.where(i[None, :] <= i[:, None], 0.0, -1.0e30).astype(np.float32)
    mnegU = np.where(i[None, :] >= i[:, None], 0.0, -1.0e30).astype(np.float32)
    return np.stack([ident, triL, triU, mnegL, mnegU])

def attn_sel():
    s = np.zeros((65, 64), np.float32); s[64, :] = 1.0
    return s


_PROGS = {}
PAIRS = [[0, 1], [2, 3], [4, 5], [6, 7]]


def _prog(key, fn):
    if key not in _PROGS:
        _PROGS[key] = fn()
    return _PROGS[key]


def _allgather(K, src, dst, rn_src, rn_dst):
    P = K.P
    P._deps('pool', [rn_src], [rn_dst], True)
    ins = K.nc.gpsimd.collective_compute("AllGather", ALU.bypass, replica_groups=PAIRS, ins=[src.opt()], outs=[dst.opt()])
    P.cnt['pool'] += 1
    ins.then_inc(P.sem['pool'], 1)
    P._record(('pool', P.cnt['pool']), [rn_src], [rn_dst])


def _build_fused(ntl):
    K = Ctx()
    Th = ntl * NT
    T = 2 * Th
    TOK = (ntl + 1) * NT
    nsub = 2 * ntl + 1
    x1T_scr0 = K.scr("x1T_scr0", [ntl + 1, D, NT])
    x2_scr0 = K.scr("x2_scr0", [nsub, 128, D])
    x1T_scr1 = K.scr("x1T_scr1", [ntl, D, NT])
    gb = dict(qm=K.scr("g_qm", [512, TOK], BF16), km=K.scr("g_km", [512, TOK], BF16), vm=K.scr("g_vm", [512, TOK], BF16),
              ka=K.scr("g_ka", [128, TOK], BF16), va=K.scr("g_va", [128, TOK], BF16), g=K.scr("g_g", [16, TOK], F32),
              qa=K.scr("l_qa", [512, TOK], BF16), om=K.scr("l_om", [512, TOK], F32), xl0T=K.scr("l_xl0T", [ntl, D, NT]),
              ya=K.scr("l_ya", [512, Th]), h=K.scr("g_h", [2 * 2 * T, 128]),
              qm_all=K.scr("a_qm", [1024, TOK], BF16), km_all=K.scr("a_km", [1024, TOK], BF16), vm_all=K.scr("a_vm", [1024, TOK], BF16),
              ka_all=K.scr("a_ka", [256, TOK], BF16), va_all=K.scr("a_va", [256, TOK], BF16), g_all=K.scr("a_g", [32, TOK], F32),
              h_all=K.scr("a_h", [2 * 2 * 2 * T, 128]))
    out_d = K.dout("x2", [2 * ntl, 128, D])
    uvb = [K.scr(f"uvb{L}", [16384, 2 * D], BF16) for L in range(2)]
    stage_conv(K, 0, uvb[0])
    stage_mixa(K, ntl + 1, [0] * ntl + [1], x1T_scr0)
    stage_peer(K, nsub, [0] * (2 * ntl) + [1], 0, x1T_scr0, x2_scr0, uvb[0])
    stage_l1a(K, ntl + 1, [0] * ntl + [1], x2_scr0, nsub, gb)
    for nm in ("qm", "km", "vm"):
        for hd in range(4):
            _allgather(K, gb[nm][hd * 128:(hd + 1) * 128, :], gb[nm + "_all"][hd * 256:(hd + 1) * 256, :], nm + "_src", nm + "_all")
    for nm in ("ka", "va", "g"):
        _allgather(K, gb[nm], gb[nm + "_all"], nm + "_src", nm + "_all")
    K.P.barrier()
    stage_scan(K, 2, Th, TOK, gb)
    stage_attn(K, Th, TOK, gb, conv=(1, uvb[1]))
    hrows = 2 * 2 * T
    PR = min(4096, hrows)
    for k in range(hrows // PR):
        _allgather(K, gb["h"][k * PR:(k + 1) * PR, :], gb["h_all"][k * 2 * PR:(k + 1) * 2 * PR, :], "h_src", "h_all")
    K.P.barrier()
    stage_l1c(K, ntl, x1T_scr1, gb, Th)
    stage_peer(K, 2 * ntl, [0] * (2 * ntl), 1, x1T_scr1, out_d, uvb[1])
    K.close()
    return K.nc


def kernel(x, c, ctx, c_ctx, mod_w, mod_b, ln_g, ln_b, ab_w_in, pool_w, pool_ls, conv_w, conv_b, conv_ln_g, conv_ln_b, ab_w_out,
           cd_w_in, mlstm_gate_b, mlstm_norm_g, q_norm_g, k_norm_g, cd_w_out, peer_w_q, peer_keys, peer_u, peer_v):
    f32 = lambda a: np.ascontiguousarray(np.asarray(a, np.float32))
    x, c, ctx, c_ctx, mod_w, mod_b, ln_g, ln_b = map(f32, (x, c, ctx, c_ctx, mod_w, mod_b, ln_g, ln_b))
    peer_w_q, peer_keys, peer_u, peer_v = map(f32, (peer_w_q, peer_keys, peer_u, peer_v))
    cd_w_in, cd_w_out, ab_w_in, ab_w_out = map(f32, (cd_w_in, cd_w_out, ab_w_in, ab_w_out))
    B, T, _ = x.shape
    Lc = ctx.shape[1]
    NH = 2
    ncores = B * NH
    assert ncores == 8 and Lc == 256
    Th = T // NH
    Lh = Lc // NH
    ntl = Th // NT
    cores = [(i // NH, i % NH) for i in range(ncores)]
    ident = np.eye(128, dtype=np.float32)
    iota16 = np.tile(np.arange(16, dtype=np.float32), (128, 1))
    nc = _prog(("fused", ntl), lambda: _build_fused(ntl))
    uv_tabs = [np.ascontiguousarray(np.concatenate([peer_u[L], peer_v[L]], axis=1)) for L in range(2)]
    w_in_p = cd_perm_cols(cd_w_in[0])
    consts = scan_consts()
    p = np.arange(128)
    maps = []
    for (b, hf_) in cores:
        xt, cm, rc = seg_tiles(x[b], hf_ * Th, ntl)
        xt2, cm2, rc2 = seg_tiles(ctx[b], hf_ * Lh, 1)
        cs, sn = rope_tiles(hf_ * Th, ntl)
        cs2, sn2 = rope_tiles(0, 1, latent=False)
        sidx = np.zeros((128, 4), np.int32)
        gidx = np.zeros((128, 4), np.int32)
        for j in range(2):
            h = 2 * hf_ + j
            for m_ in range(2):
                sidx[:, j * 2 + m_] = (h * 2 + m_) * 128 + p
                gidx[:4, j * 2 + m_] = m_ * 16 + np.arange(4) * 4 + h
        hidx = np.zeros((128, 2 * ntl * 8), np.int32)
        for s_ in range(2 * ntl):
            for hd in range(4):
                for dr in range(2):
                    m_, u_ = hd // 2, hd % 2
                    grow = (u_ * 2 + dr) * T + hf_ * Th + s_ * 128 + p
                    PR = min(4096, 4 * T)
                    hidx[:, s_ * 8 + hd * 2 + dr] = ((grow // PR) * 2 + m_) * PR + (grow % PR)
        m = dict(
            xtiles=np.concatenate([xt, xt2]), cmask=np.concatenate([cm, cm2]), rcorr=np.concatenate([rc, rc2]),
            cvec=cvec_of(c[b], c_ctx), ab_w_in=ab_w_in[0], pool_w=f32(pool_w[0]), pool_lsT=fm(pool_ls[0], 4),
            conv_wT=np.ascontiguousarray(f32(conv_w[0]).T.reshape(4, 128, 31).transpose(1, 0, 2)),
            conv_bT=fm(conv_b[0], 4), cln_gT=fm(conv_ln_g[0], 4), cln_bT=fm(conv_ln_b[0], 4),
            ab_w_out=ab_w_out[0], ln_gT00=fm(ln_g[0, 0], KC), ln_bT00=fm(ln_b[0, 0], KC),
            cosT=np.concatenate([cs, cs2]), sinT=np.concatenate([sn, sn2]), cd_w_in=w_in_p,
            gate_bT=f32(mlstm_gate_b[0]).reshape(16, 1).copy(), qn_gT=f32(q_norm_g[0]).reshape(64, 1).copy(),
            kn_gT=f32(k_norm_g[0]).reshape(64, 1).copy(), ropeP=rope_perm(),
            scan_idx=sidx, scan_gidx=gidx, consts=consts, qn_g=f32(q_norm_g[0])[None].copy(), kn_g=f32(k_norm_g[0])[None].copy(), sel=attn_sel(),
            h_idx=hidx, norm_gT=fm(mlstm_norm_g[0], 4), cd_w_out=cd_w_out[0], ln_gT10=fm(ln_g[1, 0], KC), ln_bT10=fm(ln_b[1, 0], KC),
            ident=ident, iota16=iota16)
        for L in range(2):
            m.update({f"mod_w{L}": mod_w[L], f"mod_bT{L}": fm(mod_b[L], 48), f"w_q{L}": peer_w_q[L],
                      f"keysT{L}": np.ascontiguousarray(peer_keys[L].transpose(2, 0, 1)), f"peer_uv{L}": uv_tabs[L],
                      f"ln_g{L}1": ln_g[L, 1][None].copy(), f"ln_b{L}1": ln_b[L, 1][None].copy()})
        maps.append(m)
    res = run_bass_kernel_spmd(nc, maps, core_ids=list(range(ncores))).results
    out = np.zeros((B, T, D), np.float32)
    for i, (b, hf_) in enumerate(cores):
        out[b, hf_ * Th:(hf_ + 1) * Th] = res[i]["x2"].reshape(Th, D)
    return out
```
